# Optimizing a Trainium2 kernel written in Bass

```python
import math
import jax
import jax.numpy as jnp
from jax import lax
import numpy as np

D_MODEL = 1024
BATCH = 8
SEQ = 4096
DEPTH = 2

CTX_LEN = 256
GRID_W = 64
N_MIXERS = 4
GROUP_W = D_MODEL // N_MIXERS

RW_HEADS = 4
RW_HD = GROUP_W // RW_HEADS
RW_DECAY_LORA = 32
RW_AAA_LORA = 32
RW_GATE_LORA = 64
RW_GN_EPS = 64e-5

GD_HEADS = 4
GD_HD = GROUP_W // GD_HEADS
GD_CHUNK = 64
GD_CONV = 3
GD_NORM_EPS = 1e-6

HY_CONV = 3
HY_EMB = 33
HY_BANDS = (HY_EMB - 1) // 2
HY_FILTER_HIDDEN = 64
HY_ORDER = 2
HY_SIDES = 2
HY_TARGET = 1e-2
HY_SHORT_DECAY_PCT = 0.3
HY_LONG_DECAY_PCT = 1.5

S5_CH = 16
S5_GROUPS = GROUP_W // S5_CH
S5_STATE = 64

N_EXPERTS = 64
TOP_K = 8
N_GROUPS = 8
TOPK_GROUPS = 4
D_EXPERT = 256
ROUTED_SCALE = 2.5
MOE_BLOCK = 256

LN_EPS = 1e-5
DEEPNORM_ALPHA = (2 * DEPTH) ** 0.25
DEEPNORM_BETA = (8 * DEPTH) ** -0.25

RW_COLS = 3 * GROUP_W + 2 * RW_DECAY_LORA + 2 * RW_AAA_LORA + RW_GATE_LORA
GD_COLS = 4 * GROUP_W + 4 * GD_HEADS
HY_COLS = 3 * GROUP_W
S5_COLS = GROUP_W
D_IN = RW_COLS + GD_COLS + HY_COLS + S5_COLS
MIX_SPLIT = [RW_COLS, RW_COLS + GD_COLS, RW_COLS + GD_COLS + HY_COLS]
RW_SPLIT = [GROUP_W, 2 * GROUP_W, 3 * GROUP_W, 3 * GROUP_W + 2 * RW_DECAY_LORA,
            3 * GROUP_W + 2 * RW_DECAY_LORA + 2 * RW_AAA_LORA]
GD_SPLIT = [3 * GROUP_W, 4 * GROUP_W, 4 * GROUP_W + 2 * GD_HEADS]

kernel_name = 'hybrid_flow_backbone_rwkv_gdn_hyena_s5_moe'


def layer_norm(x, g, b):
    xf = x.astype(jnp.float32)
    mu = jnp.mean(xf, axis=-1, keepdims=True)
    var = jnp.mean(jnp.square(xf - mu), axis=-1, keepdims=True)
    return ((xf - mu) * lax.rsqrt(var + LN_EPS) * g + b).astype(x.dtype)


def l2_normalize(x):
    return x * lax.rsqrt(jnp.sum(x * x, axis=-1, keepdims=True) + 1e-12)


def split_heads(t, n_heads):
    return t.reshape(t.shape[:-1] + (n_heads, t.shape[-1] // n_heads))


def merge_heads(t):
    return t.reshape(t.shape[:-2] + (t.shape[-2] * t.shape[-1],))


def dwconv_centered(x, w):
    k = w.shape[0]
    return lax.conv_general_dilated(
        x, w[:, None, :].astype(x.dtype), window_strides=(1,), padding=[(k // 2, k // 2)],
        dimension_numbers=('NWC', 'WIO', 'NWC'), feature_group_count=x.shape[-1])


def swiglu(x, w1, w3, w2):
    return (jax.nn.silu(x @ w1) * (x @ w3)) @ w2


def rwkv7_prep(z, w0, w2, a0, a2, g2, k_k, k_a):
    z = z.astype(jnp.float32)
    r, k, v, wl, al, gl = jnp.split(z, RW_SPLIT, axis=-1)
    lead = z.shape[:-1]
    wl = wl.reshape(lead + (2, RW_DECAY_LORA))
    al = al.reshape(lead + (2, RW_AAA_LORA))
    wlog = -jax.nn.softplus(-(w0 + jnp.einsum('bldr,drc->bldc', jnp.tanh(wl), w2))) - 0.5
    decay = jnp.exp(-jnp.exp(wlog))
    a = jax.nn.sigmoid(a0 + jnp.einsum('bldr,drc->bldc', al, a2))
    g = jax.nn.sigmoid(gl) @ g2
    kk = l2_normalize(split_heads(k * k_k, RW_HEADS))
    kd = k[:, :, None, :] * (1.0 + (a - 1.0) * k_a)
    return (split_heads(r, RW_HEADS), split_heads(v, RW_HEADS), kk, split_heads(kd, RW_HEADS),
            split_heads(decay, RW_HEADS), split_heads(a, RW_HEADS), g)


def rwkv7_scan(r, w, k, v, a, b, s0, reverse):
    def step(s, inp):
        rt, wt, kt, vt, at, bt = inp
        sa = jnp.einsum('bhvk,bhk->bhv', s, at)
        s = s * wt[:, :, None, :] + sa[..., None] * bt[:, :, None, :] + vt[..., None] * kt[:, :, None, :]
        return s, jnp.einsum('bhvk,bhk->bhv', s, rt)
    xs = tuple(jnp.moveaxis(t, 1, 0) for t in (r, w, k, v, a, b))
    s, ys = lax.scan(step, s0, xs, reverse=reverse)
    return jnp.moveaxis(ys, 0, 1), s


def rwkv7_mixer(zc, zl, w0, w2, a0, a2, g2, k_k, k_a, r_k, gn_g, gn_b, need_ctx):
    seqs = (rwkv7_prep(zc, w0, w2, a0, a2, g2, k_k, k_a), rwkv7_prep(zl, w0, w2, a0, a2, g2, k_k, k_a))
    s0 = jnp.zeros((zl.shape[0], RW_HEADS, RW_HD, RW_HD), jnp.float32)
    y = [0.0, 0.0]
    bonus = [0.0, 0.0]
    for d in range(2):
        state = s0
        for j, (r, v, kk, kd, dec, a, _) in enumerate(seqs):
            yj, state = rwkv7_scan(r, dec[:, :, d], kd[:, :, d], v, -kk, kk * a[:, :, d], state, d == 1)
            y[j] = y[j] + yj
            bonus[j] = bonus[j] + jnp.sum(r * kd[:, :, d] * r_k, axis=-1, keepdims=True) * v

    def post(yj, bj, g):
        mu = jnp.mean(yj, axis=-1, keepdims=True)
        var = jnp.mean(jnp.square(yj - mu), axis=-1, keepdims=True)
        yn = merge_heads((yj - mu) * lax.rsqrt(var + RW_GN_EPS)) * gn_g + gn_b
        return (yn + merge_heads(bj)) * g

    out_l = post(y[1], bonus[1], seqs[1][6])
    out_c = post(y[0], bonus[0], seqs[0][6]) if need_ctx else None
    return out_c, out_l


def gdn_prep(z, conv_w, a_log, dt_bias):
    z = z.astype(jnp.float32)
    qkv, zg, al, bl = jnp.split(z, GD_SPLIT, axis=-1)
    q, k, v = jnp.split(jax.nn.silu(dwconv_centered(qkv, conv_w)), 3, axis=-1)
    q = l2_normalize(split_heads(q, GD_HEADS)) * (GD_HD ** -0.5)
    k = l2_normalize(split_heads(k, GD_HEADS))
    v = split_heads(v, GD_HEADS)
    lead = z.shape[:-1]
    g = -jnp.exp(a_log) * jax.nn.softplus(al.reshape(lead + (2, GD_HEADS)) + dt_bias)
    beta = jax.nn.sigmoid(bl.reshape(lead + (2, GD_HEADS)))
    return q, k, v, zg, g, beta


def gdn_chunked(q, k, v, g, beta, s0):
    bsz, seq, nh, _ = q.shape
    n = seq // GD_CHUNK

    def chunk(t):
        t = jnp.swapaxes(t, 1, 2)
        return t.reshape(t.shape[:2] + (n, GD_CHUNK) + t.shape[3:])

    q, k, v, g, beta = chunk(q), chunk(k), chunk(v), chunk(g), chunk(beta)
    gc = jnp.cumsum(g, axis=-1)
    incl = jnp.tril(jnp.ones((GD_CHUNK, GD_CHUNK), bool))
    strict = jnp.tril(jnp.ones((GD_CHUNK, GD_CHUNK), bool), -1)
    diff = gc[..., :, None] - gc[..., None, :]
    decay = jnp.where(incl, jnp.exp(jnp.where(incl, diff, 0.0)), 0.0)
    kb = k * beta[..., None]
    lmat = jnp.where(strict, jnp.einsum('bhncd,bhnsd->bhncs', kb, k) * decay, 0.0)
    amat = lmat + jnp.eye(GD_CHUNK, dtype=lmat.dtype)
    u = lax.linalg.triangular_solve(amat, v * beta[..., None], left_side=True, lower=True, unit_diagonal=True)
    w = lax.linalg.triangular_solve(amat, kb * jnp.exp(gc)[..., None], left_side=True, lower=True,
                                    unit_diagonal=True)
    attn = jnp.einsum('bhncd,bhnsd->bhncs', q, k) * decay
    qg = q * jnp.exp(gc)[..., None]
    kg = k * jnp.exp(gc[..., -1:] - gc)[..., None]
    glast = jnp.exp(gc[..., -1])

    def step(s, inp):
        u_i, w_i, qg_i, kg_i, at_i, gl_i = inp
        v_new = u_i - jnp.einsum('bhcd,bhde->bhce', w_i, s)
        o = jnp.einsum('bhcd,bhde->bhce', qg_i, s) + jnp.einsum('bhcs,bhse->bhce', at_i, v_new)
        s = s * gl_i[..., None, None] + jnp.einsum('bhcd,bhce->bhde', kg_i, v_new)
        return s, o

    xs = tuple(jnp.moveaxis(t, 2, 0) for t in (u, w, qg, kg, attn, glast))
    s, o = lax.scan(step, s0, xs)
    o = jnp.moveaxis(o, 0, 2).reshape(bsz, nh, seq, -1)
    return jnp.swapaxes(o, 1, 2), s


def gdn_mixer(zc, zl, conv_w, a_log, dt_bias, norm_g, need_ctx):
    seqs = (gdn_prep(zc, conv_w, a_log, dt_bias), gdn_prep(zl, conv_w, a_log, dt_bias))
    s0 = jnp.zeros((zl.shape[0], GD_HEADS, GD_HD, GD_HD), jnp.float32)
    o = [0.0, 0.0]
    for d in range(2):
        flip = (lambda t: jnp.flip(t, axis=1)) if d == 1 else (lambda t: t)
        state = s0
        for j, (q, k, v, _, g, beta) in enumerate(seqs):
            oj, state = gdn_chunked(flip(q), flip(k), flip(v), flip(g[:, :, d]), flip(beta[:, :, d]), state)
            o[j] = o[j] + flip(oj)

    def post(oj, zg):
        on = oj * lax.rsqrt(jnp.mean(jnp.square(oj), axis=-1, keepdims=True) + GD_NORM_EPS) * norm_g
        return merge_heads(on) * jax.nn.silu(zg)

    out_l = post(o[1], seqs[1][3])
    out_c = post(o[0], seqs[0][3]) if need_ctx else None
    return out_c, out_l


def hyena_filters(length, w1, b1, f1, w2, b2, f2, w3):
    t = jnp.linspace(0.0, 1.0, length, dtype=jnp.float32)[:, None]
    ang = (2.0 * math.pi / length) * jnp.arange(length, dtype=jnp.float32)[:, None]
    bands = jnp.linspace(1e-4, HY_BANDS - 1, HY_BANDS, dtype=jnp.float32)
    feat = jnp.concatenate([t, jnp.cos(bands * ang), -jnp.sin(bands * ang)], axis=-1)
    hid = jnp.sin(f1 * (feat @ w1 + b1))
    hid = jnp.sin(f2 * (hid @ w2 + b2))
    h = (hid @ w3).astype(jnp.float32).reshape(length, HY_ORDER, HY_SIDES, GROUP_W)
    deltas = jnp.abs(jnp.linspace(math.log(HY_TARGET) / HY_LONG_DECAY_PCT, math.log(HY_TARGET) / HY_SHORT_DECAY_PCT,
                                  GROUP_W, dtype=jnp.float32))
    return h * jnp.exp(-t[:, :, None, None] * deltas)


def two_sided_fftconv(u, h_fwd, h_bwd, skip):
    length = u.shape[1]
    taps = jnp.concatenate([h_fwd, jnp.zeros_like(h_fwd[:1]), jnp.flip(h_bwd[1:], axis=0)], axis=0)
    y = jnp.fft.irfft(jnp.fft.rfft(u, n=2 * length, axis=1) * jnp.fft.rfft(taps, axis=0)[None],
                      n=2 * length, axis=1)
    return y[:, :length] + u * skip


def hyena_branch(z, conv_w, conv_b, filt, skip):
    z = dwconv_centered(z.astype(jnp.float32), conv_w) + conv_b
    x1, x2, v = jnp.split(z, 3, axis=-1)
    y = x1 * two_sided_fftconv(v, filt[:, 0, 0], filt[:, 0, 1], skip[0])
    return x2 * two_sided_fftconv(y, filt[:, 1, 0], filt[:, 1, 1], skip[1])


def hyena_mixer(zc, zl, conv_w, conv_b, w1, b1, f1, w2, b2, f2, w3, skip, need_ctx):
    out_l = hyena_branch(zl, conv_w, conv_b, hyena_filters(zl.shape[1], w1, b1, f1, w2, b2, f2, w3), skip)
    out_c = None
    if need_ctx:
        out_c = hyena_branch(zc, conv_w, conv_b, hyena_filters(zc.shape[1], w1, b1, f1, w2, b2, f2, w3), skip)
    return out_c, out_l


def s5_discretize(lam_re, lam_im, log_step, b_re, b_im):
    lam_re, lam_im = lam_re.astype(jnp.float32), lam_im.astype(jnp.float32)
    dt = jnp.exp(log_step.astype(jnp.float32))[:, None]
    mag = jnp.exp(lam_re * dt)
    ab_re, ab_im = mag * jnp.cos(lam_im * dt), mag * jnp.sin(lam_im * dt)
    den = lam_re * lam_re + lam_im * lam_im
    co_re = ((ab_re - 1.0) * lam_re + ab_im * lam_im) / den
    co_im = (ab_im * lam_re - (ab_re - 1.0) * lam_im) / den
    bb_re = co_re[..., None] * b_re - co_im[..., None] * b_im
    bb_im = co_re[..., None] * b_im + co_im[..., None] * b_re
    return ab_re, ab_im, bb_re, bb_im


def complex_affine_combine(e1, e2):
    a1r, a1i, b1r, b1i = e1
    a2r, a2i, b2r, b2i = e2
    return (a1r * a2r - a1i * a2i, a1r * a2i + a1i * a2r,
            a2r * b1r - a2i * b1i + b2r, a2r * b1i + a2i * b1r + b2i)


def s5_scan(u, ab_re, ab_im, bb_re, bb_im, s0_re, s0_im, reverse):
    length = u.shape[0]
    bu_re = jnp.einsum('lbgh,gph->lbgp', u, bb_re)
    bu_im = jnp.einsum('lbgh,gph->lbgp', u, bb_im)
    first = length - 1 if reverse else 0
    bu_re = bu_re.at[first].add(ab_re * s0_re - ab_im * s0_im)
    bu_im = bu_im.at[first].add(ab_re * s0_im + ab_im * s0_re)
    a_re = jnp.broadcast_to(ab_re, (length, 1) + ab_re.shape)
    a_im = jnp.broadcast_to(ab_im, (length, 1) + ab_im.shape)
    _, _, x_re, x_im = lax.associative_scan(complex_affine_combine, (a_re, a_im, bu_re, bu_im),
                                            reverse=reverse, axis=0)
    return x_re, x_im


def s5_mixer(uc, ul, rows, lam_re, lam_im, log_step, b_re, b_im, c_re, c_im, d_skip, glu_w, glu_b, need_ctx):
    bsz, seq, _ = ul.shape
    ul_cm = ul.reshape(bsz, rows, GRID_W, GROUP_W).transpose(0, 2, 1, 3).reshape(bsz, seq, GROUP_W)

    def to_groups(t):
        return jnp.swapaxes(t.astype(jnp.float32), 0, 1).reshape(t.shape[1], bsz, S5_GROUPS, S5_CH)

    ug = (to_groups(uc), to_groups(ul_cm))
    zero = jnp.zeros((bsz, S5_GROUPS, S5_STATE), jnp.float32)
    y = [0.0, 0.0]
    for d in range(2):
        rev = d == 1
        ab_re, ab_im, bb_re, bb_im = s5_discretize(lam_re[d], lam_im[d], log_step[d], b_re[d], b_im[d])
        xc_re, xc_im = s5_scan(ug[0], ab_re, ab_im, bb_re, bb_im, zero, zero, rev)
        fin = 0 if rev else -1
        xl_re, xl_im = s5_scan(ug[1], ab_re, ab_im, bb_re, bb_im, xc_re[fin], xc_im[fin], rev)
        y[1] = y[1] + (jnp.einsum('lbgp,ghp->lbgh', xl_re, c_re[d]) - jnp.einsum('lbgp,ghp->lbgh', xl_im, c_im[d]))
        if need_ctx:
            y[0] = y[0] + (jnp.einsum('lbgp,ghp->lbgh', xc_re, c_re[d])
                           - jnp.einsum('lbgp,ghp->lbgh', xc_im, c_im[d]))

    def post(yj, uj):
        length = yj.shape[0]
        yj = yj + uj * d_skip.reshape(S5_GROUPS, S5_CH)
        yj = jnp.swapaxes(yj.reshape(length, bsz, GROUP_W), 0, 1)
        yj = jax.nn.gelu(yj, approximate=False)
        return yj * jax.nn.sigmoid(yj @ glu_w + glu_b)

    out_l = post(y[1], ug[1]).reshape(bsz, GRID_W, rows, GROUP_W).transpose(0, 2, 1, 3).reshape(bsz, seq, GROUP_W)
    out_c = post(y[0], ug[0]) if need_ctx else None
    return out_c, out_l


def routed_experts(h, idx, wts, w1, w3, w2):
    t, d = h.shape
    n_assign = t * TOP_K
    n_blocks = -(-n_assign // MOE_BLOCK) + N_EXPERTS
    flat_e = idx.reshape(-1)
    order = jnp.argsort(flat_e)
    sorted_e = flat_e[order]
    counts = jnp.zeros((N_EXPERTS,), jnp.int32).at[flat_e].add(1)
    padded = (counts + MOE_BLOCK - 1) // MOE_BLOCK * MOE_BLOCK
    pad_end = jnp.cumsum(padded)
    pad_start = pad_end - padded
    start = jnp.cumsum(counts) - counts
    dest = pad_start[sorted_e] + jnp.arange(n_assign, dtype=jnp.int32) - start[sorted_e]
    row_tok = jnp.full((n_blocks * MOE_BLOCK,), t, jnp.int32).at[dest].set((order // TOP_K).astype(jnp.int32))
    row_w = jnp.zeros((n_blocks * MOE_BLOCK,), jnp.float32).at[dest].set(wts.reshape(-1)[order])
    block_e = jnp.minimum(jnp.searchsorted(pad_end, jnp.arange(n_blocks, dtype=jnp.int32) * MOE_BLOCK,
                                           side='right'), N_EXPERTS - 1)
    h_pad = jnp.concatenate([h, jnp.zeros((1, d), h.dtype)], axis=0)

    def body(acc, blk):
        tok, wb, e = blk
        yb = swiglu(h_pad[tok], w1[e], w3[e], w2[e]).astype(jnp.float32) * wb[:, None]
        return acc.at[tok].add(yb), None

    acc, _ = lax.scan(body, jnp.zeros((t + 1, d), jnp.float32),
                      (row_tok.reshape(n_blocks, MOE_BLOCK), row_w.reshape(n_blocks, MOE_BLOCK), block_e))
    return acc[:t].astype(h.dtype)


def moe_ffn(h, router_w, router_b, w1, w3, w2, sw1, sw3, sw2):
    t = h.shape[0]
    scores = jax.nn.sigmoid((h @ router_w).astype(jnp.float32))
    biased = scores + router_b
    grp = biased.reshape(t, N_GROUPS, N_EXPERTS // N_GROUPS)
    grp_score = jnp.sum(lax.top_k(grp, 2)[0], axis=-1)
    _, gsel = lax.top_k(grp_score, TOPK_GROUPS)
    gmask = jnp.any(gsel[:, :, None] == jnp.arange(N_GROUPS)[None, None, :], axis=1)
    emask = jnp.repeat(gmask, N_EXPERTS // N_GROUPS, axis=1)
    _, idx = lax.top_k(jnp.where(emask, biased, -jnp.inf), TOP_K)
    wts = jnp.take_along_axis(scores, idx, axis=1)
    wts = wts / jnp.sum(wts, axis=-1, keepdims=True) * ROUTED_SCALE
    return routed_experts(h, idx, wts, w1, w3, w2) + swiglu(h, sw1, sw3, sw2)


def setup_inputs(seed: int = 0) -> dict:
    key = jax.random.key(seed)
    keys = iter(jax.random.split(key, 64))

    def nrm(shape, scale):
        return jax.random.normal(next(keys), shape, jnp.float32) * scale

    def unif(shape, lo, hi):
        return jax.random.uniform(next(keys), shape, jnp.float32, lo, hi)

    D, C, E, F = D_MODEL, GROUP_W, N_EXPERTS, D_EXPERT
    H = HY_FILTER_HIDDEN
    x = nrm((BATCH, SEQ, D), 1.0)
    c = nrm((BATCH, D), 1.0)
    ctx = nrm((BATCH, CTX_LEN, D), 1.0)
    c_ctx = nrm((D,), 1.0)
    dt = jnp.exp(unif((DEPTH, 2, GD_HEADS), math.log(1e-3), math.log(1e-1)))
    return {
        'x': x,
        'c': c,
        'ctx': ctx,
        'c_ctx': c_ctx,
        'ada_w': nrm((DEPTH, D, 6 * D), 0.5 * D ** -0.5),
        'ada_b': nrm((DEPTH, 6 * D), 0.02),
        'w_in': nrm((DEPTH, D, D_IN), D ** -0.5),
        'w_out': nrm((DEPTH, D, D), DEEPNORM_BETA * D ** -0.5),
        'ln_g': 1.0 + nrm((DEPTH, 2, D), 0.02),
        'ln_b': nrm((DEPTH, 2, D), 0.02),
        'rw_w0': unif((DEPTH, 2, C), -5.0, 0.0),
        'rw_w2': nrm((DEPTH, 2, RW_DECAY_LORA, C), 0.1),
        'rw_a0': nrm((DEPTH, 2, C), 0.1),
        'rw_a2': nrm((DEPTH, 2, RW_AAA_LORA, C), 0.1),
        'rw_g2': nrm((DEPTH, RW_GATE_LORA, C), RW_GATE_LORA ** -0.5),
        'rw_kk': 0.85 + nrm((DEPTH, C), 0.02),
        'rw_ka': 1.0 + nrm((DEPTH, C), 0.02),
        'rw_rk': nrm((DEPTH, RW_HEADS, RW_HD), 0.1),
        'rw_gn_g': 1.0 + nrm((DEPTH, C), 0.02),
        'rw_gn_b': nrm((DEPTH, C), 0.02),
        'gd_conv': nrm((DEPTH, GD_CONV, 3 * C), GD_CONV ** -0.5),
        'gd_alog': jnp.log(unif((DEPTH, 2, GD_HEADS), 1.0, 16.0)),
        'gd_dtb': dt + jnp.log(-jnp.expm1(-dt)),
        'gd_norm': 1.0 + nrm((DEPTH, GD_HD), 0.02),
        'hy_conv': nrm((DEPTH, HY_CONV, 3 * C), HY_CONV ** -0.5),
        'hy_conv_b': nrm((DEPTH, 3 * C), 0.02),
        'hy_w1': nrm((DEPTH, HY_EMB, H), HY_EMB ** -0.5),
        'hy_b1': nrm((DEPTH, H), 0.1),
        'hy_f1': 1.0 + nrm((DEPTH, H), 0.02),
        'hy_w2': nrm((DEPTH, H, H), H ** -0.5),
        'hy_b2': nrm((DEPTH, H), 0.1),
        'hy_f2': 1.0 + nrm((DEPTH, H), 0.02),
        'hy_w3': nrm((DEPTH, H, HY_ORDER * HY_SIDES * C), 0.05 * H ** -0.5),
        'hy_skip': nrm((DEPTH, HY_ORDER, C), 1.0),
        's5_lre': -0.5 + nrm((DEPTH, 2, S5_GROUPS, S5_STATE), 0.01),
        's5_lim': math.pi * jnp.arange(S5_STATE, dtype=jnp.float32) + nrm((DEPTH, 2, S5_GROUPS, S5_STATE), 0.01),
        's5_logstep': unif((DEPTH, 2, S5_GROUPS), math.log(1e-3), math.log(1e-1)),
        's5_bre': nrm((DEPTH, 2, S5_GROUPS, S5_STATE, S5_CH), (2 * S5_CH) ** -0.5),
        's5_bim': nrm((DEPTH, 2, S5_GROUPS, S5_STATE, S5_CH), (2 * S5_CH) ** -0.5),
        's5_cre': nrm((DEPTH, 2, S5_GROUPS, S5_CH, S5_STATE), 0.5),
        's5_cim': nrm((DEPTH, 2, S5_GROUPS, S5_CH, S5_STATE), 0.5),
        's5_d': nrm((DEPTH, C), 1.0),
        's5_glu_w': nrm((DEPTH, C, C), C ** -0.5),
        's5_glu_b': nrm((DEPTH, C), 0.02),
        'router_w': nrm((DEPTH, D, E), D ** -0.5),
        'router_b': nrm((DEPTH, E), 0.01),
        'ex_w1': nrm((DEPTH, E, D, F), D ** -0.5),
        'ex_w3': nrm((DEPTH, E, D, F), D ** -0.5),
        'ex_w2': nrm((DEPTH, E, F, D), DEEPNORM_BETA * F ** -0.5),
        'sh_w1': nrm((DEPTH, D, F), D ** -0.5),
        'sh_w3': nrm((DEPTH, D, F), D ** -0.5),
        'sh_w2': nrm((DEPTH, F, D), DEEPNORM_BETA * F ** -0.5),
    }


def reference(x, c, ctx, c_ctx, ada_w, ada_b, w_in, w_out, ln_g, ln_b,
              rw_w0, rw_w2, rw_a0, rw_a2, rw_g2, rw_kk, rw_ka, rw_rk, rw_gn_g, rw_gn_b,
              gd_conv, gd_alog, gd_dtb, gd_norm,
              hy_conv, hy_conv_b, hy_w1, hy_b1, hy_f1, hy_w2, hy_b2, hy_f2, hy_w3, hy_skip,
              s5_lre, s5_lim, s5_logstep, s5_bre, s5_bim, s5_cre, s5_cim, s5_d, s5_glu_w, s5_glu_b,
              router_w, router_b, ex_w1, ex_w3, ex_w2, sh_w1, sh_w3, sh_w2):
    d_model = x.shape[-1]
    rows = x.shape[1] // GRID_W
    xl, xc = x, ctx
    for i in range(DEPTH):
        need_ctx = i < DEPTH - 1
        mod_l = jax.nn.silu(c) @ ada_w[i] + ada_b[i]
        mod_c = jax.nn.silu(c_ctx) @ ada_w[i] + ada_b[i]
        sh1, sc1, ga1, sh2, sc2, ga2 = jnp.split(mod_l[:, None, :], 6, axis=-1)
        csh1, csc1, cga1, csh2, csc2, cga2 = jnp.split(mod_c, 6, axis=-1)

        zl = jnp.split((xl * (1.0 + sc1) + sh1) @ w_in[i], MIX_SPLIT, axis=-1)
        zc = jnp.split((xc * (1.0 + csc1) + csh1) @ w_in[i], MIX_SPLIT, axis=-1)
        rw_c, rw_l = rwkv7_mixer(zc[0], zl[0], rw_w0[i], rw_w2[i], rw_a0[i], rw_a2[i], rw_g2[i], rw_kk[i],
                                 rw_ka[i], rw_rk[i], rw_gn_g[i], rw_gn_b[i], need_ctx)
        gd_c, gd_l = gdn_mixer(zc[1], zl[1], gd_conv[i], gd_alog[i], gd_dtb[i], gd_norm[i], need_ctx)
        hy_c, hy_l = hyena_mixer(zc[2], zl[2], hy_conv[i], hy_conv_b[i], hy_w1[i], hy_b1[i], hy_f1[i],
                                 hy_w2[i], hy_b2[i], hy_f2[i], hy_w3[i], hy_skip[i], need_ctx)
        s5_c, s5_l = s5_mixer(zc[3], zl[3], rows, s5_lre[i], s5_lim[i], s5_logstep[i], s5_bre[i], s5_bim[i],
                              s5_cre[i], s5_cim[i], s5_d[i], s5_glu_w[i], s5_glu_b[i], need_ctx)
        ol = jnp.concatenate([rw_l, gd_l, hy_l, s5_l], axis=-1).astype(xl.dtype) @ w_out[i]
        xl = layer_norm(DEEPNORM_ALPHA * xl + ga1 * ol, ln_g[i, 0], ln_b[i, 0])
        if need_ctx:
            oc = jnp.concatenate([rw_c, gd_c, hy_c, s5_c], axis=-1).astype(xc.dtype) @ w_out[i]
            xc = layer_norm(DEEPNORM_ALPHA * xc + cga1 * oc, ln_g[i, 0], ln_b[i, 0])

        hl = (xl * (1.0 + sc2) + sh2).reshape(-1, d_model)
        if need_ctx:
            hc = (xc * (1.0 + csc2) + csh2).reshape(-1, d_model)
            f = moe_ffn(jnp.concatenate([hc, hl], axis=0), router_w[i], router_b[i], ex_w1[i], ex_w3[i],
                        ex_w2[i], sh_w1[i], sh_w3[i], sh_w2[i])
            fc, fl = f[:hc.shape[0]], f[hc.shape[0]:]
            xc = layer_norm(DEEPNORM_ALPHA * xc + cga2 * fc.reshape(xc.shape), ln_g[i, 1], ln_b[i, 1])
        else:
            fl = moe_ffn(hl, router_w[i], router_b[i], ex_w1[i], ex_w3[i], ex_w2[i], sh_w1[i], sh_w3[i],
                         sh_w2[i])
        xl = layer_norm(DEEPNORM_ALPHA * xl + ga2 * fl.reshape(xl.shape), ln_g[i, 1], ln_b[i, 1])
    return xl
```

```python
import bisect
import contextlib
import math
import numpy as np
import concourse.bass as bass
import concourse.mybir as mybir
from concourse.bass_utils import run_bass_kernel_spmd

F32 = mybir.dt.float32
BF16 = mybir.dt.bfloat16
ALU = mybir.AluOpType
AF = mybir.ActivationFunctionType
AX = mybir.AxisListType

D = 1024
LC = 256
LL = 4096
T = LC + LL
NT = T // 128
DEPTH = 2
ALPHA = (2 * DEPTH) ** 0.25
ZROWS = 3200
ZR = dict(rw_r=0, rw_k=256, rw_v=512, rw_wa=768, rw_g=896, gd_q=1024, gd_k=1280, gd_v=1536, gd_zg=1792,
          gd_ab=2048, hy=2176, s5=2944)


class Prog:
    def __init__(self, nc, n_dma_sems=(('sp', 10), ('pool', 8), ('act', 4))):
        self.nc = nc
        self.E = {'pe': nc.tensor, 'dve': nc.vector, 'act': nc.scalar, 'pool': nc.gpsimd, 'sp': nc.sync}
        self.es = contextlib.ExitStack()
        self.dma_sems, self.dma_pool, self.dma_next = [], {}, {}
        for q, n in n_dma_sems:
            self.dma_pool[q] = list(range(len(self.dma_sems), len(self.dma_sems) + n))
            self.dma_next[q] = 0
            self.dma_sems += [self.es.enter_context(nc.semaphore(f"dq_{q}{i}")) for i in range(n)]
        self.dma_val = [0] * len(self.dma_sems)
        self.sem, self.cnt, self.ins, self.inc_idx, self.inc_cnt = {}, {}, {}, {}, {}
        self.nsem = 0
        for e in self.E:
            self._new_sem(e)
        self.waited, self.lastw, self.readers = {}, {}, {}

    def _new_sem(self, e):
        self.sem[e] = self.es.enter_context(self.nc.semaphore(f"es_{e}_{self.nsem}"))
        self.nsem += 1
        self.cnt[e] = 0
        self.ins[e] = []
        self.inc_idx[e] = []
        self.inc_cnt[e] = []

    def _eng_count(self, e, idx):
        lst = self.inc_idx[e]
        j = bisect.bisect_left(lst, idx)
        if j < len(lst):
            return self.inc_cnt[e][j]
        self.cnt[e] += 1
        self.ins[e][idx].then_inc(self.sem[e], 1)
        lst.append(idx)
        self.inc_cnt[e].append(self.cnt[e])
        return self.cnt[e]

    def _need(self, waiter, tok):
        if tok[0] == 'eng':
            _, e, idx, sem_id = tok
            if (e == waiter and e == 'pe') or sem_id != id(self.sem[e]):
                return None
            c = self._eng_count(e, idx)
            key = (waiter, 'eng', e, sem_id)
            if self.waited.get(key, 0) >= c:
                return None
            self.waited[key] = c
            return (self.sem[e], c)
        _, k, v = tok
        key = (waiter, 'dma', k)
        if self.waited.get(key, 0) >= v:
            return None
        self.waited[key] = v
        return (self.dma_sems[k], v)

    def _wait(self, waiter, tok):
        n = self._need(waiter, tok)
        if n is not None:
            self.E[waiter].wait_ge(n[0], n[1])

    def _waits(self, eng, deps):
        needs = [n for n in (self._need(eng, d) for d in self._order(deps)) if n is not None]
        for n in needs[:-1]:
            self.E[eng].wait_ge(n[0], n[1])
        return needs[-1] if needs else None

    def _deps(self, reads, writes):
        deps = []
        for k in reads:
            t = self.lastw.get(k)
            if t is not None:
                deps.append(t)
        for k in writes:
            t = self.lastw.get(k)
            if t is not None:
                deps.append(t)
            rd = self.readers.get(k)
            if rd:
                deps.extend(rd.values())
        return deps

    def _register(self, tok, reads, writes):
        for k in reads:
            self.readers.setdefault(k, {})[(tok[0], tok[1])] = tok
        for k in writes:
            self.lastw[k] = tok
            self.readers[k] = {}

    @staticmethod
    def _order(deps):
        return sorted(set(deps), key=lambda t: -t[2])

    def stage(self, eng, items):
        deps = []
        for (_, r, w) in items:
            deps.extend(self._deps(r, w))
        last = self._waits(eng, deps)
        for i, (fn, r, w) in enumerate(items):
            ins = fn(self.E[eng])
            if i == 0 and last is not None:
                ins._wait_ge(last[0], last[1])
            self.ins[eng].append(ins)
            self._register(('eng', eng, len(self.ins[eng]) - 1, id(self.sem[eng])), r, w)

    def op(self, eng, fn, r=(), w=()):
        last = self._waits(eng, self._deps(r, w))
        ins = fn(self.E[eng])
        if last is not None:
            ins._wait_ge(last[0], last[1])
        self.ins[eng].append(ins)
        tok = ('eng', eng, len(self.ins[eng]) - 1, id(self.sem[eng]))
        self._register(tok, r, w)
        return tok

    def dma(self, q, out, in_, r=(), w=(), **kw):
        for d in self._order(self._deps(r, w)):
            self._wait(q, d)
        k = self.dma_pool[q][self.dma_next[q]]
        self.dma_next[q] = (self.dma_next[q] + 1) % len(self.dma_pool[q])
        if self.dma_val[k] > 0:
            self._wait(q, ('dma', k, self.dma_val[k]))
        self.E[q].dma_start(out=out, in_=in_, **kw).then_inc(self.dma_sems[k], 16)
        self.dma_val[k] += 16
        tok = ('dma', k, self.dma_val[k])
        self._register(tok, r, w)
        return tok

    def barrier(self):
        toks = []
        for e in self.E:
            if self.ins[e]:
                toks.append(('eng', e, len(self.ins[e]) - 1, id(self.sem[e])))
        for k, v in enumerate(self.dma_val):
            if v > 0:
                toks.append(('dma', k, v))
        for waiter in self.E:
            for t in toks:
                self._wait(waiter, t)
        self.lastw, self.readers = {}, {}
        for e in self.E:
            if self.cnt[e] > 20000:
                self._new_sem(e)

    def finish(self):
        self.barrier()
        self.es.close()


class Ctx:
    pass


_UID = [0]


@contextlib.contextmanager
def _scope(K):
    with contextlib.ExitStack() as es2:
        yield es2
        K.p.barrier()


def _sb(nc, name, shape, dt):
    _UID[0] += 1
    return nc.sbuf_tensor(f"{name}_u{_UID[0]}", shape, dt)


def _copy_any(p, i, out, in_, r, w):
    if i % 2 == 0:
        p.op('act', lambda e: e.copy(out=out, in_=in_), r=r, w=w)
    else:
        p.op('dve', lambda e: e.tensor_copy(out=out, in_=in_), r=r, w=w)


def phase_adaln(K, li):
    p, nc = K.p, K.nc
    with contextlib.ExitStack() as es:
        c2 = es.enter_context(_sb(nc, "c2", [128, 8, 2], F32))
        cs = es.enter_context(_sb(nc, "cs", [128, 8, 2], F32))
        abT = es.enter_context(_sb(nc, "abT", [128, 48], F32))
        abrow = es.enter_context(_sb(nc, "abrow", [128, 2, 1024], F32))
        wblk = [es.enter_context(_sb(nc, f"adaw{i}", [128, 8, 1024], F32)) for i in range(2)]
        p.dma('sp', c2[:], K.inp["c2T"][:, :, :], w=["c2"])
        p.dma('sp', abT[:], K.inp["ada_bT"][li], w=["abT"])
        for gi, col in enumerate((2048, 5120)):
            p.dma('sp', abrow[:, gi, :], K.inp["ada_b"][li, col:col + 1024].partition_broadcast(128),
                  w=[f"abrow{gi}"])
        p.op('act', lambda e: e.activation(out=cs[:], in_=c2[:], func=AF.Sigmoid), r=["c2"], w=["cs"])
        p.op('dve', lambda e: e.tensor_tensor(out=cs[:], in0=cs[:], in1=c2[:], op=ALU.mult), r=["cs", "c2"],
             w=["cs"])
        aw = K.inp["ada_w"][li].rearrange("(k q) n -> q k n", q=128)
        for blk in range(6):
            wb = wblk[blk % 2]
            wk = f"adaw{blk % 2}"
            p.dma('sp', wb[:], aw[:, :, blk * 1024:(blk + 1) * 1024], w=[wk])
            for jj in range(8):
                j = blk * 8 + jj
                ps = K.ps[j % 2]
                for k in range(8):
                    p.op('pe', lambda e, k=k, jj=jj, ps=ps: e.matmul(
                        ps[:, 0:2], lhsT=wb[:, k, jj * 128:(jj + 1) * 128], rhs=cs[:, k, :],
                        start=(k == 0), stop=(k == 7)), r=[wk, "cs"], w=[f"ps{j % 2}"])
                addc = 1.0 if blk in (1, 4) else 0.0
                p.op('dve', lambda e, j=j, ps=ps, addc=addc: e.tensor_scalar(
                    out=K.modT[:, j, :], in0=ps[:, 0:2], scalar1=abT[:, j:j + 1], scalar2=addc, op0=ALU.add,
                    op1=ALU.add), r=[f"ps{j % 2}", "abT"], w=["modT"])
            if blk in (2, 5):
                gi = 0 if blk == 2 else 1
                for which in range(2):
                    for half in range(2):
                        ps = K.ps[2 + half]
                        for k in range(8):
                            p.op('pe', lambda e, k=k, ps=ps, half=half, which=which: e.matmul(
                                ps[:, :], lhsT=cs[:, k, which:which + 1].to_broadcast([128, 128]),
                                rhs=wb[:, k, half * 512:(half + 1) * 512], start=(k == 0), stop=(k == 7)),
                                r=[wk, "cs"], w=[f"ps{2 + half}"])
                        g = K.gate[(which, gi)]
                        p.op('dve', lambda e, ps=ps, g=g, half=half, gi=gi: e.tensor_tensor(
                            out=g[:, half * 512:(half + 1) * 512], in0=ps[:, :],
                            in1=abrow[:, gi, half * 512:(half + 1) * 512], op=ALU.add),
                            r=[f"ps{2 + half}", f"abrow{gi}"], w=[f"gate{which}{gi}"])
        p.barrier()


def phase_inproj(K, li):
    p, nc = K.p, K.nc
    with contextlib.ExitStack() as es:
        win = es.enter_context(_sb(nc, "win", [128, 8, ZROWS], BF16))
        wst = [es.enter_context(_sb(nc, f"wst{i}", [128, ZROWS], F32)) for i in range(2)]
        xt = [es.enter_context(_sb(nc, f"xt{i}", [128, 1024], F32)) for i in range(2)]
        xmT = [es.enter_context(_sb(nc, f"xmT{i}", [128, 8, 512], BF16)) for i in range(2)]
        zst = [es.enter_context(_sb(nc, f"zst{i}", [128, 512], F32)) for i in range(4)]
        wsrc = K.inp["w_in_p"][li].rearrange("(k q) n -> q k n", q=128)
        for k in range(8):
            p.dma('sp', wst[k % 2][:], wsrc[:, k, :], w=[f"wst{k % 2}"])
            _copy_any(p, k, win[:, k, :], wst[k % 2][:], r=[f"wst{k % 2}"], w=["win"])
        groups = [(0, 2)] + [(2 + 4 * g, 4) for g in range(8)]
        nst = 0
        ntile = 0
        for gidx, (t0, ntl) in enumerate(groups):
            which = 1 if gidx == 0 else 0
            xm = xmT[gidx % 2]
            xmk = f"xmT{gidx % 2}"
            for tl in range(ntl):
                tt = t0 + tl
                xb = xt[ntile % 2]
                xk = f"xt{ntile % 2}"
                ntile += 1
                p.dma('sp', xb[:], K.xres[tt * 128:(tt + 1) * 128, :], r=["xres"], w=[xk])
                for k in range(8):
                    bank = 4 + 2 * (ntile % 2) + (k // 4)
                    p.op('pe', lambda e, k=k, bank=bank, xb=xb: e.transpose(
                        out=K.ps[bank][:, (k % 4) * 128:(k % 4 + 1) * 128], in_=xb[:, k * 128:(k + 1) * 128],
                        identity=K.ident[:]), r=[xk, "ident"], w=[f"ps{bank}"])
                for k in range(8):
                    bank = 4 + 2 * (ntile % 2) + (k // 4)
                    p.op('act', lambda e, k=k, bank=bank, xm=xm, tl=tl, which=which: e.activation(
                        out=xm[:, k, tl * 128:(tl + 1) * 128], in_=K.ps[bank][:, (k % 4) * 128:(k % 4 + 1) * 128],
                        func=AF.Identity, scale=K.modT[:, 8 + k, which:which + 1],
                        bias=K.modT[:, k, which:which + 1]), r=[f"ps{bank}", "modT"], w=[xmk])
            ntok = ntl * 128
            for oc in range(ZROWS // 128):
                bank = oc % 4
                for k in range(8):
                    p.op('pe', lambda e, k=k, bank=bank, oc=oc, xm=xm, ntok=ntok: e.matmul(
                        K.ps[bank][:, 0:ntok], lhsT=win[:, k, oc * 128:(oc + 1) * 128], rhs=xm[:, k, 0:ntok],
                        start=(k == 0), stop=(k == 7)), r=["win", xmk], w=[f"ps{bank}"])
                zs = zst[nst % 4]
                zk = f"zst{nst % 4}"
                _copy_any(p, nst, zs[:, 0:ntok], K.ps[bank][:, 0:ntok], r=[f"ps{bank}"], w=[zk])
                p.dma('pool', K.zfm[oc * 128:(oc + 1) * 128, t0 * 128:t0 * 128 + ntok], zs[:, 0:ntok], r=[zk],
                      w=["zfm"])
                nst += 1
        p.barrier()


def _load_bcast(K, es, name, src_row, n):
    t = es.enter_context(_sb(K.nc, name, [128, n], F32))
    K.p.dma('sp', t[:], src_row.partition_broadcast(128), w=[name])
    return t


def _ln_tile(K, xt, xk, stat, sk, sq, sqk, g, gk, b, bk):
    p = K.p
    p.op('act', lambda e: e.activation(out=sq[:], in_=xt[:], func=AF.Square, accum_out=stat[:, 1:2]),
         r=[xk], w=[sqk, sk + "q"])
    p.op('dve', lambda e: e.tensor_scalar(out=stat[:, 2:3], in0=stat[:, 0:1], scalar1=1.0 / D, scalar2=None,
                                          op0=ALU.mult), r=[sk], w=[sk + "m"])
    p.op('dve', lambda e: e.tensor_tensor(out=stat[:, 3:4], in0=stat[:, 2:3], in1=stat[:, 2:3], op=ALU.mult),
         r=[sk + "m"], w=[sk + "v"])
    p.op('dve', lambda e: e.scalar_tensor_tensor(out=stat[:, 3:4], in0=stat[:, 1:2], scalar=1.0 / D,
                                                 in1=stat[:, 3:4], op0=ALU.mult, op1=ALU.subtract),
         r=[sk + "q", sk + "v"], w=[sk + "v"])
    p.op('dve', lambda e: e.tensor_scalar(out=stat[:, 3:4], in0=stat[:, 3:4], scalar1=1e-5, scalar2=None,
                                          op0=ALU.add), r=[sk + "v"], w=[sk + "v"])
    p.op('act', lambda e: e.activation(out=stat[:, 4:5], in_=stat[:, 3:4], func=AF.Sqrt), r=[sk + "v"],
         w=[sk + "r"])
    p.op('dve', lambda e: e.reciprocal(out=stat[:, 4:5], in_=stat[:, 4:5]), r=[sk + "r"], w=[sk + "r"])
    p.op('dve', lambda e: e.tensor_scalar(out=xt[:], in0=xt[:], scalar1=stat[:, 2:3], scalar2=stat[:, 4:5],
                                          op0=ALU.subtract, op1=ALU.mult), r=[xk, sk + "m", sk + "r"], w=[xk])
    p.op('dve', lambda e: e.tensor_tensor(out=xt[:], in0=xt[:], in1=g[:], op=ALU.mult), r=[xk, gk], w=[xk])
    p.op('dve', lambda e: e.tensor_tensor(out=xt[:], in0=xt[:], in1=b[:], op=ALU.add), r=[xk, bk], w=[xk])


def phase_outproj(K, li):
    p, nc = K.p, K.nc
    need_ctx = li < DEPTH - 1
    with contextlib.ExitStack() as es:
        wo = es.enter_context(_sb(nc, "wo", [128, 8, 1024], BF16))
        wst = [es.enter_context(_sb(nc, f"wost{i}", [128, 1024], F32)) for i in range(2)]
        lng = _load_bcast(K, es, "lng", K.inp["ln_g"][li, 0], D)
        lnb = _load_bcast(K, es, "lnb", K.inp["ln_b"][li, 0], D)
        mst = [es.enter_context(_sb(nc, f"mst{i}", [128, 8, 128], F32)) for i in range(2)]
        mbf = [es.enter_context(_sb(nc, f"mbf{i}", [128, 8, 128], BF16)) for i in range(2)]
        xt = [es.enter_context(_sb(nc, f"xo{i}", [128, 1024], F32)) for i in range(2)]
        t1 = [es.enter_context(_sb(nc, f"t1_{i}", [128, 1024], F32)) for i in range(2)]
        sq = es.enter_context(_sb(nc, "sqo", [128, 1024], F32))
        stat = [es.enter_context(_sb(nc, f"st{i}", [128, 8], F32)) for i in range(2)]
        wsrc = K.inp["w_out"][li].rearrange("(k q) n -> q k n", q=128)
        for k in range(8):
            p.dma('sp', wst[k % 2][:], wsrc[:, k, :], w=[f"wost{k % 2}"])
            _copy_any(p, k, wo[:, k, :], wst[k % 2][:], r=[f"wost{k % 2}"], w=["wo"])
        msrc = K.mix.rearrange("(k q) t -> q k t", q=128)
        tiles = list(range(0 if need_ctx else 2, NT))
        for n, tt in enumerate(tiles):
            i = n % 2
            which = 1 if tt < 2 else 0
            p.dma('sp', mst[i][:], msrc[:, :, tt * 128:(tt + 1) * 128], r=["mix"], w=[f"mst{i}"])
            p.dma('sp', xt[i][:], K.xres[tt * 128:(tt + 1) * 128, :], r=["xres"], w=[f"xo{i}"])
            _copy_any(p, n, mbf[i][:], mst[i][:], r=[f"mst{i}"], w=[f"mbf{i}"])
            for half in range(2):
                bank = 2 * i + half
                for k in range(8):
                    p.op('pe', lambda e: e.matmul(K.ps[bank][:, :], lhsT=mbf[i][:, k, :],
                                                  rhs=wo[:, k, half * 512:(half + 1) * 512], start=(k == 0),
                                                  stop=(k == 7)), r=[f"mbf{i}", "wo"], w=[f"ps{bank}"])
                p.op('dve', lambda e: e.tensor_tensor(out=t1[i][:, half * 512:(half + 1) * 512],
                                                      in0=K.ps[bank][:, :],
                                                      in1=K.gate[(which, 0)][:, half * 512:(half + 1) * 512],
                                                      op=ALU.mult), r=[f"ps{bank}", f"gate{which}0"], w=[f"t1_{i}"])
            p.op('dve', lambda e: e.scalar_tensor_tensor(out=xt[i][:], in0=xt[i][:], scalar=ALPHA, in1=t1[i][:],
                                                         op0=ALU.mult, op1=ALU.add, accum_out=stat[i][:, 0:1]),
                 r=[f"xo{i}", f"t1_{i}"], w=[f"xo{i}", f"st{i}"])
            _ln_tile(K, xt[i], f"xo{i}", stat[i], f"st{i}", sq, "sqo", lng, "lng", lnb, "lnb")
            p.dma('pool', K.xres[tt * 128:(tt + 1) * 128, :], xt[i][:], r=[f"xo{i}"], w=["xres"])
        p.barrier()


def phase_router(K, li):
    p, nc = K.p, K.nc
    need_ctx = li < DEPTH - 1
    BIG = 1.0e30
    with contextlib.ExitStack() as es:
        rw_ = es.enter_context(_sb(nc, "rtw", [128, 8, 64], F32))
        rb = _load_bcast(K, es, "rtb", K.inp["router_b"][li], 64)
        xt = [es.enter_context(_sb(nc, f"xr{i}", [128, 1024], F32)) for i in range(2)]
        hT = [es.enter_context(_sb(nc, f"hT{i}", [128, 8, 128], F32)) for i in range(2)]
        hb = [es.enter_context(_sb(nc, f"hb{i}", [128, 8, 128], BF16)) for i in range(2)]
        sc = es.enter_context(_sb(nc, "rsc", [128, 64], F32))
        bi = es.enter_context(_sb(nc, "rbi", [128, 64], F32))
        b2 = es.enter_context(_sb(nc, "rb2", [128, 64], F32))
        mk = es.enter_context(_sb(nc, "rmk", [128, 64], F32))
        g1 = es.enter_context(_sb(nc, "rg1", [128, 8], F32))
        g2 = es.enter_context(_sb(nc, "rg2", [128, 8], F32))
        gm = es.enter_context(_sb(nc, "rgm", [128, 8], F32))
        m8 = es.enter_context(_sb(nc, "rm8", [128, 8], F32))
        ssum = es.enter_context(_sb(nc, "rss", [128, 2], F32))
        p.dma('sp', rw_[:], K.inp["router_w"][li].rearrange("(k q) n -> q k n", q=128), w=["rtw"])
        tiles = list(range(0 if need_ctx else 2, NT))
        for n, tt in enumerate(tiles):
            i = n % 2
            which = 1 if tt < 2 else 0
            p.dma('sp', xt[i][:], K.xres[tt * 128:(tt + 1) * 128, :], r=["xres"], w=[f"xr{i}"])
            for k in range(8):
                bank = 4 * i + (k // 4)
                p.op('pe', lambda e: e.transpose(out=K.ps[bank][:, (k % 4) * 128:(k % 4 + 1) * 128],
                                                 in_=xt[i][:, k * 128:(k + 1) * 128], identity=K.ident[:]),
                     r=[f"xr{i}", "ident"], w=[f"ps{bank}"])
            for k in range(8):
                bank = 4 * i + (k // 4)
                p.op('act', lambda e: e.activation(out=hT[i][:, k, :],
                                                   in_=K.ps[bank][:, (k % 4) * 128:(k % 4 + 1) * 128],
                                                   func=AF.Identity, scale=K.modT[:, 32 + k, which:which + 1],
                                                   bias=K.modT[:, 24 + k, which:which + 1]),
                     r=[f"ps{bank}", "modT"], w=[f"hT{i}"])
            p.op('dve', lambda e: e.tensor_copy(out=hb[i][:], in_=hT[i][:]), r=[f"hT{i}"], w=[f"hb{i}"])
            p.dma('pool', K.hT.rearrange("(k q) t -> q k t", q=128)[:, :, tt * 128:(tt + 1) * 128], hb[i][:],
                  r=[f"hb{i}"], w=["hTd"])
            bank = 4 * i + 2
            for k in range(8):
                p.op('pe', lambda e: e.matmul(K.ps[bank][:, 0:64], lhsT=hT[i][:, k, :], rhs=rw_[:, k, :],
                                              start=(k == 0), stop=(k == 7)), r=[f"hT{i}", "rtw"], w=[f"ps{bank}"])
            p.op('act', lambda e: e.activation(out=sc[:], in_=K.ps[bank][:, 0:64], func=AF.Sigmoid),
                 r=[f"ps{bank}"], w=["rsc"])
            V = lambda fn, r, w: p.op('dve', fn, r=r, w=w)
            V(lambda e: e.tensor_tensor(out=bi[:], in0=sc[:], in1=rb[:], op=ALU.add), ["rsc", "rtb"], ["rbi"])
            bi3 = bi[:].rearrange("q (g j) -> q g j", j=8)
            b23 = b2[:].rearrange("q (g j) -> q g j", j=8)
            mk3 = mk[:].rearrange("q (g j) -> q g j", j=8)
            V(lambda e: e.tensor_reduce(out=g1[:], in_=bi3, axis=AX.X, op=ALU.max), ["rbi"], ["rg1"])
            V(lambda e: e.tensor_tensor(out=mk3, in0=bi3, in1=g1[:].unsqueeze(2).to_broadcast([128, 8, 8]),
                                        op=ALU.is_equal), ["rbi", "rg1"], ["rmk"])
            V(lambda e: e.scalar_tensor_tensor(out=b2[:], in0=mk[:], scalar=-BIG, in1=bi[:], op0=ALU.mult,
                                               op1=ALU.add), ["rmk", "rbi"], ["rb2"])
            V(lambda e: e.tensor_reduce(out=g2[:], in_=b23, axis=AX.X, op=ALU.max), ["rb2"], ["rg2"])
            V(lambda e: e.tensor_tensor(out=g1[:], in0=g1[:], in1=g2[:], op=ALU.add), ["rg1", "rg2"], ["rg1"])
            V(lambda e: e.max(out=m8[:], in_=g1[:]), ["rg1"], ["rm8"])
            V(lambda e: e.tensor_scalar(out=gm[:], in0=g1[:], scalar1=m8[:, 3:4], scalar2=None, op0=ALU.is_ge),
              ["rg1", "rm8"], ["rgm"])
            V(lambda e: e.tensor_tensor(out=b23, in0=bi3, in1=gm[:].unsqueeze(2).to_broadcast([128, 8, 8]),
                                        op=ALU.mult), ["rbi", "rgm"], ["rb2"])
            V(lambda e: e.tensor_scalar(out=gm[:], in0=gm[:], scalar1=BIG, scalar2=BIG, op0=ALU.mult,
                                        op1=ALU.subtract), ["rgm"], ["rgm"])
            V(lambda e: e.tensor_tensor(out=b23, in0=b23, in1=gm[:].unsqueeze(2).to_broadcast([128, 8, 8]),
                                        op=ALU.add), ["rb2", "rgm"], ["rb2"])
            V(lambda e: e.max(out=m8[:], in_=b2[:]), ["rb2"], ["rm8"])
            V(lambda e: e.tensor_scalar(out=mk[:], in0=b2[:], scalar1=m8[:, 7:8], scalar2=None, op0=ALU.is_ge),
              ["rb2", "rm8"], ["rmk"])
            V(lambda e: e.scalar_tensor_tensor(out=mk[:], in0=sc[:], scalar=1.0, in1=mk[:], op0=ALU.mult,
                                               op1=ALU.mult, accum_out=ssum[:, 0:1]), ["rsc", "rmk"],
              ["rmk", "rss"])
            V(lambda e: e.reciprocal(out=ssum[:, 1:2], in_=ssum[:, 0:1]), ["rss"], ["rss"])
            V(lambda e: e.tensor_scalar(out=K.gates[:, tt, :], in0=mk[:], scalar1=ssum[:, 1:2], scalar2=2.5,
                                        op0=ALU.mult, op1=ALU.mult), ["rmk", "rss"], ["gates"])
        p.barrier()


def phase_experts(K, li):
    p, nc = K.p, K.nc
    need_ctx = li < DEPTH - 1
    t_lo = 0 if need_ctx else 2
    parts = [(t_lo, 17), (17, NT)]
    with contextlib.ExitStack() as es:
        NTP = 17
        hT = es.enter_context(_sb(nc, "ehT", [128, 8, NTP * 128], BF16))
        acc = es.enter_context(_sb(nc, "eacc", [128, NTP, 1024], F32))
        w13 = [es.enter_context(_sb(nc, f"ew13_{i}", [128, 2, 8, 256], BF16)) for i in range(2)]
        w2 = [es.enter_context(_sb(nc, f"ew2_{i}", [128, 2, 1024], BF16)) for i in range(2)]
        g = [es.enter_context(_sb(nc, f"eg{i}", [128, 2, NTP * 128], BF16)) for i in range(2)]
        sa = [es.enter_context(_sb(nc, f"esa{i}", [128, 512], F32)) for i in range(2)]
        lng = _load_bcast(K, es, "lng2", K.inp["ln_g"][li, 1], D)
        lnb = _load_bcast(K, es, "lnb2", K.inp["ln_b"][li, 1], D)
        xt = [es.enter_context(_sb(nc, f"xe{i}", [128, 1024], F32)) for i in range(2)]
        sq = es.enter_context(_sb(nc, "sqe", [128, 1024], F32))
        stat = [es.enter_context(_sb(nc, f"ste{i}", [128, 8], F32)) for i in range(2)]
        hsrc = K.hT.rearrange("(k q) t -> q k t", q=128)
        nsa = 0
        for (ta, tb) in parts:
            ntl = tb - ta
            ntok = ntl * 128
            p.dma('sp', hT[:, :, 0:ntok], hsrc[:, :, ta * 128:tb * 128], r=["hTd"], w=["ehT"])
            for ei in range(65):
                i = ei % 2
                if ei < 64:
                    s1, s3, s2 = K.inp["ex_w1"][li, ei], K.inp["ex_w3"][li, ei], K.inp["ex_w2"][li, ei]
                else:
                    s1, s3, s2 = K.inp["sh_w1"][li], K.inp["sh_w3"][li], K.inp["sh_w2"][li]
                p.dma('pool', w13[i][:, 0], s1.rearrange("(k q) f -> q k f", q=128), w=[f"ew13_{i}"])
                p.dma('pool', w13[i][:, 1], s3.rearrange("(k q) f -> q k f", q=128), w=[f"ew13_{i}"])
                p.dma('pool', w2[i][:], s2.rearrange("(c q) n -> q c n", q=128), w=[f"ew2_{i}"])
                blocks = [(b0, min(512, ntok - b0)) for b0 in range(0, ntok, 512)]
                for fc in range(2):
                    for (b0, bn) in blocks:
                        for ab in range(2):
                            bank = 2 * (nsa % 2) + ab
                            for k in range(8):
                                p.op('pe', lambda e: e.matmul(
                                    K.ps[bank][:, 0:bn], lhsT=w13[i][:, ab, k, fc * 128:(fc + 1) * 128],
                                    rhs=hT[:, k, b0:b0 + bn], start=(k == 0), stop=(k == 7)),
                                    r=[f"ew13_{i}", "ehT"], w=[f"ps{bank}"])
                        ba = 2 * (nsa % 2)
                        sab = sa[nsa % 2]
                        p.op('act', lambda e: e.activation(out=sab[:, 0:bn], in_=K.ps[ba][:, 0:bn], func=AF.Silu),
                             r=[f"ps{ba}"], w=[f"esa{nsa % 2}"])
                        p.op('dve', lambda e: e.tensor_tensor(out=g[i][:, fc, b0:b0 + bn], in0=sab[:, 0:bn],
                                                              in1=K.ps[ba + 1][:, 0:bn], op=ALU.mult),
                             r=[f"esa{nsa % 2}", f"ps{ba + 1}"], w=[f"eg{i}"])
                        nsa += 1
                for tl in range(ntl):
                    tt = ta + tl
                    for half in range(2):
                        bank = 4 + 2 * (tl % 2) + half
                        for fc in range(2):
                            p.op('pe', lambda e: e.matmul(
                                K.ps[bank][:, :], lhsT=g[i][:, fc, tl * 128:(tl + 1) * 128],
                                rhs=w2[i][:, fc, half * 512:(half + 1) * 512], start=(fc == 0), stop=(fc == 1)),
                                r=[f"eg{i}", f"ew2_{i}"], w=[f"ps{bank}"])
                        a_out = acc[:, tl, half * 512:(half + 1) * 512]
                        if ei == 0:
                            p.op('dve', lambda e: e.tensor_scalar(out=a_out, in0=K.ps[bank][:, :],
                                                                  scalar1=K.gates[:, tt, 0:1], scalar2=None,
                                                                  op0=ALU.mult), r=[f"ps{bank}", "gates"],
                                 w=[f"eacc{tl}"])
                        else:
                            scal = K.gates[:, tt, ei:ei + 1] if ei < 64 else 1.0
                            p.op('dve', lambda e: e.scalar_tensor_tensor(out=a_out, in0=K.ps[bank][:, :],
                                                                         scalar=scal, in1=a_out, op0=ALU.mult,
                                                                         op1=ALU.add),
                                 r=[f"ps{bank}", "gates", f"eacc{tl}"], w=[f"eacc{tl}"])
            for tl in range(ntl):
                tt = ta + tl
                i = tl % 2
                which = 1 if tt < 2 else 0
                p.dma('sp', xt[i][:], K.xres[tt * 128:(tt + 1) * 128, :], r=["xres"], w=[f"xe{i}"])
                p.op('dve', lambda e: e.tensor_tensor(out=acc[:, tl, :], in0=acc[:, tl, :],
                                                      in1=K.gate[(which, 1)][:], op=ALU.mult),
                     r=[f"eacc{tl}", f"gate{which}1"], w=[f"eacc{tl}"])
                p.op('dve', lambda e: e.scalar_tensor_tensor(out=xt[i][:], in0=xt[i][:], scalar=ALPHA,
                                                             in1=acc[:, tl, :], op0=ALU.mult, op1=ALU.add,
                                                             accum_out=stat[i][:, 0:1]),
                     r=[f"xe{i}", f"eacc{tl}"], w=[f"xe{i}", f"ste{i}"])
                _ln_tile(K, xt[i], f"xe{i}", stat[i], f"ste{i}", sq, "sqe", lng, "lng2", lnb, "lnb2")
                if li == DEPTH - 1:
                    p.dma('pool', K.out[(tt - 2) * 128:(tt - 1) * 128, :], xt[i][:], r=[f"xe{i}"], w=["out"])
                else:
                    p.dma('pool', K.xres[tt * 128:(tt + 1) * 128, :], xt[i][:], r=[f"xe{i}"], w=["xres"])
        p.barrier()


TWO_PI = 2.0 * math.pi
MAGIC = 12582912.0
PI_LO = 3.1415925


def _sin(K, out, x, shift, tmp, keys_r, key_w, key_t):
    p = K.p
    p.op('dve', lambda e: e.tensor_scalar(out=out, in0=x, scalar1=shift, scalar2=None, op0=ALU.add), r=keys_r,
         w=[key_w])
    p.op('dve', lambda e: e.tensor_scalar(out=tmp, in0=out, scalar1=1.0 / TWO_PI, scalar2=MAGIC, op0=ALU.mult,
                                          op1=ALU.add), r=[key_w], w=[key_t])
    p.op('dve', lambda e: e.tensor_scalar(out=tmp, in0=tmp, scalar1=-MAGIC, scalar2=-TWO_PI, op0=ALU.add,
                                          op1=ALU.mult), r=[key_t], w=[key_t])
    p.op('dve', lambda e: e.tensor_tensor(out=tmp, in0=tmp, in1=out, op=ALU.add), r=[key_t, key_w], w=[key_t])
    p.op('dve', lambda e: e.tensor_scalar(out=tmp, in0=tmp, scalar1=PI_LO, scalar2=-PI_LO, op0=ALU.min,
                                          op1=ALU.max), r=[key_t], w=[key_t])
    p.op('act', lambda e: e.activation(out=out, in_=tmp, func=AF.Sin), r=[key_t], w=[key_w])


S5_BLOCKS0 = [(0, 256)] + [(256 + 512 * b, 512) for b in range(8)]
NDBL = 13
S5_Q = 16
S5_LQ = 4
S5_NCH = T // S5_Q
S5_LC = 9


def phase_s5(K, li):
    p, nc = K.p, K.nc
    V = lambda fn, r, w: p.op('dve', fn, r=r, w=w)
    with contextlib.ExitStack() as es:
        SB = lambda name, shape, dt=F32: es.enter_context(_sb(nc, name, shape, dt))
        lre, lim, dtt = SB("s5lre", [128, 16]), SB("s5lim", [128, 16]), SB("s5dt", [128, 16])
        ar, ai, mag = SB("s5ar", [128, 16]), SB("s5ai", [128, 16]), SB("s5mag", [128, 16])
        sn, cs_, tmp = SB("s5sn", [128, 16]), SB("s5cs", [128, 16]), SB("s5tmp", [128, 16])
        den, t2 = SB("s5den", [128, 16]), SB("s5t2", [128, 16])
        co_re, co_im, co_imn = SB("s5core", [128, 16]), SB("s5coim", [128, 16]), SB("s5coimn", [128, 16])
        pw_re, pw_im, pw_imn = (SB("s5pwre", [128, NDBL, 16]), SB("s5pwim", [128, NDBL, 16]),
                                SB("s5pwimn", [128, NDBL, 16]))
        dsk, glb = SB("s5d", [128, 2]), SB("s5glb", [128, 2])
        gluw = SB("s5gluw", [128, 2, 256], BF16)
        p.dma('sp', lre[:], K.inp["s5_lreT"][li], w=["lre"])
        p.dma('sp', lim[:], K.inp["s5_limT"][li], w=["lim"])
        p.dma('sp', dtt[:], K.inp["s5_dtT"][li], w=["dtt"])
        p.dma('sp', dsk[:], K.inp["s5_dT"][li], w=["dsk"])
        p.dma('sp', glb[:], K.inp["s5_glbT"][li], w=["glb"])
        p.dma('pool', gluw[:], K.inp["s5_glu_w"][li].rearrange("(c q) n -> q c n", q=128), w=["gluw"])
        p.op('act', lambda e: e.activation(out=dtt[:], in_=dtt[:], func=AF.Exp), r=["dtt"], w=["dtt"])
        V(lambda e: e.tensor_tensor(out=ar[:], in0=lre[:], in1=dtt[:], op=ALU.mult), ["lre", "dtt"], ["ar"])
        V(lambda e: e.tensor_tensor(out=ai[:], in0=lim[:], in1=dtt[:], op=ALU.mult), ["lim", "dtt"], ["ai"])
        p.op('act', lambda e: e.activation(out=mag[:], in_=ar[:], func=AF.Exp), r=["ar"], w=["mag"])
        _sin(K, sn[:], ai[:], 0.0, tmp[:], ["ai"], "sn", "s5tmp")
        _sin(K, cs_[:], ai[:], math.pi / 2.0, tmp[:], ["ai"], "cs", "s5tmp")
        V(lambda e: e.tensor_tensor(out=pw_re[:, 0, :], in0=mag[:], in1=cs_[:], op=ALU.mult), ["mag", "cs"], ["pw"])
        V(lambda e: e.tensor_tensor(out=pw_im[:, 0, :], in0=mag[:], in1=sn[:], op=ALU.mult), ["mag", "sn"], ["pw"])
        V(lambda e: e.tensor_tensor(out=den[:], in0=lre[:], in1=lre[:], op=ALU.mult), ["lre"], ["den"])
        V(lambda e: e.tensor_tensor(out=t2[:], in0=lim[:], in1=lim[:], op=ALU.mult), ["lim"], ["t2"])
        V(lambda e: e.tensor_tensor(out=den[:], in0=den[:], in1=t2[:], op=ALU.add), ["den", "t2"], ["den"])
        V(lambda e: e.reciprocal(out=den[:], in_=den[:]), ["den"], ["den"])
        V(lambda e: e.tensor_scalar(out=tmp[:], in0=pw_re[:, 0, :], scalar1=-1.0, scalar2=None, op0=ALU.add),
          ["pw"], ["s5tmp"])
        V(lambda e: e.tensor_tensor(out=co_re[:], in0=tmp[:], in1=lre[:], op=ALU.mult), ["s5tmp", "lre"], ["core"])
        V(lambda e: e.tensor_tensor(out=t2[:], in0=pw_im[:, 0, :], in1=lim[:], op=ALU.mult), ["pw", "lim"], ["t2"])
        V(lambda e: e.tensor_tensor(out=co_re[:], in0=co_re[:], in1=t2[:], op=ALU.add), ["core", "t2"], ["core"])
        V(lambda e: e.tensor_tensor(out=co_re[:], in0=co_re[:], in1=den[:], op=ALU.mult), ["core", "den"], ["core"])
        V(lambda e: e.tensor_tensor(out=co_im[:], in0=pw_im[:, 0, :], in1=lre[:], op=ALU.mult), ["pw", "lre"],
          ["coim"])
        V(lambda e: e.tensor_tensor(out=t2[:], in0=tmp[:], in1=lim[:], op=ALU.mult), ["s5tmp", "lim"], ["t2"])
        V(lambda e: e.tensor_tensor(out=co_im[:], in0=co_im[:], in1=t2[:], op=ALU.subtract), ["coim", "t2"],
          ["coim"])
        V(lambda e: e.tensor_tensor(out=co_im[:], in0=co_im[:], in1=den[:], op=ALU.mult), ["coim", "den"], ["coim"])
        V(lambda e: e.tensor_scalar(out=co_imn[:], in0=co_im[:], scalar1=-1.0, scalar2=None, op0=ALU.mult),
          ["coim"], ["coimn"])
        for m in range(1, NDBL):
            V(lambda e: e.tensor_tensor(out=tmp[:], in0=pw_re[:, m - 1, :], in1=pw_re[:, m - 1, :], op=ALU.mult),
              ["pw"], ["s5tmp"])
            V(lambda e: e.tensor_tensor(out=t2[:], in0=pw_im[:, m - 1, :], in1=pw_im[:, m - 1, :], op=ALU.mult),
              ["pw"], ["t2"])
            V(lambda e: e.tensor_tensor(out=pw_re[:, m, :], in0=tmp[:], in1=t2[:], op=ALU.subtract),
              ["s5tmp", "t2"], ["pw"])
            V(lambda e: e.tensor_tensor(out=tmp[:], in0=pw_re[:, m - 1, :], in1=pw_im[:, m - 1, :], op=ALU.mult),
              ["pw"], ["s5tmp"])
            V(lambda e: e.tensor_scalar(out=pw_im[:, m, :], in0=tmp[:], scalar1=2.0, scalar2=None, op0=ALU.mult),
              ["s5tmp"], ["pw"])
        V(lambda e: e.tensor_scalar(out=pw_imn[:], in0=pw_im[:], scalar1=-1.0, scalar2=None, op0=ALU.mult),
          ["pw"], ["pwn"])
        ub = SB("s5ub", [128, 2, T], BF16)
        Y = SB("s5Y", [128, 2, T])
        with _scope(K) as es2:
            ust = es2.enter_context(_sb(nc, "s5ust", [128, T], F32))
            for c in range(2):
                r0 = ZR["s5"] + c * 128
                p.dma('sp', ust[:], K.zfm[r0:r0 + 128, :], r=["zfm"], w=["ust"])
                lat_in = ust[:, LC:T].rearrange("q (r c) -> q c r", c=64)
                lat_ub = ub[:, c, LC:T].rearrange("q (c r) -> q c r", r=64)
                lat_y = Y[:, c, LC:T].rearrange("q (c r) -> q c r", r=64)
                p.op('act', lambda e: e.copy(out=ub[:, c, 0:LC], in_=ust[:, 0:LC]), r=["ust"], w=["ub"])
                p.op('act', lambda e: e.copy(out=lat_ub, in_=lat_in), r=["ust"], w=["ub"])
                V(lambda e: e.tensor_scalar(out=Y[:, c, 0:LC], in0=ust[:, 0:LC], scalar1=dsk[:, c:c + 1],
                                            scalar2=None, op0=ALU.mult), ["ust", "dsk"], [f"Y{c}"])
                V(lambda e: e.tensor_scalar(out=lat_y, in0=lat_in, scalar1=dsk[:, c:c + 1], scalar2=None,
                                            op0=ALU.mult), ["ust", "dsk"], [f"Y{c}"])
        with _scope(K) as es2:
            SB2 = lambda name, shape, dt=F32: es2.enter_context(_sb(nc, name, shape, dt))
            X = [[SB2(f"s5X{a}{ri}", [128, T]) for ri in range(2)] for a in range(2)]
            xb = [SB2(f"s5xb{ri}", [128, T], BF16) for ri in range(2)]
            bc = [SB2(f"s5bc{i}", [128, 4, 128], BF16) for i in range(2)]
            tq = [SB2(f"s5tq{i}", [128, 512]) for i in range(2)]
            Cx = [[SB2(f"s5C{a}{ri}", [128, S5_NCH]) for ri in range(2)] for a in range(2)]
            PT = [SB2(f"s5PT{ri}", [128, S5_Q]) for ri in range(2)]
            ptt = SB2("s5ptt", [128, S5_Q])
            nblk = 0
            for d in range(2):
                for j in range(8):
                    dj = d * 8 + j
                    c = j // 4
                    bi = dj % 2
                    for wi, wn in enumerate(("s5_breT", "s5_bimT", "s5_creT", "s5_cimT")):
                        p.dma('pool', bc[bi][:, wi, :], K.inp[wn][li, dj], w=[f"bc{bi}"])
                    V(lambda e: e.tensor_scalar(out=bc[bi][:, 3, :], in0=bc[bi][:, 3, :], scalar1=-1.0,
                                                scalar2=None, op0=ALU.mult), [f"bc{bi}"], [f"bc{bi}"])
                    if d == 0:
                        blocks = [(s0, n, s0) for (s0, n) in S5_BLOCKS0]
                    else:
                        blocks = [(256 + 512 * b, 512, 512 * b) for b in range(8)] + [(0, 256, LL)]
                    A = X[0]
                    for (s0, n, xp) in blocks:
                        bk = 2 * (nblk % 2)
                        for ri in range(2):
                            p.op('pe', lambda e: e.matmul(K.ps[bk + ri][:, 0:n], lhsT=bc[bi][:, ri, :],
                                                          rhs=ub[:, c, s0:s0 + n], start=True, stop=True),
                                 r=[f"bc{bi}", "ub"], w=[f"ps{bk + ri}"])
                        t_ = tq[nblk % 2]
                        tk = f"tq{nblk % 2}"
                        V(lambda e: e.tensor_scalar(out=t_[:, 0:n], in0=K.ps[bk + 1][:, 0:n],
                                                    scalar1=co_imn[:, dj:dj + 1], scalar2=None, op0=ALU.mult),
                          [f"ps{bk + 1}", "coimn"], [tk])
                        V(lambda e: e.scalar_tensor_tensor(out=A[0][:, xp:xp + n], in0=K.ps[bk][:, 0:n],
                                                           scalar=co_re[:, dj:dj + 1], in1=t_[:, 0:n],
                                                           op0=ALU.mult, op1=ALU.add),
                          [f"ps{bk}", "core", tk], ["X00"])
                        V(lambda e: e.tensor_scalar(out=t_[:, 0:n], in0=K.ps[bk][:, 0:n],
                                                    scalar1=co_im[:, dj:dj + 1], scalar2=None, op0=ALU.mult),
                          [f"ps{bk}", "coim"], [tk])
                        V(lambda e: e.scalar_tensor_tensor(out=A[1][:, xp:xp + n], in0=K.ps[bk + 1][:, 0:n],
                                                           scalar=co_re[:, dj:dj + 1], in1=t_[:, 0:n],
                                                           op0=ALU.mult, op1=ALU.add),
                          [f"ps{bk + 1}", "core", tk], ["X01"])
                        nblk += 1
                    xv = lambda t_: t_[:].rearrange("q (c j) -> q c j", j=S5_Q)
                    for m in range(S5_LQ):
                        sh = 1 << m
                        a, b = m % 2, (m + 1) % 2
                        src, dst = X[a], X[b]
                        sk = [f"X{a}0", f"X{a}1"]
                        dk = [f"X{b}0", f"X{b}1"]
                        pr, pi_, pin = pw_re[:, m, dj:dj + 1], pw_im[:, m, dj:dj + 1], pw_imn[:, m, dj:dj + 1]
                        if d == 0:
                            o_sl, i_sl, c_sl = slice(sh, S5_Q), slice(0, S5_Q - sh), slice(0, sh)
                        else:
                            o_sl, i_sl, c_sl = slice(0, S5_Q - sh), slice(sh, S5_Q), slice(S5_Q - sh, S5_Q)
                        for ri in range(2):
                            p.op('act', lambda e: e.copy(out=xv(dst[ri])[:, :, c_sl], in_=xv(src[ri])[:, :, c_sl]),
                                 r=[sk[ri]], w=[dk[ri]])
                        V(lambda e: e.scalar_tensor_tensor(out=xv(dst[0])[:, :, o_sl], in0=xv(src[0])[:, :, i_sl], scalar=pr,
                                                           in1=xv(src[0])[:, :, o_sl], op0=ALU.mult, op1=ALU.add),
                          [sk[0], "pw"], [dk[0]])
                        V(lambda e: e.scalar_tensor_tensor(out=xv(dst[0])[:, :, o_sl], in0=xv(src[1])[:, :, i_sl], scalar=pin,
                                                           in1=xv(dst[0])[:, :, o_sl], op0=ALU.mult, op1=ALU.add),
                          [sk[1], "pwn", dk[0]], [dk[0]])
                        V(lambda e: e.scalar_tensor_tensor(out=xv(dst[1])[:, :, o_sl], in0=xv(src[1])[:, :, i_sl], scalar=pr,
                                                           in1=xv(src[1])[:, :, o_sl], op0=ALU.mult, op1=ALU.add),
                          [sk[1], "pw"], [dk[1]])
                        V(lambda e: e.scalar_tensor_tensor(out=xv(dst[1])[:, :, o_sl], in0=xv(src[0])[:, :, i_sl], scalar=pi_,
                                                           in1=xv(dst[1])[:, :, o_sl], op0=ALU.mult, op1=ALU.add),
                          [sk[0], "pw", dk[1]], [dk[1]])
                    R = X[S5_LQ % 2]
                    rk = [f"X{S5_LQ % 2}0", f"X{S5_LQ % 2}1"]
                    e_col = S5_Q - 1 if d == 0 else 0
                    for ri in range(2):
                        V(lambda e: e.tensor_copy(out=Cx[0][ri][:], in_=xv(R[ri])[:, :, e_col]), [rk[ri]], [f"C0{ri}"])
                    for m in range(S5_LC):
                        sh = 1 << m
                        a, b = m % 2, (m + 1) % 2
                        src, dst = Cx[a], Cx[b]
                        sk = [f"C{a}0", f"C{a}1"]
                        dk = [f"C{b}0", f"C{b}1"]
                        mm = S5_LQ + m
                        pr, pi_, pin = pw_re[:, mm, dj:dj + 1], pw_im[:, mm, dj:dj + 1], pw_imn[:, mm, dj:dj + 1]
                        if d == 0:
                            o_sl, i_sl, c_sl = slice(sh, S5_NCH), slice(0, S5_NCH - sh), slice(0, sh)
                        else:
                            o_sl, i_sl, c_sl = slice(0, S5_NCH - sh), slice(sh, S5_NCH), slice(S5_NCH - sh, S5_NCH)
                        for ri in range(2):
                            p.op('act', lambda e: e.copy(out=dst[ri][:, c_sl], in_=src[ri][:, c_sl]), r=[sk[ri]], w=[dk[ri]])
                        V(lambda e: e.scalar_tensor_tensor(out=dst[0][:, o_sl], in0=src[0][:, i_sl], scalar=pr,
                                                           in1=src[0][:, o_sl], op0=ALU.mult, op1=ALU.add),
                          [sk[0], "pw"], [dk[0]])
                        V(lambda e: e.scalar_tensor_tensor(out=dst[0][:, o_sl], in0=src[1][:, i_sl], scalar=pin,
                                                           in1=dst[0][:, o_sl], op0=ALU.mult, op1=ALU.add),
                          [sk[1], "pwn", dk[0]], [dk[0]])
                        V(lambda e: e.scalar_tensor_tensor(out=dst[1][:, o_sl], in0=src[1][:, i_sl], scalar=pr,
                                                           in1=src[1][:, o_sl], op0=ALU.mult, op1=ALU.add),
                          [sk[1], "pw"], [dk[1]])
                        V(lambda e: e.scalar_tensor_tensor(out=dst[1][:, o_sl], in0=src[0][:, i_sl], scalar=pi_,
                                                           in1=dst[1][:, o_sl], op0=ALU.mult, op1=ALU.add),
                          [sk[0], "pw", dk[1]], [dk[1]])
                    CF = Cx[S5_LC % 2]
                    cfk = [f"C{S5_LC % 2}0", f"C{S5_LC % 2}1"]
                    j0 = 0 if d == 0 else S5_Q - 1
                    V(lambda e: e.tensor_copy(out=PT[0][:, j0:j0 + 1], in_=pw_re[:, 0, dj:dj + 1]), ["pw"], ["PT0"])
                    V(lambda e: e.tensor_copy(out=PT[1][:, j0:j0 + 1], in_=pw_im[:, 0, dj:dj + 1]), ["pw"], ["PT1"])
                    for m in range(S5_LQ):
                        sh = 1 << m
                        pr, pi_, pin = pw_re[:, m, dj:dj + 1], pw_im[:, m, dj:dj + 1], pw_imn[:, m, dj:dj + 1]
                        if d == 0:
                            s_sl, d_sl = slice(0, sh), slice(sh, 2 * sh)
                        else:
                            s_sl, d_sl = slice(S5_Q - sh, S5_Q), slice(S5_Q - 2 * sh, S5_Q - sh)
                        V(lambda e: e.tensor_scalar(out=ptt[:, 0:sh], in0=PT[1][:, s_sl], scalar1=pin, scalar2=None,
                                                    op0=ALU.mult), ["PT1", "pwn"], ["ptt"])
                        V(lambda e: e.scalar_tensor_tensor(out=PT[0][:, d_sl], in0=PT[0][:, s_sl], scalar=pr,
                                                           in1=ptt[:, 0:sh], op0=ALU.mult, op1=ALU.add),
                          ["PT0", "pw", "ptt"], ["PT0"])
                        V(lambda e: e.tensor_scalar(out=ptt[:, 0:sh], in0=PT[0][:, s_sl], scalar1=pi_, scalar2=None,
                                                    op0=ALU.mult), ["PT0", "pw"], ["ptt"])
                        V(lambda e: e.scalar_tensor_tensor(out=PT[1][:, d_sl], in0=PT[1][:, s_sl], scalar=pr,
                                                           in1=ptt[:, 0:sh], op0=ALU.mult, op1=ALU.add),
                          ["PT1", "pw", "ptt"], ["PT1"])
                    if d == 0:
                        xc_sl, cc_sl = slice(1, S5_NCH), slice(0, S5_NCH - 1)
                    else:
                        xc_sl, cc_sl = slice(0, S5_NCH - 1), slice(1, S5_NCH)
                    nco = S5_NCH - 1
                    ptb = lambda ri: PT[ri][:].unsqueeze(1).to_broadcast([128, nco, S5_Q])
                    cfb = lambda ri: CF[ri][:, cc_sl].unsqueeze(2).to_broadcast([128, nco, S5_Q])
                    tmpv = xv(X[(S5_LQ + 1) % 2][0])[:, 0:nco, :]
                    tk = f"X{(S5_LQ + 1) % 2}0"
                    for (ro, pa, ca, op_) in ((0, 0, 0, ALU.add), (0, 1, 1, ALU.subtract), (1, 0, 1, ALU.add),
                                              (1, 1, 0, ALU.add)):
                        V(lambda e: e.tensor_tensor(out=tmpv, in0=ptb(pa), in1=cfb(ca), op=ALU.mult),
                          [f"PT{pa}", cfk[ca]], [tk])
                        V(lambda e: e.tensor_tensor(out=xv(R[ro])[:, xc_sl, :], in0=xv(R[ro])[:, xc_sl, :], in1=tmpv,
                                                    op=op_), [rk[ro], tk], [rk[ro]])
                    for ri in range(2):
                        p.op('act', lambda e: e.copy(out=xb[ri][:], in_=R[ri][:]), r=[rk[ri]], w=[f"xb{ri}"])
                    for (s0, n, xp) in blocks:
                        bk = 4 + (nblk % 2)
                        for ri in range(2):
                            p.op('pe', lambda e: e.matmul(K.ps[bk][:, 0:n], lhsT=bc[bi][:, 2 + ri, :],
                                                          rhs=xb[ri][:, xp:xp + n], start=(ri == 0),
                                                          stop=(ri == 1)), r=[f"bc{bi}", f"xb{ri}"], w=[f"ps{bk}"])
                        V(lambda e: e.tensor_tensor(out=Y[:, c, s0:s0 + n], in0=Y[:, c, s0:s0 + n],
                                                    in1=K.ps[bk][:, 0:n], op=ALU.add), [f"ps{bk}", f"Y{c}"],
                          [f"Y{c}"])
                        nblk += 1
        with _scope(K) as es2:
            SB2 = lambda name, shape, dt=F32: es2.enter_context(_sb(nc, name, shape, dt))
            yb = SB2("s5yb", [128, 2, T], BF16)
            O = SB2("s5O", [128, 2, T])
            sg = [SB2(f"s5sg{i}", [128, 512]) for i in range(2)]
            for c in range(2):
                p.op('act', lambda e: e.activation(out=Y[:, c, :], in_=Y[:, c, :], func=AF.Gelu), r=[f"Y{c}"],
                     w=[f"Y{c}"])
                V(lambda e: e.tensor_copy(out=yb[:, c, :], in_=Y[:, c, :]), [f"Y{c}"], ["yb"])
            nb = 0
            for co in range(2):
                for (s0, n) in S5_BLOCKS0:
                    bk = nb % 2
                    for ci in range(2):
                        p.op('pe', lambda e: e.matmul(K.ps[bk][:, 0:n], lhsT=gluw[:, ci, co * 128:(co + 1) * 128],
                                                      rhs=yb[:, ci, s0:s0 + n], start=(ci == 0), stop=(ci == 1)),
                             r=["gluw", "yb"], w=[f"ps{bk}"])
                    p.op('act', lambda e: e.activation(out=sg[bk][:, 0:n], in_=K.ps[bk][:, 0:n], func=AF.Sigmoid,
                                                       bias=glb[:, co:co + 1]), r=[f"ps{bk}", "glb"], w=[f"sg{bk}"])
                    if s0 == 0:
                        V(lambda e: e.tensor_tensor(out=O[:, co, 0:LC], in0=Y[:, co, 0:LC], in1=sg[bk][:, 0:n],
                                                    op=ALU.mult), [f"Y{co}", f"sg{bk}"], [f"O{co}"])
                    else:
                        b8 = (s0 - LC) // 64
                        o_ap = O[:, co, LC:T].rearrange("q (r c) -> q c r", c=64)[:, b8:b8 + 8, :]
                        V(lambda e: e.tensor_tensor(out=o_ap,
                                                    in0=Y[:, co, s0:s0 + n].rearrange("q (c r) -> q c r", r=64),
                                                    in1=sg[bk][:, 0:n].rearrange("q (c r) -> q c r", r=64),
                                                    op=ALU.mult), [f"Y{co}", f"sg{bk}"], [f"O{co}"])
                    nb += 1
                p.dma('sp', K.mix[768 + co * 128:768 + (co + 1) * 128, :], O[:, co, :], r=[f"O{co}"], w=["mix"])
        p.barrier()


def _hy_seq(K, li, seq, t0, L, mixcol0):
    p, nc = K.p, K.nc
    V = lambda fn, r, w: p.op('dve', fn, r=r, w=w)
    A = lambda fn, r, w: p.op('act', fn, r=r, w=w)
    NTT = L // 128
    NFC = NTT
    N2 = 2 * L
    dft = K.inp[f"dft_{seq}"]
    featT = K.inp[f"hy_featT_{seq}"]
    dec = K.inp[f"hy_dec_{seq}"]
    with contextlib.ExitStack() as es:
        SB = lambda name, shape, dt=F32: es.enter_context(_sb(nc, name, shape, dt))
        w1, w2, w3 = SB("hyw1", [33, 64]), SB("hyw2", [64, 64]), SB("hyw3", [64, 1024])
        cf = SB("hycf", [64, 6])
        skipb = SB("hyskip", [128, 2, 256])
        alt = SB("hyalt", [128, 2], BF16)
        altrow = SB("hyaltr", [1, 128], BF16)
        wcol = SB("hywcol", [128, NFC])
        hid2 = SB("hyhid2", [64, L])
        u = SB("hyu", [128, NTT, 256], BF16)
        Hn = SB("hyHn", [1, 256])
        Pn = SB("hyPn", [1, 256], BF16)
        Un = SB("hyUn", [1, 256])
        p.dma('sp', w1[:], K.inp["hy_w1"][li], w=["hyw1"])
        p.dma('sp', w2[:], K.inp["hy_w2"][li], w=["hyw2"])
        p.dma('sp', w3[:], K.inp["hy_w3"][li], w=["hyw3"])
        for i, n_ in enumerate(("hy_b1", "hy_f1", "hy_b2", "hy_f2")):
            p.dma('sp', cf[:, i:i + 1], K.inp[n_][li].rearrange("(q o) -> q o", o=1), w=["hycf"])
        for o in range(2):
            p.dma('sp', skipb[:, o, :], K.inp["hy_skip"][li, o].partition_broadcast(128), w=["hyskip"])
        p.dma('sp', alt[:], K.inp["hy_alt"][:, :], w=["hyalt"])
        p.dma('sp', altrow[:], K.inp["hy_altrow"][:, :], w=["hyaltr"])
        V(lambda e: e.tensor_tensor(out=cf[:, 4:5], in0=cf[:, 0:1], in1=cf[:, 1:2], op=ALU.mult), ["hycf"], ["hycf"])
        V(lambda e: e.tensor_tensor(out=cf[:, 5:6], in0=cf[:, 2:3], in1=cf[:, 3:4], op=ALU.mult), ["hycf"], ["hycf"])
        V(lambda e: e.memset(wcol[:], 2.0 / N2), [], ["hywcol"])
        V(lambda e: e.memset(wcol[0:1, 0:1], 1.0 / N2), ["hywcol"], ["hywcol"])
        with _scope(K) as es2:
            ft = es2.enter_context(_sb(nc, "hyfeat", [33, L], F32))
            hid1 = es2.enter_context(_sb(nc, "hyhid1", [64, L], F32))
            tmp = es2.enter_context(_sb(nc, "hytmp", [64, 512], F32))
            p.dma('sp', ft[:], featT[:, :], w=["hyfeat"])
            nb = 0
            for (src, sk, wm, wk, kdim, dst, dk, fi, bi) in ((ft, "hyfeat", w1, "hyw1", 33, hid1, "hid1", 1, 4),
                                                             (hid1, "hid1", w2, "hyw2", 64, hid2, "hid2", 3, 5)):
                for b0 in range(0, L, 512):
                    n = min(512, L - b0)
                    bk = nb % 2
                    p.op('pe', lambda e: e.matmul(K.ps[bk][0:64, 0:n], lhsT=wm[0:kdim, :], rhs=src[0:kdim, b0:b0 + n],
                                                  start=True, stop=True), r=[wk, sk], w=[f"ps{bk}"])
                    V(lambda e: e.tensor_scalar(out=dst[:, b0:b0 + n], in0=K.ps[bk][0:64, 0:n],
                                                scalar1=cf[:, fi:fi + 1], scalar2=cf[:, bi:bi + 1], op0=ALU.mult,
                                                op1=ALU.add), [f"ps{bk}", "hycf"], [dk])
                    _sin(K, dst[:, b0:b0 + n], dst[:, b0:b0 + n], 0.0, tmp[:, 0:n], [dk], dk, "hytmp")
                    nb += 1
        with _scope(K) as es2:
            vst = [es2.enter_context(_sb(nc, f"hyvst{i}", [128, 256], F32)) for i in range(2)]
            for tt in range(NTT):
                i = tt % 2
                p.dma('sp', vst[i][:], K.hytm[2, t0 + tt * 128:t0 + (tt + 1) * 128, :], r=["hytm2"], w=[f"vst{i}"])
                _copy_any(p, tt, u[:, tt, :], vst[i][:], r=[f"vst{i}"], w=["hyu"])
        for order in range(2):
            with _scope(K) as es2:
                SB2 = lambda name, shape, dt=F32: es2.enter_context(_sb(nc, name, shape, dt))
                HP, HM = SB2("hyHP", [128, NTT, 256], BF16), SB2("hyHM", [128, NTT, 256], BF16)
                dct = [SB2(f"hydec{i}", [128, 256]) for i in range(2)]
                hf = [SB2(f"hyhf{i}", [128, 2, 256]) for i in range(2)]
                for tt in range(NTT):
                    i = tt % 2
                    bk = i
                    p.dma('sp', dct[i][:], dec[tt * 128:(tt + 1) * 128, :], w=[f"hydec{i}"])
                    p.op('pe', lambda e: e.matmul(K.ps[bk][:, :], lhsT=hid2[:, tt * 128:(tt + 1) * 128],
                                                  rhs=w3[:, order * 512:(order + 1) * 512], start=True, stop=True),
                         r=["hid2", "hyw3"], w=[f"ps{bk}"])
                    V(lambda e: e.tensor_tensor(out=hf[i][:], in0=K.ps[bk][:, :].rearrange("q (s c) -> q s c", s=2),
                                                in1=dct[i][:].unsqueeze(1).to_broadcast([128, 2, 256]), op=ALU.mult),
                      [f"ps{bk}", f"hydec{i}"], [f"hyhf{i}"])
                    if tt == 0:
                        V(lambda e: e.memset(hf[i][0:1, 1, :], 0.0), [f"hyhf{i}"], [f"hyhf{i}"])
                    V(lambda e: e.tensor_tensor(out=HP[:, tt, :], in0=hf[i][:, 0, :], in1=hf[i][:, 1, :], op=ALU.add),
                      [f"hyhf{i}"], ["HP"])
                    V(lambda e: e.tensor_tensor(out=HM[:, tt, :], in0=hf[i][:, 0, :], in1=hf[i][:, 1, :],
                                                op=ALU.subtract), [f"hyhf{i}"], ["HM"])
                Pc, Ps = SB2("hyPc", [128, NFC, 256], BF16), SB2("hyPs", [128, NFC, 256], BF16)
                slab = [[SB2(f"hysm{i}{cs}", [128, NTT * 128], BF16) for cs in range(2)] for i in range(2)]
                hcs = [[SB2(f"hyhcs{i}{cs}", [128, 256]) for cs in range(2)] for i in range(2)]
                tq = [SB2(f"hytq{i}", [128, 256]) for i in range(4)]
                xa = [SB2(f"hyxa{i}", [128, 256]) for i in range(2)]
                xv = [SB2(f"hyxv{i}", [128, 256]) for i in range(2)]
                ofm = [SB2(f"hyofm{i}", [128, 2, 128]) for i in range(2)]
                for tt in range(NTT):
                    p.op('pe', lambda e: e.matmul(K.ps[6][0:1, 0:256], lhsT=alt[:, 0:1], rhs=HP[:, tt, :],
                                                  start=(tt == 0), stop=(tt == NTT - 1)), r=["hyalt", "HP"], w=["ps6"])
                V(lambda e: e.tensor_scalar(out=Hn[:], in0=K.ps[6][0:1, 0:256], scalar1=1.0 / N2, scalar2=None,
                                            op0=ALU.mult), ["ps6"], ["Hn"])
                for fc in range(NFC):
                    i = fc % 2
                    for cs in range(2):
                        p.dma('sp', slab[i][cs][:], dft[cs, fc], w=[f"hysm{i}{cs}"])
                    for cs, (src, sk) in enumerate(((HP, "HP"), (HM, "HM"))):
                        for (rhs_t, rk_, bk) in ((src, sk, 4 * i + 2 + cs), (u, "hyu", 4 * i + cs)):
                            for tt in range(NTT):
                                p.op('pe', lambda e: e.matmul(K.ps[bk][:, 0:256],
                                                              lhsT=slab[i][cs][:, tt * 128:(tt + 1) * 128],
                                                              rhs=rhs_t[:, tt, :], start=(tt == 0), stop=(tt == NTT - 1)),
                                     r=[f"hysm{i}{cs}", rk_], w=[f"ps{bk}"])
                        A(lambda e: e.activation(out=hcs[i][cs][:], in_=K.ps[4 * i + 2 + cs][:, 0:256], func=AF.Copy,
                                                 scale=wcol[:, fc:fc + 1]), [f"ps{4 * i + 2 + cs}", "hywcol"],
                          [f"hcs{i}{cs}"])
                    C_, S_ = K.ps[4 * i][:, 0:256], K.ps[4 * i + 1][:, 0:256]
                    ck, sk = f"ps{4 * i}", f"ps{4 * i + 1}"
                    Hc_, Hs_ = hcs[i][0][:], hcs[i][1][:]
                    hck, hsk = f"hcs{i}0", f"hcs{i}1"
                    V(lambda e: e.tensor_tensor(out=tq[0][:], in0=C_, in1=Hc_, op=ALU.mult), [ck, hck], ["tq0"])
                    V(lambda e: e.tensor_tensor(out=tq[1][:], in0=S_, in1=Hs_, op=ALU.mult), [sk, hsk], ["tq1"])
                    V(lambda e: e.tensor_tensor(out=Pc[:, fc, :], in0=tq[0][:], in1=tq[1][:], op=ALU.subtract),
                      ["tq0", "tq1"], ["Pc"])
                    V(lambda e: e.tensor_tensor(out=tq[2][:], in0=C_, in1=Hs_, op=ALU.mult), [ck, hsk], ["tq2"])
                    V(lambda e: e.tensor_tensor(out=tq[3][:], in0=S_, in1=Hc_, op=ALU.mult), [sk, hck], ["tq3"])
                    V(lambda e: e.tensor_tensor(out=Ps[:, fc, :], in0=tq[2][:], in1=tq[3][:], op=ALU.add),
                      ["tq2", "tq3"], ["Ps"])
                for tt in range(NTT):
                    p.op('pe', lambda e: e.matmul(K.ps[6][0:1, 0:256], lhsT=alt[:, 0:1], rhs=u[:, tt, :],
                                                  start=(tt == 0), stop=(tt == NTT - 1)), r=["hyalt", "hyu"], w=["ps6"])
                V(lambda e: e.tensor_copy(out=Un[:], in_=K.ps[6][0:1, 0:256]), ["ps6"], ["Un"])
                V(lambda e: e.tensor_tensor(out=Pn[:], in0=Un[:], in1=Hn[:], op=ALU.mult), ["Un", "Hn"], ["Pn"])
                xslot = 0 if order == 0 else 1
                yslot = 2 if order == 0 else 0
                for tc in range(NTT):
                    i = tc % 2
                    for cs in range(2):
                        p.dma('sp', slab[i][cs][:], dft[cs, tc], w=[f"hysm{i}{cs}"])
                    rows = slice(t0 + tc * 128, t0 + (tc + 1) * 128)
                    p.dma('sp', xa[i][:], K.hytm[xslot, rows, :], r=[f"hytm{xslot}"], w=[f"hyxa{i}"])
                    p.dma('sp', xv[i][:], K.hytm[yslot, rows, :], r=[f"hytm{yslot}"], w=[f"hyxv{i}"])
                    bk = 4 + i
                    for fc in range(NFC):
                        p.op('pe', lambda e: e.matmul(K.ps[bk][:, 0:256], lhsT=slab[i][0][:, fc * 128:(fc + 1) * 128],
                                                      rhs=Pc[:, fc, :], start=(fc == 0), stop=False),
                             r=[f"hysm{i}0", "Pc"], w=[f"ps{bk}"])
                        p.op('pe', lambda e: e.matmul(K.ps[bk][:, 0:256], lhsT=slab[i][1][:, fc * 128:(fc + 1) * 128],
                                                      rhs=Ps[:, fc, :], start=False, stop=False),
                             r=[f"hysm{i}1", "Ps"], w=[f"ps{bk}"])
                    p.op('pe', lambda e: e.matmul(K.ps[bk][:, 0:256], lhsT=altrow[0:1, :], rhs=Pn[0:1, :], start=False,
                                                  stop=True), r=["hyaltr", "Pn"], w=[f"ps{bk}"])
                    V(lambda e: e.tensor_tensor(out=xv[i][:], in0=xv[i][:], in1=skipb[:, order, :], op=ALU.mult),
                      [f"hyxv{i}", "hyskip"], [f"hyxv{i}"])
                    V(lambda e: e.tensor_tensor(out=xv[i][:], in0=xv[i][:], in1=K.ps[bk][:, 0:256], op=ALU.add),
                      [f"hyxv{i}", f"ps{bk}"], [f"hyxv{i}"])
                    V(lambda e: e.tensor_tensor(out=xa[i][:], in0=xa[i][:], in1=xv[i][:], op=ALU.mult),
                      [f"hyxa{i}", f"hyxv{i}"], [f"hyxa{i}"])
                    if order == 0:
                        A(lambda e: e.copy(out=u[:, tc, :], in_=xa[i][:]), [f"hyxa{i}"], ["hyu2"])
                        p.dma('pool', K.hytm[0, rows, :], xa[i][:], r=[f"hyxa{i}"], w=["hytm0"])
                    else:
                        for c2 in range(2):
                            p.op('pe', lambda e: e.transpose(out=K.ps[7][:, c2 * 128:(c2 + 1) * 128],
                                                             in_=xa[i][:, c2 * 128:(c2 + 1) * 128], identity=K.ident[:]),
                                 r=[f"hyxa{i}", "ident"], w=["ps7"])
                        A(lambda e: e.copy(out=ofm[i][:].rearrange("q a b -> q (a b)"), in_=K.ps[7][:, 0:256]), ["ps7"],
                          [f"hyofm{i}"])
                        for c2 in range(2):
                            p.dma('pool', K.mix[512 + c2 * 128:512 + (c2 + 1) * 128,
                                                mixcol0 + tc * 128:mixcol0 + (tc + 1) * 128], ofm[i][:, c2, :],
                                  r=[f"hyofm{i}"], w=["mix"])
        p.barrier()


def phase_hyena(K, li):
    p, nc = K.p, K.nc
    V = lambda fn, r, w: p.op('dve', fn, r=r, w=w)
    need_ctx = li < DEPTH - 1
    with contextlib.ExitStack() as es:
        SB = lambda name, shape, dt=F32: es.enter_context(_sb(nc, name, shape, dt))
        cw = SB("hycw", [128, 6, 4])
        zin = [SB(f"hyzin{i}", [128, T]) for i in range(2)]
        zo = [SB(f"hyzo{i}", [128, T]) for i in range(2)]
        tst = [SB(f"hytst{i}", [128, 128]) for i in range(4)]
        for k in range(3):
            p.dma('sp', cw[:, :, k], K.inp["hy_conv"][li, k].rearrange("(c q) -> q c", q=128), w=["hycw"])
        p.dma('sp', cw[:, :, 3], K.inp["hy_conv_b"][li].rearrange("(c q) -> q c", q=128), w=["hycw"])
        nt = 0
        for ch in range(6):
            i = ch % 2
            r0 = ZR["hy"] + ch * 128
            p.dma('sp', zin[i][:], K.zfm[r0:r0 + 128, :], r=["zfm"], w=[f"zin{i}"])
            V(lambda e: e.tensor_scalar(out=zo[i][:], in0=zin[i][:], scalar1=cw[:, ch, 1:2], scalar2=cw[:, ch, 3:4],
                                        op0=ALU.mult, op1=ALU.add), [f"zin{i}", "hycw"], [f"zo{i}"])
            for (a, b) in ((0, LC), (LC, T)):
                V(lambda e: e.scalar_tensor_tensor(out=zo[i][:, a + 1:b], in0=zin[i][:, a:b - 1], scalar=cw[:, ch, 0:1],
                                                   in1=zo[i][:, a + 1:b], op0=ALU.mult, op1=ALU.add),
                  [f"zin{i}", "hycw", f"zo{i}"], [f"zo{i}"])
                V(lambda e: e.scalar_tensor_tensor(out=zo[i][:, a:b - 1], in0=zin[i][:, a + 1:b], scalar=cw[:, ch, 2:3],
                                                   in1=zo[i][:, a:b - 1], op0=ALU.mult, op1=ALU.add),
                  [f"zin{i}", "hycw", f"zo{i}"], [f"zo{i}"])
            slot, c2 = ch // 2, ch % 2
            for tt in range(0 if need_ctx else 2, NT):
                bk = nt % 4
                p.op('pe', lambda e: e.transpose(out=K.ps[bk][:, 0:128], in_=zo[i][:, tt * 128:(tt + 1) * 128],
                                                 identity=K.ident[:]), r=[f"zo{i}", "ident"], w=[f"ps{bk}"])
                _copy_any(p, nt, tst[bk][:], K.ps[bk][:, 0:128], r=[f"ps{bk}"], w=[f"tst{bk}"])
                p.dma('pool', K.hytm[slot, tt * 128:(tt + 1) * 128, c2 * 128:(c2 + 1) * 128], tst[bk][:],
                      r=[f"tst{bk}"], w=[f"hytm{slot}"])
                nt += 1
        p.barrier()
    if need_ctx:
        _hy_seq(K, li, "ctx", 0, LC, 0)
    _hy_seq(K, li, "lat", LC, LL, LC)


SCAN_W = 8
SCAN_WV = 64
SCAN_STEPS = [T]


def _tok1(s):
    return LC - 1 - s if s < LC else T + LC - 1 - s


def _ap(t, offset, dims, nparts):
    full = t[:]
    return bass.AP(full.tensor, offset, [[full.ap[0][0], nparts]] + [list(d) for d in dims])


def phase_scan(K, li):
    p, nc = K.p, K.nc
    W, WV = SCAN_W, SCAN_WV
    nsteps = SCAN_STEPS[0]
    NQ = (4, 6, 4, 4, 4)
    with contextlib.ExitStack() as es:
        SB = lambda name, shape, dt=F32: es.enter_context(_sb(nc, name, shape, dt))
        Sb = [SB(f"scS{i}", [128, 8, 64]) for i in range(2)]
        P1, Pb = SB("scP1", [128, 8, 64]), SB("scPb", [128, 8, 64])
        P4 = [SB(f"scP4{i}", [128, 8, 64]) for i in range(2)]
        sa = SB("scsa", [128, 8])
        t2 = [SB(f"sct2{b}", [128, 8, 64]) for b in range(2)]
        wsb = [SB(f"scwsb{b}", [128, 8, 64]) for b in range(2)]
        sel = SB("scsel", [6, 128], BF16)
        win = [[SB(f"scwin{o}_{b}", [6, 2 * W * 256], BF16) for b in range(2)] for o in range(5)]
        vw = [SB(f"scvw{b}", [128, 8, WV]) for b in range(2)]
        yw = [SB(f"scyw{b}", [128, 8, WV]) for b in range(2)]
        p.dma('sp', sel[:], K.inp["scan_sel"][:, :], w=["scsel"])
        p.op('dve', lambda e: e.memset(Sb[0][:], 0.0), w=["S0"])
        pend = []
        for b in range(2):
            p.op('dve', lambda e: e.memset(yw[b][:], 0.0), w=[f"yw{b}"])
        for s in range(nsteps):
            j, wi = s % W, s // W
            jv, wvi = s % WV, s // WV
            wb, vb = wi % 2, wvi % 2
            if j == 0:
                t_lo = (wi * W, _tok1(wi * W) - W + 1)
                rows_t = K.rows[:].tensor
                for o in range(5):
                    for d in range(2):
                        src = bass.AP(rows_t, (o * T + t_lo[d]) * 3072 + d * 256, [[512, NQ[o]], [3072, W], [1, 256]])
                        p.dma('sp', win[o][wb][0:NQ[o], d * W * 256:(d + 1) * W * 256], src, r=["rows"],
                              w=[f"win{o}_{wb}"])
            if jv == 0:
                v_lo = (wvi * WV, _tok1(wvi * WV) - WV + 1)
                for g in range(8):
                    d, mh = g // 4, g % 4
                    p.dma('sp', vw[vb][:, g, :], K.vfm[mh, :, v_lo[d]:v_lo[d] + WV], r=["vfm"], w=[f"vw{vb}"])
            ix = (j, W - 1 - j)
            ixv = (jv, WV - 1 - jv)
            banks = [(5 * s + o) % 8 for o in range(5)]
            for o in range(5):
                rhs = _ap(win[o][wb], ix[0] * 256, [[(W + ix[1] - ix[0]) * 256, 2], [1, 256]], NQ[o])
                p.op('pe', lambda e: e.matmul(K.ps[banks[o]][:, :], lhsT=sel[0:NQ[o], :], rhs=rhs, start=True,
                                              stop=True),
                     r=[f"win{o}_{wb}", "scsel"], w=[f"ps{banks[o]}"])
            psv = [K.ps[b][:, :].rearrange("q (g k) -> q g k", k=64) for b in banks]
            dv = ixv[1] - ixv[0]
            vcols = _ap(vw[vb], ixv[0], [[4 * WV + dv, 2], [WV, 4], [0, 64]], 128)
            ycols = _ap(yw[vb], ixv[0], [[4 * WV + dv, 2], [WV, 4]], 128)
            tb = s % 2
            items = []
            for g in range(8):
                d = g // 4
                vcol = vw[vb][:, g, ixv[d]:ixv[d] + 1]
                items.append((lambda e, g=g, vcol=vcol: e.activation(out=t2[tb][:, g, :], in_=psv[3][:, g, :],
                                                                     func=AF.Copy, scale=vcol),
                              [f"vw{vb}", f"ps{banks[3]}"], [f"sct2{tb}"]))
            p.op('act', lambda e: e.copy(out=wsb[tb][:], in_=psv[1]), r=[f"ps{banks[1]}"], w=[f"scwsb{tb}"])
            p.stage('act', items)
            V = lambda fn, r, w: p.op('dve', fn, r=r, w=w)
            G = lambda fn, r, w: p.op('pool', fn, r=r, w=w)
            cur, nxt = Sb[s % 2], Sb[(s + 1) % 2]
            ck, nk = f"S{s % 2}", f"S{(s + 1) % 2}"
            V(lambda e: e.tensor_tensor(out=P1[:], in0=cur[:], in1=psv[0], op=ALU.mult), [ck, f"ps{banks[0]}"], ["P1"])
            G(lambda e: e.tensor_tensor(out=nxt[:], in0=cur[:], in1=wsb[tb][:], op=ALU.mult), [ck, f"scwsb{tb}"], [nk])
            G(lambda e: e.tensor_tensor(out=nxt[:], in0=nxt[:], in1=t2[tb][:], op=ALU.add), [nk, f"sct2{tb}"], [nk])
            if pend:
                pend[0][0]()
            V(lambda e: e.tensor_reduce(out=sa[:], in_=P1[:], axis=AX.X, op=ALU.add), ["P1"], ["sa"])
            if pend:
                pend.pop(0)[1]()
            V(lambda e: e.tensor_tensor(out=Pb[:], in0=psv[2], in1=sa[:].unsqueeze(2).to_broadcast([128, 8, 64]),
                                        op=ALU.mult), ["sa", f"ps{banks[2]}"], ["Pb"])
            V(lambda e: e.tensor_tensor(out=nxt[:], in0=nxt[:], in1=Pb[:], op=ALU.add), [nk, "Pb"], [nk])

            def out_mul(nxt=nxt, nk=nk, r_ap=psv[4], rb=banks[4]):
                V(lambda e: e.tensor_tensor(out=P4[0][:], in0=nxt[:], in1=r_ap, op=ALU.mult), [nk, f"ps{rb}"], ["P4"])

            def out_red(ycols=ycols, vb=vb, s=s, jv=jv, wvi=wvi):
                V(lambda e: e.tensor_reduce(out=ycols, in_=P4[0][:], axis=AX.X, op=ALU.add), ["P4"], [f"yw{vb}"])
                if jv == WV - 1 or s == nsteps - 1:
                    v_lo = (wvi * WV, _tok1(wvi * WV) - WV + 1)
                    for g in range(8):
                        d = g // 4
                        p.dma('pool', K.yfm[g, :, v_lo[d]:v_lo[d] + WV], yw[vb][:, g, :], r=[f"yw{vb}"], w=["yfm"])
            pend.append((out_mul, out_red))
        pend[0][0]()
        pend[0][1]()
        p.barrier()


TOK_BLOCKS = [(0, 256)] + [(256 + 512 * b, 512) for b in range(8)]


def _split_store(K, tt, stg, stgk, SP, r1):
    p = K.p
    V = lambda fn, r, w: p.op('dve', fn, r=r, w=w)
    A = lambda fn, r, w: p.op('act', fn, r=r, w=w)
    stgk = list(stgk)
    A(lambda e: e.copy(out=SP[:, 0], in_=stg[:]), stgk, ["sp_hi"])
    V(lambda e: e.tensor_tensor(out=r1[:], in0=stg[:], in1=SP[:, 0], op=ALU.subtract), stgk + ["sp_hi"], ["sp_r1"])
    A(lambda e: e.copy(out=SP[:, 1], in_=r1[:]), ["sp_r1"], ["sp_mid"])
    V(lambda e: e.tensor_tensor(out=r1[:, 1], in0=r1[:, 1], in1=SP[:, 1, 1], op=ALU.subtract), ["sp_r1", "sp_mid"],
      ["sp_r1"])
    A(lambda e: e.copy(out=SP[:, 2, 1], in_=r1[:, 1]), ["sp_r1"], ["sp_lo"])
    for o in range(5):
        nsp = 3 if o == 1 else 2
        p.dma('sp' if o % 2 == 0 else 'pool',
              K.rows[o, tt * 128:(tt + 1) * 128, 0:nsp].rearrange("q s h d c -> q s (h d c)"),
              SP[:, 0:nsp, o].rearrange("q s h d c -> q s (h d c)"), r=["sp_hi", "sp_mid", "sp_lo"], w=["rows"])


def phase_rwprep(K, li):
    p, nc = K.p, K.nc
    V = lambda fn, r, w: p.op('dve', fn, r=r, w=w)
    A = lambda fn, r, w: p.op('act', fn, r=r, w=w)
    EM05 = math.exp(-0.5)
    with contextlib.ExitStack() as es:
        SB = lambda name, shape, dt=F32: es.enter_context(_sb(nc, name, shape, dt))
        w2p, a2p = SB("rww2p", [128, 4, 128]), SB("rwa2p", [128, 4, 128])
        g2p = SB("rwg2p", [128, 2, 128])
        blk = SB("rwblk", [128, 128])
        pT = SB("rwpT", [128, 18])
        omka = SB("rwomka", [128, 2])
        zin = {n: SB(f"rwz_{n}", [128, 2, 512]) for n in ("r", "k", "v")}
        zwa, zg_ = SB("rwz_wa", [128, 512]), SB("rwz_g", [128, 512])
        actc, sgl = SB("rwactc", [128, 512]), SB("rwsgl", [128, 512])
        dec, aa = SB("rwdec", [128, 4, 512]), SB("rwaa", [128, 4, 512])
        gout, bv = SB("rwgout", [128, 2, 512]), SB("rwbv", [128, 2, 512])
        kk, kkn, an = SB("rwkk", [128, 2, 512]), SB("rwkkn", [128, 2, 512]), SB("rwan", [128, 2, 512])
        kd, bb = SB("rwkd", [128, 4, 512]), SB("rwbb", [128, 4, 512])
        t1, t2, prs = SB("rwt1", [128, 512]), SB("rwt2", [128, 512]), SB("rwprs", [128, 512])
        stgs = [SB(f"rwstg{i}", [128, 5, 2, 2, 256]) for i in range(2)]
        SP = SB("rwSP", [128, 3, 5, 2, 2, 256], BF16)
        r1 = SB("rwr1", [128, 5, 2, 2, 256])
        p.dma('sp', w2p[:], K.inp["rw_w2p"][li].rearrange("i q c -> q i c"), w=["w2p"])
        p.dma('sp', a2p[:], K.inp["rw_a2p"][li].rearrange("i q c -> q i c"), w=["a2p"])
        p.dma('sp', g2p[:], K.inp["rw_g2p"][li].rearrange("i q c -> q i c"), w=["g2p"])
        p.dma('sp', blk[:], K.inp["blk64"][:, :], w=["blk"])
        p.dma('sp', pT[:], K.inp["rw_pT"][li], w=["pT"])
        V(lambda e: e.tensor_scalar(out=omka[:], in0=pT[:, 10:12], scalar1=-1.0, scalar2=1.0, op0=ALU.mult,
                                    op1=ALU.add), ["pT"], ["omka"])
        for hp in range(2):
            r0 = ZR["rw_v"] + hp * 128
            p.dma('pool', K.vfm[hp, :, :], K.zfm[r0:r0 + 128, :], r=["zfm"], w=["vfm"])
        nb = 0
        ncp = 0
        for (b0, n) in TOK_BLOCKS:
            for nm in ("r", "k", "v"):
                r0 = ZR["rw_" + nm]
                p.dma('sp', zin[nm][:, :, 0:n], K.zfm[r0:r0 + 256, b0:b0 + n].rearrange("(c q) t -> q c t", q=128),
                      r=["zfm"], w=["z" + nm])
            p.dma('sp', zwa[:, 0:n], K.zfm[ZR["rw_wa"]:ZR["rw_wa"] + 128, b0:b0 + n], r=["zfm"], w=["zwa"])
            p.dma('sp', zg_[:, 0:n], K.zfm[ZR["rw_g"]:ZR["rw_g"] + 128, b0:b0 + n], r=["zfm"], w=["zg"])
            A(lambda e: e.activation(out=actc[0:64, 0:n], in_=zwa[0:64, 0:n], func=AF.Tanh), ["zwa"], ["actc"])
            A(lambda e: e.copy(out=actc[64:128, 0:n], in_=zwa[64:128, 0:n]), ["zwa"], ["actc"])
            A(lambda e: e.activation(out=sgl[:, 0:n], in_=zg_[:, 0:n], func=AF.Sigmoid), ["zg"], ["sgl"])
            for i in range(4):
                bk = nb % 4
                nb += 1
                p.op('pe', lambda e: e.matmul(K.ps[bk][:, 0:n], lhsT=w2p[:, i, :], rhs=actc[:, 0:n], start=True,
                                              stop=True), r=["w2p", "actc"], w=[f"ps{bk}"])
                A(lambda e: e.activation(out=dec[:, i, 0:n], in_=K.ps[bk][:, 0:n], func=AF.Sigmoid,
                                         bias=pT[:, i:i + 1]), [f"ps{bk}", "pT"], ["dec"])
                A(lambda e: e.activation(out=dec[:, i, 0:n], in_=dec[:, i, 0:n], func=AF.Exp, scale=-EM05),
                  ["dec"], ["dec"])
                bk = nb % 4
                nb += 1
                p.op('pe', lambda e: e.matmul(K.ps[bk][:, 0:n], lhsT=a2p[:, i, :], rhs=actc[:, 0:n], start=True,
                                              stop=True), r=["a2p", "actc"], w=[f"ps{bk}"])
                A(lambda e: e.activation(out=aa[:, i, 0:n], in_=K.ps[bk][:, 0:n], func=AF.Sigmoid,
                                         bias=pT[:, 4 + i:5 + i]), [f"ps{bk}", "pT"], ["aa"])
            for cc in range(2):
                bk = nb % 4
                nb += 1
                p.op('pe', lambda e: e.matmul(K.ps[bk][:, 0:n], lhsT=g2p[:, cc, :], rhs=sgl[:, 0:n], start=True,
                                              stop=True), r=["g2p", "sgl"], w=[f"ps{bk}"])
                A(lambda e: e.copy(out=gout[:, cc, 0:n], in_=K.ps[bk][:, 0:n]), [f"ps{bk}"], ["gout"])
                V(lambda e: e.tensor_scalar(out=kk[:, cc, 0:n], in0=zin["k"][:, cc, 0:n], scalar1=pT[:, 8 + cc:9 + cc],
                                            scalar2=None, op0=ALU.mult), ["zk", "pT"], ["kk"])
                V(lambda e: e.tensor_tensor(out=t1[:, 0:n], in0=kk[:, cc, 0:n], in1=kk[:, cc, 0:n], op=ALU.mult),
                  ["kk"], ["t1"])
                bk = nb % 4
                nb += 1
                p.op('pe', lambda e: e.matmul(K.ps[bk][:, 0:n], lhsT=blk[:, :], rhs=t1[:, 0:n], start=True, stop=True),
                     r=["blk", "t1"], w=[f"ps{bk}"])
                V(lambda e: e.tensor_scalar(out=t2[:, 0:n], in0=K.ps[bk][:, 0:n], scalar1=1e-12, scalar2=None,
                                            op0=ALU.add), [f"ps{bk}"], ["t2"])
                A(lambda e: e.activation(out=t2[:, 0:n], in_=t2[:, 0:n], func=AF.Sqrt), ["t2"], ["t2"])
                V(lambda e: e.reciprocal(out=t2[:, 0:n], in_=t2[:, 0:n]), ["t2"], ["t2"])
                V(lambda e: e.tensor_tensor(out=kkn[:, cc, 0:n], in0=kk[:, cc, 0:n], in1=t2[:, 0:n], op=ALU.mult),
                  ["kk", "t2"], ["kkn"])
                V(lambda e: e.tensor_scalar(out=an[:, cc, 0:n], in0=kkn[:, cc, 0:n], scalar1=-1.0, scalar2=None,
                                            op0=ALU.mult), ["kkn"], ["an"])
                for d in range(2):
                    i = d * 2 + cc
                    V(lambda e: e.tensor_scalar(out=t1[:, 0:n], in0=aa[:, i, 0:n], scalar1=pT[:, 10 + cc:11 + cc],
                                                scalar2=omka[:, cc:cc + 1], op0=ALU.mult, op1=ALU.add),
                      ["aa", "pT", "omka"], ["t1"])
                    V(lambda e: e.tensor_tensor(out=kd[:, i, 0:n], in0=t1[:, 0:n], in1=zin["k"][:, cc, 0:n],
                                                op=ALU.mult), ["t1", "zk"], ["kd"])
                    V(lambda e: e.tensor_tensor(out=bb[:, i, 0:n], in0=kkn[:, cc, 0:n], in1=aa[:, i, 0:n],
                                                op=ALU.mult), ["kkn", "aa"], ["bb"])
                V(lambda e: e.tensor_tensor(out=t1[:, 0:n], in0=kd[:, cc, 0:n], in1=kd[:, 2 + cc, 0:n], op=ALU.add),
                  ["kd"], ["t1"])
                V(lambda e: e.scalar_tensor_tensor(out=prs[:, 0:n], in0=zin["r"][:, cc, 0:n],
                                                   scalar=pT[:, 12 + cc:13 + cc], in1=t1[:, 0:n], op0=ALU.mult,
                                                   op1=ALU.mult), ["zr", "pT", "t1"], ["prs"])
                bk = nb % 4
                nb += 1
                p.op('pe', lambda e: e.matmul(K.ps[bk][:, 0:n], lhsT=blk[:, :], rhs=prs[:, 0:n], start=True,
                                              stop=True), r=["blk", "prs"], w=[f"ps{bk}"])
                V(lambda e: e.tensor_tensor(out=bv[:, cc, 0:n], in0=K.ps[bk][:, 0:n], in1=zin["v"][:, cc, 0:n],
                                            op=ALU.mult), [f"ps{bk}", "zv"], ["bv"])
            p.dma('pool', K.rwg.rearrange("(c q) t -> q c t", q=128)[:, :, b0:b0 + n], gout[:, :, 0:n], r=["gout"],
                  w=["rwg"])
            p.dma('pool', K.rwbv.rearrange("(c q) t -> q c t", q=128)[:, :, b0:b0 + n], bv[:, :, 0:n], r=["bv"],
                  w=["rwbv"])
            for tl in range(n // 128):
                tt = (b0 // 128) + tl
                cols = slice(tl * 128, (tl + 1) * 128)
                stg = stgs[tt % 2]
                sb_ = tt % 2
                skeys = [f"st{sb_}:gd"]
                p.dma('sp', stg[:].rearrange("q o h d c -> q (o h d) c")[:, :, 128:256],
                      K.gdstg[tt * 128:(tt + 1) * 128, :].rearrange("q (x c) -> q x c", c=128), r=["gdstgd"],
                      w=[f"st{sb_}:gd"])
                for cc in range(2):
                    srcs = [(0, None, an[:, cc, cols], "an"), (4, None, zin["r"][:, cc, cols], "zr")]
                    for d in range(2):
                        i = d * 2 + cc
                        srcs += [(1, d, dec[:, i, cols], "dec"), (2, d, bb[:, i, cols], "bb"), (3, d, kd[:, i, cols], "kd")]
                    for (o, d, src, sk) in srcs:
                        bk = 4 + (ncp % 4)
                        p.op('pe', lambda e: e.transpose(out=K.ps[bk][:, 0:128], in_=src, identity=K.ident[:]),
                             r=[sk, "ident"], w=[f"ps{bk}"])
                        pin = K.ps[bk][:, 0:128].rearrange("q (h k) -> q h k", k=64)
                        if d is None:
                            out_ap = stg[:, o, :, :, cc * 64:(cc + 1) * 64]
                            in_ap = pin.unsqueeze(2).to_broadcast([128, 2, 2, 64])
                        else:
                            out_ap = stg[:, o, :, d, cc * 64:(cc + 1) * 64]
                            in_ap = pin
                        wk = f"st{sb_}:{o}:{cc}:{d}"
                        skeys.append(wk)
                        _copy_any(p, ncp, out_ap, in_ap, r=[f"ps{bk}"], w=[wk])
                        ncp += 1
                _split_store(K, tt, stg, skeys, SP, r1)
        p.barrier()


def phase_gdprep(K, li):
    p, nc = K.p, K.nc
    V = lambda fn, r, w: p.op('dve', fn, r=r, w=w)
    A = lambda fn, r, w: p.op('act', fn, r=r, w=w)
    with contextlib.ExitStack() as es:
        SB = lambda name, shape, dt=F32: es.enter_context(_sb(nc, name, shape, dt))
        cw = SB("gdcw", [128, 6, 3])
        gpar = SB("gdpar", [128, 2, 8])
        zin = [SB(f"gdzin{i}", [128, T]) for i in range(2)]
        qk = SB("gdqk", [128, 4, T])
        zo = SB("gdzo", [128, T])
        ab = SB("gdab", [16, T])
        for k in range(3):
            p.dma('sp', cw[:, :, k], K.inp["gd_conv"][li, k].rearrange("(c q) -> q c", q=128), w=["cw"])
        p.dma('sp', gpar[:, 0, :], K.inp["gd_dtb"][li].rearrange("d h -> (d h)").partition_broadcast(128), w=["gpar"])
        p.dma('sp', gpar[:, 1, :], K.inp["gd_alog"][li].rearrange("d h -> (d h)").partition_broadcast(128), w=["gpar"])
        A(lambda e: e.activation(out=gpar[:, 1, :], in_=gpar[:, 1, :], func=AF.Exp), ["gpar"], ["gpar"])
        V(lambda e: e.tensor_scalar(out=gpar[:, 1, :], in0=gpar[:, 1, :], scalar1=-1.0, scalar2=None, op0=ALU.mult),
          ["gpar"], ["gpar"])
        p.dma('sp', ab[:], K.zfm[ZR["gd_ab"]:ZR["gd_ab"] + 16, :], r=["zfm"], w=["gdab"])
        for ch in range(6):
            i = ch % 2
            r0 = ZR["gd_q"] + ch * 128
            p.dma('sp', zin[i][:], K.zfm[r0:r0 + 128, :], r=["zfm"], w=[f"gdzin{i}"])
            dst = qk[:, ch, :] if ch < 4 else zo[:]
            dk = f"gdqk{ch}" if ch < 4 else "gdzo"
            V(lambda e: e.tensor_scalar(out=dst, in0=zin[i][:], scalar1=cw[:, ch, 1:2], scalar2=None, op0=ALU.mult),
              [f"gdzin{i}", "cw"], [dk])
            for (a, b) in ((0, LC), (LC, T)):
                V(lambda e: e.scalar_tensor_tensor(out=dst[:, a + 1:b], in0=zin[i][:, a:b - 1], scalar=cw[:, ch, 0:1],
                                                   in1=dst[:, a + 1:b], op0=ALU.mult, op1=ALU.add),
                  [f"gdzin{i}", "cw", dk], [dk])
                V(lambda e: e.scalar_tensor_tensor(out=dst[:, a:b - 1], in0=zin[i][:, a + 1:b], scalar=cw[:, ch, 2:3],
                                                   in1=dst[:, a:b - 1], op0=ALU.mult, op1=ALU.add),
                  [f"gdzin{i}", "cw", dk], [dk])
            A(lambda e: e.activation(out=dst, in_=dst, func=AF.Silu), [dk], [dk])
            if ch >= 4:
                p.dma('pool', K.vfm[2 + (ch - 4), :, :], zo[:], r=["gdzo"], w=["vfm"])
        with _scope(K) as es2:
            SB2 = lambda name, shape, dt=F32: es2.enter_context(_sb(nc, name, shape, dt))
            qt, kt = SB2("gdqt", [128, 4, 64]), SB2("gdkt", [128, 4, 64])
            gt = SB2("gdgt", [128, 16])
            sq = SB2("gdsq", [128, 4, 64])
            nrm = SB2("gdnrm", [128, 8])
            agd, nagd, beta = SB2("gdagd", [128, 8]), SB2("gdnagd", [128, 8]), SB2("gdbeta", [128, 8])
            stgs = [SB2(f"gdstg{i}", [128, 5, 2, 2, 128]) for i in range(2)]

            def hv(ap):
                return ap.rearrange("q (hp hl) k -> q hl hp k", hl=2)

            def sv(o, d):
                return stg[:, o, :, d, :].rearrange("q hl (hp k) -> q hl hp k", k=64)
            for tt in range(NT):
                cols = slice(tt * 128, (tt + 1) * 128)
                stg = stgs[tt % 2]
                sgk = f"gdstg{tt % 2}"
                for ch in range(4):
                    bk = ch
                    p.op('pe', lambda e: e.transpose(out=K.ps[bk][:, 0:128], in_=qk[:, ch, cols], identity=K.ident[:]),
                         r=[f"gdqk{ch}", "ident"], w=[f"ps{bk}"])
                    dst = (qt if ch < 2 else kt)[:, 2 * (ch % 2):2 * (ch % 2) + 2, :]
                    _copy_any(p, ch, dst.rearrange("q h k -> q (h k)"), K.ps[bk][:, 0:128], r=[f"ps{bk}"],
                              w=["gdqt" if ch < 2 else "gdkt"])
                p.op('pe', lambda e: e.transpose(out=K.ps[4][:, 0:16], in_=ab[:, cols], identity=K.ident[0:16, 0:16]),
                     r=["gdab", "ident"], w=["ps4"])
                V(lambda e: e.tensor_copy(out=gt[:], in_=K.ps[4][:, 0:16]), ["ps4"], ["gdgt"])
                V(lambda e: e.tensor_tensor(out=agd[:], in0=gt[:, 0:8], in1=gpar[:, 0, :], op=ALU.add), ["gdgt", "gpar"],
                  ["agd"])
                A(lambda e: e.activation(out=agd[:], in_=agd[:], func=AF.Exp), ["agd"], ["agd"])
                A(lambda e: e.activation(out=agd[:], in_=agd[:], func=AF.Ln, bias=1.0), ["agd"], ["agd"])
                V(lambda e: e.tensor_tensor(out=agd[:], in0=agd[:], in1=gpar[:, 1, :], op=ALU.mult), ["agd", "gpar"],
                  ["agd"])
                A(lambda e: e.activation(out=agd[:], in_=agd[:], func=AF.Exp), ["agd"], ["agd"])
                V(lambda e: e.tensor_scalar(out=nagd[:], in0=agd[:], scalar1=-1.0, scalar2=None, op0=ALU.mult), ["agd"],
                  ["nagd"])
                A(lambda e: e.activation(out=beta[:], in_=gt[:, 8:16], func=AF.Sigmoid), ["gdgt"], ["beta"])
                for (src, sk, col, scl) in ((qt, "gdqt", 0, 64.0), (kt, "gdkt", 4, 1.0)):
                    V(lambda e: e.tensor_tensor(out=sq[:], in0=src[:], in1=src[:], op=ALU.mult), [sk], ["gdsq"])
                    V(lambda e: e.tensor_reduce(out=nrm[:, col:col + 4], in_=sq[:], axis=AX.X, op=ALU.add), ["gdsq"],
                      ["gdnrm"])
                    V(lambda e: e.tensor_scalar(out=nrm[:, col:col + 4], in0=nrm[:, col:col + 4], scalar1=scl,
                                                scalar2=scl * 1e-12, op0=ALU.mult, op1=ALU.add), ["gdnrm"], ["gdnrm"])
                    A(lambda e: e.activation(out=nrm[:, col:col + 4], in_=nrm[:, col:col + 4], func=AF.Sqrt),
                      ["gdnrm"], ["gdnrm"])
                    V(lambda e: e.reciprocal(out=nrm[:, col:col + 4], in_=nrm[:, col:col + 4]), ["gdnrm"], ["gdnrm"])
                    V(lambda e: e.tensor_tensor(out=src[:], in0=src[:],
                                                in1=nrm[:, col:col + 4].unsqueeze(2).to_broadcast([128, 4, 64]),
                                                op=ALU.mult), [sk, "gdnrm"], [sk])
                for d in range(2):
                    V(lambda e: e.tensor_copy(out=sv(0, d), in_=hv(kt[:])), ["gdkt"], [sgk])
                    V(lambda e: e.tensor_copy(out=sv(4, d), in_=hv(qt[:])), ["gdqt"], [sgk])
                    bcol = lambda t_: hv(t_[:, d * 4:(d + 1) * 4].unsqueeze(2).to_broadcast([128, 4, 64]))
                    V(lambda e: e.tensor_copy(out=sv(1, d), in_=bcol(agd)), ["agd"], [sgk])
                    V(lambda e: e.tensor_tensor(out=sv(3, d), in0=hv(kt[:]), in1=bcol(beta), op=ALU.mult),
                      ["gdkt", "beta"], [sgk])
                    V(lambda e: e.tensor_tensor(out=sv(2, d), in0=sv(3, d), in1=bcol(nagd), op=ALU.mult),
                      [sgk, "nagd"], [sgk])
                p.dma('pool', K.gdstg[tt * 128:(tt + 1) * 128, :], stg[:].rearrange("q o h d c -> q (o h d c)"),
                      r=[sgk], w=["gdstgd"])
        p.barrier()


def phase_rwgd_post(K, li):
    p, nc = K.p, K.nc
    V = lambda fn, r, w: p.op('dve', fn, r=r, w=w)
    A = lambda fn, r, w: p.op('act', fn, r=r, w=w)
    with contextlib.ExitStack() as es:
        SB = lambda name, shape, dt=F32: es.enter_context(_sb(nc, name, shape, dt))
        blk = SB("poblk", [128, 128])
        pT = SB("popT", [128, 18])
        gnT = SB("pognT", [128, 1])
        y0, y1 = SB("poy0", [128, 512]), SB("poy1", [128, 512])
        ysq, mean, var = SB("poysq", [128, 512]), SB("pomean", [128, 512]), SB("povar", [128, 512])
        ga, bvv = SB("poga", [128, 512]), SB("pobv", [128, 512])
        p.dma('sp', blk[:], K.inp["blk64"][:, :], w=["blk"])
        p.dma('sp', pT[:], K.inp["rw_pT"][li], w=["pT"])
        p.dma('sp', gnT[:], K.inp["gd_normT"][li].rearrange("(q o) -> q o", o=1), w=["gnT"])
        V(lambda e: e.tensor_scalar(out=blk[:], in0=blk[:], scalar1=1.0 / 64.0, scalar2=None, op0=ALU.mult), ["blk"],
          ["blk"])
        for m in range(2):
            for hp in range(2):
                for (b0, n) in TOK_BLOCKS:
                    p.dma('sp', y0[:, 0:n], K.yfm[m * 2 + hp, :, b0:b0 + n], r=["yfm"], w=["y0"])
                    p.dma('sp', y1[:, 0:n], K.yfm[4 + m * 2 + hp, :, b0:b0 + n], r=["yfm"], w=["y1"])
                    rowsl = slice(hp * 128, (hp + 1) * 128)
                    if m == 0:
                        p.dma('sp', ga[:, 0:n], K.rwg[rowsl, b0:b0 + n], r=["rwg"], w=["ga"])
                        p.dma('sp', bvv[:, 0:n], K.rwbv[rowsl, b0:b0 + n], r=["rwbv"], w=["bvv"])
                    else:
                        r0 = ZR["gd_zg"] + hp * 128
                        p.dma('sp', ga[:, 0:n], K.zfm[r0:r0 + 128, b0:b0 + n], r=["zfm"], w=["ga"])
                    V(lambda e: e.tensor_tensor(out=y0[:, 0:n], in0=y0[:, 0:n], in1=y1[:, 0:n], op=ALU.add),
                      ["y0", "y1"], ["y0"])
                    A(lambda e: e.activation(out=ysq[:, 0:n], in_=y0[:, 0:n], func=AF.Square), ["y0"], ["ysq"])
                    p.op('pe', lambda e: e.matmul(K.ps[0][:, 0:n], lhsT=blk[:, :], rhs=ysq[:, 0:n], start=True,
                                                  stop=True), r=["blk", "ysq"], w=["ps0"])
                    if m == 0:
                        p.op('pe', lambda e: e.matmul(K.ps[1][:, 0:n], lhsT=blk[:, :], rhs=y0[:, 0:n], start=True,
                                                      stop=True), r=["blk", "y0"], w=["ps1"])
                        V(lambda e: e.tensor_copy(out=mean[:, 0:n], in_=K.ps[1][:, 0:n]), ["ps1"], ["mean"])
                        V(lambda e: e.tensor_tensor(out=var[:, 0:n], in0=mean[:, 0:n], in1=mean[:, 0:n], op=ALU.mult),
                          ["mean"], ["var"])
                        V(lambda e: e.tensor_tensor(out=var[:, 0:n], in0=K.ps[0][:, 0:n], in1=var[:, 0:n],
                                                    op=ALU.subtract), ["ps0", "var"], ["var"])
                        V(lambda e: e.tensor_scalar(out=var[:, 0:n], in0=var[:, 0:n], scalar1=64e-5, scalar2=None,
                                                    op0=ALU.add), ["var"], ["var"])
                        A(lambda e: e.activation(out=var[:, 0:n], in_=var[:, 0:n], func=AF.Sqrt), ["var"], ["var"])
                        V(lambda e: e.reciprocal(out=var[:, 0:n], in_=var[:, 0:n]), ["var"], ["var"])
                        V(lambda e: e.tensor_tensor(out=y0[:, 0:n], in0=y0[:, 0:n], in1=mean[:, 0:n], op=ALU.subtract),
                          ["y0", "mean"], ["y0"])
                        V(lambda e: e.tensor_tensor(out=y0[:, 0:n], in0=y0[:, 0:n], in1=var[:, 0:n], op=ALU.mult),
                          ["y0", "var"], ["y0"])
                        V(lambda e: e.tensor_scalar(out=y0[:, 0:n], in0=y0[:, 0:n], scalar1=pT[:, 14 + hp:15 + hp],
                                                    scalar2=pT[:, 16 + hp:17 + hp], op0=ALU.mult, op1=ALU.add),
                          ["y0", "pT"], ["y0"])
                        V(lambda e: e.tensor_tensor(out=y0[:, 0:n], in0=y0[:, 0:n], in1=bvv[:, 0:n], op=ALU.add),
                          ["y0", "bvv"], ["y0"])
                        V(lambda e: e.tensor_tensor(out=y0[:, 0:n], in0=y0[:, 0:n], in1=ga[:, 0:n], op=ALU.mult),
                          ["y0", "ga"], ["y0"])
                    else:
                        V(lambda e: e.tensor_scalar(out=var[:, 0:n], in0=K.ps[0][:, 0:n], scalar1=1e-6, scalar2=None,
                                                    op0=ALU.add), ["ps0"], ["var"])
                        A(lambda e: e.activation(out=var[:, 0:n], in_=var[:, 0:n], func=AF.Sqrt), ["var"], ["var"])
                        V(lambda e: e.reciprocal(out=var[:, 0:n], in_=var[:, 0:n]), ["var"], ["var"])
                        V(lambda e: e.scalar_tensor_tensor(out=y0[:, 0:n], in0=y0[:, 0:n], scalar=gnT[:, 0:1],
                                                           in1=var[:, 0:n], op0=ALU.mult, op1=ALU.mult),
                          ["y0", "gnT", "var"], ["y0"])
                        A(lambda e: e.activation(out=ga[:, 0:n], in_=ga[:, 0:n], func=AF.Silu), ["ga"], ["ga"])
                        V(lambda e: e.tensor_tensor(out=y0[:, 0:n], in0=y0[:, 0:n], in1=ga[:, 0:n], op=ALU.mult),
                          ["y0", "ga"], ["y0"])
                    r0 = m * 256 + hp * 128
                    p.dma('pool', K.mix[r0:r0 + 128, b0:b0 + n], y0[:, 0:n], r=["y0"], w=["mix"])
        p.barrier()


LAST_INPUT_NAMES = []
PHASES = {}


def build_program(phases=None, debug=(), dbg_in=()):
    del LAST_INPUT_NAMES[:]
    nc = bass.Bass("TRN2", target_bir_lowering=False)
    K = Ctx()
    K.nc = nc
    K.inp = {}

    def inp(name, shape, dt=F32):
        K.inp[name] = nc.dram_tensor(name, list(shape), dt, kind="ExternalInput").ap()
        LAST_INPUT_NAMES.append(name)

    def scratch(name, shape, dt=F32):
        if name in dbg_in:
            LAST_INPUT_NAMES.append(name)
            return nc.dram_tensor(name, list(shape), dt, kind="ExternalInput").ap()
        kind = "ExternalOutput" if name in debug else "Internal"
        return nc.dram_tensor(name, list(shape), dt, kind=kind).ap()

    if phases is None:
        phases = [(n, li) for li in range(DEPTH) for n in PHASE_ORDER]
    names = set(n for n, _ in phases)
    inp("xin", [T, D])
    inp("c2T", [128, 8, 2])
    inp("ident", [128, 128])
    inp("ada_w", [DEPTH, D, 6 * D])
    inp("ada_b", [DEPTH, 6 * D])
    inp("ada_bT", [DEPTH, 128, 48])
    if "inproj" in names:
        inp("w_in_p", [DEPTH, D, ZROWS])
    if "outproj" in names:
        inp("w_out", [DEPTH, D, D])
    inp("ln_g", [DEPTH, 2, D])
    inp("ln_b", [DEPTH, 2, D])
    if "s5" in names:
        for n_ in ("s5_lreT", "s5_limT", "s5_dtT"):
            inp(n_, [DEPTH, 128, 16])
        inp("s5_dT", [DEPTH, 128, 2])
        inp("s5_glbT", [DEPTH, 128, 2])
        inp("s5_glu_w", [DEPTH, 256, 256])
        for n_ in ("s5_breT", "s5_bimT", "s5_creT", "s5_cimT"):
            inp(n_, [DEPTH, 16, 128, 128])
    if "hyena" in names:
        inp("hy_conv", [DEPTH, 3, 768])
        inp("hy_conv_b", [DEPTH, 768])
        inp("hy_w1", [DEPTH, 33, 64])
        inp("hy_w2", [DEPTH, 64, 64])
        inp("hy_w3", [DEPTH, 64, 1024])
        for n_ in ("hy_b1", "hy_f1", "hy_b2", "hy_f2"):
            inp(n_, [DEPTH, 64])
        inp("hy_skip", [DEPTH, 2, 256])
        inp("hy_alt", [128, 2], BF16)
        inp("hy_altrow", [1, 128], BF16)
        for sq_, L_ in (("ctx", LC), ("lat", LL)):
            inp(f"hy_featT_{sq_}", [33, L_])
            inp(f"hy_dec_{sq_}", [L_, 256])
            inp(f"dft_{sq_}", [2, L_ // 128, 128, L_], BF16)
    if "scan" in names:
        inp("scan_sel", [6, 128], BF16)
    if names & {"rwprep", "gdprep", "rwgdpost"}:
        inp("rw_w2p", [DEPTH, 4, 128, 128])
        inp("rw_a2p", [DEPTH, 4, 128, 128])
        inp("rw_g2p", [DEPTH, 2, 128, 128])
        inp("rw_pT", [DEPTH, 128, 18])
        inp("blk64", [128, 128])
        inp("gd_conv", [DEPTH, 3, 768])
        inp("gd_dtb", [DEPTH, 2, 4])
        inp("gd_alog", [DEPTH, 2, 4])
        inp("gd_normT", [DEPTH, 128])
    if "router" in names:
        inp("router_w", [DEPTH, D, 64])
        inp("router_b", [DEPTH, 64])
    if "experts" in names:
        inp("ex_w1", [DEPTH, 64, D, 256])
        inp("ex_w3", [DEPTH, 64, D, 256])
        inp("ex_w2", [DEPTH, 64, 256, D])
        inp("sh_w1", [DEPTH, D, 256])
        inp("sh_w3", [DEPTH, D, 256])
        inp("sh_w2", [DEPTH, 256, D])
    K.out = nc.dram_tensor("out", [LL, D], F32, kind="ExternalOutput").ap()
    K.xres = scratch("xres", [T, D])
    K.zfm = scratch("zfm", [ZROWS, T])
    K.mix = scratch("mix", [D, T])
    K.hT = scratch("hT", [D, T], BF16)
    K.hytm = scratch("hytm", [3, T, 256])
    K.rows = scratch("rows", [5, T, 3, 2, 2, 256], BF16)
    K.gdstg = scratch("gdstg", [T, 5 * 2 * 2 * 128])
    K.vfm = scratch("vfm", [4, 128, T])
    K.yfm = scratch("yfm", [8, 128, T])
    K.rwg = scratch("rwg", [256, T])
    K.rwbv = scratch("rwbv", [256, T])
    with contextlib.ExitStack() as es:
        es.enter_context(nc.allow_non_contiguous_dma(reason="tiny per-channel parameter loads"))
        K.p = p = Prog(nc)
        K.ps = [es.enter_context(nc.psum_tensor(f"psb{i}", [128, 512], F32)) for i in range(8)]
        K.ident = es.enter_context(_sb(nc, "ident_sb", [128, 128], F32))
        K.modT = es.enter_context(_sb(nc, "modT", [128, 48, 2], F32))
        K.gates = es.enter_context(_sb(nc, "gates", [128, NT, 64], F32))
        K.gate = {(w, g): es.enter_context(_sb(nc, f"gate{w}{g}", [128, 1024], F32))
                  for w in range(2) for g in range(2)}
        p.dma('sp', K.ident[:], K.inp["ident"][:, :], w=["ident"])
        if "xres" not in dbg_in:
            p.dma('sp', K.xres[:, :], K.inp["xin"][:, :], w=["xres"])
        p.barrier()
        for (name, li) in phases:
            PHASES[name](K, li)
        if "xres_out" in debug:
            xo = nc.dram_tensor("xres_out", [T, D], F32, kind="ExternalOutput").ap()
            p.dma('sp', xo[:, :], K.xres[:, :], r=["xres"])
        p.finish()
    return nc


PHASE_ORDER = ["adaln", "inproj", "gdprep", "rwprep", "scan", "rwgdpost", "hyena", "s5", "outproj", "router", "experts"]
PHASES.update(rwprep=phase_rwprep, gdprep=phase_gdprep, rwgdpost=phase_rwgd_post, scan=phase_scan, hyena=phase_hyena, s5=phase_s5, adaln=phase_adaln, inproj=phase_inproj, outproj=phase_outproj, router=phase_router,
              experts=phase_experts)


_HYC = {}


def _hy_consts():
    if _HYC:
        return _HYC
    import ml_dtypes
    alt = np.where(np.arange(128) % 2 == 0, 1.0, -1.0).astype(np.float32)
    _HYC["hy_alt"] = np.stack([alt, alt], axis=1).astype(ml_dtypes.bfloat16)
    _HYC["hy_altrow"] = alt[None, :].astype(ml_dtypes.bfloat16)
    for sq_, L in (("ctx", LC), ("lat", LL)):
        t = np.linspace(0.0, 1.0, L, dtype=np.float32)[:, None]
        ang = (np.float32(2.0 * math.pi / L) * np.arange(L, dtype=np.float32))[:, None]
        bands = np.linspace(1e-4, 15.0, 16, dtype=np.float32)
        feat = np.concatenate([t, np.cos(bands * ang), -np.sin(bands * ang)], axis=-1).astype(np.float32)
        _HYC[f"hy_featT_{sq_}"] = np.ascontiguousarray(feat.T)
        deltas = np.abs(np.linspace(math.log(1e-2) / 1.5, math.log(1e-2) / 0.3, 256, dtype=np.float32))
        _HYC[f"hy_dec_{sq_}"] = np.exp(-t * deltas[None, :]).astype(np.float32)
        n = np.arange(L, dtype=np.int64)
        prod = (n[:, None] * n[None, :]) % (2 * L)
        ang2 = prod.astype(np.float64) * (2.0 * math.pi / (2 * L))
        nt = L // 128
        tabs = []
        for fn in (np.cos, np.sin):
            tb = fn(ang2).astype(np.float32).reshape(nt, 128, nt, 128)
            tabs.append(tb.transpose(2, 1, 0, 3).reshape(nt, 128, L))
        _HYC[f"dft_{sq_}"] = np.stack(tabs, axis=0).astype(ml_dtypes.bfloat16)
    return _HYC


def host_inputs(inputs, b):
    f = lambda a: np.ascontiguousarray(a, dtype=np.float32)
    m = {}
    m["xin"] = f(np.concatenate([inputs["ctx"][b], inputs["x"][b]], axis=0))
    c2 = np.stack([inputs["c"][b], inputs["c_ctx"]], axis=0)
    m["c2T"] = f(c2.reshape(2, 8, 128).transpose(2, 1, 0))
    m["ident"] = np.eye(128, dtype=np.float32)
    m["ada_bT"] = f(np.asarray(inputs["ada_b"]).reshape(DEPTH, 48, 128).transpose(0, 2, 1))
    w_in = np.asarray(inputs["w_in"])
    wp = np.zeros((DEPTH, D, ZROWS), np.float32)
    wp[:, :, 0:960] = w_in[:, :, 0:960]
    wp[:, :, 1024:2048] = w_in[:, :, 960:1984]
    wp[:, :, 2048:2064] = w_in[:, :, 1984:2000]
    wp[:, :, 2176:2944] = w_in[:, :, 2000:2768]
    wp[:, :, 2944:3200] = w_in[:, :, 2768:3024]
    m["w_in_p"] = wp
    def st(a):
        a = np.asarray(a).reshape(DEPTH, 2, 8, 2, 64)
        return f(a.transpose(0, 3, 4, 1, 2).reshape(DEPTH, 128, 16))
    m["s5_lreT"] = st(inputs["s5_lre"])
    m["s5_limT"] = st(inputs["s5_lim"])
    m["s5_dtT"] = st(np.broadcast_to(np.asarray(inputs["s5_logstep"])[..., None], (DEPTH, 2, 16, 64)))
    m["s5_dT"] = f(np.asarray(inputs["s5_d"]).reshape(DEPTH, 2, 128).transpose(0, 2, 1))
    m["s5_glbT"] = f(np.asarray(inputs["s5_glu_b"]).reshape(DEPTH, 2, 128).transpose(0, 2, 1))
    m["s5_glu_w"] = f(inputs["s5_glu_w"])

    def bT(a):
        a = np.asarray(a)
        o = np.zeros((DEPTH, 2, 8, 128, 128), np.float32)
        for j in range(8):
            for gg in range(2):
                g = 2 * j + gg
                r0 = (g % 8) * 16
                o[:, :, j, r0:r0 + 16, gg * 64:(gg + 1) * 64] = a[:, :, g].transpose(0, 1, 3, 2)
        return o.reshape(DEPTH, 16, 128, 128)

    def cT(a, sign=1.0):
        a = np.asarray(a)
        o = np.zeros((DEPTH, 2, 8, 128, 128), np.float32)
        for j in range(8):
            for gg in range(2):
                g = 2 * j + gg
                c0 = (g % 8) * 16
                o[:, :, j, gg * 64:(gg + 1) * 64, c0:c0 + 16] = a[:, :, g].transpose(0, 1, 3, 2)
        return o.reshape(DEPTH, 16, 128, 128)
    m["s5_breT"] = bT(inputs["s5_bre"])
    m["s5_bimT"] = bT(inputs["s5_bim"])
    m["s5_creT"] = cT(inputs["s5_cre"])
    m["s5_cimT"] = cT(inputs["s5_cim"])
    m.update(_hy_consts())
    import ml_dtypes
    w2, a2, g2 = np.asarray(inputs["rw_w2"]), np.asarray(inputs["rw_a2"]), np.asarray(inputs["rw_g2"])
    w2p = np.zeros((DEPTH, 4, 128, 128), np.float32)
    a2p = np.zeros((DEPTH, 4, 128, 128), np.float32)
    g2p = np.zeros((DEPTH, 2, 128, 128), np.float32)
    for d in range(2):
        for cc in range(2):
            w2p[:, d * 2 + cc, d * 32:(d + 1) * 32, :] = w2[:, d, :, cc * 128:(cc + 1) * 128]
            a2p[:, d * 2 + cc, 64 + d * 32:64 + (d + 1) * 32, :] = a2[:, d, :, cc * 128:(cc + 1) * 128]
    for cc in range(2):
        g2p[:, cc, 0:64, :] = g2[:, :, cc * 128:(cc + 1) * 128]
    m["rw_w2p"], m["rw_a2p"], m["rw_g2p"] = w2p, a2p, g2p
    cols = []
    for nm in ("rw_w0", "rw_a0"):
        a_ = np.asarray(inputs[nm])
        for d in range(2):
            for cc in range(2):
                cols.append(a_[:, d, cc * 128:(cc + 1) * 128])
    for nm in ("rw_kk", "rw_ka", "rw_rk", "rw_gn_g", "rw_gn_b"):
        a_ = np.asarray(inputs[nm]).reshape(DEPTH, 256)
        for cc in range(2):
            cols.append(a_[:, cc * 128:(cc + 1) * 128])
    m["rw_pT"] = f(np.stack(cols, axis=-1))
    blk = np.zeros((128, 128), np.float32)
    blk[0:64, 0:64] = 1.0
    blk[64:128, 64:128] = 1.0
    m["blk64"] = blk
    m["gd_normT"] = f(np.tile(np.asarray(inputs["gd_norm"]), (1, 2)))
    for k in ("gd_conv", "gd_dtb", "gd_alog"):
        m[k] = f(inputs[k])
    sel = np.zeros((6, 128), np.float32)
    for q in range(6):
        sel[q, (q % 2) * 64:(q % 2 + 1) * 64] = 1.0
    m["scan_sel"] = sel.astype(ml_dtypes.bfloat16)
    for k in ("hy_conv", "hy_conv_b", "hy_w1", "hy_w2", "hy_w3", "hy_b1", "hy_f1", "hy_b2", "hy_f2", "hy_skip"):
        m[k] = f(inputs[k])
    for k in ("ada_w", "ada_b", "w_out", "ln_g", "ln_b", "router_w", "router_b", "ex_w1", "ex_w3", "ex_w2",
              "sh_w1", "sh_w3", "sh_w2"):
        m[k] = f(inputs[k])
    return m


def kernel(**inputs):
    nc = build_program()
    names = set(LAST_INPUT_NAMES)
    in_maps = [{k: v for k, v in host_inputs(inputs, b).items() if k in names} for b in range(8)]
    res = run_bass_kernel_spmd(nc, in_maps, core_ids=list(range(8)))
    return np.stack([r["out"] for r in res.results], axis=0)
```

```python
import bisect
import contextlib
import math
import numpy as np
import concourse.bass as bass
import concourse.mybir as mybir
from concourse.bass_utils import run_bass_kernel_spmd

F32 = mybir.dt.float32
BF16 = mybir.dt.bfloat16
ALU = mybir.AluOpType
AF = mybir.ActivationFunctionType
AX = mybir.AxisListType

D = 1024
LC = 256
LL = 4096
T = LC + LL
NT = T // 128
DEPTH = 2
ALPHA = (2 * DEPTH) ** 0.25
ZROWS = 3200
ZR = dict(rw_r=0, rw_k=256, rw_v=512, rw_wa=768, rw_g=896, gd_q=1024, gd_k=1280, gd_v=1536, gd_zg=1792,
          gd_ab=2048, hy=2176, s5=2944)


class Prog:
    def __init__(self, nc, n_dma_sems=(('sp', 10), ('pool', 8), ('act', 4))):
        self.nc = nc
        self.E = {'pe': nc.tensor, 'dve': nc.vector, 'act': nc.scalar, 'pool': nc.gpsimd, 'sp': nc.sync}
        self.es = contextlib.ExitStack()
        self.dma_sems, self.dma_pool, self.dma_next = [], {}, {}
        for q, n in n_dma_sems:
            self.dma_pool[q] = list(range(len(self.dma_sems), len(self.dma_sems) + n))
            self.dma_next[q] = 0
            self.dma_sems += [self.es.enter_context(nc.semaphore(f"dq_{q}{i}")) for i in range(n)]
        self.dma_val = [0] * len(self.dma_sems)
        self.sem, self.cnt, self.ins, self.inc_idx, self.inc_cnt = {}, {}, {}, {}, {}
        self.nsem = 0
        for e in self.E:
            self._new_sem(e)
        self.waited, self.lastw, self.readers = {}, {}, {}

    def _new_sem(self, e):
        self.sem[e] = self.es.enter_context(self.nc.semaphore(f"es_{e}_{self.nsem}"))
        self.nsem += 1
        self.cnt[e] = 0
        self.ins[e] = []
        self.inc_idx[e] = []
        self.inc_cnt[e] = []

    def _eng_count(self, e, idx):
        lst = self.inc_idx[e]
        j = bisect.bisect_left(lst, idx)
        if j < len(lst):
            return self.inc_cnt[e][j]
        self.cnt[e] += 1
        self.ins[e][idx].then_inc(self.sem[e], 1)
        lst.append(idx)
        self.inc_cnt[e].append(self.cnt[e])
        return self.cnt[e]

    def _need(self, waiter, tok):
        if tok[0] == 'eng':
            _, e, idx, sem_id = tok
            if (e == waiter and e == 'pe') or sem_id != id(self.sem[e]):
                return None
            c = self._eng_count(e, idx)
            key = (waiter, 'eng', e, sem_id)
            if self.waited.get(key, 0) >= c:
                return None
            self.waited[key] = c
            return (self.sem[e], c)
        _, k, v = tok
        key = (waiter, 'dma', k)
        if self.waited.get(key, 0) >= v:
            return None
        self.waited[key] = v
        return (self.dma_sems[k], v)

    def _wait(self, waiter, tok):
        n = self._need(waiter, tok)
        if n is not None:
            self.E[waiter].wait_ge(n[0], n[1])

    def _waits(self, eng, deps):
        needs = [n for n in (self._need(eng, d) for d in self._order(deps)) if n is not None]
        for n in needs[:-1]:
            self.E[eng].wait_ge(n[0], n[1])
        return needs[-1] if needs else None

    def _deps(self, reads, writes):
        deps = []
        for k in reads:
            t = self.lastw.get(k)
            if t is not None:
                deps.append(t)
        for k in writes:
            t = self.lastw.get(k)
            if t is not None:
                deps.append(t)
            rd = self.readers.get(k)
            if rd:
                deps.extend(rd.values())
        return deps

    def _register(self, tok, reads, writes):
        for k in reads:
            self.readers.setdefault(k, {})[(tok[0], tok[1])] = tok
        for k in writes:
            self.lastw[k] = tok
            self.readers[k] = {}

    @staticmethod
    def _order(deps):
        return sorted(set(deps), key=lambda t: -t[2])

    def stage(self, eng, items):
        deps = []
        for (_, r, w) in items:
            deps.extend(self._deps(r, w))
        last = self._waits(eng, deps)
        for i, (fn, r, w) in enumerate(items):
            ins = fn(self.E[eng])
            if i == 0 and last is not None:
                ins._wait_ge(last[0], last[1])
            self.ins[eng].append(ins)
            self._register(('eng', eng, len(self.ins[eng]) - 1, id(self.sem[eng])), r, w)

    def op(self, eng, fn, r=(), w=()):
        last = self._waits(eng, self._deps(r, w))
        ins = fn(self.E[eng])
        if last is not None:
            ins._wait_ge(last[0], last[1])
        self.ins[eng].append(ins)
        tok = ('eng', eng, len(self.ins[eng]) - 1, id(self.sem[eng]))
        self._register(tok, r, w)
        return tok

    def dma(self, q, out, in_, r=(), w=(), **kw):
        for d in self._order(self._deps(r, w)):
            self._wait(q, d)
        k = self.dma_pool[q][self.dma_next[q]]
        self.dma_next[q] = (self.dma_next[q] + 1) % len(self.dma_pool[q])
        if self.dma_val[k] > 0:
            self._wait(q, ('dma', k, self.dma_val[k]))
        self.E[q].dma_start(out=out, in_=in_, **kw).then_inc(self.dma_sems[k], 16)
        self.dma_val[k] += 16
        tok = ('dma', k, self.dma_val[k])
        self._register(tok, r, w)
        return tok

    def barrier(self):
        toks = []
        for e in self.E:
            if self.ins[e]:
                toks.append(('eng', e, len(self.ins[e]) - 1, id(self.sem[e])))
        for k, v in enumerate(self.dma_val):
            if v > 0:
                toks.append(('dma', k, v))
        for waiter in self.E:
            for t in toks:
                self._wait(waiter, t)
        self.lastw, self.readers = {}, {}
        for e in self.E:
            if self.cnt[e] > 20000:
                self._new_sem(e)

    def finish(self):
        self.barrier()
        self.es.close()


class Ctx:
    pass


_UID = [0]


@contextlib.contextmanager
def _scope(K):
    with contextlib.ExitStack() as es2:
        yield es2
        K.p.barrier()


def _sb(nc, name, shape, dt):
    _UID[0] += 1
    return nc.sbuf_tensor(f"{name}_u{_UID[0]}", shape, dt)


def _copy_any(p, i, out, in_, r, w):
    if i % 2 == 0:
        p.op('act', lambda e: e.copy(out=out, in_=in_), r=r, w=w)
    else:
        p.op('dve', lambda e: e.tensor_copy(out=out, in_=in_), r=r, w=w)


def phase_adaln(K, li):
    p, nc = K.p, K.nc
    with contextlib.ExitStack() as es:
        c2 = es.enter_context(_sb(nc, "c2", [128, 8, 2], F32))
        cs = es.enter_context(_sb(nc, "cs", [128, 8, 2], F32))
        abT = es.enter_context(_sb(nc, "abT", [128, 48], F32))
        abrow = es.enter_context(_sb(nc, "abrow", [128, 2, 1024], F32))
        wblk = [es.enter_context(_sb(nc, f"adaw{i}", [128, 8, 1024], F32)) for i in range(2)]
        p.dma('sp', c2[:], K.inp["c2T"][:, :, :], w=["c2"])
        p.dma('sp', abT[:], K.inp["ada_bT"][li], w=["abT"])
        for gi, col in enumerate((2048, 5120)):
            p.dma('sp', abrow[:, gi, :], K.inp["ada_b"][li, col:col + 1024].partition_broadcast(128),
                  w=[f"abrow{gi}"])
        p.op('act', lambda e: e.activation(out=cs[:], in_=c2[:], func=AF.Sigmoid), r=["c2"], w=["cs"])
        p.op('dve', lambda e: e.tensor_tensor(out=cs[:], in0=cs[:], in1=c2[:], op=ALU.mult), r=["cs", "c2"],
             w=["cs"])
        aw = K.inp["ada_w"][li].rearrange("(k q) n -> q k n", q=128)
        for blk in range(6):
            wb = wblk[blk % 2]
            wk = f"adaw{blk % 2}"
            p.dma('sp', wb[:], aw[:, :, blk * 1024:(blk + 1) * 1024], w=[wk])
            for jj in range(8):
                j = blk * 8 + jj
                ps = K.ps[j % 2]
                for k in range(8):
                    p.op('pe', lambda e, k=k, jj=jj, ps=ps: e.matmul(
                        ps[:, 0:2], lhsT=wb[:, k, jj * 128:(jj + 1) * 128], rhs=cs[:, k, :],
                        start=(k == 0), stop=(k == 7)), r=[wk, "cs"], w=[f"ps{j % 2}"])
                addc = 1.0 if blk in (1, 4) else 0.0
                p.op('dve', lambda e, j=j, ps=ps, addc=addc: e.tensor_scalar(
                    out=K.modT[:, j, :], in0=ps[:, 0:2], scalar1=abT[:, j:j + 1], scalar2=addc, op0=ALU.add,
                    op1=ALU.add), r=[f"ps{j % 2}", "abT"], w=["modT"])
            if blk in (2, 5):
                gi = 0 if blk == 2 else 1
                for which in range(2):
                    for half in range(2):
                        ps = K.ps[2 + half]
                        for k in range(8):
                            p.op('pe', lambda e, k=k, ps=ps, half=half, which=which: e.matmul(
                                ps[:, :], lhsT=cs[:, k, which:which + 1].to_broadcast([128, 128]),
                                rhs=wb[:, k, half * 512:(half + 1) * 512], start=(k == 0), stop=(k == 7)),
                                r=[wk, "cs"], w=[f"ps{2 + half}"])
                        g = K.gate[(which, gi)]
                        p.op('dve', lambda e, ps=ps, g=g, half=half, gi=gi: e.tensor_tensor(
                            out=g[:, half * 512:(half + 1) * 512], in0=ps[:, :],
                            in1=abrow[:, gi, half * 512:(half + 1) * 512], op=ALU.add),
                            r=[f"ps{2 + half}", f"abrow{gi}"], w=[f"gate{which}{gi}"])
        p.barrier()


def phase_inproj(K, li):
    p, nc = K.p, K.nc
    with contextlib.ExitStack() as es:
        win = es.enter_context(_sb(nc, "win", [128, 8, ZROWS], BF16))
        wst = [es.enter_context(_sb(nc, f"wst{i}", [128, ZROWS], F32)) for i in range(2)]
        xt = [es.enter_context(_sb(nc, f"xt{i}", [128, 1024], F32)) for i in range(2)]
        xmT = [es.enter_context(_sb(nc, f"xmT{i}", [128, 8, 512], BF16)) for i in range(2)]
        zst = [es.enter_context(_sb(nc, f"zst{i}", [128, 512], F32)) for i in range(4)]
        wsrc = K.inp["w_in_p"][li].rearrange("(k q) n -> q k n", q=128)
        for k in range(8):
            p.dma('sp', wst[k % 2][:], wsrc[:, k, :], w=[f"wst{k % 2}"])
            _copy_any(p, k, win[:, k, :], wst[k % 2][:], r=[f"wst{k % 2}"], w=["win"])
        groups = [(0, 2)] + [(2 + 4 * g, 4) for g in range(8)]
        nst = 0
        ntile = 0
        for gidx, (t0, ntl) in enumerate(groups):
            which = 1 if gidx == 0 else 0
            xm = xmT[gidx % 2]
            xmk = f"xmT{gidx % 2}"
            for tl in range(ntl):
                tt = t0 + tl
                xb = xt[ntile % 2]
                xk = f"xt{ntile % 2}"
                ntile += 1
                p.dma('sp', xb[:], K.xres[tt * 128:(tt + 1) * 128, :], r=["xres"], w=[xk])
                for k in range(8):
                    bank = 4 + 2 * (ntile % 2) + (k // 4)
                    p.op('pe', lambda e, k=k, bank=bank, xb=xb: e.transpose(
                        out=K.ps[bank][:, (k % 4) * 128:(k % 4 + 1) * 128], in_=xb[:, k * 128:(k + 1) * 128],
                        identity=K.ident[:]), r=[xk, "ident"], w=[f"ps{bank}"])
                for k in range(8):
                    bank = 4 + 2 * (ntile % 2) + (k // 4)
                    p.op('act', lambda e, k=k, bank=bank, xm=xm, tl=tl, which=which: e.activation(
                        out=xm[:, k, tl * 128:(tl + 1) * 128], in_=K.ps[bank][:, (k % 4) * 128:(k % 4 + 1) * 128],
                        func=AF.Identity, scale=K.modT[:, 8 + k, which:which + 1],
                        bias=K.modT[:, k, which:which + 1]), r=[f"ps{bank}", "modT"], w=[xmk])
            ntok = ntl * 128
            for oc in range(ZROWS // 128):
                bank = oc % 4
                for k in range(8):
                    p.op('pe', lambda e, k=k, bank=bank, oc=oc, xm=xm, ntok=ntok: e.matmul(
                        K.ps[bank][:, 0:ntok], lhsT=win[:, k, oc * 128:(oc + 1) * 128], rhs=xm[:, k, 0:ntok],
                        start=(k == 0), stop=(k == 7)), r=["win", xmk], w=[f"ps{bank}"])
                zs = zst[nst % 4]
                zk = f"zst{nst % 4}"
                _copy_any(p, nst, zs[:, 0:ntok], K.ps[bank][:, 0:ntok], r=[f"ps{bank}"], w=[zk])
                p.dma('pool', K.zfm[oc * 128:(oc + 1) * 128, t0 * 128:t0 * 128 + ntok], zs[:, 0:ntok], r=[zk],
                      w=["zfm"])
                nst += 1
        p.barrier()


def _load_bcast(K, es, name, src_row, n):
    t = es.enter_context(_sb(K.nc, name, [128, n], F32))
    K.p.dma('sp', t[:], src_row.partition_broadcast(128), w=[name])
    return t


def _ln_tile(K, xt, xk, stat, sk, sq, sqk, g, gk, b, bk):
    p = K.p
    p.op('act', lambda e: e.activation(out=sq[:], in_=xt[:], func=AF.Square, accum_out=stat[:, 1:2]),
         r=[xk], w=[sqk, sk + "q"])
    p.op('dve', lambda e: e.tensor_scalar(out=stat[:, 2:3], in0=stat[:, 0:1], scalar1=1.0 / D, scalar2=None,
                                          op0=ALU.mult), r=[sk], w=[sk + "m"])
    p.op('dve', lambda e: e.tensor_tensor(out=stat[:, 3:4], in0=stat[:, 2:3], in1=stat[:, 2:3], op=ALU.mult),
         r=[sk + "m"], w=[sk + "v"])
    p.op('dve', lambda e: e.scalar_tensor_tensor(out=stat[:, 3:4], in0=stat[:, 1:2], scalar=1.0 / D,
                                                 in1=stat[:, 3:4], op0=ALU.mult, op1=ALU.subtract),
         r=[sk + "q", sk + "v"], w=[sk + "v"])
    p.op('dve', lambda e: e.tensor_scalar(out=stat[:, 3:4], in0=stat[:, 3:4], scalar1=1e-5, scalar2=None,
                                          op0=ALU.add), r=[sk + "v"], w=[sk + "v"])
    p.op('act', lambda e: e.activation(out=stat[:, 4:5], in_=stat[:, 3:4], func=AF.Sqrt), r=[sk + "v"],
         w=[sk + "r"])
    p.op('dve', lambda e: e.reciprocal(out=stat[:, 4:5], in_=stat[:, 4:5]), r=[sk + "r"], w=[sk + "r"])
    p.op('dve', lambda e: e.tensor_scalar(out=xt[:], in0=xt[:], scalar1=stat[:, 2:3], scalar2=stat[:, 4:5],
                                          op0=ALU.subtract, op1=ALU.mult), r=[xk, sk + "m", sk + "r"], w=[xk])
    p.op('dve', lambda e: e.tensor_tensor(out=xt[:], in0=xt[:], in1=g[:], op=ALU.mult), r=[xk, gk], w=[xk])
    p.op('dve', lambda e: e.tensor_tensor(out=xt[:], in0=xt[:], in1=b[:], op=ALU.add), r=[xk, bk], w=[xk])


def phase_outproj(K, li):
    p, nc = K.p, K.nc
    need_ctx = li < DEPTH - 1
    with contextlib.ExitStack() as es:
        wo = es.enter_context(_sb(nc, "wo", [128, 8, 1024], BF16))
        wst = [es.enter_context(_sb(nc, f"wost{i}", [128, 1024], F32)) for i in range(2)]
        lng = _load_bcast(K, es, "lng", K.inp["ln_g"][li, 0], D)
        lnb = _load_bcast(K, es, "lnb", K.inp["ln_b"][li, 0], D)
        mst = [es.enter_context(_sb(nc, f"mst{i}", [128, 8, 128], F32)) for i in range(2)]
        mbf = [es.enter_context(_sb(nc, f"mbf{i}", [128, 8, 128], BF16)) for i in range(2)]
        xt = [es.enter_context(_sb(nc, f"xo{i}", [128, 1024], F32)) for i in range(2)]
        t1 = [es.enter_context(_sb(nc, f"t1_{i}", [128, 1024], F32)) for i in range(2)]
        sq = es.enter_context(_sb(nc, "sqo", [128, 1024], F32))
        stat = [es.enter_context(_sb(nc, f"st{i}", [128, 8], F32)) for i in range(2)]
        wsrc = K.inp["w_out"][li].rearrange("(k q) n -> q k n", q=128)
        for k in range(8):
            p.dma('sp', wst[k % 2][:], wsrc[:, k, :], w=[f"wost{k % 2}"])
            _copy_any(p, k, wo[:, k, :], wst[k % 2][:], r=[f"wost{k % 2}"], w=["wo"])
        msrc = K.mix.rearrange("(k q) t -> q k t", q=128)
        tiles = list(range(0 if need_ctx else 2, NT))
        for n, tt in enumerate(tiles):
            i = n % 2
            which = 1 if tt < 2 else 0
            p.dma('sp', mst[i][:], msrc[:, :, tt * 128:(tt + 1) * 128], r=["mix"], w=[f"mst{i}"])
            p.dma('sp', xt[i][:], K.xres[tt * 128:(tt + 1) * 128, :], r=["xres"], w=[f"xo{i}"])
            _copy_any(p, n, mbf[i][:], mst[i][:], r=[f"mst{i}"], w=[f"mbf{i}"])
            for half in range(2):
                bank = 2 * i + half
                for k in range(8):
                    p.op('pe', lambda e: e.matmul(K.ps[bank][:, :], lhsT=mbf[i][:, k, :],
                                                  rhs=wo[:, k, half * 512:(half + 1) * 512], start=(k == 0),
                                                  stop=(k == 7)), r=[f"mbf{i}", "wo"], w=[f"ps{bank}"])
                p.op('dve', lambda e: e.tensor_tensor(out=t1[i][:, half * 512:(half + 1) * 512],
                                                      in0=K.ps[bank][:, :],
                                                      in1=K.gate[(which, 0)][:, half * 512:(half + 1) * 512],
                                                      op=ALU.mult), r=[f"ps{bank}", f"gate{which}0"], w=[f"t1_{i}"])
            p.op('dve', lambda e: e.scalar_tensor_tensor(out=xt[i][:], in0=xt[i][:], scalar=ALPHA, in1=t1[i][:],
                                                         op0=ALU.mult, op1=ALU.add, accum_out=stat[i][:, 0:1]),
                 r=[f"xo{i}", f"t1_{i}"], w=[f"xo{i}", f"st{i}"])
            _ln_tile(K, xt[i], f"xo{i}", stat[i], f"st{i}", sq, "sqo", lng, "lng", lnb, "lnb")
            p.dma('pool', K.xres[tt * 128:(tt + 1) * 128, :], xt[i][:], r=[f"xo{i}"], w=["xres"])
        p.barrier()


def phase_router(K, li):
    p, nc = K.p, K.nc
    need_ctx = li < DEPTH - 1
    BIG = 1.0e30
    with contextlib.ExitStack() as es:
        rw_ = es.enter_context(_sb(nc, "rtw", [128, 8, 64], F32))
        rb = _load_bcast(K, es, "rtb", K.inp["router_b"][li], 64)
        xt = [es.enter_context(_sb(nc, f"xr{i}", [128, 1024], F32)) for i in range(2)]
        hT = [es.enter_context(_sb(nc, f"hT{i}", [128, 8, 128], F32)) for i in range(2)]
        hb = [es.enter_context(_sb(nc, f"hb{i}", [128, 8, 128], BF16)) for i in range(2)]
        sc = es.enter_context(_sb(nc, "rsc", [128, 64], F32))
        bi = es.enter_context(_sb(nc, "rbi", [128, 64], F32))
        b2 = es.enter_context(_sb(nc, "rb2", [128, 64], F32))
        mk = es.enter_context(_sb(nc, "rmk", [128, 64], F32))
        g1 = es.enter_context(_sb(nc, "rg1", [128, 8], F32))
        g2 = es.enter_context(_sb(nc, "rg2", [128, 8], F32))
        gm = es.enter_context(_sb(nc, "rgm", [128, 8], F32))
        m8 = es.enter_context(_sb(nc, "rm8", [128, 8], F32))
        ssum = es.enter_context(_sb(nc, "rss", [128, 2], F32))
        p.dma('sp', rw_[:], K.inp["router_w"][li].rearrange("(k q) n -> q k n", q=128), w=["rtw"])
        tiles = list(range(0 if need_ctx else 2, NT))
        for n, tt in enumerate(tiles):
            i = n % 2
            which = 1 if tt < 2 else 0
            p.dma('sp', xt[i][:], K.xres[tt * 128:(tt + 1) * 128, :], r=["xres"], w=[f"xr{i}"])
            for k in range(8):
                bank = 4 * i + (k // 4)
                p.op('pe', lambda e: e.transpose(out=K.ps[bank][:, (k % 4) * 128:(k % 4 + 1) * 128],
                                                 in_=xt[i][:, k * 128:(k + 1) * 128], identity=K.ident[:]),
                     r=[f"xr{i}", "ident"], w=[f"ps{bank}"])
            for k in range(8):
                bank = 4 * i + (k // 4)
                p.op('act', lambda e: e.activation(out=hT[i][:, k, :],
                                                   in_=K.ps[bank][:, (k % 4) * 128:(k % 4 + 1) * 128],
                                                   func=AF.Identity, scale=K.modT[:, 32 + k, which:which + 1],
                                                   bias=K.modT[:, 24 + k, which:which + 1]),
                     r=[f"ps{bank}", "modT"], w=[f"hT{i}"])
            p.op('dve', lambda e: e.tensor_copy(out=hb[i][:], in_=hT[i][:]), r=[f"hT{i}"], w=[f"hb{i}"])
            p.dma('pool', K.hT.rearrange("(k q) t -> q k t", q=128)[:, :, tt * 128:(tt + 1) * 128], hb[i][:],
                  r=[f"hb{i}"], w=["hTd"])
            bank = 4 * i + 2
            for k in range(8):
                p.op('pe', lambda e: e.matmul(K.ps[bank][:, 0:64], lhsT=hT[i][:, k, :], rhs=rw_[:, k, :],
                                              start=(k == 0), stop=(k == 7)), r=[f"hT{i}", "rtw"], w=[f"ps{bank}"])
            p.op('act', lambda e: e.activation(out=sc[:], in_=K.ps[bank][:, 0:64], func=AF.Sigmoid),
                 r=[f"ps{bank}"], w=["rsc"])
            V = lambda fn, r, w: p.op('dve', fn, r=r, w=w)
            V(lambda e: e.tensor_tensor(out=bi[:], in0=sc[:], in1=rb[:], op=ALU.add), ["rsc", "rtb"], ["rbi"])
            bi3 = bi[:].rearrange("q (g j) -> q g j", j=8)
            b23 = b2[:].rearrange("q (g j) -> q g j", j=8)
            mk3 = mk[:].rearrange("q (g j) -> q g j", j=8)
            V(lambda e: e.tensor_reduce(out=g1[:], in_=bi3, axis=AX.X, op=ALU.max), ["rbi"], ["rg1"])
            V(lambda e: e.tensor_tensor(out=mk3, in0=bi3, in1=g1[:].unsqueeze(2).to_broadcast([128, 8, 8]),
                                        op=ALU.is_equal), ["rbi", "rg1"], ["rmk"])
            V(lambda e: e.scalar_tensor_tensor(out=b2[:], in0=mk[:], scalar=-BIG, in1=bi[:], op0=ALU.mult,
                                               op1=ALU.add), ["rmk", "rbi"], ["rb2"])
            V(lambda e: e.tensor_reduce(out=g2[:], in_=b23, axis=AX.X, op=ALU.max), ["rb2"], ["rg2"])
            V(lambda e: e.tensor_tensor(out=g1[:], in0=g1[:], in1=g2[:], op=ALU.add), ["rg1", "rg2"], ["rg1"])
            V(lambda e: e.max(out=m8[:], in_=g1[:]), ["rg1"], ["rm8"])
            V(lambda e: e.tensor_scalar(out=gm[:], in0=g1[:], scalar1=m8[:, 3:4], scalar2=None, op0=ALU.is_ge),
              ["rg1", "rm8"], ["rgm"])
            V(lambda e: e.tensor_tensor(out=b23, in0=bi3, in1=gm[:].unsqueeze(2).to_broadcast([128, 8, 8]),
                                        op=ALU.mult), ["rbi", "rgm"], ["rb2"])
            V(lambda e: e.tensor_scalar(out=gm[:], in0=gm[:], scalar1=BIG, scalar2=BIG, op0=ALU.mult,
                                        op1=ALU.subtract), ["rgm"], ["rgm"])
            V(lambda e: e.tensor_tensor(out=b23, in0=b23, in1=gm[:].unsqueeze(2).to_broadcast([128, 8, 8]),
                                        op=ALU.add), ["rb2", "rgm"], ["rb2"])
            V(lambda e: e.max(out=m8[:], in_=b2[:]), ["rb2"], ["rm8"])
            V(lambda e: e.tensor_scalar(out=mk[:], in0=b2[:], scalar1=m8[:, 7:8], scalar2=None, op0=ALU.is_ge),
              ["rb2", "rm8"], ["rmk"])
            V(lambda e: e.scalar_tensor_tensor(out=mk[:], in0=sc[:], scalar=1.0, in1=mk[:], op0=ALU.mult,
                                               op1=ALU.mult, accum_out=ssum[:, 0:1]), ["rsc", "rmk"],
              ["rmk", "rss"])
            V(lambda e: e.reciprocal(out=ssum[:, 1:2], in_=ssum[:, 0:1]), ["rss"], ["rss"])
            V(lambda e: e.tensor_scalar(out=K.gates[:, tt, :], in0=mk[:], scalar1=ssum[:, 1:2], scalar2=2.5,
                                        op0=ALU.mult, op1=ALU.mult), ["rmk", "rss"], ["gates"])
        p.barrier()


def phase_experts(K, li):
    p, nc = K.p, K.nc
    need_ctx = li < DEPTH - 1
    t_lo = 0 if need_ctx else 2
    parts = [(t_lo, 17), (17, NT)]
    with contextlib.ExitStack() as es:
        NTP = 17
        hT = es.enter_context(_sb(nc, "ehT", [128, 8, NTP * 128], BF16))
        acc = es.enter_context(_sb(nc, "eacc", [128, NTP, 1024], F32))
        w13 = [es.enter_context(_sb(nc, f"ew13_{i}", [128, 2, 8, 256], BF16)) for i in range(2)]
        w2 = [es.enter_context(_sb(nc, f"ew2_{i}", [128, 2, 1024], BF16)) for i in range(2)]
        g = [es.enter_context(_sb(nc, f"eg{i}", [128, 2, NTP * 128], BF16)) for i in range(2)]
        sa = [es.enter_context(_sb(nc, f"esa{i}", [128, 512], F32)) for i in range(2)]
        lng = _load_bcast(K, es, "lng2", K.inp["ln_g"][li, 1], D)
        lnb = _load_bcast(K, es, "lnb2", K.inp["ln_b"][li, 1], D)
        xt = [es.enter_context(_sb(nc, f"xe{i}", [128, 1024], F32)) for i in range(2)]
        sq = es.enter_context(_sb(nc, "sqe", [128, 1024], F32))
        stat = [es.enter_context(_sb(nc, f"ste{i}", [128, 8], F32)) for i in range(2)]
        hsrc = K.hT.rearrange("(k q) t -> q k t", q=128)
        nsa = 0
        for (ta, tb) in parts:
            ntl = tb - ta
            ntok = ntl * 128
            p.dma('sp', hT[:, :, 0:ntok], hsrc[:, :, ta * 128:tb * 128], r=["hTd"], w=["ehT"])
            for ei in range(65):
                i = ei % 2
                if ei < 64:
                    s1, s3, s2 = K.inp["ex_w1"][li, ei], K.inp["ex_w3"][li, ei], K.inp["ex_w2"][li, ei]
                else:
                    s1, s3, s2 = K.inp["sh_w1"][li], K.inp["sh_w3"][li], K.inp["sh_w2"][li]
                p.dma('pool', w13[i][:, 0], s1.rearrange("(k q) f -> q k f", q=128), w=[f"ew13_{i}"])
                p.dma('pool', w13[i][:, 1], s3.rearrange("(k q) f -> q k f", q=128), w=[f"ew13_{i}"])
                p.dma('pool', w2[i][:], s2.rearrange("(c q) n -> q c n", q=128), w=[f"ew2_{i}"])
                blocks = [(b0, min(512, ntok - b0)) for b0 in range(0, ntok, 512)]
                for fc in range(2):
                    for (b0, bn) in blocks:
                        for ab in range(2):
                            bank = 2 * (nsa % 2) + ab
                            for k in range(8):
                                p.op('pe', lambda e: e.matmul(
                                    K.ps[bank][:, 0:bn], lhsT=w13[i][:, ab, k, fc * 128:(fc + 1) * 128],
                                    rhs=hT[:, k, b0:b0 + bn], start=(k == 0), stop=(k == 7)),
                                    r=[f"ew13_{i}", "ehT"], w=[f"ps{bank}"])
                        ba = 2 * (nsa % 2)
                        sab = sa[nsa % 2]
                        p.op('act', lambda e: e.activation(out=sab[:, 0:bn], in_=K.ps[ba][:, 0:bn], func=AF.Silu),
                             r=[f"ps{ba}"], w=[f"esa{nsa % 2}"])
                        p.op('dve', lambda e: e.tensor_tensor(out=g[i][:, fc, b0:b0 + bn], in0=sab[:, 0:bn],
                                                              in1=K.ps[ba + 1][:, 0:bn], op=ALU.mult),
                             r=[f"esa{nsa % 2}", f"ps{ba + 1}"], w=[f"eg{i}"])
                        nsa += 1
                for tl in range(ntl):
                    tt = ta + tl
                    for half in range(2):
                        bank = 4 + 2 * (tl % 2) + half
                        for fc in range(2):
                            p.op('pe', lambda e: e.matmul(
                                K.ps[bank][:, :], lhsT=g[i][:, fc, tl * 128:(tl + 1) * 128],
                                rhs=w2[i][:, fc, half * 512:(half + 1) * 512], start=(fc == 0), stop=(fc == 1)),
                                r=[f"eg{i}", f"ew2_{i}"], w=[f"ps{bank}"])
                        a_out = acc[:, tl, half * 512:(half + 1) * 512]
                        if ei == 0:
                            p.op('dve', lambda e: e.tensor_scalar(out=a_out, in0=K.ps[bank][:, :],
                                                                  scalar1=K.gates[:, tt, 0:1], scalar2=None,
                                                                  op0=ALU.mult), r=[f"ps{bank}", "gates"],
                                 w=[f"eacc{tl}"])
                        else:
                            scal = K.gates[:, tt, ei:ei + 1] if ei < 64 else 1.0
                            p.op('dve', lambda e: e.scalar_tensor_tensor(out=a_out, in0=K.ps[bank][:, :],
                                                                         scalar=scal, in1=a_out, op0=ALU.mult,
                                                                         op1=ALU.add),
                                 r=[f"ps{bank}", "gates", f"eacc{tl}"], w=[f"eacc{tl}"])
            for tl in range(ntl):
                tt = ta + tl
                i = tl % 2
                which = 1 if tt < 2 else 0
                p.dma('sp', xt[i][:], K.xres[tt * 128:(tt + 1) * 128, :], r=["xres"], w=[f"xe{i}"])
                p.op('dve', lambda e: e.tensor_tensor(out=acc[:, tl, :], in0=acc[:, tl, :],
                                                      in1=K.gate[(which, 1)][:], op=ALU.mult),
                     r=[f"eacc{tl}", f"gate{which}1"], w=[f"eacc{tl}"])
                p.op('dve', lambda e: e.scalar_tensor_tensor(out=xt[i][:], in0=xt[i][:], scalar=ALPHA,
                                                             in1=acc[:, tl, :], op0=ALU.mult, op1=ALU.add,
                                                             accum_out=stat[i][:, 0:1]),
                     r=[f"xe{i}", f"eacc{tl}"], w=[f"xe{i}", f"ste{i}"])
                _ln_tile(K, xt[i], f"xe{i}", stat[i], f"ste{i}", sq, "sqe", lng, "lng2", lnb, "lnb2")
                if li == DEPTH - 1:
                    p.dma('pool', K.out[(tt - 2) * 128:(tt - 1) * 128, :], xt[i][:], r=[f"xe{i}"], w=["out"])
                else:
                    p.dma('pool', K.xres[tt * 128:(tt + 1) * 128, :], xt[i][:], r=[f"xe{i}"], w=["xres"])
        p.barrier()


TWO_PI = 2.0 * math.pi
MAGIC = 12582912.0
PI_LO = 3.1415925


def _sin(K, out, x, shift, tmp, keys_r, key_w, key_t):
    p = K.p
    p.op('dve', lambda e: e.tensor_scalar(out=out, in0=x, scalar1=shift, scalar2=None, op0=ALU.add), r=keys_r,
         w=[key_w])
    p.op('dve', lambda e: e.tensor_scalar(out=tmp, in0=out, scalar1=1.0 / TWO_PI, scalar2=MAGIC, op0=ALU.mult,
                                          op1=ALU.add), r=[key_w], w=[key_t])
    p.op('dve', lambda e: e.tensor_scalar(out=tmp, in0=tmp, scalar1=-MAGIC, scalar2=-TWO_PI, op0=ALU.add,
                                          op1=ALU.mult), r=[key_t], w=[key_t])
    p.op('dve', lambda e: e.tensor_tensor(out=tmp, in0=tmp, in1=out, op=ALU.add), r=[key_t, key_w], w=[key_t])
    p.op('dve', lambda e: e.tensor_scalar(out=tmp, in0=tmp, scalar1=PI_LO, scalar2=-PI_LO, op0=ALU.min,
                                          op1=ALU.max), r=[key_t], w=[key_t])
    p.op('act', lambda e: e.activation(out=out, in_=tmp, func=AF.Sin), r=[key_t], w=[key_w])


S5_BLOCKS0 = [(0, 256)] + [(256 + 512 * b, 512) for b in range(8)]
NDBL = 13
S5_Q = 16
S5_LQ = 4
S5_NCH = T // S5_Q
S5_LC = 9


def phase_s5(K, li):
    p, nc = K.p, K.nc
    V = lambda fn, r, w: p.op('dve', fn, r=r, w=w)
    with contextlib.ExitStack() as es:
        SB = lambda name, shape, dt=F32: es.enter_context(_sb(nc, name, shape, dt))
        lre, lim, dtt = SB("s5lre", [128, 16]), SB("s5lim", [128, 16]), SB("s5dt", [128, 16])
        ar, ai, mag = SB("s5ar", [128, 16]), SB("s5ai", [128, 16]), SB("s5mag", [128, 16])
        sn, cs_, tmp = SB("s5sn", [128, 16]), SB("s5cs", [128, 16]), SB("s5tmp", [128, 16])
        den, t2 = SB("s5den", [128, 16]), SB("s5t2", [128, 16])
        co_re, co_im, co_imn = SB("s5core", [128, 16]), SB("s5coim", [128, 16]), SB("s5coimn", [128, 16])
        pw_re, pw_im, pw_imn = (SB("s5pwre", [128, NDBL, 16]), SB("s5pwim", [128, NDBL, 16]),
                                SB("s5pwimn", [128, NDBL, 16]))
        dsk, glb = SB("s5d", [128, 2]), SB("s5glb", [128, 2])
        gluw = SB("s5gluw", [128, 2, 256], BF16)
        p.dma('sp', lre[:], K.inp["s5_lreT"][li], w=["lre"])
        p.dma('sp', lim[:], K.inp["s5_limT"][li], w=["lim"])
        p.dma('sp', dtt[:], K.inp["s5_dtT"][li], w=["dtt"])
        p.dma('sp', dsk[:], K.inp["s5_dT"][li], w=["dsk"])
        p.dma('sp', glb[:], K.inp["s5_glbT"][li], w=["glb"])
        p.dma('pool', gluw[:], K.inp["s5_glu_w"][li].rearrange("(c q) n -> q c n", q=128), w=["gluw"])
        p.op('act', lambda e: e.activation(out=dtt[:], in_=dtt[:], func=AF.Exp), r=["dtt"], w=["dtt"])
        V(lambda e: e.tensor_tensor(out=ar[:], in0=lre[:], in1=dtt[:], op=ALU.mult), ["lre", "dtt"], ["ar"])
        V(lambda e: e.tensor_tensor(out=ai[:], in0=lim[:], in1=dtt[:], op=ALU.mult), ["lim", "dtt"], ["ai"])
        p.op('act', lambda e: e.activation(out=mag[:], in_=ar[:], func=AF.Exp), r=["ar"], w=["mag"])
        _sin(K, sn[:], ai[:], 0.0, tmp[:], ["ai"], "sn", "s5tmp")
        _sin(K, cs_[:], ai[:], math.pi / 2.0, tmp[:], ["ai"], "cs", "s5tmp")
        V(lambda e: e.tensor_tensor(out=pw_re[:, 0, :], in0=mag[:], in1=cs_[:], op=ALU.mult), ["mag", "cs"], ["pw"])
        V(lambda e: e.tensor_tensor(out=pw_im[:, 0, :], in0=mag[:], in1=sn[:], op=ALU.mult), ["mag", "sn"], ["pw"])
        V(lambda e: e.tensor_tensor(out=den[:], in0=lre[:], in1=lre[:], op=ALU.mult), ["lre"], ["den"])
        V(lambda e: e.tensor_tensor(out=t2[:], in0=lim[:], in1=lim[:], op=ALU.mult), ["lim"], ["t2"])
        V(lambda e: e.tensor_tensor(out=den[:], in0=den[:], in1=t2[:], op=ALU.add), ["den", "t2"], ["den"])
        V(lambda e: e.reciprocal(out=den[:], in_=den[:]), ["den"], ["den"])
        V(lambda e: e.tensor_scalar(out=tmp[:], in0=pw_re[:, 0, :], scalar1=-1.0, scalar2=None, op0=ALU.add),
          ["pw"], ["s5tmp"])
        V(lambda e: e.tensor_tensor(out=co_re[:], in0=tmp[:], in1=lre[:], op=ALU.mult), ["s5tmp", "lre"], ["core"])
        V(lambda e: e.tensor_tensor(out=t2[:], in0=pw_im[:, 0, :], in1=lim[:], op=ALU.mult), ["pw", "lim"], ["t2"])
        V(lambda e: e.tensor_tensor(out=co_re[:], in0=co_re[:], in1=t2[:], op=ALU.add), ["core", "t2"], ["core"])
        V(lambda e: e.tensor_tensor(out=co_re[:], in0=co_re[:], in1=den[:], op=ALU.mult), ["core", "den"], ["core"])
        V(lambda e: e.tensor_tensor(out=co_im[:], in0=pw_im[:, 0, :], in1=lre[:], op=ALU.mult), ["pw", "lre"],
          ["coim"])
        V(lambda e: e.tensor_tensor(out=t2[:], in0=tmp[:], in1=lim[:], op=ALU.mult), ["s5tmp", "lim"], ["t2"])
        V(lambda e: e.tensor_tensor(out=co_im[:], in0=co_im[:], in1=t2[:], op=ALU.subtract), ["coim", "t2"],
          ["coim"])
        V(lambda e: e.tensor_tensor(out=co_im[:], in0=co_im[:], in1=den[:], op=ALU.mult), ["coim", "den"], ["coim"])
        V(lambda e: e.tensor_scalar(out=co_imn[:], in0=co_im[:], scalar1=-1.0, scalar2=None, op0=ALU.mult),
          ["coim"], ["coimn"])
        for m in range(1, NDBL):
            V(lambda e: e.tensor_tensor(out=tmp[:], in0=pw_re[:, m - 1, :], in1=pw_re[:, m - 1, :], op=ALU.mult),
              ["pw"], ["s5tmp"])
            V(lambda e: e.tensor_tensor(out=t2[:], in0=pw_im[:, m - 1, :], in1=pw_im[:, m - 1, :], op=ALU.mult),
              ["pw"], ["t2"])
            V(lambda e: e.tensor_tensor(out=pw_re[:, m, :], in0=tmp[:], in1=t2[:], op=ALU.subtract),
              ["s5tmp", "t2"], ["pw"])
            V(lambda e: e.tensor_tensor(out=tmp[:], in0=pw_re[:, m - 1, :], in1=pw_im[:, m - 1, :], op=ALU.mult),
              ["pw"], ["s5tmp"])
            V(lambda e: e.tensor_scalar(out=pw_im[:, m, :], in0=tmp[:], scalar1=2.0, scalar2=None, op0=ALU.mult),
              ["s5tmp"], ["pw"])
        V(lambda e: e.tensor_scalar(out=pw_imn[:], in0=pw_im[:], scalar1=-1.0, scalar2=None, op0=ALU.mult),
          ["pw"], ["pwn"])
        ub = SB("s5ub", [128, 2, T], BF16)
        Y = SB("s5Y", [128, 2, T])
        with _scope(K) as es2:
            ust = es2.enter_context(_sb(nc, "s5ust", [128, T], F32))
            for c in range(2):
                r0 = ZR["s5"] + c * 128
                p.dma('sp', ust[:], K.zfm[r0:r0 + 128, :], r=["zfm"], w=["ust"])
                lat_in = ust[:, LC:T].rearrange("q (r c) -> q c r", c=64)
                lat_ub = ub[:, c, LC:T].rearrange("q (c r) -> q c r", r=64)
                lat_y = Y[:, c, LC:T].rearrange("q (c r) -> q c r", r=64)
                p.op('act', lambda e: e.copy(out=ub[:, c, 0:LC], in_=ust[:, 0:LC]), r=["ust"], w=["ub"])
                p.op('act', lambda e: e.copy(out=lat_ub, in_=lat_in), r=["ust"], w=["ub"])
                V(lambda e: e.tensor_scalar(out=Y[:, c, 0:LC], in0=ust[:, 0:LC], scalar1=dsk[:, c:c + 1],
                                            scalar2=None, op0=ALU.mult), ["ust", "dsk"], [f"Y{c}"])
                V(lambda e: e.tensor_scalar(out=lat_y, in0=lat_in, scalar1=dsk[:, c:c + 1], scalar2=None,
                                            op0=ALU.mult), ["ust", "dsk"], [f"Y{c}"])
        with _scope(K) as es2:
            SB2 = lambda name, shape, dt=F32: es2.enter_context(_sb(nc, name, shape, dt))
            X = [[SB2(f"s5X{a}{ri}", [128, T]) for ri in range(2)] for a in range(2)]
            xb = [SB2(f"s5xb{ri}", [128, T], BF16) for ri in range(2)]
            bc = [SB2(f"s5bc{i}", [128, 4, 128], BF16) for i in range(2)]
            tq = [SB2(f"s5tq{i}", [128, 512]) for i in range(2)]
            Cx = [[SB2(f"s5C{a}{ri}", [128, S5_NCH]) for ri in range(2)] for a in range(2)]
            PT = [SB2(f"s5PT{ri}", [128, S5_Q]) for ri in range(2)]
            ptt = SB2("s5ptt", [128, S5_Q])
            nblk = 0
            for d in range(2):
                for j in range(8):
                    dj = d * 8 + j
                    c = j // 4
                    bi = dj % 2
                    for wi, wn in enumerate(("s5_breT", "s5_bimT", "s5_creT", "s5_cimT")):
                        p.dma('pool', bc[bi][:, wi, :], K.inp[wn][li, dj], w=[f"bc{bi}"])
                    V(lambda e: e.tensor_scalar(out=bc[bi][:, 3, :], in0=bc[bi][:, 3, :], scalar1=-1.0,
                                                scalar2=None, op0=ALU.mult), [f"bc{bi}"], [f"bc{bi}"])
                    if d == 0:
                        blocks = [(s0, n, s0) for (s0, n) in S5_BLOCKS0]
                    else:
                        blocks = [(256 + 512 * b, 512, 512 * b) for b in range(8)] + [(0, 256, LL)]
                    A = X[0]
                    for (s0, n, xp) in blocks:
                        bk = 2 * (nblk % 2)
                        for ri in range(2):
                            p.op('pe', lambda e: e.matmul(K.ps[bk + ri][:, 0:n], lhsT=bc[bi][:, ri, :],
                                                          rhs=ub[:, c, s0:s0 + n], start=True, stop=True),
                                 r=[f"bc{bi}", "ub"], w=[f"ps{bk + ri}"])
                        t_ = tq[nblk % 2]
                        tk = f"tq{nblk % 2}"
                        V(lambda e: e.tensor_scalar(out=t_[:, 0:n], in0=K.ps[bk + 1][:, 0:n],
                                                    scalar1=co_imn[:, dj:dj + 1], scalar2=None, op0=ALU.mult),
                          [f"ps{bk + 1}", "coimn"], [tk])
                        V(lambda e: e.scalar_tensor_tensor(out=A[0][:, xp:xp + n], in0=K.ps[bk][:, 0:n],
                                                           scalar=co_re[:, dj:dj + 1], in1=t_[:, 0:n],
                                                           op0=ALU.mult, op1=ALU.add),
                          [f"ps{bk}", "core", tk], ["X00"])
                        V(lambda e: e.tensor_scalar(out=t_[:, 0:n], in0=K.ps[bk][:, 0:n],
                                                    scalar1=co_im[:, dj:dj + 1], scalar2=None, op0=ALU.mult),
                          [f"ps{bk}", "coim"], [tk])
                        V(lambda e: e.scalar_tensor_tensor(out=A[1][:, xp:xp + n], in0=K.ps[bk + 1][:, 0:n],
                                                           scalar=co_re[:, dj:dj + 1], in1=t_[:, 0:n],
                                                           op0=ALU.mult, op1=ALU.add),
                          [f"ps{bk + 1}", "core", tk], ["X01"])
                        nblk += 1
                    xv = lambda t_: t_[:].rearrange("q (c j) -> q c j", j=S5_Q)
                    for m in range(S5_LQ):
                        sh = 1 << m
                        a, b = m % 2, (m + 1) % 2
                        src, dst = X[a], X[b]
                        sk = [f"X{a}0", f"X{a}1"]
                        dk = [f"X{b}0", f"X{b}1"]
                        pr, pi_, pin = pw_re[:, m, dj:dj + 1], pw_im[:, m, dj:dj + 1], pw_imn[:, m, dj:dj + 1]
                        if d == 0:
                            o_sl, i_sl, c_sl = slice(sh, S5_Q), slice(0, S5_Q - sh), slice(0, sh)
                        else:
                            o_sl, i_sl, c_sl = slice(0, S5_Q - sh), slice(sh, S5_Q), slice(S5_Q - sh, S5_Q)
                        for ri in range(2):
                            p.op('act', lambda e: e.copy(out=xv(dst[ri])[:, :, c_sl], in_=xv(src[ri])[:, :, c_sl]),
                                 r=[sk[ri]], w=[dk[ri]])
                        V(lambda e: e.scalar_tensor_tensor(out=xv(dst[0])[:, :, o_sl], in0=xv(src[0])[:, :, i_sl], scalar=pr,
                                                           in1=xv(src[0])[:, :, o_sl], op0=ALU.mult, op1=ALU.add),
                          [sk[0], "pw"], [dk[0]])
                        V(lambda e: e.scalar_tensor_tensor(out=xv(dst[0])[:, :, o_sl], in0=xv(src[1])[:, :, i_sl], scalar=pin,
                                                           in1=xv(dst[0])[:, :, o_sl], op0=ALU.mult, op1=ALU.add),
                          [sk[1], "pwn", dk[0]], [dk[0]])
                        V(lambda e: e.scalar_tensor_tensor(out=xv(dst[1])[:, :, o_sl], in0=xv(src[1])[:, :, i_sl], scalar=pr,
                                                           in1=xv(src[1])[:, :, o_sl], op0=ALU.mult, op1=ALU.add),
                          [sk[1], "pw"], [dk[1]])
                        V(lambda e: e.scalar_tensor_tensor(out=xv(dst[1])[:, :, o_sl], in0=xv(src[0])[:, :, i_sl], scalar=pi_,
                                                           in1=xv(dst[1])[:, :, o_sl], op0=ALU.mult, op1=ALU.add),
                          [sk[0], "pw", dk[1]], [dk[1]])
                    R = X[S5_LQ % 2]
                    rk = [f"X{S5_LQ % 2}0", f"X{S5_LQ % 2}1"]
                    e_col = S5_Q - 1 if d == 0 else 0
                    for ri in range(2):
                        V(lambda e: e.tensor_copy(out=Cx[0][ri][:], in_=xv(R[ri])[:, :, e_col]), [rk[ri]], [f"C0{ri}"])
                    for m in range(S5_LC):
                        sh = 1 << m
                        a, b = m % 2, (m + 1) % 2
                        src, dst = Cx[a], Cx[b]
                        sk = [f"C{a}0", f"C{a}1"]
                        dk = [f"C{b}0", f"C{b}1"]
                        mm = S5_LQ + m
                        pr, pi_, pin = pw_re[:, mm, dj:dj + 1], pw_im[:, mm, dj:dj + 1], pw_imn[:, mm, dj:dj + 1]
                        if d == 0:
                            o_sl, i_sl, c_sl = slice(sh, S5_NCH), slice(0, S5_NCH - sh), slice(0, sh)
                        else:
                            o_sl, i_sl, c_sl = slice(0, S5_NCH - sh), slice(sh, S5_NCH), slice(S5_NCH - sh, S5_NCH)
                        for ri in range(2):
                            p.op('act', lambda e: e.copy(out=dst[ri][:, c_sl], in_=src[ri][:, c_sl]), r=[sk[ri]], w=[dk[ri]])
                        V(lambda e: e.scalar_tensor_tensor(out=dst[0][:, o_sl], in0=src[0][:, i_sl], scalar=pr,
                                                           in1=src[0][:, o_sl], op0=ALU.mult, op1=ALU.add),
                          [sk[0], "pw"], [dk[0]])
                        V(lambda e: e.scalar_tensor_tensor(out=dst[0][:, o_sl], in0=src[1][:, i_sl], scalar=pin,
                                                           in1=dst[0][:, o_sl], op0=ALU.mult, op1=ALU.add),
                          [sk[1], "pwn", dk[0]], [dk[0]])
                        V(lambda e: e.scalar_tensor_tensor(out=dst[1][:, o_sl], in0=src[1][:, i_sl], scalar=pr,
                                                           in1=src[1][:, o_sl], op0=ALU.mult, op1=ALU.add),
                          [sk[1], "pw"], [dk[1]])
                        V(lambda e: e.scalar_tensor_tensor(out=dst[1][:, o_sl], in0=src[0][:, i_sl], scalar=pi_,
                                                           in1=dst[1][:, o_sl], op0=ALU.mult, op1=ALU.add),
                          [sk[0], "pw", dk[1]], [dk[1]])
                    CF = Cx[S5_LC % 2]
                    cfk = [f"C{S5_LC % 2}0", f"C{S5_LC % 2}1"]
                    j0 = 0 if d == 0 else S5_Q - 1
                    V(lambda e: e.tensor_copy(out=PT[0][:, j0:j0 + 1], in_=pw_re[:, 0, dj:dj + 1]), ["pw"], ["PT0"])
                    V(lambda e: e.tensor_copy(out=PT[1][:, j0:j0 + 1], in_=pw_im[:, 0, dj:dj + 1]), ["pw"], ["PT1"])
                    for m in range(S5_LQ):
                        sh = 1 << m
                        pr, pi_, pin = pw_re[:, m, dj:dj + 1], pw_im[:, m, dj:dj + 1], pw_imn[:, m, dj:dj + 1]
                        if d == 0:
                            s_sl, d_sl = slice(0, sh), slice(sh, 2 * sh)
                        else:
                            s_sl, d_sl = slice(S5_Q - sh, S5_Q), slice(S5_Q - 2 * sh, S5_Q - sh)
                        V(lambda e: e.tensor_scalar(out=ptt[:, 0:sh], in0=PT[1][:, s_sl], scalar1=pin, scalar2=None,
                                                    op0=ALU.mult), ["PT1", "pwn"], ["ptt"])
                        V(lambda e: e.scalar_tensor_tensor(out=PT[0][:, d_sl], in0=PT[0][:, s_sl], scalar=pr,
                                                           in1=ptt[:, 0:sh], op0=ALU.mult, op1=ALU.add),
                          ["PT0", "pw", "ptt"], ["PT0"])
                        V(lambda e: e.tensor_scalar(out=ptt[:, 0:sh], in0=PT[0][:, s_sl], scalar1=pi_, scalar2=None,
                                                    op0=ALU.mult), ["PT0", "pw"], ["ptt"])
                        V(lambda e: e.scalar_tensor_tensor(out=PT[1][:, d_sl], in0=PT[1][:, s_sl], scalar=pr,
                                                           in1=ptt[:, 0:sh], op0=ALU.mult, op1=ALU.add),
                          ["PT1", "pw", "ptt"], ["PT1"])
                    if d == 0:
                        xc_sl, cc_sl = slice(1, S5_NCH), slice(0, S5_NCH - 1)
                    else:
                        xc_sl, cc_sl = slice(0, S5_NCH - 1), slice(1, S5_NCH)
                    nco = S5_NCH - 1
                    ptb = lambda ri: PT[ri][:].unsqueeze(1).to_broadcast([128, nco, S5_Q])
                    cfb = lambda ri: CF[ri][:, cc_sl].unsqueeze(2).to_broadcast([128, nco, S5_Q])
                    tmpv = xv(X[(S5_LQ + 1) % 2][0])[:, 0:nco, :]
                    tk = f"X{(S5_LQ + 1) % 2}0"
                    for (ro, pa, ca, op_) in ((0, 0, 0, ALU.add), (0, 1, 1, ALU.subtract), (1, 0, 1, ALU.add),
                                              (1, 1, 0, ALU.add)):
                        V(lambda e: e.tensor_tensor(out=tmpv, in0=ptb(pa), in1=cfb(ca), op=ALU.mult),
                          [f"PT{pa}", cfk[ca]], [tk])
                        V(lambda e: e.tensor_tensor(out=xv(R[ro])[:, xc_sl, :], in0=xv(R[ro])[:, xc_sl, :], in1=tmpv,
                                                    op=op_), [rk[ro], tk], [rk[ro]])
                    for ri in range(2):
                        p.op('act', lambda e: e.copy(out=xb[ri][:], in_=R[ri][:]), r=[rk[ri]], w=[f"xb{ri}"])
                    for (s0, n, xp) in blocks:
                        bk = 4 + (nblk % 2)
                        for ri in range(2):
                            p.op('pe', lambda e: e.matmul(K.ps[bk][:, 0:n], lhsT=bc[bi][:, 2 + ri, :],
                                                          rhs=xb[ri][:, xp:xp + n], start=(ri == 0),
                                                          stop=(ri == 1)), r=[f"bc{bi}", f"xb{ri}"], w=[f"ps{bk}"])
                        V(lambda e: e.tensor_tensor(out=Y[:, c, s0:s0 + n], in0=Y[:, c, s0:s0 + n],
                                                    in1=K.ps[bk][:, 0:n], op=ALU.add), [f"ps{bk}", f"Y{c}"],
                          [f"Y{c}"])
                        nblk += 1
        with _scope(K) as es2:
            SB2 = lambda name, shape, dt=F32: es2.enter_context(_sb(nc, name, shape, dt))
            yb = SB2("s5yb", [128, 2, T], BF16)
            O = SB2("s5O", [128, 2, T])
            sg = [SB2(f"s5sg{i}", [128, 512]) for i in range(2)]
            for c in range(2):
                p.op('act', lambda e: e.activation(out=Y[:, c, :], in_=Y[:, c, :], func=AF.Gelu), r=[f"Y{c}"],
                     w=[f"Y{c}"])
                V(lambda e: e.tensor_copy(out=yb[:, c, :], in_=Y[:, c, :]), [f"Y{c}"], ["yb"])
            nb = 0
            for co in range(2):
                for (s0, n) in S5_BLOCKS0:
                    bk = nb % 2
                    for ci in range(2):
                        p.op('pe', lambda e: e.matmul(K.ps[bk][:, 0:n], lhsT=gluw[:, ci, co * 128:(co + 1) * 128],
                                                      rhs=yb[:, ci, s0:s0 + n], start=(ci == 0), stop=(ci == 1)),
                             r=["gluw", "yb"], w=[f"ps{bk}"])
                    p.op('act', lambda e: e.activation(out=sg[bk][:, 0:n], in_=K.ps[bk][:, 0:n], func=AF.Sigmoid,
                                                       bias=glb[:, co:co + 1]), r=[f"ps{bk}", "glb"], w=[f"sg{bk}"])
                    if s0 == 0:
                        V(lambda e: e.tensor_tensor(out=O[:, co, 0:LC], in0=Y[:, co, 0:LC], in1=sg[bk][:, 0:n],
                                                    op=ALU.mult), [f"Y{co}", f"sg{bk}"], [f"O{co}"])
                    else:
                        b8 = (s0 - LC) // 64
                        o_ap = O[:, co, LC:T].rearrange("q (r c) -> q c r", c=64)[:, b8:b8 + 8, :]
                        V(lambda e: e.tensor_tensor(out=o_ap,
                                                    in0=Y[:, co, s0:s0 + n].rearrange("q (c r) -> q c r", r=64),
                                                    in1=sg[bk][:, 0:n].rearrange("q (c r) -> q c r", r=64),
                                                    op=ALU.mult), [f"Y{co}", f"sg{bk}"], [f"O{co}"])
                    nb += 1
                p.dma('sp', K.mix[768 + co * 128:768 + (co + 1) * 128, :], O[:, co, :], r=[f"O{co}"], w=["mix"])
        p.barrier()


def _hy_seq(K, li, seq, t0, L, mixcol0):
    p, nc = K.p, K.nc
    V = lambda fn, r, w: p.op('dve', fn, r=r, w=w)
    A = lambda fn, r, w: p.op('act', fn, r=r, w=w)
    NTT = L // 128
    NFC = NTT
    N2 = 2 * L
    dft = K.inp[f"dft_{seq}"]
    featT = K.inp[f"hy_featT_{seq}"]
    dec = K.inp[f"hy_dec_{seq}"]
    with contextlib.ExitStack() as es:
        SB = lambda name, shape, dt=F32: es.enter_context(_sb(nc, name, shape, dt))
        w1, w2, w3 = SB("hyw1", [33, 64]), SB("hyw2", [64, 64]), SB("hyw3", [64, 1024])
        cf = SB("hycf", [64, 6])
        skipb = SB("hyskip", [128, 2, 256])
        alt = SB("hyalt", [128, 2], BF16)
        altrow = SB("hyaltr", [1, 128], BF16)
        wcol = SB("hywcol", [128, NFC])
        hid2 = SB("hyhid2", [64, L])
        u = SB("hyu", [128, NTT, 256], BF16)
        Hn = SB("hyHn", [1, 256])
        Pn = SB("hyPn", [1, 256], BF16)
        Un = SB("hyUn", [1, 256])
        p.dma('sp', w1[:], K.inp["hy_w1"][li], w=["hyw1"])
        p.dma('sp', w2[:], K.inp["hy_w2"][li], w=["hyw2"])
        p.dma('sp', w3[:], K.inp["hy_w3"][li], w=["hyw3"])
        for i, n_ in enumerate(("hy_b1", "hy_f1", "hy_b2", "hy_f2")):
            p.dma('sp', cf[:, i:i + 1], K.inp[n_][li].rearrange("(q o) -> q o", o=1), w=["hycf"])
        for o in range(2):
            p.dma('sp', skipb[:, o, :], K.inp["hy_skip"][li, o].partition_broadcast(128), w=["hyskip"])
        p.dma('sp', alt[:], K.inp["hy_alt"][:, :], w=["hyalt"])
        p.dma('sp', altrow[:], K.inp["hy_altrow"][:, :], w=["hyaltr"])
        V(lambda e: e.tensor_tensor(out=cf[:, 4:5], in0=cf[:, 0:1], in1=cf[:, 1:2], op=ALU.mult), ["hycf"], ["hycf"])
        V(lambda e: e.tensor_tensor(out=cf[:, 5:6], in0=cf[:, 2:3], in1=cf[:, 3:4], op=ALU.mult), ["hycf"], ["hycf"])
        V(lambda e: e.memset(wcol[:], 2.0 / N2), [], ["hywcol"])
        V(lambda e: e.memset(wcol[0:1, 0:1], 1.0 / N2), ["hywcol"], ["hywcol"])
        with _scope(K) as es2:
            ft = es2.enter_context(_sb(nc, "hyfeat", [33, L], F32))
            hid1 = es2.enter_context(_sb(nc, "hyhid1", [64, L], F32))
            tmp = es2.enter_context(_sb(nc, "hytmp", [64, 512], F32))
            p.dma('sp', ft[:], featT[:, :], w=["hyfeat"])
            nb = 0
            for (src, sk, wm, wk, kdim, dst, dk, fi, bi) in ((ft, "hyfeat", w1, "hyw1", 33, hid1, "hid1", 1, 4),
                                                             (hid1, "hid1", w2, "hyw2", 64, hid2, "hid2", 3, 5)):
                for b0 in range(0, L, 512):
                    n = min(512, L - b0)
                    bk = nb % 2
                    p.op('pe', lambda e: e.matmul(K.ps[bk][0:64, 0:n], lhsT=wm[0:kdim, :], rhs=src[0:kdim, b0:b0 + n],
                                                  start=True, stop=True), r=[wk, sk], w=[f"ps{bk}"])
                    V(lambda e: e.tensor_scalar(out=dst[:, b0:b0 + n], in0=K.ps[bk][0:64, 0:n],
                                                scalar1=cf[:, fi:fi + 1], scalar2=cf[:, bi:bi + 1], op0=ALU.mult,
                                                op1=ALU.add), [f"ps{bk}", "hycf"], [dk])
                    _sin(K, dst[:, b0:b0 + n], dst[:, b0:b0 + n], 0.0, tmp[:, 0:n], [dk], dk, "hytmp")
                    nb += 1
        with _scope(K) as es2:
            vst = [es2.enter_context(_sb(nc, f"hyvst{i}", [128, 256], F32)) for i in range(2)]
            for tt in range(NTT):
                i = tt % 2
                p.dma('sp', vst[i][:], K.hytm[2, t0 + tt * 128:t0 + (tt + 1) * 128, :], r=["hytm2"], w=[f"vst{i}"])
                _copy_any(p, tt, u[:, tt, :], vst[i][:], r=[f"vst{i}"], w=["hyu"])
        for order in range(2):
            with _scope(K) as es2:
                SB2 = lambda name, shape, dt=F32: es2.enter_context(_sb(nc, name, shape, dt))
                HP, HM = SB2("hyHP", [128, NTT, 256], BF16), SB2("hyHM", [128, NTT, 256], BF16)
                dct = [SB2(f"hydec{i}", [128, 256]) for i in range(2)]
                hf = [SB2(f"hyhf{i}", [128, 2, 256]) for i in range(2)]
                for tt in range(NTT):
                    i = tt % 2
                    bk = i
                    p.dma('sp', dct[i][:], dec[tt * 128:(tt + 1) * 128, :], w=[f"hydec{i}"])
                    p.op('pe', lambda e: e.matmul(K.ps[bk][:, :], lhsT=hid2[:, tt * 128:(tt + 1) * 128],
                                                  rhs=w3[:, order * 512:(order + 1) * 512], start=True, stop=True),
                         r=["hid2", "hyw3"], w=[f"ps{bk}"])
                    V(lambda e: e.tensor_tensor(out=hf[i][:], in0=K.ps[bk][:, :].rearrange("q (s c) -> q s c", s=2),
                                                in1=dct[i][:].unsqueeze(1).to_broadcast([128, 2, 256]), op=ALU.mult),
                      [f"ps{bk}", f"hydec{i}"], [f"hyhf{i}"])
                    if tt == 0:
                        V(lambda e: e.memset(hf[i][0:1, 1, :], 0.0), [f"hyhf{i}"], [f"hyhf{i}"])
                    V(lambda e: e.tensor_tensor(out=HP[:, tt, :], in0=hf[i][:, 0, :], in1=hf[i][:, 1, :], op=ALU.add),
                      [f"hyhf{i}"], ["HP"])
                    V(lambda e: e.tensor_tensor(out=HM[:, tt, :], in0=hf[i][:, 0, :], in1=hf[i][:, 1, :],
                                                op=ALU.subtract), [f"hyhf{i}"], ["HM"])
                Pc, Ps = SB2("hyPc", [128, NFC, 256], BF16), SB2("hyPs", [128, NFC, 256], BF16)
                slab = [[SB2(f"hysm{i}{cs}", [128, NTT * 128], BF16) for cs in range(2)] for i in range(2)]
                hcs = [[SB2(f"hyhcs{i}{cs}", [128, 256]) for cs in range(2)] for i in range(2)]
                tq = [SB2(f"hytq{i}", [128, 256]) for i in range(4)]
                xa = [SB2(f"hyxa{i}", [128, 256]) for i in range(2)]
                xv = [SB2(f"hyxv{i}", [128, 256]) for i in range(2)]
                ofm = [SB2(f"hyofm{i}", [128, 2, 128]) for i in range(2)]
                for tt in range(NTT):
                    p.op('pe', lambda e: e.matmul(K.ps[6][0:1, 0:256], lhsT=alt[:, 0:1], rhs=HP[:, tt, :],
                                                  start=(tt == 0), stop=(tt == NTT - 1)), r=["hyalt", "HP"], w=["ps6"])
                V(lambda e: e.tensor_scalar(out=Hn[:], in0=K.ps[6][0:1, 0:256], scalar1=1.0 / N2, scalar2=None,
                                            op0=ALU.mult), ["ps6"], ["Hn"])
                for fc in range(NFC):
                    i = fc % 2
                    for cs in range(2):
                        p.dma('sp', slab[i][cs][:], dft[cs, fc], w=[f"hysm{i}{cs}"])
                    for cs, (src, sk) in enumerate(((HP, "HP"), (HM, "HM"))):
                        for (rhs_t, rk_, bk) in ((src, sk, 4 * i + 2 + cs), (u, "hyu", 4 * i + cs)):
                            for tt in range(NTT):
                                p.op('pe', lambda e: e.matmul(K.ps[bk][:, 0:256],
                                                              lhsT=slab[i][cs][:, tt * 128:(tt + 1) * 128],
                                                              rhs=rhs_t[:, tt, :], start=(tt == 0), stop=(tt == NTT - 1)),
                                     r=[f"hysm{i}{cs}", rk_], w=[f"ps{bk}"])
                        A(lambda e: e.activation(out=hcs[i][cs][:], in_=K.ps[4 * i + 2 + cs][:, 0:256], func=AF.Copy,
                                                 scale=wcol[:, fc:fc + 1]), [f"ps{4 * i + 2 + cs}", "hywcol"],
                          [f"hcs{i}{cs}"])
                    C_, S_ = K.ps[4 * i][:, 0:256], K.ps[4 * i + 1][:, 0:256]
                    ck, sk = f"ps{4 * i}", f"ps{4 * i + 1}"
                    Hc_, Hs_ = hcs[i][0][:], hcs[i][1][:]
                    hck, hsk = f"hcs{i}0", f"hcs{i}1"
                    V(lambda e: e.tensor_tensor(out=tq[0][:], in0=C_, in1=Hc_, op=ALU.mult), [ck, hck], ["tq0"])
                    V(lambda e: e.tensor_tensor(out=tq[1][:], in0=S_, in1=Hs_, op=ALU.mult), [sk, hsk], ["tq1"])
                    V(lambda e: e.tensor_tensor(out=Pc[:, fc, :], in0=tq[0][:], in1=tq[1][:], op=ALU.subtract),
                      ["tq0", "tq1"], ["Pc"])
                    V(lambda e: e.tensor_tensor(out=tq[2][:], in0=C_, in1=Hs_, op=ALU.mult), [ck, hsk], ["tq2"])
                    V(lambda e: e.tensor_tensor(out=tq[3][:], in0=S_, in1=Hc_, op=ALU.mult), [sk, hck], ["tq3"])
                    V(lambda e: e.tensor_tensor(out=Ps[:, fc, :], in0=tq[2][:], in1=tq[3][:], op=ALU.add),
                      ["tq2", "tq3"], ["Ps"])
                for tt in range(NTT):
                    p.op('pe', lambda e: e.matmul(K.ps[6][0:1, 0:256], lhsT=alt[:, 0:1], rhs=u[:, tt, :],
                                                  start=(tt == 0), stop=(tt == NTT - 1)), r=["hyalt", "hyu"], w=["ps6"])
                V(lambda e: e.tensor_copy(out=Un[:], in_=K.ps[6][0:1, 0:256]), ["ps6"], ["Un"])
                V(lambda e: e.tensor_tensor(out=Pn[:], in0=Un[:], in1=Hn[:], op=ALU.mult), ["Un", "Hn"], ["Pn"])
                xslot = 0 if order == 0 else 1
                yslot = 2 if order == 0 else 0
                for tc in range(NTT):
                    i = tc % 2
                    for cs in range(2):
                        p.dma('sp', slab[i][cs][:], dft[cs, tc], w=[f"hysm{i}{cs}"])
                    rows = slice(t0 + tc * 128, t0 + (tc + 1) * 128)
                    p.dma('sp', xa[i][:], K.hytm[xslot, rows, :], r=[f"hytm{xslot}"], w=[f"hyxa{i}"])
                    p.dma('sp', xv[i][:], K.hytm[yslot, rows, :], r=[f"hytm{yslot}"], w=[f"hyxv{i}"])
                    bk = 4 + i
                    for fc in range(NFC):
                        p.op('pe', lambda e: e.matmul(K.ps[bk][:, 0:256], lhsT=slab[i][0][:, fc * 128:(fc + 1) * 128],
                                                      rhs=Pc[:, fc, :], start=(fc == 0), stop=False),
                             r=[f"hysm{i}0", "Pc"], w=[f"ps{bk}"])
                        p.op('pe', lambda e: e.matmul(K.ps[bk][:, 0:256], lhsT=slab[i][1][:, fc * 128:(fc + 1) * 128],
                                                      rhs=Ps[:, fc, :], start=False, stop=False),
                             r=[f"hysm{i}1", "Ps"], w=[f"ps{bk}"])
                    p.op('pe', lambda e: e.matmul(K.ps[bk][:, 0:256], lhsT=altrow[0:1, :], rhs=Pn[0:1, :], start=False,
                                                  stop=True), r=["hyaltr", "Pn"], w=[f"ps{bk}"])
                    V(lambda e: e.tensor_tensor(out=xv[i][:], in0=xv[i][:], in1=skipb[:, order, :], op=ALU.mult),
                      [f"hyxv{i}", "hyskip"], [f"hyxv{i}"])
                    V(lambda e: e.tensor_tensor(out=xv[i][:], in0=xv[i][:], in1=K.ps[bk][:, 0:256], op=ALU.add),
                      [f"hyxv{i}", f"ps{bk}"], [f"hyxv{i}"])
                    V(lambda e: e.tensor_tensor(out=xa[i][:], in0=xa[i][:], in1=xv[i][:], op=ALU.mult),
                      [f"hyxa{i}", f"hyxv{i}"], [f"hyxa{i}"])
                    if order == 0:
                        A(lambda e: e.copy(out=u[:, tc, :], in_=xa[i][:]), [f"hyxa{i}"], ["hyu2"])
                        p.dma('pool', K.hytm[0, rows, :], xa[i][:], r=[f"hyxa{i}"], w=["hytm0"])
                    else:
                        for c2 in range(2):
                            p.op('pe', lambda e: e.transpose(out=K.ps[7][:, c2 * 128:(c2 + 1) * 128],
                                                             in_=xa[i][:, c2 * 128:(c2 + 1) * 128], identity=K.ident[:]),
                                 r=[f"hyxa{i}", "ident"], w=["ps7"])
                        A(lambda e: e.copy(out=ofm[i][:].rearrange("q a b -> q (a b)"), in_=K.ps[7][:, 0:256]), ["ps7"],
                          [f"hyofm{i}"])
                        for c2 in range(2):
                            p.dma('pool', K.mix[512 + c2 * 128:512 + (c2 + 1) * 128,
                                                mixcol0 + tc * 128:mixcol0 + (tc + 1) * 128], ofm[i][:, c2, :],
                                  r=[f"hyofm{i}"], w=["mix"])
        p.barrier()


def phase_hyena(K, li):
    p, nc = K.p, K.nc
    V = lambda fn, r, w: p.op('dve', fn, r=r, w=w)
    need_ctx = li < DEPTH - 1
    with contextlib.ExitStack() as es:
        SB = lambda name, shape, dt=F32: es.enter_context(_sb(nc, name, shape, dt))
        cw = SB("hycw", [128, 6, 4])
        zin = [SB(f"hyzin{i}", [128, T]) for i in range(2)]
        zo = [SB(f"hyzo{i}", [128, T]) for i in range(2)]
        tst = [SB(f"hytst{i}", [128, 128]) for i in range(4)]
        for k in range(3):
            p.dma('sp', cw[:, :, k], K.inp["hy_conv"][li, k].rearrange("(c q) -> q c", q=128), w=["hycw"])
        p.dma('sp', cw[:, :, 3], K.inp["hy_conv_b"][li].rearrange("(c q) -> q c", q=128), w=["hycw"])
        nt = 0
        for ch in range(6):
            i = ch % 2
            r0 = ZR["hy"] + ch * 128
            p.dma('sp', zin[i][:], K.zfm[r0:r0 + 128, :], r=["zfm"], w=[f"zin{i}"])
            V(lambda e: e.tensor_scalar(out=zo[i][:], in0=zin[i][:], scalar1=cw[:, ch, 1:2], scalar2=cw[:, ch, 3:4],
                                        op0=ALU.mult, op1=ALU.add), [f"zin{i}", "hycw"], [f"zo{i}"])
            for (a, b) in ((0, LC), (LC, T)):
                V(lambda e: e.scalar_tensor_tensor(out=zo[i][:, a + 1:b], in0=zin[i][:, a:b - 1], scalar=cw[:, ch, 0:1],
                                                   in1=zo[i][:, a + 1:b], op0=ALU.mult, op1=ALU.add),
                  [f"zin{i}", "hycw", f"zo{i}"], [f"zo{i}"])
                V(lambda e: e.scalar_tensor_tensor(out=zo[i][:, a:b - 1], in0=zin[i][:, a + 1:b], scalar=cw[:, ch, 2:3],
                                                   in1=zo[i][:, a:b - 1], op0=ALU.mult, op1=ALU.add),
                  [f"zin{i}", "hycw", f"zo{i}"], [f"zo{i}"])
            slot, c2 = ch // 2, ch % 2
            for tt in range(0 if need_ctx else 2, NT):
                bk = nt % 4
                p.op('pe', lambda e: e.transpose(out=K.ps[bk][:, 0:128], in_=zo[i][:, tt * 128:(tt + 1) * 128],
                                                 identity=K.ident[:]), r=[f"zo{i}", "ident"], w=[f"ps{bk}"])
                _copy_any(p, nt, tst[bk][:], K.ps[bk][:, 0:128], r=[f"ps{bk}"], w=[f"tst{bk}"])
                p.dma('pool', K.hytm[slot, tt * 128:(tt + 1) * 128, c2 * 128:(c2 + 1) * 128], tst[bk][:],
                      r=[f"tst{bk}"], w=[f"hytm{slot}"])
                nt += 1
        p.barrier()
    if need_ctx:
        _hy_seq(K, li, "ctx", 0, LC, 0)
    _hy_seq(K, li, "lat", LC, LL, LC)


SCAN_W = 8
SCAN_WV = 64
SCAN_STEPS = [T]


def _tok1(s):
    return LC - 1 - s if s < LC else T + LC - 1 - s


def _ap(t, offset, dims, nparts):
    full = t[:]
    return bass.AP(full.tensor, offset, [[full.ap[0][0], nparts]] + [list(d) for d in dims])


def phase_scan(K, li):
    p, nc = K.p, K.nc
    W, WV = SCAN_W, SCAN_WV
    nsteps = SCAN_STEPS[0]
    NQ = (4, 6, 4, 4, 4)
    with contextlib.ExitStack() as es:
        SB = lambda name, shape, dt=F32: es.enter_context(_sb(nc, name, shape, dt))
        Sb = [SB(f"scS{i}", [128, 8, 64]) for i in range(2)]
        P1, Pb = SB("scP1", [128, 8, 64]), SB("scPb", [128, 8, 64])
        P4 = [SB(f"scP4{i}", [128, 8, 64]) for i in range(2)]
        sa = SB("scsa", [128, 8])
        t2 = [SB(f"sct2{b}", [128, 8, 64]) for b in range(2)]
        wsb = [SB(f"scwsb{b}", [128, 8, 64]) for b in range(2)]
        sel = SB("scsel", [6, 128], BF16)
        win = [[SB(f"scwin{o}_{b}", [6, 2 * W * 256], BF16) for b in range(2)] for o in range(5)]
        vw = [SB(f"scvw{b}", [128, 8, WV]) for b in range(2)]
        yw = [SB(f"scyw{b}", [128, 8, WV]) for b in range(2)]
        p.dma('sp', sel[:], K.inp["scan_sel"][:, :], w=["scsel"])
        p.op('dve', lambda e: e.memset(Sb[0][:], 0.0), w=["S0"])
        pend = []
        for b in range(2):
            p.op('dve', lambda e: e.memset(yw[b][:], 0.0), w=[f"yw{b}"])
        for s in range(nsteps):
            j, wi = s % W, s // W
            jv, wvi = s % WV, s // WV
            wb, vb = wi % 2, wvi % 2
            if j == 0:
                t_lo = (wi * W, _tok1(wi * W) - W + 1)
                rows_t = K.rows[:].tensor
                for o in range(5):
                    for d in range(2):
                        src = bass.AP(rows_t, (o * T + t_lo[d]) * 3072 + d * 256, [[512, NQ[o]], [3072, W], [1, 256]])
                        p.dma('sp', win[o][wb][0:NQ[o], d * W * 256:(d + 1) * W * 256], src, r=["rows"],
                              w=[f"win{o}_{wb}"])
            if jv == 0:
                v_lo = (wvi * WV, _tok1(wvi * WV) - WV + 1)
                for g in range(8):
                    d, mh = g // 4, g % 4
                    p.dma('sp', vw[vb][:, g, :], K.vfm[mh, :, v_lo[d]:v_lo[d] + WV], r=["vfm"], w=[f"vw{vb}"])
            ix = (j, W - 1 - j)
            ixv = (jv, WV - 1 - jv)
            banks = [(5 * s + o) % 8 for o in range(5)]
            for o in range(5):
                rhs = _ap(win[o][wb], ix[0] * 256, [[(W + ix[1] - ix[0]) * 256, 2], [1, 256]], NQ[o])
                p.op('pe', lambda e: e.matmul(K.ps[banks[o]][:, :], lhsT=sel[0:NQ[o], :], rhs=rhs, start=True,
                                              stop=True),
                     r=[f"win{o}_{wb}", "scsel"], w=[f"ps{banks[o]}"])
            psv = [K.ps[b][:, :].rearrange("q (g k) -> q g k", k=64) for b in banks]
            dv = ixv[1] - ixv[0]
            vcols = _ap(vw[vb], ixv[0], [[4 * WV + dv, 2], [WV, 4], [0, 64]], 128)
            ycols = _ap(yw[vb], ixv[0], [[4 * WV + dv, 2], [WV, 4]], 128)
            tb = s % 2
            items = []
            for g in range(8):
                d = g // 4
                vcol = vw[vb][:, g, ixv[d]:ixv[d] + 1]
                items.append((lambda e, g=g, vcol=vcol: e.activation(out=t2[tb][:, g, :], in_=psv[3][:, g, :],
                                                                     func=AF.Copy, scale=vcol),
                              [f"vw{vb}", f"ps{banks[3]}"], [f"sct2{tb}"]))
            p.op('act', lambda e: e.copy(out=wsb[tb][:], in_=psv[1]), r=[f"ps{banks[1]}"], w=[f"scwsb{tb}"])
            p.stage('act', items)
            V = lambda fn, r, w: p.op('dve', fn, r=r, w=w)
            G = lambda fn, r, w: p.op('pool', fn, r=r, w=w)
            cur, nxt = Sb[s % 2], Sb[(s + 1) % 2]
            ck, nk = f"S{s % 2}", f"S{(s + 1) % 2}"
            V(lambda e: e.tensor_tensor(out=P1[:], in0=cur[:], in1=psv[0], op=ALU.mult), [ck, f"ps{banks[0]}"], ["P1"])
            G(lambda e: e.tensor_tensor(out=nxt[:], in0=cur[:], in1=wsb[tb][:], op=ALU.mult), [ck, f"scwsb{tb}"], [nk])
            G(lambda e: e.tensor_tensor(out=nxt[:], in0=nxt[:], in1=t2[tb][:], op=ALU.add), [nk, f"sct2{tb}"], [nk])
            if pend:
                pend[0][0]()
            V(lambda e: e.tensor_reduce(out=sa[:], in_=P1[:], axis=AX.X, op=ALU.add), ["P1"], ["sa"])
            if pend:
                pend.pop(0)[1]()
            V(lambda e: e.tensor_tensor(out=Pb[:], in0=psv[2], in1=sa[:].unsqueeze(2).to_broadcast([128, 8, 64]),
                                        op=ALU.mult), ["sa", f"ps{banks[2]}"], ["Pb"])
            V(lambda e: e.tensor_tensor(out=nxt[:], in0=nxt[:], in1=Pb[:], op=ALU.add), [nk, "Pb"], [nk])

            def out_mul(nxt=nxt, nk=nk, r_ap=psv[4], rb=banks[4]):
                V(lambda e: e.tensor_tensor(out=P4[0][:], in0=nxt[:], in1=r_ap, op=ALU.mult), [nk, f"ps{rb}"], ["P4"])

            def out_red(ycols=ycols, vb=vb, s=s, jv=jv, wvi=wvi):
                V(lambda e: e.tensor_reduce(out=ycols, in_=P4[0][:], axis=AX.X, op=ALU.add), ["P4"], [f"yw{vb}"])
                if jv == WV - 1 or s == nsteps - 1:
                    v_lo = (wvi * WV, _tok1(wvi * WV) - WV + 1)
                    for g in range(8):
                        d = g // 4
                        p.dma('pool', K.yfm[g, :, v_lo[d]:v_lo[d] + WV], yw[vb][:, g, :], r=[f"yw{vb}"], w=["yfm"])
            if li < DEPTH - 1 or s >= LC or nsteps < T:
                pend.append((out_mul, out_red))
        pend[0][0]()
        pend[0][1]()
        p.barrier()


TOK_BLOCKS = [(0, 256)] + [(256 + 512 * b, 512) for b in range(8)]


def _split_store(K, tt, stg, stgk, SP, r1):
    p = K.p
    V = lambda fn, r, w: p.op('dve', fn, r=r, w=w)
    A = lambda fn, r, w: p.op('act', fn, r=r, w=w)
    stgk = list(stgk)
    A(lambda e: e.copy(out=SP[:, 0], in_=stg[:]), stgk, ["sp_hi"])
    V(lambda e: e.tensor_tensor(out=r1[:], in0=stg[:], in1=SP[:, 0], op=ALU.subtract), stgk + ["sp_hi"], ["sp_r1"])
    A(lambda e: e.copy(out=SP[:, 1], in_=r1[:]), ["sp_r1"], ["sp_mid"])
    V(lambda e: e.tensor_tensor(out=r1[:, 1], in0=r1[:, 1], in1=SP[:, 1, 1], op=ALU.subtract), ["sp_r1", "sp_mid"],
      ["sp_r1"])
    A(lambda e: e.copy(out=SP[:, 2, 1], in_=r1[:, 1]), ["sp_r1"], ["sp_lo"])
    for o in range(5):
        nsp = 3 if o == 1 else 2
        p.dma('sp' if o % 2 == 0 else 'pool',
              K.rows[o, tt * 128:(tt + 1) * 128, 0:nsp].rearrange("q s h d c -> q s (h d c)"),
              SP[:, 0:nsp, o].rearrange("q s h d c -> q s (h d c)"), r=["sp_hi", "sp_mid", "sp_lo"], w=["rows"])


def phase_rwprep(K, li):
    p, nc = K.p, K.nc
    V = lambda fn, r, w: p.op('dve', fn, r=r, w=w)
    A = lambda fn, r, w: p.op('act', fn, r=r, w=w)
    EM05 = math.exp(-0.5)
    with contextlib.ExitStack() as es:
        SB = lambda name, shape, dt=F32: es.enter_context(_sb(nc, name, shape, dt))
        w2p, a2p = SB("rww2p", [128, 4, 128]), SB("rwa2p", [128, 4, 128])
        g2p = SB("rwg2p", [128, 2, 128])
        blk = SB("rwblk", [128, 128])
        pT = SB("rwpT", [128, 18])
        omka = SB("rwomka", [128, 2])
        zin = {n: SB(f"rwz_{n}", [128, 2, 512]) for n in ("r", "k", "v")}
        zwa, zg_ = SB("rwz_wa", [128, 512]), SB("rwz_g", [128, 512])
        actc, sgl = SB("rwactc", [128, 512]), SB("rwsgl", [128, 512])
        dec, aa = SB("rwdec", [128, 4, 512]), SB("rwaa", [128, 4, 512])
        gout, bv = SB("rwgout", [128, 2, 512]), SB("rwbv", [128, 2, 512])
        kk, kkn, an = SB("rwkk", [128, 2, 512]), SB("rwkkn", [128, 2, 512]), SB("rwan", [128, 2, 512])
        kd, bb = SB("rwkd", [128, 4, 512]), SB("rwbb", [128, 4, 512])
        t1, t2, prs = SB("rwt1", [128, 512]), SB("rwt2", [128, 512]), SB("rwprs", [128, 512])
        stgs = [SB(f"rwstg{i}", [128, 5, 2, 2, 256]) for i in range(2)]
        SP = SB("rwSP", [128, 3, 5, 2, 2, 256], BF16)
        r1 = SB("rwr1", [128, 5, 2, 2, 256])
        p.dma('sp', w2p[:], K.inp["rw_w2p"][li].rearrange("i q c -> q i c"), w=["w2p"])
        p.dma('sp', a2p[:], K.inp["rw_a2p"][li].rearrange("i q c -> q i c"), w=["a2p"])
        p.dma('sp', g2p[:], K.inp["rw_g2p"][li].rearrange("i q c -> q i c"), w=["g2p"])
        p.dma('sp', blk[:], K.inp["blk64"][:, :], w=["blk"])
        p.dma('sp', pT[:], K.inp["rw_pT"][li], w=["pT"])
        V(lambda e: e.tensor_scalar(out=omka[:], in0=pT[:, 10:12], scalar1=-1.0, scalar2=1.0, op0=ALU.mult,
                                    op1=ALU.add), ["pT"], ["omka"])
        for hp in range(2):
            r0 = ZR["rw_v"] + hp * 128
            p.dma('pool', K.vfm[hp, :, :], K.zfm[r0:r0 + 128, :], r=["zfm"], w=["vfm"])
        nb = 0
        ncp = 0
        for (b0, n) in TOK_BLOCKS:
            for nm in ("r", "k", "v"):
                r0 = ZR["rw_" + nm]
                p.dma('sp', zin[nm][:, :, 0:n], K.zfm[r0:r0 + 256, b0:b0 + n].rearrange("(c q) t -> q c t", q=128),
                      r=["zfm"], w=["z" + nm])
            p.dma('sp', zwa[:, 0:n], K.zfm[ZR["rw_wa"]:ZR["rw_wa"] + 128, b0:b0 + n], r=["zfm"], w=["zwa"])
            p.dma('sp', zg_[:, 0:n], K.zfm[ZR["rw_g"]:ZR["rw_g"] + 128, b0:b0 + n], r=["zfm"], w=["zg"])
            A(lambda e: e.activation(out=actc[0:64, 0:n], in_=zwa[0:64, 0:n], func=AF.Tanh), ["zwa"], ["actc"])
            A(lambda e: e.copy(out=actc[64:128, 0:n], in_=zwa[64:128, 0:n]), ["zwa"], ["actc"])
            A(lambda e: e.activation(out=sgl[:, 0:n], in_=zg_[:, 0:n], func=AF.Sigmoid), ["zg"], ["sgl"])
            for i in range(4):
                bk = nb % 4
                nb += 1
                p.op('pe', lambda e: e.matmul(K.ps[bk][:, 0:n], lhsT=w2p[:, i, :], rhs=actc[:, 0:n], start=True,
                                              stop=True), r=["w2p", "actc"], w=[f"ps{bk}"])
                A(lambda e: e.activation(out=dec[:, i, 0:n], in_=K.ps[bk][:, 0:n], func=AF.Sigmoid,
                                         bias=pT[:, i:i + 1]), [f"ps{bk}", "pT"], ["dec"])
                A(lambda e: e.activation(out=dec[:, i, 0:n], in_=dec[:, i, 0:n], func=AF.Exp, scale=-EM05),
                  ["dec"], ["dec"])
                bk = nb % 4
                nb += 1
                p.op('pe', lambda e: e.matmul(K.ps[bk][:, 0:n], lhsT=a2p[:, i, :], rhs=actc[:, 0:n], start=True,
                                              stop=True), r=["a2p", "actc"], w=[f"ps{bk}"])
                A(lambda e: e.activation(out=aa[:, i, 0:n], in_=K.ps[bk][:, 0:n], func=AF.Sigmoid,
                                         bias=pT[:, 4 + i:5 + i]), [f"ps{bk}", "pT"], ["aa"])
            for cc in range(2):
                bk = nb % 4
                nb += 1
                p.op('pe', lambda e: e.matmul(K.ps[bk][:, 0:n], lhsT=g2p[:, cc, :], rhs=sgl[:, 0:n], start=True,
                                              stop=True), r=["g2p", "sgl"], w=[f"ps{bk}"])
                A(lambda e: e.copy(out=gout[:, cc, 0:n], in_=K.ps[bk][:, 0:n]), [f"ps{bk}"], ["gout"])
                V(lambda e: e.tensor_scalar(out=kk[:, cc, 0:n], in0=zin["k"][:, cc, 0:n], scalar1=pT[:, 8 + cc:9 + cc],
                                            scalar2=None, op0=ALU.mult), ["zk", "pT"], ["kk"])
                V(lambda e: e.tensor_tensor(out=t1[:, 0:n], in0=kk[:, cc, 0:n], in1=kk[:, cc, 0:n], op=ALU.mult),
                  ["kk"], ["t1"])
                bk = nb % 4
                nb += 1
                p.op('pe', lambda e: e.matmul(K.ps[bk][:, 0:n], lhsT=blk[:, :], rhs=t1[:, 0:n], start=True, stop=True),
                     r=["blk", "t1"], w=[f"ps{bk}"])
                V(lambda e: e.tensor_scalar(out=t2[:, 0:n], in0=K.ps[bk][:, 0:n], scalar1=1e-12, scalar2=None,
                                            op0=ALU.add), [f"ps{bk}"], ["t2"])
                A(lambda e: e.activation(out=t2[:, 0:n], in_=t2[:, 0:n], func=AF.Sqrt), ["t2"], ["t2"])
                V(lambda e: e.reciprocal(out=t2[:, 0:n], in_=t2[:, 0:n]), ["t2"], ["t2"])
                V(lambda e: e.tensor_tensor(out=kkn[:, cc, 0:n], in0=kk[:, cc, 0:n], in1=t2[:, 0:n], op=ALU.mult),
                  ["kk", "t2"], ["kkn"])
                V(lambda e: e.tensor_scalar(out=an[:, cc, 0:n], in0=kkn[:, cc, 0:n], scalar1=-1.0, scalar2=None,
                                            op0=ALU.mult), ["kkn"], ["an"])
                for d in range(2):
                    i = d * 2 + cc
                    V(lambda e: e.tensor_scalar(out=t1[:, 0:n], in0=aa[:, i, 0:n], scalar1=pT[:, 10 + cc:11 + cc],
                                                scalar2=omka[:, cc:cc + 1], op0=ALU.mult, op1=ALU.add),
                      ["aa", "pT", "omka"], ["t1"])
                    V(lambda e: e.tensor_tensor(out=kd[:, i, 0:n], in0=t1[:, 0:n], in1=zin["k"][:, cc, 0:n],
                                                op=ALU.mult), ["t1", "zk"], ["kd"])
                    V(lambda e: e.tensor_tensor(out=bb[:, i, 0:n], in0=kkn[:, cc, 0:n], in1=aa[:, i, 0:n],
                                                op=ALU.mult), ["kkn", "aa"], ["bb"])
                V(lambda e: e.tensor_tensor(out=t1[:, 0:n], in0=kd[:, cc, 0:n], in1=kd[:, 2 + cc, 0:n], op=ALU.add),
                  ["kd"], ["t1"])
                V(lambda e: e.scalar_tensor_tensor(out=prs[:, 0:n], in0=zin["r"][:, cc, 0:n],
                                                   scalar=pT[:, 12 + cc:13 + cc], in1=t1[:, 0:n], op0=ALU.mult,
                                                   op1=ALU.mult), ["zr", "pT", "t1"], ["prs"])
                bk = nb % 4
                nb += 1
                p.op('pe', lambda e: e.matmul(K.ps[bk][:, 0:n], lhsT=blk[:, :], rhs=prs[:, 0:n], start=True,
                                              stop=True), r=["blk", "prs"], w=[f"ps{bk}"])
                V(lambda e: e.tensor_tensor(out=bv[:, cc, 0:n], in0=K.ps[bk][:, 0:n], in1=zin["v"][:, cc, 0:n],
                                            op=ALU.mult), [f"ps{bk}", "zv"], ["bv"])
            p.dma('pool', K.rwg.rearrange("(c q) t -> q c t", q=128)[:, :, b0:b0 + n], gout[:, :, 0:n], r=["gout"],
                  w=["rwg"])
            p.dma('pool', K.rwbv.rearrange("(c q) t -> q c t", q=128)[:, :, b0:b0 + n], bv[:, :, 0:n], r=["bv"],
                  w=["rwbv"])
            for tl in range(n // 128):
                tt = (b0 // 128) + tl
                cols = slice(tl * 128, (tl + 1) * 128)
                stg = stgs[tt % 2]
                sb_ = tt % 2
                skeys = [f"st{sb_}:gd"]
                p.dma('sp', stg[:].rearrange("q o h d c -> q (o h d) c")[:, :, 128:256],
                      K.gdstg[tt * 128:(tt + 1) * 128, :].rearrange("q (x c) -> q x c", c=128), r=["gdstgd"],
                      w=[f"st{sb_}:gd"])
                for cc in range(2):
                    srcs = [(0, None, an[:, cc, cols], "an"), (4, None, zin["r"][:, cc, cols], "zr")]
                    for d in range(2):
                        i = d * 2 + cc
                        srcs += [(1, d, dec[:, i, cols], "dec"), (2, d, bb[:, i, cols], "bb"), (3, d, kd[:, i, cols], "kd")]
                    for (o, d, src, sk) in srcs:
                        bk = 4 + (ncp % 4)
                        p.op('pe', lambda e: e.transpose(out=K.ps[bk][:, 0:128], in_=src, identity=K.ident[:]),
                             r=[sk, "ident"], w=[f"ps{bk}"])
                        pin = K.ps[bk][:, 0:128].rearrange("q (h k) -> q h k", k=64)
                        if d is None:
                            out_ap = stg[:, o, :, :, cc * 64:(cc + 1) * 64]
                            in_ap = pin.unsqueeze(2).to_broadcast([128, 2, 2, 64])
                        else:
                            out_ap = stg[:, o, :, d, cc * 64:(cc + 1) * 64]
                            in_ap = pin
                        wk = f"st{sb_}:{o}:{cc}:{d}"
                        skeys.append(wk)
                        _copy_any(p, ncp, out_ap, in_ap, r=[f"ps{bk}"], w=[wk])
                        ncp += 1
                _split_store(K, tt, stg, skeys, SP, r1)
        p.barrier()


def phase_gdprep(K, li):
    p, nc = K.p, K.nc
    V = lambda fn, r, w: p.op('dve', fn, r=r, w=w)
    A = lambda fn, r, w: p.op('act', fn, r=r, w=w)
    with contextlib.ExitStack() as es:
        SB = lambda name, shape, dt=F32: es.enter_context(_sb(nc, name, shape, dt))
        cw = SB("gdcw", [128, 6, 3])
        gpar = SB("gdpar", [128, 2, 8])
        zin = [SB(f"gdzin{i}", [128, T]) for i in range(2)]
        qk = SB("gdqk", [128, 4, T])
        zo = SB("gdzo", [128, T])
        ab = SB("gdab", [16, T])
        for k in range(3):
            p.dma('sp', cw[:, :, k], K.inp["gd_conv"][li, k].rearrange("(c q) -> q c", q=128), w=["cw"])
        p.dma('sp', gpar[:, 0, :], K.inp["gd_dtb"][li].rearrange("d h -> (d h)").partition_broadcast(128), w=["gpar"])
        p.dma('sp', gpar[:, 1, :], K.inp["gd_alog"][li].rearrange("d h -> (d h)").partition_broadcast(128), w=["gpar"])
        A(lambda e: e.activation(out=gpar[:, 1, :], in_=gpar[:, 1, :], func=AF.Exp), ["gpar"], ["gpar"])
        V(lambda e: e.tensor_scalar(out=gpar[:, 1, :], in0=gpar[:, 1, :], scalar1=-1.0, scalar2=None, op0=ALU.mult),
          ["gpar"], ["gpar"])
        p.dma('sp', ab[:], K.zfm[ZR["gd_ab"]:ZR["gd_ab"] + 16, :], r=["zfm"], w=["gdab"])
        for ch in range(6):
            i = ch % 2
            r0 = ZR["gd_q"] + ch * 128
            p.dma('sp', zin[i][:], K.zfm[r0:r0 + 128, :], r=["zfm"], w=[f"gdzin{i}"])
            dst = qk[:, ch, :] if ch < 4 else zo[:]
            dk = f"gdqk{ch}" if ch < 4 else "gdzo"
            V(lambda e: e.tensor_scalar(out=dst, in0=zin[i][:], scalar1=cw[:, ch, 1:2], scalar2=None, op0=ALU.mult),
              [f"gdzin{i}", "cw"], [dk])
            for (a, b) in ((0, LC), (LC, T)):
                V(lambda e: e.scalar_tensor_tensor(out=dst[:, a + 1:b], in0=zin[i][:, a:b - 1], scalar=cw[:, ch, 0:1],
                                                   in1=dst[:, a + 1:b], op0=ALU.mult, op1=ALU.add),
                  [f"gdzin{i}", "cw", dk], [dk])
                V(lambda e: e.scalar_tensor_tensor(out=dst[:, a:b - 1], in0=zin[i][:, a + 1:b], scalar=cw[:, ch, 2:3],
                                                   in1=dst[:, a:b - 1], op0=ALU.mult, op1=ALU.add),
                  [f"gdzin{i}", "cw", dk], [dk])
            A(lambda e: e.activation(out=dst, in_=dst, func=AF.Silu), [dk], [dk])
            if ch >= 4:
                p.dma('pool', K.vfm[2 + (ch - 4), :, :], zo[:], r=["gdzo"], w=["vfm"])
        with _scope(K) as es2:
            SB2 = lambda name, shape, dt=F32: es2.enter_context(_sb(nc, name, shape, dt))
            qt, kt = SB2("gdqt", [128, 4, 64]), SB2("gdkt", [128, 4, 64])
            gt = SB2("gdgt", [128, 16])
            sq = SB2("gdsq", [128, 4, 64])
            nrm = SB2("gdnrm", [128, 8])
            agd, nagd, beta = SB2("gdagd", [128, 8]), SB2("gdnagd", [128, 8]), SB2("gdbeta", [128, 8])
            stgs = [SB2(f"gdstg{i}", [128, 5, 2, 2, 128]) for i in range(2)]

            def hv(ap):
                return ap.rearrange("q (hp hl) k -> q hl hp k", hl=2)

            def sv(o, d):
                return stg[:, o, :, d, :].rearrange("q hl (hp k) -> q hl hp k", k=64)
            for tt in range(NT):
                cols = slice(tt * 128, (tt + 1) * 128)
                stg = stgs[tt % 2]
                sgk = f"gdstg{tt % 2}"
                for ch in range(4):
                    bk = ch
                    p.op('pe', lambda e: e.transpose(out=K.ps[bk][:, 0:128], in_=qk[:, ch, cols], identity=K.ident[:]),
                         r=[f"gdqk{ch}", "ident"], w=[f"ps{bk}"])
                    dst = (qt if ch < 2 else kt)[:, 2 * (ch % 2):2 * (ch % 2) + 2, :]
                    _copy_any(p, ch, dst.rearrange("q h k -> q (h k)"), K.ps[bk][:, 0:128], r=[f"ps{bk}"],
                              w=["gdqt" if ch < 2 else "gdkt"])
                p.op('pe', lambda e: e.transpose(out=K.ps[4][:, 0:16], in_=ab[:, cols], identity=K.ident[0:16, 0:16]),
                     r=["gdab", "ident"], w=["ps4"])
                V(lambda e: e.tensor_copy(out=gt[:], in_=K.ps[4][:, 0:16]), ["ps4"], ["gdgt"])
                V(lambda e: e.tensor_tensor(out=agd[:], in0=gt[:, 0:8], in1=gpar[:, 0, :], op=ALU.add), ["gdgt", "gpar"],
                  ["agd"])
                A(lambda e: e.activation(out=agd[:], in_=agd[:], func=AF.Exp), ["agd"], ["agd"])
                A(lambda e: e.activation(out=agd[:], in_=agd[:], func=AF.Ln, bias=1.0), ["agd"], ["agd"])
                V(lambda e: e.tensor_tensor(out=agd[:], in0=agd[:], in1=gpar[:, 1, :], op=ALU.mult), ["agd", "gpar"],
                  ["agd"])
                A(lambda e: e.activation(out=agd[:], in_=agd[:], func=AF.Exp), ["agd"], ["agd"])
                V(lambda e: e.tensor_scalar(out=nagd[:], in0=agd[:], scalar1=-1.0, scalar2=None, op0=ALU.mult), ["agd"],
                  ["nagd"])
                A(lambda e: e.activation(out=beta[:], in_=gt[:, 8:16], func=AF.Sigmoid), ["gdgt"], ["beta"])
                for (src, sk, col, scl) in ((qt, "gdqt", 0, 64.0), (kt, "gdkt", 4, 1.0)):
                    V(lambda e: e.tensor_tensor(out=sq[:], in0=src[:], in1=src[:], op=ALU.mult), [sk], ["gdsq"])
                    V(lambda e: e.tensor_reduce(out=nrm[:, col:col + 4], in_=sq[:], axis=AX.X, op=ALU.add), ["gdsq"],
                      ["gdnrm"])
                    V(lambda e: e.tensor_scalar(out=nrm[:, col:col + 4], in0=nrm[:, col:col + 4], scalar1=scl,
                                                scalar2=scl * 1e-12, op0=ALU.mult, op1=ALU.add), ["gdnrm"], ["gdnrm"])
                    A(lambda e: e.activation(out=nrm[:, col:col + 4], in_=nrm[:, col:col + 4], func=AF.Sqrt),
                      ["gdnrm"], ["gdnrm"])
                    V(lambda e: e.reciprocal(out=nrm[:, col:col + 4], in_=nrm[:, col:col + 4]), ["gdnrm"], ["gdnrm"])
                    V(lambda e: e.tensor_tensor(out=src[:], in0=src[:],
                                                in1=nrm[:, col:col + 4].unsqueeze(2).to_broadcast([128, 4, 64]),
                                                op=ALU.mult), [sk, "gdnrm"], [sk])
                for d in range(2):
                    V(lambda e: e.tensor_copy(out=sv(0, d), in_=hv(kt[:])), ["gdkt"], [sgk])
                    V(lambda e: e.tensor_copy(out=sv(4, d), in_=hv(qt[:])), ["gdqt"], [sgk])
                    bcol = lambda t_: hv(t_[:, d * 4:(d + 1) * 4].unsqueeze(2).to_broadcast([128, 4, 64]))
                    V(lambda e: e.tensor_copy(out=sv(1, d), in_=bcol(agd)), ["agd"], [sgk])
                    V(lambda e: e.tensor_tensor(out=sv(3, d), in0=hv(kt[:]), in1=bcol(beta), op=ALU.mult),
                      ["gdkt", "beta"], [sgk])
                    V(lambda e: e.tensor_tensor(out=sv(2, d), in0=sv(3, d), in1=bcol(nagd), op=ALU.mult),
                      [sgk, "nagd"], [sgk])
                p.dma('pool', K.gdstg[tt * 128:(tt + 1) * 128, :], stg[:].rearrange("q o h d c -> q (o h d c)"),
                      r=[sgk], w=["gdstgd"])
        p.barrier()


def phase_rwgd_post(K, li):
    p, nc = K.p, K.nc
    V = lambda fn, r, w: p.op('dve', fn, r=r, w=w)
    A = lambda fn, r, w: p.op('act', fn, r=r, w=w)
    with contextlib.ExitStack() as es:
        SB = lambda name, shape, dt=F32: es.enter_context(_sb(nc, name, shape, dt))
        blk = SB("poblk", [128, 128])
        pT = SB("popT", [128, 18])
        gnT = SB("pognT", [128, 1])
        y0, y1 = SB("poy0", [128, 512]), SB("poy1", [128, 512])
        ysq, mean, var = SB("poysq", [128, 512]), SB("pomean", [128, 512]), SB("povar", [128, 512])
        ga, bvv = SB("poga", [128, 512]), SB("pobv", [128, 512])
        p.dma('sp', blk[:], K.inp["blk64"][:, :], w=["blk"])
        p.dma('sp', pT[:], K.inp["rw_pT"][li], w=["pT"])
        p.dma('sp', gnT[:], K.inp["gd_normT"][li].rearrange("(q o) -> q o", o=1), w=["gnT"])
        V(lambda e: e.tensor_scalar(out=blk[:], in0=blk[:], scalar1=1.0 / 64.0, scalar2=None, op0=ALU.mult), ["blk"],
          ["blk"])
        blocks = TOK_BLOCKS if li < DEPTH - 1 else TOK_BLOCKS[1:]
        for m in range(2):
            for hp in range(2):
                for (b0, n) in blocks:
                    p.dma('sp', y0[:, 0:n], K.yfm[m * 2 + hp, :, b0:b0 + n], r=["yfm"], w=["y0"])
                    p.dma('sp', y1[:, 0:n], K.yfm[4 + m * 2 + hp, :, b0:b0 + n], r=["yfm"], w=["y1"])
                    rowsl = slice(hp * 128, (hp + 1) * 128)
                    if m == 0:
                        p.dma('sp', ga[:, 0:n], K.rwg[rowsl, b0:b0 + n], r=["rwg"], w=["ga"])
                        p.dma('sp', bvv[:, 0:n], K.rwbv[rowsl, b0:b0 + n], r=["rwbv"], w=["bvv"])
                    else:
                        r0 = ZR["gd_zg"] + hp * 128
                        p.dma('sp', ga[:, 0:n], K.zfm[r0:r0 + 128, b0:b0 + n], r=["zfm"], w=["ga"])
                    V(lambda e: e.tensor_tensor(out=y0[:, 0:n], in0=y0[:, 0:n], in1=y1[:, 0:n], op=ALU.add),
                      ["y0", "y1"], ["y0"])
                    A(lambda e: e.activation(out=ysq[:, 0:n], in_=y0[:, 0:n], func=AF.Square), ["y0"], ["ysq"])
                    p.op('pe', lambda e: e.matmul(K.ps[0][:, 0:n], lhsT=blk[:, :], rhs=ysq[:, 0:n], start=True,
                                                  stop=True), r=["blk", "ysq"], w=["ps0"])
                    if m == 0:
                        p.op('pe', lambda e: e.matmul(K.ps[1][:, 0:n], lhsT=blk[:, :], rhs=y0[:, 0:n], start=True,
                                                      stop=True), r=["blk", "y0"], w=["ps1"])
                        V(lambda e: e.tensor_copy(out=mean[:, 0:n], in_=K.ps[1][:, 0:n]), ["ps1"], ["mean"])
                        V(lambda e: e.tensor_tensor(out=var[:, 0:n], in0=mean[:, 0:n], in1=mean[:, 0:n], op=ALU.mult),
                          ["mean"], ["var"])
                        V(lambda e: e.tensor_tensor(out=var[:, 0:n], in0=K.ps[0][:, 0:n], in1=var[:, 0:n],
                                                    op=ALU.subtract), ["ps0", "var"], ["var"])
                        V(lambda e: e.tensor_scalar(out=var[:, 0:n], in0=var[:, 0:n], scalar1=64e-5, scalar2=None,
                                                    op0=ALU.add), ["var"], ["var"])
                        A(lambda e: e.activation(out=var[:, 0:n], in_=var[:, 0:n], func=AF.Sqrt), ["var"], ["var"])
                        V(lambda e: e.reciprocal(out=var[:, 0:n], in_=var[:, 0:n]), ["var"], ["var"])
                        V(lambda e: e.tensor_tensor(out=y0[:, 0:n], in0=y0[:, 0:n], in1=mean[:, 0:n], op=ALU.subtract),
                          ["y0", "mean"], ["y0"])
                        V(lambda e: e.tensor_tensor(out=y0[:, 0:n], in0=y0[:, 0:n], in1=var[:, 0:n], op=ALU.mult),
                          ["y0", "var"], ["y0"])
                        V(lambda e: e.tensor_scalar(out=y0[:, 0:n], in0=y0[:, 0:n], scalar1=pT[:, 14 + hp:15 + hp],
                                                    scalar2=pT[:, 16 + hp:17 + hp], op0=ALU.mult, op1=ALU.add),
                          ["y0", "pT"], ["y0"])
                        V(lambda e: e.tensor_tensor(out=y0[:, 0:n], in0=y0[:, 0:n], in1=bvv[:, 0:n], op=ALU.add),
                          ["y0", "bvv"], ["y0"])
                        V(lambda e: e.tensor_tensor(out=y0[:, 0:n], in0=y0[:, 0:n], in1=ga[:, 0:n], op=ALU.mult),
                          ["y0", "ga"], ["y0"])
                    else:
                        V(lambda e: e.tensor_scalar(out=var[:, 0:n], in0=K.ps[0][:, 0:n], scalar1=1e-6, scalar2=None,
                                                    op0=ALU.add), ["ps0"], ["var"])
                        A(lambda e: e.activation(out=var[:, 0:n], in_=var[:, 0:n], func=AF.Sqrt), ["var"], ["var"])
                        V(lambda e: e.reciprocal(out=var[:, 0:n], in_=var[:, 0:n]), ["var"], ["var"])
                        V(lambda e: e.scalar_tensor_tensor(out=y0[:, 0:n], in0=y0[:, 0:n], scalar=gnT[:, 0:1],
                                                           in1=var[:, 0:n], op0=ALU.mult, op1=ALU.mult),
                          ["y0", "gnT", "var"], ["y0"])
                        A(lambda e: e.activation(out=ga[:, 0:n], in_=ga[:, 0:n], func=AF.Silu), ["ga"], ["ga"])
                        V(lambda e: e.tensor_tensor(out=y0[:, 0:n], in0=y0[:, 0:n], in1=ga[:, 0:n], op=ALU.mult),
                          ["y0", "ga"], ["y0"])
                    r0 = m * 256 + hp * 128
                    p.dma('pool', K.mix[r0:r0 + 128, b0:b0 + n], y0[:, 0:n], r=["y0"], w=["mix"])
        p.barrier()


LAST_INPUT_NAMES = []
PHASES = {}


def build_program(phases=None, debug=(), dbg_in=()):
    del LAST_INPUT_NAMES[:]
    nc = bass.Bass("TRN2", target_bir_lowering=False)
    K = Ctx()
    K.nc = nc
    K.inp = {}

    def inp(name, shape, dt=F32):
        K.inp[name] = nc.dram_tensor(name, list(shape), dt, kind="ExternalInput").ap()
        LAST_INPUT_NAMES.append(name)

    def scratch(name, shape, dt=F32):
        if name in dbg_in:
            LAST_INPUT_NAMES.append(name)
            return nc.dram_tensor(name, list(shape), dt, kind="ExternalInput").ap()
        kind = "ExternalOutput" if name in debug else "Internal"
        return nc.dram_tensor(name, list(shape), dt, kind=kind).ap()

    if phases is None:
        phases = [(n, li) for li in range(DEPTH) for n in PHASE_ORDER]
    names = set(n for n, _ in phases)
    inp("xin", [T, D])
    inp("c2T", [128, 8, 2])
    inp("ident", [128, 128])
    inp("ada_w", [DEPTH, D, 6 * D])
    inp("ada_b", [DEPTH, 6 * D])
    inp("ada_bT", [DEPTH, 128, 48])
    if "inproj" in names:
        inp("w_in_p", [DEPTH, D, ZROWS])
    if "outproj" in names:
        inp("w_out", [DEPTH, D, D])
    inp("ln_g", [DEPTH, 2, D])
    inp("ln_b", [DEPTH, 2, D])
    if "s5" in names:
        for n_ in ("s5_lreT", "s5_limT", "s5_dtT"):
            inp(n_, [DEPTH, 128, 16])
        inp("s5_dT", [DEPTH, 128, 2])
        inp("s5_glbT", [DEPTH, 128, 2])
        inp("s5_glu_w", [DEPTH, 256, 256])
        for n_ in ("s5_breT", "s5_bimT", "s5_creT", "s5_cimT"):
            inp(n_, [DEPTH, 16, 128, 128])
    if "hyena" in names:
        inp("hy_conv", [DEPTH, 3, 768])
        inp("hy_conv_b", [DEPTH, 768])
        inp("hy_w1", [DEPTH, 33, 64])
        inp("hy_w2", [DEPTH, 64, 64])
        inp("hy_w3", [DEPTH, 64, 1024])
        for n_ in ("hy_b1", "hy_f1", "hy_b2", "hy_f2"):
            inp(n_, [DEPTH, 64])
        inp("hy_skip", [DEPTH, 2, 256])
        inp("hy_alt", [128, 2], BF16)
        inp("hy_altrow", [1, 128], BF16)
        for sq_, L_ in (("ctx", LC), ("lat", LL)):
            inp(f"hy_featT_{sq_}", [33, L_])
            inp(f"hy_dec_{sq_}", [L_, 256])
            inp(f"dft_{sq_}", [2, L_ // 128, 128, L_], BF16)
    if "scan" in names:
        inp("scan_sel", [6, 128], BF16)
    if names & {"rwprep", "gdprep", "rwgdpost"}:
        inp("rw_w2p", [DEPTH, 4, 128, 128])
        inp("rw_a2p", [DEPTH, 4, 128, 128])
        inp("rw_g2p", [DEPTH, 2, 128, 128])
        inp("rw_pT", [DEPTH, 128, 18])
        inp("blk64", [128, 128])
        inp("gd_conv", [DEPTH, 3, 768])
        inp("gd_dtb", [DEPTH, 2, 4])
        inp("gd_alog", [DEPTH, 2, 4])
        inp("gd_normT", [DEPTH, 128])
    if "router" in names:
        inp("router_w", [DEPTH, D, 64])
        inp("router_b", [DEPTH, 64])
    if "experts" in names:
        inp("ex_w1", [DEPTH, 64, D, 256])
        inp("ex_w3", [DEPTH, 64, D, 256])
        inp("ex_w2", [DEPTH, 64, 256, D])
        inp("sh_w1", [DEPTH, D, 256])
        inp("sh_w3", [DEPTH, D, 256])
        inp("sh_w2", [DEPTH, 256, D])
    K.out = nc.dram_tensor("out", [LL, D], F32, kind="ExternalOutput").ap()
    K.xres = scratch("xres", [T, D])
    K.zfm = scratch("zfm", [ZROWS, T])
    K.mix = scratch("mix", [D, T])
    K.hT = scratch("hT", [D, T], BF16)
    K.hytm = scratch("hytm", [3, T, 256])
    K.rows = scratch("rows", [5, T, 3, 2, 2, 256], BF16)
    K.gdstg = scratch("gdstg", [T, 5 * 2 * 2 * 128])
    K.vfm = scratch("vfm", [4, 128, T])
    K.yfm = scratch("yfm", [8, 128, T])
    K.rwg = scratch("rwg", [256, T])
    K.rwbv = scratch("rwbv", [256, T])
    with contextlib.ExitStack() as es:
        es.enter_context(nc.allow_non_contiguous_dma(reason="tiny per-channel parameter loads"))
        K.p = p = Prog(nc)
        K.ps = [es.enter_context(nc.psum_tensor(f"psb{i}", [128, 512], F32)) for i in range(8)]
        K.ident = es.enter_context(_sb(nc, "ident_sb", [128, 128], F32))
        K.modT = es.enter_context(_sb(nc, "modT", [128, 48, 2], F32))
        K.gates = es.enter_context(_sb(nc, "gates", [128, NT, 64], F32))
        K.gate = {(w, g): es.enter_context(_sb(nc, f"gate{w}{g}", [128, 1024], F32))
                  for w in range(2) for g in range(2)}
        p.dma('sp', K.ident[:], K.inp["ident"][:, :], w=["ident"])
        if "xres" not in dbg_in:
            p.dma('sp', K.xres[:, :], K.inp["xin"][:, :], w=["xres"])
        p.barrier()
        for (name, li) in phases:
            PHASES[name](K, li)
        if "xres_out" in debug:
            xo = nc.dram_tensor("xres_out", [T, D], F32, kind="ExternalOutput").ap()
            p.dma('sp', xo[:, :], K.xres[:, :], r=["xres"])
        p.finish()
    return nc


PHASE_ORDER = ["adaln", "inproj", "gdprep", "rwprep", "scan", "rwgdpost", "hyena", "s5", "outproj", "router", "experts"]
PHASES.update(rwprep=phase_rwprep, gdprep=phase_gdprep, rwgdpost=phase_rwgd_post, scan=phase_scan, hyena=phase_hyena, s5=phase_s5, adaln=phase_adaln, inproj=phase_inproj, outproj=phase_outproj, router=phase_router,
              experts=phase_experts)


_HYC = {}


def _hy_consts():
    if _HYC:
        return _HYC
    import ml_dtypes
    alt = np.where(np.arange(128) % 2 == 0, 1.0, -1.0).astype(np.float32)
    _HYC["hy_alt"] = np.stack([alt, alt], axis=1).astype(ml_dtypes.bfloat16)
    _HYC["hy_altrow"] = alt[None, :].astype(ml_dtypes.bfloat16)
    for sq_, L in (("ctx", LC), ("lat", LL)):
        t = np.linspace(0.0, 1.0, L, dtype=np.float32)[:, None]
        ang = (np.float32(2.0 * math.pi / L) * np.arange(L, dtype=np.float32))[:, None]
        bands = np.linspace(1e-4, 15.0, 16, dtype=np.float32)
        feat = np.concatenate([t, np.cos(bands * ang), -np.sin(bands * ang)], axis=-1).astype(np.float32)
        _HYC[f"hy_featT_{sq_}"] = np.ascontiguousarray(feat.T)
        deltas = np.abs(np.linspace(math.log(1e-2) / 1.5, math.log(1e-2) / 0.3, 256, dtype=np.float32))
        _HYC[f"hy_dec_{sq_}"] = np.exp(-t * deltas[None, :]).astype(np.float32)
        n = np.arange(L, dtype=np.int64)
        prod = (n[:, None] * n[None, :]) % (2 * L)
        ang2 = prod.astype(np.float64) * (2.0 * math.pi / (2 * L))
        nt = L // 128
        tabs = []
        for fn in (np.cos, np.sin):
            tb = fn(ang2).astype(np.float32).reshape(nt, 128, nt, 128)
            tabs.append(tb.transpose(2, 1, 0, 3).reshape(nt, 128, L))
        _HYC[f"dft_{sq_}"] = np.stack(tabs, axis=0).astype(ml_dtypes.bfloat16)
    return _HYC


def host_inputs(inputs, b):
    f = lambda a: np.ascontiguousarray(a, dtype=np.float32)
    m = {}
    m["xin"] = f(np.concatenate([inputs["ctx"][b], inputs["x"][b]], axis=0))
    c2 = np.stack([inputs["c"][b], inputs["c_ctx"]], axis=0)
    m["c2T"] = f(c2.reshape(2, 8, 128).transpose(2, 1, 0))
    m["ident"] = np.eye(128, dtype=np.float32)
    m["ada_bT"] = f(np.asarray(inputs["ada_b"]).reshape(DEPTH, 48, 128).transpose(0, 2, 1))
    w_in = np.asarray(inputs["w_in"])
    wp = np.zeros((DEPTH, D, ZROWS), np.float32)
    wp[:, :, 0:960] = w_in[:, :, 0:960]
    wp[:, :, 1024:2048] = w_in[:, :, 960:1984]
    wp[:, :, 2048:2064] = w_in[:, :, 1984:2000]
    wp[:, :, 2176:2944] = w_in[:, :, 2000:2768]
    wp[:, :, 2944:3200] = w_in[:, :, 2768:3024]
    m["w_in_p"] = wp
    def st(a):
        a = np.asarray(a).reshape(DEPTH, 2, 8, 2, 64)
        return f(a.transpose(0, 3, 4, 1, 2).reshape(DEPTH, 128, 16))
    m["s5_lreT"] = st(inputs["s5_lre"])
    m["s5_limT"] = st(inputs["s5_lim"])
    m["s5_dtT"] = st(np.broadcast_to(np.asarray(inputs["s5_logstep"])[..., None], (DEPTH, 2, 16, 64)))
    m["s5_dT"] = f(np.asarray(inputs["s5_d"]).reshape(DEPTH, 2, 128).transpose(0, 2, 1))
    m["s5_glbT"] = f(np.asarray(inputs["s5_glu_b"]).reshape(DEPTH, 2, 128).transpose(0, 2, 1))
    m["s5_glu_w"] = f(inputs["s5_glu_w"])

    def bT(a):
        a = np.asarray(a)
        o = np.zeros((DEPTH, 2, 8, 128, 128), np.float32)
        for j in range(8):
            for gg in range(2):
                g = 2 * j + gg
                r0 = (g % 8) * 16
                o[:, :, j, r0:r0 + 16, gg * 64:(gg + 1) * 64] = a[:, :, g].transpose(0, 1, 3, 2)
        return o.reshape(DEPTH, 16, 128, 128)

    def cT(a, sign=1.0):
        a = np.asarray(a)
        o = np.zeros((DEPTH, 2, 8, 128, 128), np.float32)
        for j in range(8):
            for gg in range(2):
                g = 2 * j + gg
                c0 = (g % 8) * 16
                o[:, :, j, gg * 64:(gg + 1) * 64, c0:c0 + 16] = a[:, :, g].transpose(0, 1, 3, 2)
        return o.reshape(DEPTH, 16, 128, 128)
    m["s5_breT"] = bT(inputs["s5_bre"])
    m["s5_bimT"] = bT(inputs["s5_bim"])
    m["s5_creT"] = cT(inputs["s5_cre"])
    m["s5_cimT"] = cT(inputs["s5_cim"])
    m.update(_hy_consts())
    import ml_dtypes
    w2, a2, g2 = np.asarray(inputs["rw_w2"]), np.asarray(inputs["rw_a2"]), np.asarray(inputs["rw_g2"])
    w2p = np.zeros((DEPTH, 4, 128, 128), np.float32)
    a2p = np.zeros((DEPTH, 4, 128, 128), np.float32)
    g2p = np.zeros((DEPTH, 2, 128, 128), np.float32)
    for d in range(2):
        for cc in range(2):
            w2p[:, d * 2 + cc, d * 32:(d + 1) * 32, :] = w2[:, d, :, cc * 128:(cc + 1) * 128]
            a2p[:, d * 2 + cc, 64 + d * 32:64 + (d + 1) * 32, :] = a2[:, d, :, cc * 128:(cc + 1) * 128]
    for cc in range(2):
        g2p[:, cc, 0:64, :] = g2[:, :, cc * 128:(cc + 1) * 128]
    m["rw_w2p"], m["rw_a2p"], m["rw_g2p"] = w2p, a2p, g2p
    cols = []
    for nm in ("rw_w0", "rw_a0"):
        a_ = np.asarray(inputs[nm])
        for d in range(2):
            for cc in range(2):
                cols.append(a_[:, d, cc * 128:(cc + 1) * 128])
    for nm in ("rw_kk", "rw_ka", "rw_rk", "rw_gn_g", "rw_gn_b"):
        a_ = np.asarray(inputs[nm]).reshape(DEPTH, 256)
        for cc in range(2):
            cols.append(a_[:, cc * 128:(cc + 1) * 128])
    m["rw_pT"] = f(np.stack(cols, axis=-1))
    blk = np.zeros((128, 128), np.float32)
    blk[0:64, 0:64] = 1.0
    blk[64:128, 64:128] = 1.0
    m["blk64"] = blk
    m["gd_normT"] = f(np.tile(np.asarray(inputs["gd_norm"]), (1, 2)))
    for k in ("gd_conv", "gd_dtb", "gd_alog"):
        m[k] = f(inputs[k])
    sel = np.zeros((6, 128), np.float32)
    for q in range(6):
        sel[q, (q % 2) * 64:(q % 2 + 1) * 64] = 1.0
    m["scan_sel"] = sel.astype(ml_dtypes.bfloat16)
    for k in ("hy_conv", "hy_conv_b", "hy_w1", "hy_w2", "hy_w3", "hy_b1", "hy_f1", "hy_b2", "hy_f2", "hy_skip"):
        m[k] = f(inputs[k])
    for k in ("ada_w", "ada_b", "w_out", "ln_g", "ln_b", "router_w", "router_b", "ex_w1", "ex_w3", "ex_w2",
              "sh_w1", "sh_w3", "sh_w2"):
        m[k] = f(inputs[k])
    return m


def kernel(**inputs):
    nc = build_program()
    names = set(LAST_INPUT_NAMES)
    in_maps = [{k: v for k, v in host_inputs(inputs, b).items() if k in names} for b in range(8)]
    res = run_bass_kernel_spmd(nc, in_maps, core_ids=list(range(8)))
    return np.stack([r["out"] for r in res.results], axis=0)
```

```python
import bisect
import contextlib
import math
import numpy as np
import concourse.bass as bass
import concourse.mybir as mybir
from concourse.bass_utils import run_bass_kernel_spmd

F32 = mybir.dt.float32
BF16 = mybir.dt.bfloat16
ALU = mybir.AluOpType
AF = mybir.ActivationFunctionType
AX = mybir.AxisListType

D = 1024
LC = 256
LL = 4096
T = LC + LL
NT = T // 128
DEPTH = 2
ALPHA = (2 * DEPTH) ** 0.25
ZROWS = 3200
ZR = dict(rw_r=0, rw_k=256, rw_v=512, rw_wa=768, rw_g=896, gd_q=1024, gd_k=1280, gd_v=1536, gd_zg=1792,
          gd_ab=2048, hy=2176, s5=2944)


class Prog:
    def __init__(self, nc, n_dma_sems=(('sp', 10), ('pool', 8), ('act', 4))):
        self.nc = nc
        self.E = {'pe': nc.tensor, 'dve': nc.vector, 'act': nc.scalar, 'pool': nc.gpsimd, 'sp': nc.sync}
        self.es = contextlib.ExitStack()
        self.dma_sems, self.dma_pool, self.dma_next = [], {}, {}
        for q, n in n_dma_sems:
            self.dma_pool[q] = list(range(len(self.dma_sems), len(self.dma_sems) + n))
            self.dma_next[q] = 0
            self.dma_sems += [self.es.enter_context(nc.semaphore(f"dq_{q}{i}")) for i in range(n)]
        self.dma_val = [0] * len(self.dma_sems)
        self.sem, self.cnt, self.ins, self.inc_idx, self.inc_cnt = {}, {}, {}, {}, {}
        self.nsem = 0
        for e in self.E:
            self._new_sem(e)
        self.waited, self.lastw, self.readers = {}, {}, {}

    def _new_sem(self, e):
        self.sem[e] = self.es.enter_context(self.nc.semaphore(f"es_{e}_{self.nsem}"))
        self.nsem += 1
        self.cnt[e] = 0
        self.ins[e] = []
        self.inc_idx[e] = []
        self.inc_cnt[e] = []

    def _eng_count(self, e, idx):
        lst = self.inc_idx[e]
        j = bisect.bisect_left(lst, idx)
        if j < len(lst):
            return self.inc_cnt[e][j]
        self.cnt[e] += 1
        self.ins[e][idx].then_inc(self.sem[e], 1)
        lst.append(idx)
        self.inc_cnt[e].append(self.cnt[e])
        return self.cnt[e]

    def _need(self, waiter, tok):
        if tok[0] == 'eng':
            _, e, idx, sem_id = tok
            if (e == waiter and e == 'pe') or sem_id != id(self.sem[e]):
                return None
            c = self._eng_count(e, idx)
            key = (waiter, 'eng', e, sem_id)
            if self.waited.get(key, 0) >= c:
                return None
            self.waited[key] = c
            return (self.sem[e], c)
        _, k, v = tok
        key = (waiter, 'dma', k)
        if self.waited.get(key, 0) >= v:
            return None
        self.waited[key] = v
        return (self.dma_sems[k], v)

    def _wait(self, waiter, tok):
        n = self._need(waiter, tok)
        if n is not None:
            self.E[waiter].wait_ge(n[0], n[1])

    def _waits(self, eng, deps):
        needs = [n for n in (self._need(eng, d) for d in self._order(deps)) if n is not None]
        for n in needs[:-1]:
            self.E[eng].wait_ge(n[0], n[1])
        return needs[-1] if needs else None

    def _deps(self, reads, writes):
        deps = []
        for k in reads:
            t = self.lastw.get(k)
            if t is not None:
                deps.append(t)
        for k in writes:
            t = self.lastw.get(k)
            if t is not None:
                deps.append(t)
            rd = self.readers.get(k)
            if rd:
                deps.extend(rd.values())
        return deps

    def _register(self, tok, reads, writes):
        for k in reads:
            self.readers.setdefault(k, {})[(tok[0], tok[1])] = tok
        for k in writes:
            self.lastw[k] = tok
            self.readers[k] = {}

    @staticmethod
    def _order(deps):
        return sorted(set(deps), key=lambda t: -t[2])

    def stage(self, eng, items):
        deps = []
        for (_, r, w) in items:
            deps.extend(self._deps(r, w))
        last = self._waits(eng, deps)
        for i, (fn, r, w) in enumerate(items):
            ins = fn(self.E[eng])
            if i == 0 and last is not None:
                ins._wait_ge(last[0], last[1])
            self.ins[eng].append(ins)
            self._register(('eng', eng, len(self.ins[eng]) - 1, id(self.sem[eng])), r, w)

    def op(self, eng, fn, r=(), w=()):
        last = self._waits(eng, self._deps(r, w))
        ins = fn(self.E[eng])
        if last is not None:
            ins._wait_ge(last[0], last[1])
        self.ins[eng].append(ins)
        tok = ('eng', eng, len(self.ins[eng]) - 1, id(self.sem[eng]))
        self._register(tok, r, w)
        return tok

    def dma(self, q, out, in_, r=(), w=(), **kw):
        for d in self._order(self._deps(r, w)):
            self._wait(q, d)
        k = self.dma_pool[q][self.dma_next[q]]
        self.dma_next[q] = (self.dma_next[q] + 1) % len(self.dma_pool[q])
        if self.dma_val[k] > 0:
            self._wait(q, ('dma', k, self.dma_val[k]))
        self.E[q].dma_start(out=out, in_=in_, **kw).then_inc(self.dma_sems[k], 16)
        self.dma_val[k] += 16
        tok = ('dma', k, self.dma_val[k])
        self._register(tok, r, w)
        return tok

    def barrier(self):
        toks = []
        for e in self.E:
            if self.ins[e]:
                toks.append(('eng', e, len(self.ins[e]) - 1, id(self.sem[e])))
        for k, v in enumerate(self.dma_val):
            if v > 0:
                toks.append(('dma', k, v))
        for waiter in self.E:
            for t in toks:
                self._wait(waiter, t)
        self.lastw, self.readers = {}, {}
        for e in self.E:
            if self.cnt[e] > 20000:
                self._new_sem(e)

    def finish(self):
        self.barrier()
        self.es.close()


class Ctx:
    pass


_UID = [0]


@contextlib.contextmanager
def _scope(K):
    with contextlib.ExitStack() as es2:
        yield es2
        K.p.barrier()


def _sb(nc, name, shape, dt):
    _UID[0] += 1
    return nc.sbuf_tensor(f"{name}_u{_UID[0]}", shape, dt)


def _copy_any(p, i, out, in_, r, w):
    if i % 2 == 0:
        p.op('act', lambda e: e.copy(out=out, in_=in_), r=r, w=w)
    else:
        p.op('dve', lambda e: e.tensor_copy(out=out, in_=in_), r=r, w=w)


def phase_adaln(K, li):
    p, nc = K.p, K.nc
    with contextlib.ExitStack() as es:
        c2 = es.enter_context(_sb(nc, "c2", [128, 8, 2], F32))
        cs = es.enter_context(_sb(nc, "cs", [128, 8, 2], F32))
        abT = es.enter_context(_sb(nc, "abT", [128, 48], F32))
        abrow = es.enter_context(_sb(nc, "abrow", [128, 2, 1024], F32))
        wblk = [es.enter_context(_sb(nc, f"adaw{i}", [128, 8, 1024], F32)) for i in range(2)]
        p.dma('sp', c2[:], K.inp["c2T"][:, :, :], w=["c2"])
        p.dma('sp', abT[:], K.inp["ada_bT"][li], w=["abT"])
        for gi, col in enumerate((2048, 5120)):
            p.dma('sp', abrow[:, gi, :], K.inp["ada_b"][li, col:col + 1024].partition_broadcast(128),
                  w=[f"abrow{gi}"])
        p.op('act', lambda e: e.activation(out=cs[:], in_=c2[:], func=AF.Sigmoid), r=["c2"], w=["cs"])
        p.op('dve', lambda e: e.tensor_tensor(out=cs[:], in0=cs[:], in1=c2[:], op=ALU.mult), r=["cs", "c2"],
             w=["cs"])
        aw = K.inp["ada_w"][li].rearrange("(k q) n -> q k n", q=128)
        for blk in range(6):
            wb = wblk[blk % 2]
            wk = f"adaw{blk % 2}"
            p.dma('sp', wb[:], aw[:, :, blk * 1024:(blk + 1) * 1024], w=[wk])
            for jj in range(8):
                j = blk * 8 + jj
                ps = K.ps[j % 2]
                for k in range(8):
                    p.op('pe', lambda e, k=k, jj=jj, ps=ps: e.matmul(
                        ps[:, 0:2], lhsT=wb[:, k, jj * 128:(jj + 1) * 128], rhs=cs[:, k, :],
                        start=(k == 0), stop=(k == 7)), r=[wk, "cs"], w=[f"ps{j % 2}"])
                addc = 1.0 if blk in (1, 4) else 0.0
                p.op('dve', lambda e, j=j, ps=ps, addc=addc: e.tensor_scalar(
                    out=K.modT[:, j, :], in0=ps[:, 0:2], scalar1=abT[:, j:j + 1], scalar2=addc, op0=ALU.add,
                    op1=ALU.add), r=[f"ps{j % 2}", "abT"], w=["modT"])
            if blk in (2, 5):
                gi = 0 if blk == 2 else 1
                for which in range(2):
                    for half in range(2):
                        ps = K.ps[2 + half]
                        for k in range(8):
                            p.op('pe', lambda e, k=k, ps=ps, half=half, which=which: e.matmul(
                                ps[:, :], lhsT=cs[:, k, which:which + 1].to_broadcast([128, 128]),
                                rhs=wb[:, k, half * 512:(half + 1) * 512], start=(k == 0), stop=(k == 7)),
                                r=[wk, "cs"], w=[f"ps{2 + half}"])
                        g = K.gate[(which, gi)]
                        p.op('dve', lambda e, ps=ps, g=g, half=half, gi=gi: e.tensor_tensor(
                            out=g[:, half * 512:(half + 1) * 512], in0=ps[:, :],
                            in1=abrow[:, gi, half * 512:(half + 1) * 512], op=ALU.add),
                            r=[f"ps{2 + half}", f"abrow{gi}"], w=[f"gate{which}{gi}"])
        p.barrier()


def phase_inproj(K, li):
    p, nc = K.p, K.nc
    with contextlib.ExitStack() as es:
        win = es.enter_context(_sb(nc, "win", [128, 8, ZROWS], BF16))
        wst = [es.enter_context(_sb(nc, f"wst{i}", [128, ZROWS], F32)) for i in range(2)]
        xt = [es.enter_context(_sb(nc, f"xt{i}", [128, 1024], F32)) for i in range(2)]
        xmT = [es.enter_context(_sb(nc, f"xmT{i}", [128, 8, 512], BF16)) for i in range(2)]
        zst = [es.enter_context(_sb(nc, f"zst{i}", [128, 512], F32)) for i in range(4)]
        wsrc = K.inp["w_in_p"][li].rearrange("(k q) n -> q k n", q=128)
        for k in range(8):
            p.dma('sp', wst[k % 2][:], wsrc[:, k, :], w=[f"wst{k % 2}"])
            _copy_any(p, k, win[:, k, :], wst[k % 2][:], r=[f"wst{k % 2}"], w=["win"])
        groups = [(0, 2)] + [(2 + 4 * g, 4) for g in range(8)]
        nst = 0
        ntile = 0
        for gidx, (t0, ntl) in enumerate(groups):
            which = 1 if gidx == 0 else 0
            xm = xmT[gidx % 2]
            xmk = f"xmT{gidx % 2}"
            for tl in range(ntl):
                tt = t0 + tl
                xb = xt[ntile % 2]
                xk = f"xt{ntile % 2}"
                ntile += 1
                p.dma('sp', xb[:], K.xres[tt * 128:(tt + 1) * 128, :], r=["xres"], w=[xk])
                for k in range(8):
                    bank = 4 + 2 * (ntile % 2) + (k // 4)
                    p.op('pe', lambda e, k=k, bank=bank, xb=xb: e.transpose(
                        out=K.ps[bank][:, (k % 4) * 128:(k % 4 + 1) * 128], in_=xb[:, k * 128:(k + 1) * 128],
                        identity=K.ident[:]), r=[xk, "ident"], w=[f"ps{bank}"])
                for k in range(8):
                    bank = 4 + 2 * (ntile % 2) + (k // 4)
                    p.op('act', lambda e, k=k, bank=bank, xm=xm, tl=tl, which=which: e.activation(
                        out=xm[:, k, tl * 128:(tl + 1) * 128], in_=K.ps[bank][:, (k % 4) * 128:(k % 4 + 1) * 128],
                        func=AF.Identity, scale=K.modT[:, 8 + k, which:which + 1],
                        bias=K.modT[:, k, which:which + 1]), r=[f"ps{bank}", "modT"], w=[xmk])
            ntok = ntl * 128
            for oc in range(ZROWS // 128):
                bank = oc % 4
                for k in range(8):
                    p.op('pe', lambda e, k=k, bank=bank, oc=oc, xm=xm, ntok=ntok: e.matmul(
                        K.ps[bank][:, 0:ntok], lhsT=win[:, k, oc * 128:(oc + 1) * 128], rhs=xm[:, k, 0:ntok],
                        start=(k == 0), stop=(k == 7)), r=["win", xmk], w=[f"ps{bank}"])
                zs = zst[nst % 4]
                zk = f"zst{nst % 4}"
                _copy_any(p, nst, zs[:, 0:ntok], K.ps[bank][:, 0:ntok], r=[f"ps{bank}"], w=[zk])
                p.dma('pool', K.zfm[oc * 128:(oc + 1) * 128, t0 * 128:t0 * 128 + ntok], zs[:, 0:ntok], r=[zk],
                      w=["zfm"])
                nst += 1
        p.barrier()


def _load_bcast(K, es, name, src_row, n):
    t = es.enter_context(_sb(K.nc, name, [128, n], F32))
    K.p.dma('sp', t[:], src_row.partition_broadcast(128), w=[name])
    return t


def _ln_tile(K, xt, xk, stat, sk, sq, sqk, g, gk, b, bk):
    p = K.p
    p.op('act', lambda e: e.activation(out=sq[:], in_=xt[:], func=AF.Square, accum_out=stat[:, 1:2]),
         r=[xk], w=[sqk, sk + "q"])
    p.op('dve', lambda e: e.tensor_scalar(out=stat[:, 2:3], in0=stat[:, 0:1], scalar1=1.0 / D, scalar2=None,
                                          op0=ALU.mult), r=[sk], w=[sk + "m"])
    p.op('dve', lambda e: e.tensor_tensor(out=stat[:, 3:4], in0=stat[:, 2:3], in1=stat[:, 2:3], op=ALU.mult),
         r=[sk + "m"], w=[sk + "v"])
    p.op('dve', lambda e: e.scalar_tensor_tensor(out=stat[:, 3:4], in0=stat[:, 1:2], scalar=1.0 / D,
                                                 in1=stat[:, 3:4], op0=ALU.mult, op1=ALU.subtract),
         r=[sk + "q", sk + "v"], w=[sk + "v"])
    p.op('dve', lambda e: e.tensor_scalar(out=stat[:, 3:4], in0=stat[:, 3:4], scalar1=1e-5, scalar2=None,
                                          op0=ALU.add), r=[sk + "v"], w=[sk + "v"])
    p.op('act', lambda e: e.activation(out=stat[:, 4:5], in_=stat[:, 3:4], func=AF.Sqrt), r=[sk + "v"],
         w=[sk + "r"])
    p.op('dve', lambda e: e.reciprocal(out=stat[:, 4:5], in_=stat[:, 4:5]), r=[sk + "r"], w=[sk + "r"])
    p.op('dve', lambda e: e.tensor_scalar(out=xt[:], in0=xt[:], scalar1=stat[:, 2:3], scalar2=stat[:, 4:5],
                                          op0=ALU.subtract, op1=ALU.mult), r=[xk, sk + "m", sk + "r"], w=[xk])
    p.op('dve', lambda e: e.tensor_tensor(out=xt[:], in0=xt[:], in1=g[:], op=ALU.mult), r=[xk, gk], w=[xk])
    p.op('dve', lambda e: e.tensor_tensor(out=xt[:], in0=xt[:], in1=b[:], op=ALU.add), r=[xk, bk], w=[xk])


def phase_outproj(K, li):
    p, nc = K.p, K.nc
    need_ctx = li < DEPTH - 1
    with contextlib.ExitStack() as es:
        wo = es.enter_context(_sb(nc, "wo", [128, 8, 1024], BF16))
        wst = [es.enter_context(_sb(nc, f"wost{i}", [128, 1024], F32)) for i in range(2)]
        lng = _load_bcast(K, es, "lng", K.inp["ln_g"][li, 0], D)
        lnb = _load_bcast(K, es, "lnb", K.inp["ln_b"][li, 0], D)
        mst = [es.enter_context(_sb(nc, f"mst{i}", [128, 8, 128], F32)) for i in range(2)]
        mbf = [es.enter_context(_sb(nc, f"mbf{i}", [128, 8, 128], BF16)) for i in range(2)]
        xt = [es.enter_context(_sb(nc, f"xo{i}", [128, 1024], F32)) for i in range(2)]
        t1 = [es.enter_context(_sb(nc, f"t1_{i}", [128, 1024], F32)) for i in range(2)]
        sq = es.enter_context(_sb(nc, "sqo", [128, 1024], F32))
        stat = [es.enter_context(_sb(nc, f"st{i}", [128, 8], F32)) for i in range(2)]
        wsrc = K.inp["w_out"][li].rearrange("(k q) n -> q k n", q=128)
        for k in range(8):
            p.dma('sp', wst[k % 2][:], wsrc[:, k, :], w=[f"wost{k % 2}"])
            _copy_any(p, k, wo[:, k, :], wst[k % 2][:], r=[f"wost{k % 2}"], w=["wo"])
        msrc = K.mix.rearrange("(k q) t -> q k t", q=128)
        tiles = list(range(0 if need_ctx else 2, NT))
        for n, tt in enumerate(tiles):
            i = n % 2
            which = 1 if tt < 2 else 0
            p.dma('sp', mst[i][:], msrc[:, :, tt * 128:(tt + 1) * 128], r=["mix"], w=[f"mst{i}"])
            p.dma('sp', xt[i][:], K.xres[tt * 128:(tt + 1) * 128, :], r=["xres"], w=[f"xo{i}"])
            _copy_any(p, n, mbf[i][:], mst[i][:], r=[f"mst{i}"], w=[f"mbf{i}"])
            for half in range(2):
                bank = 2 * i + half
                for k in range(8):
                    p.op('pe', lambda e: e.matmul(K.ps[bank][:, :], lhsT=mbf[i][:, k, :],
                                                  rhs=wo[:, k, half * 512:(half + 1) * 512], start=(k == 0),
                                                  stop=(k == 7)), r=[f"mbf{i}", "wo"], w=[f"ps{bank}"])
                p.op('dve', lambda e: e.tensor_tensor(out=t1[i][:, half * 512:(half + 1) * 512],
                                                      in0=K.ps[bank][:, :],
                                                      in1=K.gate[(which, 0)][:, half * 512:(half + 1) * 512],
                                                      op=ALU.mult), r=[f"ps{bank}", f"gate{which}0"], w=[f"t1_{i}"])
            p.op('dve', lambda e: e.scalar_tensor_tensor(out=xt[i][:], in0=xt[i][:], scalar=ALPHA, in1=t1[i][:],
                                                         op0=ALU.mult, op1=ALU.add, accum_out=stat[i][:, 0:1]),
                 r=[f"xo{i}", f"t1_{i}"], w=[f"xo{i}", f"st{i}"])
            _ln_tile(K, xt[i], f"xo{i}", stat[i], f"st{i}", sq, "sqo", lng, "lng", lnb, "lnb")
            p.dma('pool', K.xres[tt * 128:(tt + 1) * 128, :], xt[i][:], r=[f"xo{i}"], w=["xres"])
        p.barrier()


def phase_router(K, li):
    p, nc = K.p, K.nc
    need_ctx = li < DEPTH - 1
    BIG = 1.0e30
    with contextlib.ExitStack() as es:
        rw_ = es.enter_context(_sb(nc, "rtw", [128, 8, 64], F32))
        rb = _load_bcast(K, es, "rtb", K.inp["router_b"][li], 64)
        xt = [es.enter_context(_sb(nc, f"xr{i}", [128, 1024], F32)) for i in range(2)]
        hT = [es.enter_context(_sb(nc, f"hT{i}", [128, 8, 128], F32)) for i in range(2)]
        hb = [es.enter_context(_sb(nc, f"hb{i}", [128, 8, 128], BF16)) for i in range(2)]
        sc = es.enter_context(_sb(nc, "rsc", [128, 64], F32))
        bi = es.enter_context(_sb(nc, "rbi", [128, 64], F32))
        b2 = es.enter_context(_sb(nc, "rb2", [128, 64], F32))
        mk = es.enter_context(_sb(nc, "rmk", [128, 64], F32))
        g1 = es.enter_context(_sb(nc, "rg1", [128, 8], F32))
        g2 = es.enter_context(_sb(nc, "rg2", [128, 8], F32))
        gm = es.enter_context(_sb(nc, "rgm", [128, 8], F32))
        m8 = es.enter_context(_sb(nc, "rm8", [128, 8], F32))
        ssum = es.enter_context(_sb(nc, "rss", [128, 2], F32))
        p.dma('sp', rw_[:], K.inp["router_w"][li].rearrange("(k q) n -> q k n", q=128), w=["rtw"])
        tiles = list(range(0 if need_ctx else 2, NT))
        for n, tt in enumerate(tiles):
            i = n % 2
            which = 1 if tt < 2 else 0
            p.dma('sp', xt[i][:], K.xres[tt * 128:(tt + 1) * 128, :], r=["xres"], w=[f"xr{i}"])
            for k in range(8):
                bank = 4 * i + (k // 4)
                p.op('pe', lambda e: e.transpose(out=K.ps[bank][:, (k % 4) * 128:(k % 4 + 1) * 128],
                                                 in_=xt[i][:, k * 128:(k + 1) * 128], identity=K.ident[:]),
                     r=[f"xr{i}", "ident"], w=[f"ps{bank}"])
            for k in range(8):
                bank = 4 * i + (k // 4)
                p.op('act', lambda e: e.activation(out=hT[i][:, k, :],
                                                   in_=K.ps[bank][:, (k % 4) * 128:(k % 4 + 1) * 128],
                                                   func=AF.Identity, scale=K.modT[:, 32 + k, which:which + 1],
                                                   bias=K.modT[:, 24 + k, which:which + 1]),
                     r=[f"ps{bank}", "modT"], w=[f"hT{i}"])
            p.op('dve', lambda e: e.tensor_copy(out=hb[i][:], in_=hT[i][:]), r=[f"hT{i}"], w=[f"hb{i}"])
            p.dma('pool', K.hT.rearrange("(k q) t -> q k t", q=128)[:, :, tt * 128:(tt + 1) * 128], hb[i][:],
                  r=[f"hb{i}"], w=["hTd"])
            bank = 4 * i + 2
            for k in range(8):
                p.op('pe', lambda e: e.matmul(K.ps[bank][:, 0:64], lhsT=hT[i][:, k, :], rhs=rw_[:, k, :],
                                              start=(k == 0), stop=(k == 7)), r=[f"hT{i}", "rtw"], w=[f"ps{bank}"])
            p.op('act', lambda e: e.activation(out=sc[:], in_=K.ps[bank][:, 0:64], func=AF.Sigmoid),
                 r=[f"ps{bank}"], w=["rsc"])
            V = lambda fn, r, w: p.op('dve', fn, r=r, w=w)
            V(lambda e: e.tensor_tensor(out=bi[:], in0=sc[:], in1=rb[:], op=ALU.add), ["rsc", "rtb"], ["rbi"])
            bi3 = bi[:].rearrange("q (g j) -> q g j", j=8)
            b23 = b2[:].rearrange("q (g j) -> q g j", j=8)
            mk3 = mk[:].rearrange("q (g j) -> q g j", j=8)
            V(lambda e: e.tensor_reduce(out=g1[:], in_=bi3, axis=AX.X, op=ALU.max), ["rbi"], ["rg1"])
            V(lambda e: e.tensor_tensor(out=mk3, in0=bi3, in1=g1[:].unsqueeze(2).to_broadcast([128, 8, 8]),
                                        op=ALU.is_equal), ["rbi", "rg1"], ["rmk"])
            V(lambda e: e.scalar_tensor_tensor(out=b2[:], in0=mk[:], scalar=-BIG, in1=bi[:], op0=ALU.mult,
                                               op1=ALU.add), ["rmk", "rbi"], ["rb2"])
            V(lambda e: e.tensor_reduce(out=g2[:], in_=b23, axis=AX.X, op=ALU.max), ["rb2"], ["rg2"])
            V(lambda e: e.tensor_tensor(out=g1[:], in0=g1[:], in1=g2[:], op=ALU.add), ["rg1", "rg2"], ["rg1"])
            V(lambda e: e.max(out=m8[:], in_=g1[:]), ["rg1"], ["rm8"])
            V(lambda e: e.tensor_scalar(out=gm[:], in0=g1[:], scalar1=m8[:, 3:4], scalar2=None, op0=ALU.is_ge),
              ["rg1", "rm8"], ["rgm"])
            V(lambda e: e.tensor_tensor(out=b23, in0=bi3, in1=gm[:].unsqueeze(2).to_broadcast([128, 8, 8]),
                                        op=ALU.mult), ["rbi", "rgm"], ["rb2"])
            V(lambda e: e.tensor_scalar(out=gm[:], in0=gm[:], scalar1=BIG, scalar2=BIG, op0=ALU.mult,
                                        op1=ALU.subtract), ["rgm"], ["rgm"])
            V(lambda e: e.tensor_tensor(out=b23, in0=b23, in1=gm[:].unsqueeze(2).to_broadcast([128, 8, 8]),
                                        op=ALU.add), ["rb2", "rgm"], ["rb2"])
            V(lambda e: e.max(out=m8[:], in_=b2[:]), ["rb2"], ["rm8"])
            V(lambda e: e.tensor_scalar(out=mk[:], in0=b2[:], scalar1=m8[:, 7:8], scalar2=None, op0=ALU.is_ge),
              ["rb2", "rm8"], ["rmk"])
            V(lambda e: e.scalar_tensor_tensor(out=mk[:], in0=sc[:], scalar=1.0, in1=mk[:], op0=ALU.mult,
                                               op1=ALU.mult, accum_out=ssum[:, 0:1]), ["rsc", "rmk"],
              ["rmk", "rss"])
            V(lambda e: e.reciprocal(out=ssum[:, 1:2], in_=ssum[:, 0:1]), ["rss"], ["rss"])
            V(lambda e: e.tensor_scalar(out=K.gates[:, tt, :], in0=mk[:], scalar1=ssum[:, 1:2], scalar2=2.5,
                                        op0=ALU.mult, op1=ALU.mult), ["rmk", "rss"], ["gates"])
        p.barrier()


def phase_experts(K, li):
    p, nc = K.p, K.nc
    need_ctx = li < DEPTH - 1
    t_lo = 0 if need_ctx else 2
    parts = [(t_lo, 17), (17, NT)]
    with contextlib.ExitStack() as es:
        NTP = 17
        hT = es.enter_context(_sb(nc, "ehT", [128, 8, NTP * 128], BF16))
        acc = es.enter_context(_sb(nc, "eacc", [128, NTP, 1024], F32))
        w13 = [es.enter_context(_sb(nc, f"ew13_{i}", [128, 2, 8, 256], BF16)) for i in range(2)]
        w2 = [es.enter_context(_sb(nc, f"ew2_{i}", [128, 2, 1024], BF16)) for i in range(2)]
        g = [es.enter_context(_sb(nc, f"eg{i}", [128, 2, NTP * 128], BF16)) for i in range(2)]
        sa = [es.enter_context(_sb(nc, f"esa{i}", [128, 512], F32)) for i in range(2)]
        lng = _load_bcast(K, es, "lng2", K.inp["ln_g"][li, 1], D)
        lnb = _load_bcast(K, es, "lnb2", K.inp["ln_b"][li, 1], D)
        xt = [es.enter_context(_sb(nc, f"xe{i}", [128, 1024], F32)) for i in range(2)]
        sq = es.enter_context(_sb(nc, "sqe", [128, 1024], F32))
        stat = [es.enter_context(_sb(nc, f"ste{i}", [128, 8], F32)) for i in range(2)]
        hsrc = K.hT.rearrange("(k q) t -> q k t", q=128)
        nsa = 0
        for (ta, tb) in parts:
            ntl = tb - ta
            ntok = ntl * 128
            p.dma('sp', hT[:, :, 0:ntok], hsrc[:, :, ta * 128:tb * 128], r=["hTd"], w=["ehT"])
            for ei in range(65):
                i = ei % 2
                if ei < 64:
                    s1, s3, s2 = K.inp["ex_w1"][li, ei], K.inp["ex_w3"][li, ei], K.inp["ex_w2"][li, ei]
                else:
                    s1, s3, s2 = K.inp["sh_w1"][li], K.inp["sh_w3"][li], K.inp["sh_w2"][li]
                p.dma('pool', w13[i][:, 0], s1.rearrange("(k q) f -> q k f", q=128), w=[f"ew13_{i}"])
                p.dma('pool', w13[i][:, 1], s3.rearrange("(k q) f -> q k f", q=128), w=[f"ew13_{i}"])
                p.dma('pool', w2[i][:], s2.rearrange("(c q) n -> q c n", q=128), w=[f"ew2_{i}"])
                blocks = [(b0, min(512, ntok - b0)) for b0 in range(0, ntok, 512)]
                for fc in range(2):
                    for (b0, bn) in blocks:
                        for ab in range(2):
                            bank = 2 * (nsa % 2) + ab
                            for k in range(8):
                                p.op('pe', lambda e: e.matmul(
                                    K.ps[bank][:, 0:bn], lhsT=w13[i][:, ab, k, fc * 128:(fc + 1) * 128],
                                    rhs=hT[:, k, b0:b0 + bn], start=(k == 0), stop=(k == 7)),
                                    r=[f"ew13_{i}", "ehT"], w=[f"ps{bank}"])
                        ba = 2 * (nsa % 2)
                        sab = sa[nsa % 2]
                        p.op('act', lambda e: e.activation(out=sab[:, 0:bn], in_=K.ps[ba][:, 0:bn], func=AF.Silu),
                             r=[f"ps{ba}"], w=[f"esa{nsa % 2}"])
                        p.op('dve', lambda e: e.tensor_tensor(out=g[i][:, fc, b0:b0 + bn], in0=sab[:, 0:bn],
                                                              in1=K.ps[ba + 1][:, 0:bn], op=ALU.mult),
                             r=[f"esa{nsa % 2}", f"ps{ba + 1}"], w=[f"eg{i}"])
                        nsa += 1
                for tl in range(ntl):
                    tt = ta + tl
                    for half in range(2):
                        bank = 4 + 2 * (tl % 2) + half
                        for fc in range(2):
                            p.op('pe', lambda e: e.matmul(
                                K.ps[bank][:, :], lhsT=g[i][:, fc, tl * 128:(tl + 1) * 128],
                                rhs=w2[i][:, fc, half * 512:(half + 1) * 512], start=(fc == 0), stop=(fc == 1)),
                                r=[f"eg{i}", f"ew2_{i}"], w=[f"ps{bank}"])
                        a_out = acc[:, tl, half * 512:(half + 1) * 512]
                        if ei == 0:
                            p.op('dve', lambda e: e.tensor_scalar(out=a_out, in0=K.ps[bank][:, :],
                                                                  scalar1=K.gates[:, tt, 0:1], scalar2=None,
                                                                  op0=ALU.mult), r=[f"ps{bank}", "gates"],
                                 w=[f"eacc{tl}"])
                        else:
                            scal = K.gates[:, tt, ei:ei + 1] if ei < 64 else 1.0
                            p.op('dve', lambda e: e.scalar_tensor_tensor(out=a_out, in0=K.ps[bank][:, :],
                                                                         scalar=scal, in1=a_out, op0=ALU.mult,
                                                                         op1=ALU.add),
                                 r=[f"ps{bank}", "gates", f"eacc{tl}"], w=[f"eacc{tl}"])
            for tl in range(ntl):
                tt = ta + tl
                i = tl % 2
                which = 1 if tt < 2 else 0
                p.dma('sp', xt[i][:], K.xres[tt * 128:(tt + 1) * 128, :], r=["xres"], w=[f"xe{i}"])
                p.op('dve', lambda e: e.tensor_tensor(out=acc[:, tl, :], in0=acc[:, tl, :],
                                                      in1=K.gate[(which, 1)][:], op=ALU.mult),
                     r=[f"eacc{tl}", f"gate{which}1"], w=[f"eacc{tl}"])
                p.op('dve', lambda e: e.scalar_tensor_tensor(out=xt[i][:], in0=xt[i][:], scalar=ALPHA,
                                                             in1=acc[:, tl, :], op0=ALU.mult, op1=ALU.add,
                                                             accum_out=stat[i][:, 0:1]),
                     r=[f"xe{i}", f"eacc{tl}"], w=[f"xe{i}", f"ste{i}"])
                _ln_tile(K, xt[i], f"xe{i}", stat[i], f"ste{i}", sq, "sqe", lng, "lng2", lnb, "lnb2")
                if li == DEPTH - 1:
                    p.dma('pool', K.out[(tt - 2) * 128:(tt - 1) * 128, :], xt[i][:], r=[f"xe{i}"], w=["out"])
                else:
                    p.dma('pool', K.xres[tt * 128:(tt + 1) * 128, :], xt[i][:], r=[f"xe{i}"], w=["xres"])
        p.barrier()


TWO_PI = 2.0 * math.pi
MAGIC = 12582912.0
PI_LO = 3.1415925


def _sin(K, out, x, shift, tmp, keys_r, key_w, key_t):
    p = K.p
    p.op('dve', lambda e: e.tensor_scalar(out=out, in0=x, scalar1=shift, scalar2=None, op0=ALU.add), r=keys_r,
         w=[key_w])
    p.op('dve', lambda e: e.tensor_scalar(out=tmp, in0=out, scalar1=1.0 / TWO_PI, scalar2=MAGIC, op0=ALU.mult,
                                          op1=ALU.add), r=[key_w], w=[key_t])
    p.op('dve', lambda e: e.tensor_scalar(out=tmp, in0=tmp, scalar1=-MAGIC, scalar2=-TWO_PI, op0=ALU.add,
                                          op1=ALU.mult), r=[key_t], w=[key_t])
    p.op('dve', lambda e: e.tensor_tensor(out=tmp, in0=tmp, in1=out, op=ALU.add), r=[key_t, key_w], w=[key_t])
    p.op('dve', lambda e: e.tensor_scalar(out=tmp, in0=tmp, scalar1=PI_LO, scalar2=-PI_LO, op0=ALU.min,
                                          op1=ALU.max), r=[key_t], w=[key_t])
    p.op('act', lambda e: e.activation(out=out, in_=tmp, func=AF.Sin), r=[key_t], w=[key_w])


S5_BLOCKS0 = [(0, 256)] + [(256 + 512 * b, 512) for b in range(8)]
NDBL = 13
S5_Q = 16
S5_LQ = 4
S5_NCH = T // S5_Q
S5_LC = 9


def phase_s5(K, li):
    p, nc = K.p, K.nc
    V = lambda fn, r, w: p.op('dve', fn, r=r, w=w)
    with contextlib.ExitStack() as es:
        SB = lambda name, shape, dt=F32: es.enter_context(_sb(nc, name, shape, dt))
        lre, lim, dtt = SB("s5lre", [128, 16]), SB("s5lim", [128, 16]), SB("s5dt", [128, 16])
        ar, ai, mag = SB("s5ar", [128, 16]), SB("s5ai", [128, 16]), SB("s5mag", [128, 16])
        sn, cs_, tmp = SB("s5sn", [128, 16]), SB("s5cs", [128, 16]), SB("s5tmp", [128, 16])
        den, t2 = SB("s5den", [128, 16]), SB("s5t2", [128, 16])
        co_re, co_im, co_imn = SB("s5core", [128, 16]), SB("s5coim", [128, 16]), SB("s5coimn", [128, 16])
        pw_re, pw_im, pw_imn = (SB("s5pwre", [128, NDBL, 16]), SB("s5pwim", [128, NDBL, 16]),
                                SB("s5pwimn", [128, NDBL, 16]))
        dsk, glb = SB("s5d", [128, 2]), SB("s5glb", [128, 2])
        gluw = SB("s5gluw", [128, 2, 256], BF16)
        p.dma('sp', lre[:], K.inp["s5_lreT"][li], w=["lre"])
        p.dma('sp', lim[:], K.inp["s5_limT"][li], w=["lim"])
        p.dma('sp', dtt[:], K.inp["s5_dtT"][li], w=["dtt"])
        p.dma('sp', dsk[:], K.inp["s5_dT"][li], w=["dsk"])
        p.dma('sp', glb[:], K.inp["s5_glbT"][li], w=["glb"])
        p.dma('pool', gluw[:], K.inp["s5_glu_w"][li].rearrange("(c q) n -> q c n", q=128), w=["gluw"])
        p.op('act', lambda e: e.activation(out=dtt[:], in_=dtt[:], func=AF.Exp), r=["dtt"], w=["dtt"])
        V(lambda e: e.tensor_tensor(out=ar[:], in0=lre[:], in1=dtt[:], op=ALU.mult), ["lre", "dtt"], ["ar"])
        V(lambda e: e.tensor_tensor(out=ai[:], in0=lim[:], in1=dtt[:], op=ALU.mult), ["lim", "dtt"], ["ai"])
        p.op('act', lambda e: e.activation(out=mag[:], in_=ar[:], func=AF.Exp), r=["ar"], w=["mag"])
        _sin(K, sn[:], ai[:], 0.0, tmp[:], ["ai"], "sn", "s5tmp")
        _sin(K, cs_[:], ai[:], math.pi / 2.0, tmp[:], ["ai"], "cs", "s5tmp")
        V(lambda e: e.tensor_tensor(out=pw_re[:, 0, :], in0=mag[:], in1=cs_[:], op=ALU.mult), ["mag", "cs"], ["pw"])
        V(lambda e: e.tensor_tensor(out=pw_im[:, 0, :], in0=mag[:], in1=sn[:], op=ALU.mult), ["mag", "sn"], ["pw"])
        V(lambda e: e.tensor_tensor(out=den[:], in0=lre[:], in1=lre[:], op=ALU.mult), ["lre"], ["den"])
        V(lambda e: e.tensor_tensor(out=t2[:], in0=lim[:], in1=lim[:], op=ALU.mult), ["lim"], ["t2"])
        V(lambda e: e.tensor_tensor(out=den[:], in0=den[:], in1=t2[:], op=ALU.add), ["den", "t2"], ["den"])
        V(lambda e: e.reciprocal(out=den[:], in_=den[:]), ["den"], ["den"])
        V(lambda e: e.tensor_scalar(out=tmp[:], in0=pw_re[:, 0, :], scalar1=-1.0, scalar2=None, op0=ALU.add),
          ["pw"], ["s5tmp"])
        V(lambda e: e.tensor_tensor(out=co_re[:], in0=tmp[:], in1=lre[:], op=ALU.mult), ["s5tmp", "lre"], ["core"])
        V(lambda e: e.tensor_tensor(out=t2[:], in0=pw_im[:, 0, :], in1=lim[:], op=ALU.mult), ["pw", "lim"], ["t2"])
        V(lambda e: e.tensor_tensor(out=co_re[:], in0=co_re[:], in1=t2[:], op=ALU.add), ["core", "t2"], ["core"])
        V(lambda e: e.tensor_tensor(out=co_re[:], in0=co_re[:], in1=den[:], op=ALU.mult), ["core", "den"], ["core"])
        V(lambda e: e.tensor_tensor(out=co_im[:], in0=pw_im[:, 0, :], in1=lre[:], op=ALU.mult), ["pw", "lre"],
          ["coim"])
        V(lambda e: e.tensor_tensor(out=t2[:], in0=tmp[:], in1=lim[:], op=ALU.mult), ["s5tmp", "lim"], ["t2"])
        V(lambda e: e.tensor_tensor(out=co_im[:], in0=co_im[:], in1=t2[:], op=ALU.subtract), ["coim", "t2"],
          ["coim"])
        V(lambda e: e.tensor_tensor(out=co_im[:], in0=co_im[:], in1=den[:], op=ALU.mult), ["coim", "den"], ["coim"])
        V(lambda e: e.tensor_scalar(out=co_imn[:], in0=co_im[:], scalar1=-1.0, scalar2=None, op0=ALU.mult),
          ["coim"], ["coimn"])
        for m in range(1, NDBL):
            V(lambda e: e.tensor_tensor(out=tmp[:], in0=pw_re[:, m - 1, :], in1=pw_re[:, m - 1, :], op=ALU.mult),
              ["pw"], ["s5tmp"])
            V(lambda e: e.tensor_tensor(out=t2[:], in0=pw_im[:, m - 1, :], in1=pw_im[:, m - 1, :], op=ALU.mult),
              ["pw"], ["t2"])
            V(lambda e: e.tensor_tensor(out=pw_re[:, m, :], in0=tmp[:], in1=t2[:], op=ALU.subtract),
              ["s5tmp", "t2"], ["pw"])
            V(lambda e: e.tensor_tensor(out=tmp[:], in0=pw_re[:, m - 1, :], in1=pw_im[:, m - 1, :], op=ALU.mult),
              ["pw"], ["s5tmp"])
            V(lambda e: e.tensor_scalar(out=pw_im[:, m, :], in0=tmp[:], scalar1=2.0, scalar2=None, op0=ALU.mult),
              ["s5tmp"], ["pw"])
        V(lambda e: e.tensor_scalar(out=pw_imn[:], in0=pw_im[:], scalar1=-1.0, scalar2=None, op0=ALU.mult),
          ["pw"], ["pwn"])
        ub = SB("s5ub", [128, 2, T], BF16)
        Y = SB("s5Y", [128, 2, T])
        with _scope(K) as es2:
            ust = es2.enter_context(_sb(nc, "s5ust", [128, T], F32))
            for c in range(2):
                r0 = ZR["s5"] + c * 128
                p.dma('sp', ust[:], K.zfm[r0:r0 + 128, :], r=["zfm"], w=["ust"])
                lat_in = ust[:, LC:T].rearrange("q (r c) -> q c r", c=64)
                lat_ub = ub[:, c, LC:T].rearrange("q (c r) -> q c r", r=64)
                lat_y = Y[:, c, LC:T].rearrange("q (c r) -> q c r", r=64)
                p.op('act', lambda e: e.copy(out=ub[:, c, 0:LC], in_=ust[:, 0:LC]), r=["ust"], w=["ub"])
                p.op('act', lambda e: e.copy(out=lat_ub, in_=lat_in), r=["ust"], w=["ub"])
                V(lambda e: e.tensor_scalar(out=Y[:, c, 0:LC], in0=ust[:, 0:LC], scalar1=dsk[:, c:c + 1],
                                            scalar2=None, op0=ALU.mult), ["ust", "dsk"], [f"Y{c}"])
                V(lambda e: e.tensor_scalar(out=lat_y, in0=lat_in, scalar1=dsk[:, c:c + 1], scalar2=None,
                                            op0=ALU.mult), ["ust", "dsk"], [f"Y{c}"])
        with _scope(K) as es2:
            SB2 = lambda name, shape, dt=F32: es2.enter_context(_sb(nc, name, shape, dt))
            X = [[SB2(f"s5X{a}{ri}", [128, T]) for ri in range(2)] for a in range(2)]
            xb = [SB2(f"s5xb{ri}", [128, T], BF16) for ri in range(2)]
            bc = [SB2(f"s5bc{i}", [128, 4, 128], BF16) for i in range(2)]
            tq = [SB2(f"s5tq{i}", [128, 512]) for i in range(2)]
            Cx = [[SB2(f"s5C{a}{ri}", [128, S5_NCH]) for ri in range(2)] for a in range(2)]
            PT = [SB2(f"s5PT{ri}", [128, S5_Q]) for ri in range(2)]
            ptt = SB2("s5ptt", [128, S5_Q])
            nblk = 0
            for d in range(2):
                for j in range(8):
                    dj = d * 8 + j
                    c = j // 4
                    bi = dj % 2
                    for wi, wn in enumerate(("s5_breT", "s5_bimT", "s5_creT", "s5_cimT")):
                        p.dma('pool', bc[bi][:, wi, :], K.inp[wn][li, dj], w=[f"bc{bi}"])
                    V(lambda e: e.tensor_scalar(out=bc[bi][:, 3, :], in0=bc[bi][:, 3, :], scalar1=-1.0,
                                                scalar2=None, op0=ALU.mult), [f"bc{bi}"], [f"bc{bi}"])
                    if d == 0:
                        blocks = [(s0, n, s0) for (s0, n) in S5_BLOCKS0]
                    else:
                        blocks = [(256 + 512 * b, 512, 512 * b) for b in range(8)] + [(0, 256, LL)]
                    A = X[0]
                    for (s0, n, xp) in blocks:
                        bk = 2 * (nblk % 2)
                        for ri in range(2):
                            p.op('pe', lambda e: e.matmul(K.ps[bk + ri][:, 0:n], lhsT=bc[bi][:, ri, :],
                                                          rhs=ub[:, c, s0:s0 + n], start=True, stop=True),
                                 r=[f"bc{bi}", "ub"], w=[f"ps{bk + ri}"])
                        t_ = tq[nblk % 2]
                        tk = f"tq{nblk % 2}"
                        V(lambda e: e.tensor_scalar(out=t_[:, 0:n], in0=K.ps[bk + 1][:, 0:n],
                                                    scalar1=co_imn[:, dj:dj + 1], scalar2=None, op0=ALU.mult),
                          [f"ps{bk + 1}", "coimn"], [tk])
                        V(lambda e: e.scalar_tensor_tensor(out=A[0][:, xp:xp + n], in0=K.ps[bk][:, 0:n],
                                                           scalar=co_re[:, dj:dj + 1], in1=t_[:, 0:n],
                                                           op0=ALU.mult, op1=ALU.add),
                          [f"ps{bk}", "core", tk], ["X00"])
                        V(lambda e: e.tensor_scalar(out=t_[:, 0:n], in0=K.ps[bk][:, 0:n],
                                                    scalar1=co_im[:, dj:dj + 1], scalar2=None, op0=ALU.mult),
                          [f"ps{bk}", "coim"], [tk])
                        V(lambda e: e.scalar_tensor_tensor(out=A[1][:, xp:xp + n], in0=K.ps[bk + 1][:, 0:n],
                                                           scalar=co_re[:, dj:dj + 1], in1=t_[:, 0:n],
                                                           op0=ALU.mult, op1=ALU.add),
                          [f"ps{bk + 1}", "core", tk], ["X01"])
                        nblk += 1
                    xv = lambda t_: t_[:].rearrange("q (c j) -> q c j", j=S5_Q)
                    for m in range(S5_LQ):
                        sh = 1 << m
                        a, b = m % 2, (m + 1) % 2
                        src, dst = X[a], X[b]
                        sk = [f"X{a}0", f"X{a}1"]
                        dk = [f"X{b}0", f"X{b}1"]
                        pr, pi_, pin = pw_re[:, m, dj:dj + 1], pw_im[:, m, dj:dj + 1], pw_imn[:, m, dj:dj + 1]
                        if d == 0:
                            o_sl, i_sl, c_sl = slice(sh, S5_Q), slice(0, S5_Q - sh), slice(0, sh)
                        else:
                            o_sl, i_sl, c_sl = slice(0, S5_Q - sh), slice(sh, S5_Q), slice(S5_Q - sh, S5_Q)
                        for ri in range(2):
                            p.op('act', lambda e: e.copy(out=xv(dst[ri])[:, :, c_sl], in_=xv(src[ri])[:, :, c_sl]),
                                 r=[sk[ri]], w=[dk[ri]])
                        V(lambda e: e.scalar_tensor_tensor(out=xv(dst[0])[:, :, o_sl], in0=xv(src[0])[:, :, i_sl], scalar=pr,
                                                           in1=xv(src[0])[:, :, o_sl], op0=ALU.mult, op1=ALU.add),
                          [sk[0], "pw"], [dk[0]])
                        V(lambda e: e.scalar_tensor_tensor(out=xv(dst[0])[:, :, o_sl], in0=xv(src[1])[:, :, i_sl], scalar=pin,
                                                           in1=xv(dst[0])[:, :, o_sl], op0=ALU.mult, op1=ALU.add),
                          [sk[1], "pwn", dk[0]], [dk[0]])
                        V(lambda e: e.scalar_tensor_tensor(out=xv(dst[1])[:, :, o_sl], in0=xv(src[1])[:, :, i_sl], scalar=pr,
                                                           in1=xv(src[1])[:, :, o_sl], op0=ALU.mult, op1=ALU.add),
                          [sk[1], "pw"], [dk[1]])
                        V(lambda e: e.scalar_tensor_tensor(out=xv(dst[1])[:, :, o_sl], in0=xv(src[0])[:, :, i_sl], scalar=pi_,
                                                           in1=xv(dst[1])[:, :, o_sl], op0=ALU.mult, op1=ALU.add),
                          [sk[0], "pw", dk[1]], [dk[1]])
                    R = X[S5_LQ % 2]
                    rk = [f"X{S5_LQ % 2}0", f"X{S5_LQ % 2}1"]
                    e_col = S5_Q - 1 if d == 0 else 0
                    for ri in range(2):
                        V(lambda e: e.tensor_copy(out=Cx[0][ri][:], in_=xv(R[ri])[:, :, e_col]), [rk[ri]], [f"C0{ri}"])
                    for m in range(S5_LC):
                        sh = 1 << m
                        a, b = m % 2, (m + 1) % 2
                        src, dst = Cx[a], Cx[b]
                        sk = [f"C{a}0", f"C{a}1"]
                        dk = [f"C{b}0", f"C{b}1"]
                        mm = S5_LQ + m
                        pr, pi_, pin = pw_re[:, mm, dj:dj + 1], pw_im[:, mm, dj:dj + 1], pw_imn[:, mm, dj:dj + 1]
                        if d == 0:
                            o_sl, i_sl, c_sl = slice(sh, S5_NCH), slice(0, S5_NCH - sh), slice(0, sh)
                        else:
                            o_sl, i_sl, c_sl = slice(0, S5_NCH - sh), slice(sh, S5_NCH), slice(S5_NCH - sh, S5_NCH)
                        for ri in range(2):
                            p.op('act', lambda e: e.copy(out=dst[ri][:, c_sl], in_=src[ri][:, c_sl]), r=[sk[ri]], w=[dk[ri]])
                        V(lambda e: e.scalar_tensor_tensor(out=dst[0][:, o_sl], in0=src[0][:, i_sl], scalar=pr,
                                                           in1=src[0][:, o_sl], op0=ALU.mult, op1=ALU.add),
                          [sk[0], "pw"], [dk[0]])
                        V(lambda e: e.scalar_tensor_tensor(out=dst[0][:, o_sl], in0=src[1][:, i_sl], scalar=pin,
                                                           in1=dst[0][:, o_sl], op0=ALU.mult, op1=ALU.add),
                          [sk[1], "pwn", dk[0]], [dk[0]])
                        V(lambda e: e.scalar_tensor_tensor(out=dst[1][:, o_sl], in0=src[1][:, i_sl], scalar=pr,
                                                           in1=src[1][:, o_sl], op0=ALU.mult, op1=ALU.add),
                          [sk[1], "pw"], [dk[1]])
                        V(lambda e: e.scalar_tensor_tensor(out=dst[1][:, o_sl], in0=src[0][:, i_sl], scalar=pi_,
                                                           in1=dst[1][:, o_sl], op0=ALU.mult, op1=ALU.add),
                          [sk[0], "pw", dk[1]], [dk[1]])
                    CF = Cx[S5_LC % 2]
                    cfk = [f"C{S5_LC % 2}0", f"C{S5_LC % 2}1"]
                    j0 = 0 if d == 0 else S5_Q - 1
                    V(lambda e: e.tensor_copy(out=PT[0][:, j0:j0 + 1], in_=pw_re[:, 0, dj:dj + 1]), ["pw"], ["PT0"])
                    V(lambda e: e.tensor_copy(out=PT[1][:, j0:j0 + 1], in_=pw_im[:, 0, dj:dj + 1]), ["pw"], ["PT1"])
                    for m in range(S5_LQ):
                        sh = 1 << m
                        pr, pi_, pin = pw_re[:, m, dj:dj + 1], pw_im[:, m, dj:dj + 1], pw_imn[:, m, dj:dj + 1]
                        if d == 0:
                            s_sl, d_sl = slice(0, sh), slice(sh, 2 * sh)
                        else:
                            s_sl, d_sl = slice(S5_Q - sh, S5_Q), slice(S5_Q - 2 * sh, S5_Q - sh)
                        V(lambda e: e.tensor_scalar(out=ptt[:, 0:sh], in0=PT[1][:, s_sl], scalar1=pin, scalar2=None,
                                                    op0=ALU.mult), ["PT1", "pwn"], ["ptt"])
                        V(lambda e: e.scalar_tensor_tensor(out=PT[0][:, d_sl], in0=PT[0][:, s_sl], scalar=pr,
                                                           in1=ptt[:, 0:sh], op0=ALU.mult, op1=ALU.add),
                          ["PT0", "pw", "ptt"], ["PT0"])
                        V(lambda e: e.tensor_scalar(out=ptt[:, 0:sh], in0=PT[0][:, s_sl], scalar1=pi_, scalar2=None,
                                                    op0=ALU.mult), ["PT0", "pw"], ["ptt"])
                        V(lambda e: e.scalar_tensor_tensor(out=PT[1][:, d_sl], in0=PT[1][:, s_sl], scalar=pr,
                                                           in1=ptt[:, 0:sh], op0=ALU.mult, op1=ALU.add),
                          ["PT1", "pw", "ptt"], ["PT1"])
                    if d == 0:
                        xc_sl, cc_sl = slice(1, S5_NCH), slice(0, S5_NCH - 1)
                    else:
                        xc_sl, cc_sl = slice(0, S5_NCH - 1), slice(1, S5_NCH)
                    nco = S5_NCH - 1
                    ptb = lambda ri: PT[ri][:].unsqueeze(1).to_broadcast([128, nco, S5_Q])
                    cfb = lambda ri: CF[ri][:, cc_sl].unsqueeze(2).to_broadcast([128, nco, S5_Q])
                    tmpv = xv(X[(S5_LQ + 1) % 2][0])[:, 0:nco, :]
                    tk = f"X{(S5_LQ + 1) % 2}0"
                    for (ro, pa, ca, op_) in ((0, 0, 0, ALU.add), (0, 1, 1, ALU.subtract), (1, 0, 1, ALU.add),
                                              (1, 1, 0, ALU.add)):
                        V(lambda e: e.tensor_tensor(out=tmpv, in0=ptb(pa), in1=cfb(ca), op=ALU.mult),
                          [f"PT{pa}", cfk[ca]], [tk])
                        V(lambda e: e.tensor_tensor(out=xv(R[ro])[:, xc_sl, :], in0=xv(R[ro])[:, xc_sl, :], in1=tmpv,
                                                    op=op_), [rk[ro], tk], [rk[ro]])
                    for ri in range(2):
                        p.op('act', lambda e: e.copy(out=xb[ri][:], in_=R[ri][:]), r=[rk[ri]], w=[f"xb{ri}"])
                    for (s0, n, xp) in blocks:
                        bk = 4 + (nblk % 2)
                        for ri in range(2):
                            p.op('pe', lambda e: e.matmul(K.ps[bk][:, 0:n], lhsT=bc[bi][:, 2 + ri, :],
                                                          rhs=xb[ri][:, xp:xp + n], start=(ri == 0),
                                                          stop=(ri == 1)), r=[f"bc{bi}", f"xb{ri}"], w=[f"ps{bk}"])
                        V(lambda e: e.tensor_tensor(out=Y[:, c, s0:s0 + n], in0=Y[:, c, s0:s0 + n],
                                                    in1=K.ps[bk][:, 0:n], op=ALU.add), [f"ps{bk}", f"Y{c}"],
                          [f"Y{c}"])
                        nblk += 1
        with _scope(K) as es2:
            SB2 = lambda name, shape, dt=F32: es2.enter_context(_sb(nc, name, shape, dt))
            yb = SB2("s5yb", [128, 2, T], BF16)
            O = SB2("s5O", [128, 2, T])
            sg = [SB2(f"s5sg{i}", [128, 512]) for i in range(2)]
            for c in range(2):
                p.op('act', lambda e: e.activation(out=Y[:, c, :], in_=Y[:, c, :], func=AF.Gelu), r=[f"Y{c}"],
                     w=[f"Y{c}"])
                V(lambda e: e.tensor_copy(out=yb[:, c, :], in_=Y[:, c, :]), [f"Y{c}"], ["yb"])
            nb = 0
            for co in range(2):
                for (s0, n) in S5_BLOCKS0:
                    bk = nb % 2
                    for ci in range(2):
                        p.op('pe', lambda e: e.matmul(K.ps[bk][:, 0:n], lhsT=gluw[:, ci, co * 128:(co + 1) * 128],
                                                      rhs=yb[:, ci, s0:s0 + n], start=(ci == 0), stop=(ci == 1)),
                             r=["gluw", "yb"], w=[f"ps{bk}"])
                    p.op('act', lambda e: e.activation(out=sg[bk][:, 0:n], in_=K.ps[bk][:, 0:n], func=AF.Sigmoid,
                                                       bias=glb[:, co:co + 1]), r=[f"ps{bk}", "glb"], w=[f"sg{bk}"])
                    if s0 == 0:
                        V(lambda e: e.tensor_tensor(out=O[:, co, 0:LC], in0=Y[:, co, 0:LC], in1=sg[bk][:, 0:n],
                                                    op=ALU.mult), [f"Y{co}", f"sg{bk}"], [f"O{co}"])
                    else:
                        b8 = (s0 - LC) // 64
                        o_ap = O[:, co, LC:T].rearrange("q (r c) -> q c r", c=64)[:, b8:b8 + 8, :]
                        V(lambda e: e.tensor_tensor(out=o_ap,
                                                    in0=Y[:, co, s0:s0 + n].rearrange("q (c r) -> q c r", r=64),
                                                    in1=sg[bk][:, 0:n].rearrange("q (c r) -> q c r", r=64),
                                                    op=ALU.mult), [f"Y{co}", f"sg{bk}"], [f"O{co}"])
                    nb += 1
                p.dma('sp', K.mix[768 + co * 128:768 + (co + 1) * 128, :], O[:, co, :], r=[f"O{co}"], w=["mix"])
        p.barrier()


def _hy_seq(K, li, seq, t0, L, mixcol0):
    p, nc = K.p, K.nc
    V = lambda fn, r, w: p.op('dve', fn, r=r, w=w)
    A = lambda fn, r, w: p.op('act', fn, r=r, w=w)
    NTT = L // 128
    NFC = NTT
    N2 = 2 * L
    dft = K.inp[f"dft_{seq}"]
    featT = K.inp[f"hy_featT_{seq}"]
    dec = K.inp[f"hy_dec_{seq}"]
    with contextlib.ExitStack() as es:
        SB = lambda name, shape, dt=F32: es.enter_context(_sb(nc, name, shape, dt))
        w1, w2, w3 = SB("hyw1", [33, 64]), SB("hyw2", [64, 64]), SB("hyw3", [64, 1024])
        cf = SB("hycf", [64, 6])
        skipb = SB("hyskip", [128, 2, 256])
        alt = SB("hyalt", [128, 2], BF16)
        altrow = SB("hyaltr", [1, 128], BF16)
        wcol = SB("hywcol", [128, NFC])
        hid2 = SB("hyhid2", [64, L])
        u = SB("hyu", [128, NTT, 256], BF16)
        Hn = SB("hyHn", [1, 256])
        Pn = SB("hyPn", [1, 256], BF16)
        Un = SB("hyUn", [1, 256])
        p.dma('sp', w1[:], K.inp["hy_w1"][li], w=["hyw1"])
        p.dma('sp', w2[:], K.inp["hy_w2"][li], w=["hyw2"])
        p.dma('sp', w3[:], K.inp["hy_w3"][li], w=["hyw3"])
        for i, n_ in enumerate(("hy_b1", "hy_f1", "hy_b2", "hy_f2")):
            p.dma('sp', cf[:, i:i + 1], K.inp[n_][li].rearrange("(q o) -> q o", o=1), w=["hycf"])
        for o in range(2):
            p.dma('sp', skipb[:, o, :], K.inp["hy_skip"][li, o].partition_broadcast(128), w=["hyskip"])
        p.dma('sp', alt[:], K.inp["hy_alt"][:, :], w=["hyalt"])
        p.dma('sp', altrow[:], K.inp["hy_altrow"][:, :], w=["hyaltr"])
        V(lambda e: e.tensor_tensor(out=cf[:, 4:5], in0=cf[:, 0:1], in1=cf[:, 1:2], op=ALU.mult), ["hycf"], ["hycf"])
        V(lambda e: e.tensor_tensor(out=cf[:, 5:6], in0=cf[:, 2:3], in1=cf[:, 3:4], op=ALU.mult), ["hycf"], ["hycf"])
        V(lambda e: e.memset(wcol[:], 2.0 / N2), [], ["hywcol"])
        V(lambda e: e.memset(wcol[0:1, 0:1], 1.0 / N2), ["hywcol"], ["hywcol"])
        with _scope(K) as es2:
            ft = es2.enter_context(_sb(nc, "hyfeat", [33, L], F32))
            hid1 = es2.enter_context(_sb(nc, "hyhid1", [64, L], F32))
            tmp = es2.enter_context(_sb(nc, "hytmp", [64, 512], F32))
            p.dma('sp', ft[:], featT[:, :], w=["hyfeat"])
            nb = 0
            for (src, sk, wm, wk, kdim, dst, dk, fi, bi) in ((ft, "hyfeat", w1, "hyw1", 33, hid1, "hid1", 1, 4),
                                                             (hid1, "hid1", w2, "hyw2", 64, hid2, "hid2", 3, 5)):
                for b0 in range(0, L, 512):
                    n = min(512, L - b0)
                    bk = nb % 2
                    p.op('pe', lambda e: e.matmul(K.ps[bk][0:64, 0:n], lhsT=wm[0:kdim, :], rhs=src[0:kdim, b0:b0 + n],
                                                  start=True, stop=True), r=[wk, sk], w=[f"ps{bk}"])
                    V(lambda e: e.tensor_scalar(out=dst[:, b0:b0 + n], in0=K.ps[bk][0:64, 0:n],
                                                scalar1=cf[:, fi:fi + 1], scalar2=cf[:, bi:bi + 1], op0=ALU.mult,
                                                op1=ALU.add), [f"ps{bk}", "hycf"], [dk])
                    _sin(K, dst[:, b0:b0 + n], dst[:, b0:b0 + n], 0.0, tmp[:, 0:n], [dk], dk, "hytmp")
                    nb += 1
        with _scope(K) as es2:
            vst = [es2.enter_context(_sb(nc, f"hyvst{i}", [128, 256], F32)) for i in range(2)]
            for tt in range(NTT):
                i = tt % 2
                p.dma('sp', vst[i][:], K.hytm[2, t0 + tt * 128:t0 + (tt + 1) * 128, :], r=["hytm2"], w=[f"vst{i}"])
                _copy_any(p, tt, u[:, tt, :], vst[i][:], r=[f"vst{i}"], w=["hyu"])
        for order in range(2):
            with _scope(K) as es2:
                SB2 = lambda name, shape, dt=F32: es2.enter_context(_sb(nc, name, shape, dt))
                HP, HM = SB2("hyHP", [128, NTT, 256], BF16), SB2("hyHM", [128, NTT, 256], BF16)
                dct = [SB2(f"hydec{i}", [128, 256]) for i in range(2)]
                hf = [SB2(f"hyhf{i}", [128, 2, 256]) for i in range(2)]
                for tt in range(NTT):
                    i = tt % 2
                    bk = i
                    p.dma('sp', dct[i][:], dec[tt * 128:(tt + 1) * 128, :], w=[f"hydec{i}"])
                    p.op('pe', lambda e: e.matmul(K.ps[bk][:, :], lhsT=hid2[:, tt * 128:(tt + 1) * 128],
                                                  rhs=w3[:, order * 512:(order + 1) * 512], start=True, stop=True),
                         r=["hid2", "hyw3"], w=[f"ps{bk}"])
                    V(lambda e: e.tensor_tensor(out=hf[i][:], in0=K.ps[bk][:, :].rearrange("q (s c) -> q s c", s=2),
                                                in1=dct[i][:].unsqueeze(1).to_broadcast([128, 2, 256]), op=ALU.mult),
                      [f"ps{bk}", f"hydec{i}"], [f"hyhf{i}"])
                    if tt == 0:
                        V(lambda e: e.memset(hf[i][0:1, 1, :], 0.0), [f"hyhf{i}"], [f"hyhf{i}"])
                    V(lambda e: e.tensor_tensor(out=HP[:, tt, :], in0=hf[i][:, 0, :], in1=hf[i][:, 1, :], op=ALU.add),
                      [f"hyhf{i}"], ["HP"])
                    V(lambda e: e.tensor_tensor(out=HM[:, tt, :], in0=hf[i][:, 0, :], in1=hf[i][:, 1, :],
                                                op=ALU.subtract), [f"hyhf{i}"], ["HM"])
                Pc, Ps = SB2("hyPc", [128, NFC, 256], BF16), SB2("hyPs", [128, NFC, 256], BF16)
                slab = [[SB2(f"hysm{i}{cs}", [128, NTT * 128], BF16) for cs in range(2)] for i in range(2)]
                hcs = [[SB2(f"hyhcs{i}{cs}", [128, 256]) for cs in range(2)] for i in range(2)]
                tq = [SB2(f"hytq{i}", [128, 256]) for i in range(4)]
                xa = [SB2(f"hyxa{i}", [128, 256]) for i in range(2)]
                xv = [SB2(f"hyxv{i}", [128, 256]) for i in range(2)]
                ofm = [SB2(f"hyofm{i}", [128, 2, 128]) for i in range(2)]
                for tt in range(NTT):
                    p.op('pe', lambda e: e.matmul(K.ps[6][0:1, 0:256], lhsT=alt[:, 0:1], rhs=HP[:, tt, :],
                                                  start=(tt == 0), stop=(tt == NTT - 1)), r=["hyalt", "HP"], w=["ps6"])
                V(lambda e: e.tensor_scalar(out=Hn[:], in0=K.ps[6][0:1, 0:256], scalar1=1.0 / N2, scalar2=None,
                                            op0=ALU.mult), ["ps6"], ["Hn"])
                for fc in range(NFC):
                    i = fc % 2
                    for cs in range(2):
                        p.dma('sp', slab[i][cs][:], dft[cs, fc], w=[f"hysm{i}{cs}"])
                    for cs, (src, sk) in enumerate(((HP, "HP"), (HM, "HM"))):
                        for (rhs_t, rk_, bk) in ((src, sk, 4 * i + 2 + cs), (u, "hyu", 4 * i + cs)):
                            for tt in range(NTT):
                                p.op('pe', lambda e: e.matmul(K.ps[bk][:, 0:256],
                                                              lhsT=slab[i][cs][:, tt * 128:(tt + 1) * 128],
                                                              rhs=rhs_t[:, tt, :], start=(tt == 0), stop=(tt == NTT - 1)),
                                     r=[f"hysm{i}{cs}", rk_], w=[f"ps{bk}"])
                        A(lambda e: e.activation(out=hcs[i][cs][:], in_=K.ps[4 * i + 2 + cs][:, 0:256], func=AF.Copy,
                                                 scale=wcol[:, fc:fc + 1]), [f"ps{4 * i + 2 + cs}", "hywcol"],
                          [f"hcs{i}{cs}"])
                    C_, S_ = K.ps[4 * i][:, 0:256], K.ps[4 * i + 1][:, 0:256]
                    ck, sk = f"ps{4 * i}", f"ps{4 * i + 1}"
                    Hc_, Hs_ = hcs[i][0][:], hcs[i][1][:]
                    hck, hsk = f"hcs{i}0", f"hcs{i}1"
                    V(lambda e: e.tensor_tensor(out=tq[0][:], in0=C_, in1=Hc_, op=ALU.mult), [ck, hck], ["tq0"])
                    V(lambda e: e.tensor_tensor(out=tq[1][:], in0=S_, in1=Hs_, op=ALU.mult), [sk, hsk], ["tq1"])
                    V(lambda e: e.tensor_tensor(out=Pc[:, fc, :], in0=tq[0][:], in1=tq[1][:], op=ALU.subtract),
                      ["tq0", "tq1"], ["Pc"])
                    V(lambda e: e.tensor_tensor(out=tq[2][:], in0=C_, in1=Hs_, op=ALU.mult), [ck, hsk], ["tq2"])
                    V(lambda e: e.tensor_tensor(out=tq[3][:], in0=S_, in1=Hc_, op=ALU.mult), [sk, hck], ["tq3"])
                    V(lambda e: e.tensor_tensor(out=Ps[:, fc, :], in0=tq[2][:], in1=tq[3][:], op=ALU.add),
                      ["tq2", "tq3"], ["Ps"])
                for tt in range(NTT):
                    p.op('pe', lambda e: e.matmul(K.ps[6][0:1, 0:256], lhsT=alt[:, 0:1], rhs=u[:, tt, :],
                                                  start=(tt == 0), stop=(tt == NTT - 1)), r=["hyalt", "hyu"], w=["ps6"])
                V(lambda e: e.tensor_copy(out=Un[:], in_=K.ps[6][0:1, 0:256]), ["ps6"], ["Un"])
                V(lambda e: e.tensor_tensor(out=Pn[:], in0=Un[:], in1=Hn[:], op=ALU.mult), ["Un", "Hn"], ["Pn"])
                xslot = 0 if order == 0 else 1
                yslot = 2 if order == 0 else 0
                for tc in range(NTT):
                    i = tc % 2
                    for cs in range(2):
                        p.dma('sp', slab[i][cs][:], dft[cs, tc], w=[f"hysm{i}{cs}"])
                    rows = slice(t0 + tc * 128, t0 + (tc + 1) * 128)
                    p.dma('sp', xa[i][:], K.hytm[xslot, rows, :], r=[f"hytm{xslot}"], w=[f"hyxa{i}"])
                    p.dma('sp', xv[i][:], K.hytm[yslot, rows, :], r=[f"hytm{yslot}"], w=[f"hyxv{i}"])
                    bk = 4 + i
                    for fc in range(NFC):
                        p.op('pe', lambda e: e.matmul(K.ps[bk][:, 0:256], lhsT=slab[i][0][:, fc * 128:(fc + 1) * 128],
                                                      rhs=Pc[:, fc, :], start=(fc == 0), stop=False),
                             r=[f"hysm{i}0", "Pc"], w=[f"ps{bk}"])
                        p.op('pe', lambda e: e.matmul(K.ps[bk][:, 0:256], lhsT=slab[i][1][:, fc * 128:(fc + 1) * 128],
                                                      rhs=Ps[:, fc, :], start=False, stop=False),
                             r=[f"hysm{i}1", "Ps"], w=[f"ps{bk}"])
                    p.op('pe', lambda e: e.matmul(K.ps[bk][:, 0:256], lhsT=altrow[0:1, :], rhs=Pn[0:1, :], start=False,
                                                  stop=True), r=["hyaltr", "Pn"], w=[f"ps{bk}"])
                    V(lambda e: e.tensor_tensor(out=xv[i][:], in0=xv[i][:], in1=skipb[:, order, :], op=ALU.mult),
                      [f"hyxv{i}", "hyskip"], [f"hyxv{i}"])
                    V(lambda e: e.tensor_tensor(out=xv[i][:], in0=xv[i][:], in1=K.ps[bk][:, 0:256], op=ALU.add),
                      [f"hyxv{i}", f"ps{bk}"], [f"hyxv{i}"])
                    V(lambda e: e.tensor_tensor(out=xa[i][:], in0=xa[i][:], in1=xv[i][:], op=ALU.mult),
                      [f"hyxa{i}", f"hyxv{i}"], [f"hyxa{i}"])
                    if order == 0:
                        A(lambda e: e.copy(out=u[:, tc, :], in_=xa[i][:]), [f"hyxa{i}"], ["hyu2"])
                        p.dma('pool', K.hytm[0, rows, :], xa[i][:], r=[f"hyxa{i}"], w=["hytm0"])
                    else:
                        for c2 in range(2):
                            p.op('pe', lambda e: e.transpose(out=K.ps[7][:, c2 * 128:(c2 + 1) * 128],
                                                             in_=xa[i][:, c2 * 128:(c2 + 1) * 128], identity=K.ident[:]),
                                 r=[f"hyxa{i}", "ident"], w=["ps7"])
                        A(lambda e: e.copy(out=ofm[i][:].rearrange("q a b -> q (a b)"), in_=K.ps[7][:, 0:256]), ["ps7"],
                          [f"hyofm{i}"])
                        for c2 in range(2):
                            p.dma('pool', K.mix[512 + c2 * 128:512 + (c2 + 1) * 128,
                                                mixcol0 + tc * 128:mixcol0 + (tc + 1) * 128], ofm[i][:, c2, :],
                                  r=[f"hyofm{i}"], w=["mix"])
        p.barrier()


def phase_hyena(K, li):
    p, nc = K.p, K.nc
    V = lambda fn, r, w: p.op('dve', fn, r=r, w=w)
    need_ctx = li < DEPTH - 1
    with contextlib.ExitStack() as es:
        SB = lambda name, shape, dt=F32: es.enter_context(_sb(nc, name, shape, dt))
        cw = SB("hycw", [128, 6, 4])
        zin = [SB(f"hyzin{i}", [128, T]) for i in range(2)]
        zo = [SB(f"hyzo{i}", [128, T]) for i in range(2)]
        tst = [SB(f"hytst{i}", [128, 128]) for i in range(4)]
        for k in range(3):
            p.dma('sp', cw[:, :, k], K.inp["hy_conv"][li, k].rearrange("(c q) -> q c", q=128), w=["hycw"])
        p.dma('sp', cw[:, :, 3], K.inp["hy_conv_b"][li].rearrange("(c q) -> q c", q=128), w=["hycw"])
        nt = 0
        for ch in range(6):
            i = ch % 2
            r0 = ZR["hy"] + ch * 128
            p.dma('sp', zin[i][:], K.zfm[r0:r0 + 128, :], r=["zfm"], w=[f"zin{i}"])
            V(lambda e: e.tensor_scalar(out=zo[i][:], in0=zin[i][:], scalar1=cw[:, ch, 1:2], scalar2=cw[:, ch, 3:4],
                                        op0=ALU.mult, op1=ALU.add), [f"zin{i}", "hycw"], [f"zo{i}"])
            for (a, b) in ((0, LC), (LC, T)):
                V(lambda e: e.scalar_tensor_tensor(out=zo[i][:, a + 1:b], in0=zin[i][:, a:b - 1], scalar=cw[:, ch, 0:1],
                                                   in1=zo[i][:, a + 1:b], op0=ALU.mult, op1=ALU.add),
                  [f"zin{i}", "hycw", f"zo{i}"], [f"zo{i}"])
                V(lambda e: e.scalar_tensor_tensor(out=zo[i][:, a:b - 1], in0=zin[i][:, a + 1:b], scalar=cw[:, ch, 2:3],
                                                   in1=zo[i][:, a:b - 1], op0=ALU.mult, op1=ALU.add),
                  [f"zin{i}", "hycw", f"zo{i}"], [f"zo{i}"])
            slot, c2 = ch // 2, ch % 2
            for tt in range(0 if need_ctx else 2, NT):
                bk = nt % 4
                p.op('pe', lambda e: e.transpose(out=K.ps[bk][:, 0:128], in_=zo[i][:, tt * 128:(tt + 1) * 128],
                                                 identity=K.ident[:]), r=[f"zo{i}", "ident"], w=[f"ps{bk}"])
                _copy_any(p, nt, tst[bk][:], K.ps[bk][:, 0:128], r=[f"ps{bk}"], w=[f"tst{bk}"])
                p.dma('pool', K.hytm[slot, tt * 128:(tt + 1) * 128, c2 * 128:(c2 + 1) * 128], tst[bk][:],
                      r=[f"tst{bk}"], w=[f"hytm{slot}"])
                nt += 1
        p.barrier()
    if need_ctx:
        _hy_seq(K, li, "ctx", 0, LC, 0)
    _hy_seq(K, li, "lat", LC, LL, LC)


SCAN_W = 8
SCAN_WV = 64
SCAN_STEPS = [T]


def _tok1(s):
    return LC - 1 - s if s < LC else T + LC - 1 - s


def _ap(t, offset, dims, nparts):
    full = t[:]
    return bass.AP(full.tensor, offset, [[full.ap[0][0], nparts]] + [list(d) for d in dims])


def phase_scan(K, li):
    p, nc = K.p, K.nc
    W, WV = SCAN_W, SCAN_WV
    nsteps = SCAN_STEPS[0]
    NQ = (4, 6, 4, 4, 4)
    with contextlib.ExitStack() as es:
        SB = lambda name, shape, dt=F32: es.enter_context(_sb(nc, name, shape, dt))
        Sb = [SB(f"scS{i}", [128, 8, 64]) for i in range(2)]
        P1, Pb = SB("scP1", [128, 8, 64]), SB("scPb", [128, 8, 64])
        P4 = [SB(f"scP4{i}", [128, 8, 64]) for i in range(2)]
        sa = SB("scsa", [128, 8])
        t2 = [SB(f"sct2{b}", [128, 8, 64]) for b in range(2)]
        wsb = [SB(f"scwsb{b}", [128, 8, 64]) for b in range(2)]
        sel = SB("scsel", [6, 128], BF16)
        win = [[SB(f"scwin{o}_{b}", [6, 2 * W * 256], BF16) for b in range(2)] for o in range(5)]
        vw = [SB(f"scvw{b}", [128, 8, WV]) for b in range(2)]
        yw = [SB(f"scyw{b}", [128, 8, WV]) for b in range(2)]
        p.dma('sp', sel[:], K.inp["scan_sel"][:, :], w=["scsel"])
        p.op('dve', lambda e: e.memset(Sb[0][:], 0.0), w=["S0"])
        pend = []
        for b in range(2):
            p.op('dve', lambda e: e.memset(yw[b][:], 0.0), w=[f"yw{b}"])
        for s in range(nsteps):
            j, wi = s % W, s // W
            jv, wvi = s % WV, s // WV
            wb, vb = wi % 2, wvi % 2
            if j == 0:
                t_lo = (wi * W, _tok1(wi * W) - W + 1)
                rows_t = K.rows[:].tensor
                for o in range(5):
                    for d in range(2):
                        src = bass.AP(rows_t, (o * T + t_lo[d]) * 3072 + d * 256, [[512, NQ[o]], [3072, W], [1, 256]])
                        p.dma('sp', win[o][wb][0:NQ[o], d * W * 256:(d + 1) * W * 256], src, r=["rows"],
                              w=[f"win{o}_{wb}"])
            if jv == 0:
                v_lo = (wvi * WV, _tok1(wvi * WV) - WV + 1)
                for g in range(8):
                    d, mh = g // 4, g % 4
                    p.dma('sp', vw[vb][:, g, :], K.vfm[mh, :, v_lo[d]:v_lo[d] + WV], r=["vfm"], w=[f"vw{vb}"])
            ix = (j, W - 1 - j)
            ixv = (jv, WV - 1 - jv)
            banks = [(5 * s + o) % 8 for o in range(5)]
            for o in range(5):
                rhs = _ap(win[o][wb], ix[0] * 256, [[(W + ix[1] - ix[0]) * 256, 2], [1, 256]], NQ[o])
                p.op('pe', lambda e: e.matmul(K.ps[banks[o]][:, :], lhsT=sel[0:NQ[o], :], rhs=rhs, start=True,
                                              stop=True),
                     r=[f"win{o}_{wb}", "scsel"], w=[f"ps{banks[o]}"])
            psv = [K.ps[b][:, :].rearrange("q (g k) -> q g k", k=64) for b in banks]
            dv = ixv[1] - ixv[0]
            vcols = _ap(vw[vb], ixv[0], [[4 * WV + dv, 2], [WV, 4], [0, 64]], 128)
            ycols = _ap(yw[vb], ixv[0], [[4 * WV + dv, 2], [WV, 4]], 128)
            tb = s % 2
            items = []
            for g in range(8):
                d = g // 4
                vcol = vw[vb][:, g, ixv[d]:ixv[d] + 1]
                items.append((lambda e, g=g, vcol=vcol: e.activation(out=t2[tb][:, g, :], in_=psv[3][:, g, :],
                                                                     func=AF.Copy, scale=vcol),
                              [f"vw{vb}", f"ps{banks[3]}"], [f"sct2{tb}"]))
            p.op('act', lambda e: e.copy(out=wsb[tb][:], in_=psv[1]), r=[f"ps{banks[1]}"], w=[f"scwsb{tb}"])
            p.stage('act', items)
            V = lambda fn, r, w: p.op('dve', fn, r=r, w=w)
            G = lambda fn, r, w: p.op('pool', fn, r=r, w=w)
            cur, nxt = Sb[s % 2], Sb[(s + 1) % 2]
            ck, nk = f"S{s % 2}", f"S{(s + 1) % 2}"
            V(lambda e: e.tensor_tensor(out=P1[:], in0=cur[:], in1=psv[0], op=ALU.mult), [ck, f"ps{banks[0]}"], ["P1"])
            G(lambda e: e.tensor_tensor(out=nxt[:], in0=cur[:], in1=wsb[tb][:], op=ALU.mult), [ck, f"scwsb{tb}"], [nk])
            G(lambda e: e.tensor_tensor(out=nxt[:], in0=nxt[:], in1=t2[tb][:], op=ALU.add), [nk, f"sct2{tb}"], [nk])
            if pend:
                pend[0][0]()
            V(lambda e: e.tensor_reduce(out=sa[:], in_=P1[:], axis=AX.X, op=ALU.add), ["P1"], ["sa"])
            if pend:
                pend.pop(0)[1]()
            V(lambda e: e.tensor_tensor(out=Pb[:], in0=psv[2], in1=sa[:].unsqueeze(2).to_broadcast([128, 8, 64]),
                                        op=ALU.mult), ["sa", f"ps{banks[2]}"], ["Pb"])
            V(lambda e: e.tensor_tensor(out=nxt[:], in0=nxt[:], in1=Pb[:], op=ALU.add), [nk, "Pb"], [nk])

            def out_mul(nxt=nxt, nk=nk, r_ap=psv[4], rb=banks[4]):
                V(lambda e: e.tensor_tensor(out=P4[0][:], in0=nxt[:], in1=r_ap, op=ALU.mult), [nk, f"ps{rb}"], ["P4"])

            def out_red(ycols=ycols, vb=vb, s=s, jv=jv, wvi=wvi):
                V(lambda e: e.tensor_reduce(out=ycols, in_=P4[0][:], axis=AX.X, op=ALU.add), ["P4"], [f"yw{vb}"])
                if jv == WV - 1 or s == nsteps - 1:
                    v_lo = (wvi * WV, _tok1(wvi * WV) - WV + 1)
                    for g in range(8):
                        d = g // 4
                        p.dma('sp', K.yfm[g, :, v_lo[d]:v_lo[d] + WV], yw[vb][:, g, :], r=[f"yw{vb}"], w=["yfm"])
            if li < DEPTH - 1 or s >= LC or nsteps < T:
                pend.append((out_mul, out_red))
        pend[0][0]()
        pend[0][1]()
        p.barrier()


TOK_BLOCKS = [(0, 256)] + [(256 + 512 * b, 512) for b in range(8)]


def _split_store(K, tt, stg, stgk, SP, r1):
    p = K.p
    V = lambda fn, r, w: p.op('dve', fn, r=r, w=w)
    A = lambda fn, r, w: p.op('act', fn, r=r, w=w)
    stgk = list(stgk)
    A(lambda e: e.copy(out=SP[:, 0], in_=stg[:]), stgk, ["sp_hi"])
    V(lambda e: e.tensor_tensor(out=r1[:], in0=stg[:], in1=SP[:, 0], op=ALU.subtract), stgk + ["sp_hi"], ["sp_r1"])
    A(lambda e: e.copy(out=SP[:, 1], in_=r1[:]), ["sp_r1"], ["sp_mid"])
    V(lambda e: e.tensor_tensor(out=r1[:, 1], in0=r1[:, 1], in1=SP[:, 1, 1], op=ALU.subtract), ["sp_r1", "sp_mid"],
      ["sp_r1"])
    A(lambda e: e.copy(out=SP[:, 2, 1], in_=r1[:, 1]), ["sp_r1"], ["sp_lo"])
    for o in range(5):
        nsp = 3 if o == 1 else 2
        p.dma('sp' if o % 2 == 0 else 'pool',
              K.rows[o, tt * 128:(tt + 1) * 128, 0:nsp].rearrange("q s h d c -> q s (h d c)"),
              SP[:, 0:nsp, o].rearrange("q s h d c -> q s (h d c)"), r=["sp_hi", "sp_mid", "sp_lo"], w=["rows"])


def phase_rwprep(K, li):
    p, nc = K.p, K.nc
    V = lambda fn, r, w: p.op('dve', fn, r=r, w=w)
    A = lambda fn, r, w: p.op('act', fn, r=r, w=w)
    EM05 = math.exp(-0.5)
    with contextlib.ExitStack() as es:
        SB = lambda name, shape, dt=F32: es.enter_context(_sb(nc, name, shape, dt))
        w2p, a2p = SB("rww2p", [128, 4, 128]), SB("rwa2p", [128, 4, 128])
        g2p = SB("rwg2p", [128, 2, 128])
        blk = SB("rwblk", [128, 128])
        pT = SB("rwpT", [128, 18])
        omka = SB("rwomka", [128, 2])
        zin = {n: SB(f"rwz_{n}", [128, 2, 512]) for n in ("r", "k", "v")}
        zwa, zg_ = SB("rwz_wa", [128, 512]), SB("rwz_g", [128, 512])
        actc, sgl = SB("rwactc", [128, 512]), SB("rwsgl", [128, 512])
        dec, aa = SB("rwdec", [128, 4, 512]), SB("rwaa", [128, 4, 512])
        gout, bv = SB("rwgout", [128, 2, 512]), SB("rwbv", [128, 2, 512])
        kk, kkn, an = SB("rwkk", [128, 2, 512]), SB("rwkkn", [128, 2, 512]), SB("rwan", [128, 2, 512])
        kd, bb = SB("rwkd", [128, 4, 512]), SB("rwbb", [128, 4, 512])
        t1, t2, prs = SB("rwt1", [128, 512]), SB("rwt2", [128, 512]), SB("rwprs", [128, 512])
        stgs = [SB(f"rwstg{i}", [128, 5, 2, 2, 256]) for i in range(2)]
        SP = SB("rwSP", [128, 3, 5, 2, 2, 256], BF16)
        r1 = SB("rwr1", [128, 5, 2, 2, 256])
        p.dma('sp', w2p[:], K.inp["rw_w2p"][li].rearrange("i q c -> q i c"), w=["w2p"])
        p.dma('sp', a2p[:], K.inp["rw_a2p"][li].rearrange("i q c -> q i c"), w=["a2p"])
        p.dma('sp', g2p[:], K.inp["rw_g2p"][li].rearrange("i q c -> q i c"), w=["g2p"])
        p.dma('sp', blk[:], K.inp["blk64"][:, :], w=["blk"])
        p.dma('sp', pT[:], K.inp["rw_pT"][li], w=["pT"])
        V(lambda e: e.tensor_scalar(out=omka[:], in0=pT[:, 10:12], scalar1=-1.0, scalar2=1.0, op0=ALU.mult,
                                    op1=ALU.add), ["pT"], ["omka"])
        for hp in range(2):
            r0 = ZR["rw_v"] + hp * 128
            p.dma('pool', K.vfm[hp, :, :], K.zfm[r0:r0 + 128, :], r=["zfm"], w=["vfm"])
        nb = 0
        ncp = 0
        for (b0, n) in TOK_BLOCKS:
            for nm in ("r", "k", "v"):
                r0 = ZR["rw_" + nm]
                p.dma('sp', zin[nm][:, :, 0:n], K.zfm[r0:r0 + 256, b0:b0 + n].rearrange("(c q) t -> q c t", q=128),
                      r=["zfm"], w=["z" + nm])
            p.dma('sp', zwa[:, 0:n], K.zfm[ZR["rw_wa"]:ZR["rw_wa"] + 128, b0:b0 + n], r=["zfm"], w=["zwa"])
            p.dma('sp', zg_[:, 0:n], K.zfm[ZR["rw_g"]:ZR["rw_g"] + 128, b0:b0 + n], r=["zfm"], w=["zg"])
            A(lambda e: e.activation(out=actc[0:64, 0:n], in_=zwa[0:64, 0:n], func=AF.Tanh), ["zwa"], ["actc"])
            A(lambda e: e.copy(out=actc[64:128, 0:n], in_=zwa[64:128, 0:n]), ["zwa"], ["actc"])
            A(lambda e: e.activation(out=sgl[:, 0:n], in_=zg_[:, 0:n], func=AF.Sigmoid), ["zg"], ["sgl"])
            for i in range(4):
                bk = nb % 4
                nb += 1
                p.op('pe', lambda e: e.matmul(K.ps[bk][:, 0:n], lhsT=w2p[:, i, :], rhs=actc[:, 0:n], start=True,
                                              stop=True), r=["w2p", "actc"], w=[f"ps{bk}"])
                A(lambda e: e.activation(out=dec[:, i, 0:n], in_=K.ps[bk][:, 0:n], func=AF.Sigmoid,
                                         bias=pT[:, i:i + 1]), [f"ps{bk}", "pT"], ["dec"])
                A(lambda e: e.activation(out=dec[:, i, 0:n], in_=dec[:, i, 0:n], func=AF.Exp, scale=-EM05),
                  ["dec"], ["dec"])
                bk = nb % 4
                nb += 1
                p.op('pe', lambda e: e.matmul(K.ps[bk][:, 0:n], lhsT=a2p[:, i, :], rhs=actc[:, 0:n], start=True,
                                              stop=True), r=["a2p", "actc"], w=[f"ps{bk}"])
                A(lambda e: e.activation(out=aa[:, i, 0:n], in_=K.ps[bk][:, 0:n], func=AF.Sigmoid,
                                         bias=pT[:, 4 + i:5 + i]), [f"ps{bk}", "pT"], ["aa"])
            for cc in range(2):
                bk = nb % 4
                nb += 1
                p.op('pe', lambda e: e.matmul(K.ps[bk][:, 0:n], lhsT=g2p[:, cc, :], rhs=sgl[:, 0:n], start=True,
                                              stop=True), r=["g2p", "sgl"], w=[f"ps{bk}"])
                A(lambda e: e.copy(out=gout[:, cc, 0:n], in_=K.ps[bk][:, 0:n]), [f"ps{bk}"], ["gout"])
                V(lambda e: e.tensor_scalar(out=kk[:, cc, 0:n], in0=zin["k"][:, cc, 0:n], scalar1=pT[:, 8 + cc:9 + cc],
                                            scalar2=None, op0=ALU.mult), ["zk", "pT"], ["kk"])
                V(lambda e: e.tensor_tensor(out=t1[:, 0:n], in0=kk[:, cc, 0:n], in1=kk[:, cc, 0:n], op=ALU.mult),
                  ["kk"], ["t1"])
                bk = nb % 4
                nb += 1
                p.op('pe', lambda e: e.matmul(K.ps[bk][:, 0:n], lhsT=blk[:, :], rhs=t1[:, 0:n], start=True, stop=True),
                     r=["blk", "t1"], w=[f"ps{bk}"])
                V(lambda e: e.tensor_scalar(out=t2[:, 0:n], in0=K.ps[bk][:, 0:n], scalar1=1e-12, scalar2=None,
                                            op0=ALU.add), [f"ps{bk}"], ["t2"])
                A(lambda e: e.activation(out=t2[:, 0:n], in_=t2[:, 0:n], func=AF.Sqrt), ["t2"], ["t2"])
                V(lambda e: e.reciprocal(out=t2[:, 0:n], in_=t2[:, 0:n]), ["t2"], ["t2"])
                V(lambda e: e.tensor_tensor(out=kkn[:, cc, 0:n], in0=kk[:, cc, 0:n], in1=t2[:, 0:n], op=ALU.mult),
                  ["kk", "t2"], ["kkn"])
                V(lambda e: e.tensor_scalar(out=an[:, cc, 0:n], in0=kkn[:, cc, 0:n], scalar1=-1.0, scalar2=None,
                                            op0=ALU.mult), ["kkn"], ["an"])
                for d in range(2):
                    i = d * 2 + cc
                    V(lambda e: e.tensor_scalar(out=t1[:, 0:n], in0=aa[:, i, 0:n], scalar1=pT[:, 10 + cc:11 + cc],
                                                scalar2=omka[:, cc:cc + 1], op0=ALU.mult, op1=ALU.add),
                      ["aa", "pT", "omka"], ["t1"])
                    V(lambda e: e.tensor_tensor(out=kd[:, i, 0:n], in0=t1[:, 0:n], in1=zin["k"][:, cc, 0:n],
                                                op=ALU.mult), ["t1", "zk"], ["kd"])
                    V(lambda e: e.tensor_tensor(out=bb[:, i, 0:n], in0=kkn[:, cc, 0:n], in1=aa[:, i, 0:n],
                                                op=ALU.mult), ["kkn", "aa"], ["bb"])
                V(lambda e: e.tensor_tensor(out=t1[:, 0:n], in0=kd[:, cc, 0:n], in1=kd[:, 2 + cc, 0:n], op=ALU.add),
                  ["kd"], ["t1"])
                V(lambda e: e.scalar_tensor_tensor(out=prs[:, 0:n], in0=zin["r"][:, cc, 0:n],
                                                   scalar=pT[:, 12 + cc:13 + cc], in1=t1[:, 0:n], op0=ALU.mult,
                                                   op1=ALU.mult), ["zr", "pT", "t1"], ["prs"])
                bk = nb % 4
                nb += 1
                p.op('pe', lambda e: e.matmul(K.ps[bk][:, 0:n], lhsT=blk[:, :], rhs=prs[:, 0:n], start=True,
                                              stop=True), r=["blk", "prs"], w=[f"ps{bk}"])
                V(lambda e: e.tensor_tensor(out=bv[:, cc, 0:n], in0=K.ps[bk][:, 0:n], in1=zin["v"][:, cc, 0:n],
                                            op=ALU.mult), [f"ps{bk}", "zv"], ["bv"])
            p.dma('pool', K.rwg.rearrange("(c q) t -> q c t", q=128)[:, :, b0:b0 + n], gout[:, :, 0:n], r=["gout"],
                  w=["rwg"])
            p.dma('pool', K.rwbv.rearrange("(c q) t -> q c t", q=128)[:, :, b0:b0 + n], bv[:, :, 0:n], r=["bv"],
                  w=["rwbv"])
            for tl in range(n // 128):
                tt = (b0 // 128) + tl
                cols = slice(tl * 128, (tl + 1) * 128)
                stg = stgs[tt % 2]
                sb_ = tt % 2
                skeys = [f"st{sb_}:gd"]
                p.dma('sp', stg[:].rearrange("q o h d c -> q (o h d) c")[:, :, 128:256],
                      K.gdstg[tt * 128:(tt + 1) * 128, :].rearrange("q (x c) -> q x c", c=128), r=["gdstgd"],
                      w=[f"st{sb_}:gd"])
                for cc in range(2):
                    srcs = [(0, None, an[:, cc, cols], "an"), (4, None, zin["r"][:, cc, cols], "zr")]
                    for d in range(2):
                        i = d * 2 + cc
                        srcs += [(1, d, dec[:, i, cols], "dec"), (2, d, bb[:, i, cols], "bb"), (3, d, kd[:, i, cols], "kd")]
                    for (o, d, src, sk) in srcs:
                        bk = 4 + (ncp % 4)
                        p.op('pe', lambda e: e.transpose(out=K.ps[bk][:, 0:128], in_=src, identity=K.ident[:]),
                             r=[sk, "ident"], w=[f"ps{bk}"])
                        pin = K.ps[bk][:, 0:128].rearrange("q (h k) -> q h k", k=64)
                        if d is None:
                            out_ap = stg[:, o, :, :, cc * 64:(cc + 1) * 64]
                            in_ap = pin.unsqueeze(2).to_broadcast([128, 2, 2, 64])
                        else:
                            out_ap = stg[:, o, :, d, cc * 64:(cc + 1) * 64]
                            in_ap = pin
                        wk = f"st{sb_}:{o}:{cc}:{d}"
                        skeys.append(wk)
                        _copy_any(p, ncp, out_ap, in_ap, r=[f"ps{bk}"], w=[wk])
                        ncp += 1
                _split_store(K, tt, stg, skeys, SP, r1)
        p.barrier()


def phase_gdprep(K, li):
    p, nc = K.p, K.nc
    V = lambda fn, r, w: p.op('dve', fn, r=r, w=w)
    A = lambda fn, r, w: p.op('act', fn, r=r, w=w)
    with contextlib.ExitStack() as es:
        SB = lambda name, shape, dt=F32: es.enter_context(_sb(nc, name, shape, dt))
        cw = SB("gdcw", [128, 6, 3])
        gpar = SB("gdpar", [128, 2, 8])
        zin = [SB(f"gdzin{i}", [128, T]) for i in range(2)]
        qk = SB("gdqk", [128, 4, T])
        zo = SB("gdzo", [128, T])
        ab = SB("gdab", [16, T])
        for k in range(3):
            p.dma('sp', cw[:, :, k], K.inp["gd_conv"][li, k].rearrange("(c q) -> q c", q=128), w=["cw"])
        p.dma('sp', gpar[:, 0, :], K.inp["gd_dtb"][li].rearrange("d h -> (d h)").partition_broadcast(128), w=["gpar"])
        p.dma('sp', gpar[:, 1, :], K.inp["gd_alog"][li].rearrange("d h -> (d h)").partition_broadcast(128), w=["gpar"])
        A(lambda e: e.activation(out=gpar[:, 1, :], in_=gpar[:, 1, :], func=AF.Exp), ["gpar"], ["gpar"])
        V(lambda e: e.tensor_scalar(out=gpar[:, 1, :], in0=gpar[:, 1, :], scalar1=-1.0, scalar2=None, op0=ALU.mult),
          ["gpar"], ["gpar"])
        p.dma('sp', ab[:], K.zfm[ZR["gd_ab"]:ZR["gd_ab"] + 16, :], r=["zfm"], w=["gdab"])
        for ch in range(6):
            i = ch % 2
            r0 = ZR["gd_q"] + ch * 128
            p.dma('sp', zin[i][:], K.zfm[r0:r0 + 128, :], r=["zfm"], w=[f"gdzin{i}"])
            dst = qk[:, ch, :] if ch < 4 else zo[:]
            dk = f"gdqk{ch}" if ch < 4 else "gdzo"
            V(lambda e: e.tensor_scalar(out=dst, in0=zin[i][:], scalar1=cw[:, ch, 1:2], scalar2=None, op0=ALU.mult),
              [f"gdzin{i}", "cw"], [dk])
            for (a, b) in ((0, LC), (LC, T)):
                V(lambda e: e.scalar_tensor_tensor(out=dst[:, a + 1:b], in0=zin[i][:, a:b - 1], scalar=cw[:, ch, 0:1],
                                                   in1=dst[:, a + 1:b], op0=ALU.mult, op1=ALU.add),
                  [f"gdzin{i}", "cw", dk], [dk])
                V(lambda e: e.scalar_tensor_tensor(out=dst[:, a:b - 1], in0=zin[i][:, a + 1:b], scalar=cw[:, ch, 2:3],
                                                   in1=dst[:, a:b - 1], op0=ALU.mult, op1=ALU.add),
                  [f"gdzin{i}", "cw", dk], [dk])
            A(lambda e: e.activation(out=dst, in_=dst, func=AF.Silu), [dk], [dk])
            if ch >= 4:
                p.dma('pool', K.vfm[2 + (ch - 4), :, :], zo[:], r=["gdzo"], w=["vfm"])
        with _scope(K) as es2:
            SB2 = lambda name, shape, dt=F32: es2.enter_context(_sb(nc, name, shape, dt))
            qt, kt = SB2("gdqt", [128, 4, 64]), SB2("gdkt", [128, 4, 64])
            gt = SB2("gdgt", [128, 16])
            sq = SB2("gdsq", [128, 4, 64])
            nrm = SB2("gdnrm", [128, 8])
            agd, nagd, beta = SB2("gdagd", [128, 8]), SB2("gdnagd", [128, 8]), SB2("gdbeta", [128, 8])
            stgs = [SB2(f"gdstg{i}", [128, 5, 2, 2, 128]) for i in range(2)]

            def hv(ap):
                return ap.rearrange("q (hp hl) k -> q hl hp k", hl=2)

            def sv(o, d):
                return stg[:, o, :, d, :].rearrange("q hl (hp k) -> q hl hp k", k=64)
            for tt in range(NT):
                cols = slice(tt * 128, (tt + 1) * 128)
                stg = stgs[tt % 2]
                sgk = f"gdstg{tt % 2}"
                for ch in range(4):
                    bk = ch
                    p.op('pe', lambda e: e.transpose(out=K.ps[bk][:, 0:128], in_=qk[:, ch, cols], identity=K.ident[:]),
                         r=[f"gdqk{ch}", "ident"], w=[f"ps{bk}"])
                    dst = (qt if ch < 2 else kt)[:, 2 * (ch % 2):2 * (ch % 2) + 2, :]
                    _copy_any(p, ch, dst.rearrange("q h k -> q (h k)"), K.ps[bk][:, 0:128], r=[f"ps{bk}"],
                              w=["gdqt" if ch < 2 else "gdkt"])
                p.op('pe', lambda e: e.transpose(out=K.ps[4][:, 0:16], in_=ab[:, cols], identity=K.ident[0:16, 0:16]),
                     r=["gdab", "ident"], w=["ps4"])
                V(lambda e: e.tensor_copy(out=gt[:], in_=K.ps[4][:, 0:16]), ["ps4"], ["gdgt"])
                V(lambda e: e.tensor_tensor(out=agd[:], in0=gt[:, 0:8], in1=gpar[:, 0, :], op=ALU.add), ["gdgt", "gpar"],
                  ["agd"])
                A(lambda e: e.activation(out=agd[:], in_=agd[:], func=AF.Exp), ["agd"], ["agd"])
                A(lambda e: e.activation(out=agd[:], in_=agd[:], func=AF.Ln, bias=1.0), ["agd"], ["agd"])
                V(lambda e: e.tensor_tensor(out=agd[:], in0=agd[:], in1=gpar[:, 1, :], op=ALU.mult), ["agd", "gpar"],
                  ["agd"])
                A(lambda e: e.activation(out=agd[:], in_=agd[:], func=AF.Exp), ["agd"], ["agd"])
                V(lambda e: e.tensor_scalar(out=nagd[:], in0=agd[:], scalar1=-1.0, scalar2=None, op0=ALU.mult), ["agd"],
                  ["nagd"])
                A(lambda e: e.activation(out=beta[:], in_=gt[:, 8:16], func=AF.Sigmoid), ["gdgt"], ["beta"])
                for (src, sk, col, scl) in ((qt, "gdqt", 0, 64.0), (kt, "gdkt", 4, 1.0)):
                    V(lambda e: e.tensor_tensor(out=sq[:], in0=src[:], in1=src[:], op=ALU.mult), [sk], ["gdsq"])
                    V(lambda e: e.tensor_reduce(out=nrm[:, col:col + 4], in_=sq[:], axis=AX.X, op=ALU.add), ["gdsq"],
                      ["gdnrm"])
                    V(lambda e: e.tensor_scalar(out=nrm[:, col:col + 4], in0=nrm[:, col:col + 4], scalar1=scl,
                                                scalar2=scl * 1e-12, op0=ALU.mult, op1=ALU.add), ["gdnrm"], ["gdnrm"])
                    A(lambda e: e.activation(out=nrm[:, col:col + 4], in_=nrm[:, col:col + 4], func=AF.Sqrt),
                      ["gdnrm"], ["gdnrm"])
                    V(lambda e: e.reciprocal(out=nrm[:, col:col + 4], in_=nrm[:, col:col + 4]), ["gdnrm"], ["gdnrm"])
                    V(lambda e: e.tensor_tensor(out=src[:], in0=src[:],
                                                in1=nrm[:, col:col + 4].unsqueeze(2).to_broadcast([128, 4, 64]),
                                                op=ALU.mult), [sk, "gdnrm"], [sk])
                for d in range(2):
                    V(lambda e: e.tensor_copy(out=sv(0, d), in_=hv(kt[:])), ["gdkt"], [sgk])
                    V(lambda e: e.tensor_copy(out=sv(4, d), in_=hv(qt[:])), ["gdqt"], [sgk])
                    bcol = lambda t_: hv(t_[:, d * 4:(d + 1) * 4].unsqueeze(2).to_broadcast([128, 4, 64]))
                    V(lambda e: e.tensor_copy(out=sv(1, d), in_=bcol(agd)), ["agd"], [sgk])
                    V(lambda e: e.tensor_tensor(out=sv(3, d), in0=hv(kt[:]), in1=bcol(beta), op=ALU.mult),
                      ["gdkt", "beta"], [sgk])
                    V(lambda e: e.tensor_tensor(out=sv(2, d), in0=sv(3, d), in1=bcol(nagd), op=ALU.mult),
                      [sgk, "nagd"], [sgk])
                p.dma('pool', K.gdstg[tt * 128:(tt + 1) * 128, :], stg[:].rearrange("q o h d c -> q (o h d c)"),
                      r=[sgk], w=["gdstgd"])
        p.barrier()


def phase_rwgd_post(K, li):
    p, nc = K.p, K.nc
    V = lambda fn, r, w: p.op('dve', fn, r=r, w=w)
    A = lambda fn, r, w: p.op('act', fn, r=r, w=w)
    with contextlib.ExitStack() as es:
        SB = lambda name, shape, dt=F32: es.enter_context(_sb(nc, name, shape, dt))
        blk = SB("poblk", [128, 128])
        pT = SB("popT", [128, 18])
        gnT = SB("pognT", [128, 1])
        y0, y1 = SB("poy0", [128, 512]), SB("poy1", [128, 512])
        ysq, mean, var = SB("poysq", [128, 512]), SB("pomean", [128, 512]), SB("povar", [128, 512])
        ga, bvv = SB("poga", [128, 512]), SB("pobv", [128, 512])
        p.dma('sp', blk[:], K.inp["blk64"][:, :], w=["blk"])
        p.dma('sp', pT[:], K.inp["rw_pT"][li], w=["pT"])
        p.dma('sp', gnT[:], K.inp["gd_normT"][li].rearrange("(q o) -> q o", o=1), w=["gnT"])
        V(lambda e: e.tensor_scalar(out=blk[:], in0=blk[:], scalar1=1.0 / 64.0, scalar2=None, op0=ALU.mult), ["blk"],
          ["blk"])
        blocks = TOK_BLOCKS if li < DEPTH - 1 else TOK_BLOCKS[1:]
        for m in range(2):
            for hp in range(2):
                for (b0, n) in blocks:
                    p.dma('sp', y0[:, 0:n], K.yfm[m * 2 + hp, :, b0:b0 + n], r=["yfm"], w=["y0"])
                    p.dma('sp', y1[:, 0:n], K.yfm[4 + m * 2 + hp, :, b0:b0 + n], r=["yfm"], w=["y1"])
                    rowsl = slice(hp * 128, (hp + 1) * 128)
                    if m == 0:
                        p.dma('sp', ga[:, 0:n], K.rwg[rowsl, b0:b0 + n], r=["rwg"], w=["ga"])
                        p.dma('sp', bvv[:, 0:n], K.rwbv[rowsl, b0:b0 + n], r=["rwbv"], w=["bvv"])
                    else:
                        r0 = ZR["gd_zg"] + hp * 128
                        p.dma('sp', ga[:, 0:n], K.zfm[r0:r0 + 128, b0:b0 + n], r=["zfm"], w=["ga"])
                    V(lambda e: e.tensor_tensor(out=y0[:, 0:n], in0=y0[:, 0:n], in1=y1[:, 0:n], op=ALU.add),
                      ["y0", "y1"], ["y0"])
                    A(lambda e: e.activation(out=ysq[:, 0:n], in_=y0[:, 0:n], func=AF.Square), ["y0"], ["ysq"])
                    p.op('pe', lambda e: e.matmul(K.ps[0][:, 0:n], lhsT=blk[:, :], rhs=ysq[:, 0:n], start=True,
                                                  stop=True), r=["blk", "ysq"], w=["ps0"])
                    if m == 0:
                        p.op('pe', lambda e: e.matmul(K.ps[1][:, 0:n], lhsT=blk[:, :], rhs=y0[:, 0:n], start=True,
                                                      stop=True), r=["blk", "y0"], w=["ps1"])
                        V(lambda e: e.tensor_copy(out=mean[:, 0:n], in_=K.ps[1][:, 0:n]), ["ps1"], ["mean"])
                        V(lambda e: e.tensor_tensor(out=var[:, 0:n], in0=mean[:, 0:n], in1=mean[:, 0:n], op=ALU.mult),
                          ["mean"], ["var"])
                        V(lambda e: e.tensor_tensor(out=var[:, 0:n], in0=K.ps[0][:, 0:n], in1=var[:, 0:n],
                                                    op=ALU.subtract), ["ps0", "var"], ["var"])
                        V(lambda e: e.tensor_scalar(out=var[:, 0:n], in0=var[:, 0:n], scalar1=64e-5, scalar2=None,
                                                    op0=ALU.add), ["var"], ["var"])
                        A(lambda e: e.activation(out=var[:, 0:n], in_=var[:, 0:n], func=AF.Sqrt), ["var"], ["var"])
                        V(lambda e: e.reciprocal(out=var[:, 0:n], in_=var[:, 0:n]), ["var"], ["var"])
                        V(lambda e: e.tensor_tensor(out=y0[:, 0:n], in0=y0[:, 0:n], in1=mean[:, 0:n], op=ALU.subtract),
                          ["y0", "mean"], ["y0"])
                        V(lambda e: e.tensor_tensor(out=y0[:, 0:n], in0=y0[:, 0:n], in1=var[:, 0:n], op=ALU.mult),
                          ["y0", "var"], ["y0"])
                        V(lambda e: e.tensor_scalar(out=y0[:, 0:n], in0=y0[:, 0:n], scalar1=pT[:, 14 + hp:15 + hp],
                                                    scalar2=pT[:, 16 + hp:17 + hp], op0=ALU.mult, op1=ALU.add),
                          ["y0", "pT"], ["y0"])
                        V(lambda e: e.tensor_tensor(out=y0[:, 0:n], in0=y0[:, 0:n], in1=bvv[:, 0:n], op=ALU.add),
                          ["y0", "bvv"], ["y0"])
                        V(lambda e: e.tensor_tensor(out=y0[:, 0:n], in0=y0[:, 0:n], in1=ga[:, 0:n], op=ALU.mult),
                          ["y0", "ga"], ["y0"])
                    else:
                        V(lambda e: e.tensor_scalar(out=var[:, 0:n], in0=K.ps[0][:, 0:n], scalar1=1e-6, scalar2=None,
                                                    op0=ALU.add), ["ps0"], ["var"])
                        A(lambda e: e.activation(out=var[:, 0:n], in_=var[:, 0:n], func=AF.Sqrt), ["var"], ["var"])
                        V(lambda e: e.reciprocal(out=var[:, 0:n], in_=var[:, 0:n]), ["var"], ["var"])
                        V(lambda e: e.scalar_tensor_tensor(out=y0[:, 0:n], in0=y0[:, 0:n], scalar=gnT[:, 0:1],
                                                           in1=var[:, 0:n], op0=ALU.mult, op1=ALU.mult),
                          ["y0", "gnT", "var"], ["y0"])
                        A(lambda e: e.activation(out=ga[:, 0:n], in_=ga[:, 0:n], func=AF.Silu), ["ga"], ["ga"])
                        V(lambda e: e.tensor_tensor(out=y0[:, 0:n], in0=y0[:, 0:n], in1=ga[:, 0:n], op=ALU.mult),
                          ["y0", "ga"], ["y0"])
                    r0 = m * 256 + hp * 128
                    p.dma('pool', K.mix[r0:r0 + 128, b0:b0 + n], y0[:, 0:n], r=["y0"], w=["mix"])
        p.barrier()


LAST_INPUT_NAMES = []
PHASES = {}


def build_program(phases=None, debug=(), dbg_in=()):
    del LAST_INPUT_NAMES[:]
    nc = bass.Bass("TRN2", target_bir_lowering=False)
    K = Ctx()
    K.nc = nc
    K.inp = {}

    def inp(name, shape, dt=F32):
        K.inp[name] = nc.dram_tensor(name, list(shape), dt, kind="ExternalInput").ap()
        LAST_INPUT_NAMES.append(name)

    def scratch(name, shape, dt=F32):
        if name in dbg_in:
            LAST_INPUT_NAMES.append(name)
            return nc.dram_tensor(name, list(shape), dt, kind="ExternalInput").ap()
        kind = "ExternalOutput" if name in debug else "Internal"
        return nc.dram_tensor(name, list(shape), dt, kind=kind).ap()

    if phases is None:
        phases = [(n, li) for li in range(DEPTH) for n in PHASE_ORDER]
    names = set(n for n, _ in phases)
    inp("xin", [T, D])
    inp("c2T", [128, 8, 2])
    inp("ident", [128, 128])
    inp("ada_w", [DEPTH, D, 6 * D])
    inp("ada_b", [DEPTH, 6 * D])
    inp("ada_bT", [DEPTH, 128, 48])
    if "inproj" in names:
        inp("w_in_p", [DEPTH, D, ZROWS])
    if "outproj" in names:
        inp("w_out", [DEPTH, D, D])
    inp("ln_g", [DEPTH, 2, D])
    inp("ln_b", [DEPTH, 2, D])
    if "s5" in names:
        for n_ in ("s5_lreT", "s5_limT", "s5_dtT"):
            inp(n_, [DEPTH, 128, 16])
        inp("s5_dT", [DEPTH, 128, 2])
        inp("s5_glbT", [DEPTH, 128, 2])
        inp("s5_glu_w", [DEPTH, 256, 256])
        for n_ in ("s5_breT", "s5_bimT", "s5_creT", "s5_cimT"):
            inp(n_, [DEPTH, 16, 128, 128])
    if "hyena" in names:
        inp("hy_conv", [DEPTH, 3, 768])
        inp("hy_conv_b", [DEPTH, 768])
        inp("hy_w1", [DEPTH, 33, 64])
        inp("hy_w2", [DEPTH, 64, 64])
        inp("hy_w3", [DEPTH, 64, 1024])
        for n_ in ("hy_b1", "hy_f1", "hy_b2", "hy_f2"):
            inp(n_, [DEPTH, 64])
        inp("hy_skip", [DEPTH, 2, 256])
        inp("hy_alt", [128, 2], BF16)
        inp("hy_altrow", [1, 128], BF16)
        for sq_, L_ in (("ctx", LC), ("lat", LL)):
            inp(f"hy_featT_{sq_}", [33, L_])
            inp(f"hy_dec_{sq_}", [L_, 256])
            inp(f"dft_{sq_}", [2, L_ // 128, 128, L_], BF16)
    if "scan" in names:
        inp("scan_sel", [6, 128], BF16)
    if names & {"rwprep", "gdprep", "rwgdpost"}:
        inp("rw_w2p", [DEPTH, 4, 128, 128])
        inp("rw_a2p", [DEPTH, 4, 128, 128])
        inp("rw_g2p", [DEPTH, 2, 128, 128])
        inp("rw_pT", [DEPTH, 128, 18])
        inp("blk64", [128, 128])
        inp("gd_conv", [DEPTH, 3, 768])
        inp("gd_dtb", [DEPTH, 2, 4])
        inp("gd_alog", [DEPTH, 2, 4])
        inp("gd_normT", [DEPTH, 128])
    if "router" in names:
        inp("router_w", [DEPTH, D, 64])
        inp("router_b", [DEPTH, 64])
    if "experts" in names:
        inp("ex_w1", [DEPTH, 64, D, 256])
        inp("ex_w3", [DEPTH, 64, D, 256])
        inp("ex_w2", [DEPTH, 64, 256, D])
        inp("sh_w1", [DEPTH, D, 256])
        inp("sh_w3", [DEPTH, D, 256])
        inp("sh_w2", [DEPTH, 256, D])
    K.out = nc.dram_tensor("out", [LL, D], F32, kind="ExternalOutput").ap()
    K.xres = scratch("xres", [T, D])
    K.zfm = scratch("zfm", [ZROWS, T])
    K.mix = scratch("mix", [D, T])
    K.hT = scratch("hT", [D, T], BF16)
    K.hytm = scratch("hytm", [3, T, 256])
    K.rows = scratch("rows", [5, T, 3, 2, 2, 256], BF16)
    K.gdstg = scratch("gdstg", [T, 5 * 2 * 2 * 128])
    K.vfm = scratch("vfm", [4, 128, T])
    K.yfm = scratch("yfm", [8, 128, T])
    K.rwg = scratch("rwg", [256, T])
    K.rwbv = scratch("rwbv", [256, T])
    with contextlib.ExitStack() as es:
        es.enter_context(nc.allow_non_contiguous_dma(reason="tiny per-channel parameter loads"))
        K.p = p = Prog(nc)
        K.ps = [es.enter_context(nc.psum_tensor(f"psb{i}", [128, 512], F32)) for i in range(8)]
        K.ident = es.enter_context(_sb(nc, "ident_sb", [128, 128], F32))
        K.modT = es.enter_context(_sb(nc, "modT", [128, 48, 2], F32))
        K.gates = es.enter_context(_sb(nc, "gates", [128, NT, 64], F32))
        K.gate = {(w, g): es.enter_context(_sb(nc, f"gate{w}{g}", [128, 1024], F32))
                  for w in range(2) for g in range(2)}
        p.dma('sp', K.ident[:], K.inp["ident"][:, :], w=["ident"])
        if "xres" not in dbg_in:
            p.dma('sp', K.xres[:, :], K.inp["xin"][:, :], w=["xres"])
        p.barrier()
        for (name, li) in phases:
            PHASES[name](K, li)
        if "xres_out" in debug:
            xo = nc.dram_tensor("xres_out", [T, D], F32, kind="ExternalOutput").ap()
            p.dma('sp', xo[:, :], K.xres[:, :], r=["xres"])
        p.finish()
    return nc


PHASE_ORDER = ["adaln", "inproj", "gdprep", "rwprep", "scan", "rwgdpost", "hyena", "s5", "outproj", "router", "experts"]
PHASES.update(rwprep=phase_rwprep, gdprep=phase_gdprep, rwgdpost=phase_rwgd_post, scan=phase_scan, hyena=phase_hyena, s5=phase_s5, adaln=phase_adaln, inproj=phase_inproj, outproj=phase_outproj, router=phase_router,
              experts=phase_experts)


_HYC = {}


def _hy_consts():
    if _HYC:
        return _HYC
    import ml_dtypes
    alt = np.where(np.arange(128) % 2 == 0, 1.0, -1.0).astype(np.float32)
    _HYC["hy_alt"] = np.stack([alt, alt], axis=1).astype(ml_dtypes.bfloat16)
    _HYC["hy_altrow"] = alt[None, :].astype(ml_dtypes.bfloat16)
    for sq_, L in (("ctx", LC), ("lat", LL)):
        t = np.linspace(0.0, 1.0, L, dtype=np.float32)[:, None]
        ang = (np.float32(2.0 * math.pi / L) * np.arange(L, dtype=np.float32))[:, None]
        bands = np.linspace(1e-4, 15.0, 16, dtype=np.float32)
        feat = np.concatenate([t, np.cos(bands * ang), -np.sin(bands * ang)], axis=-1).astype(np.float32)
        _HYC[f"hy_featT_{sq_}"] = np.ascontiguousarray(feat.T)
        deltas = np.abs(np.linspace(math.log(1e-2) / 1.5, math.log(1e-2) / 0.3, 256, dtype=np.float32))
        _HYC[f"hy_dec_{sq_}"] = np.exp(-t * deltas[None, :]).astype(np.float32)
        n = np.arange(L, dtype=np.int64)
        prod = (n[:, None] * n[None, :]) % (2 * L)
        ang2 = prod.astype(np.float64) * (2.0 * math.pi / (2 * L))
        nt = L // 128
        tabs = []
        for fn in (np.cos, np.sin):
            tb = fn(ang2).astype(np.float32).reshape(nt, 128, nt, 128)
            tabs.append(tb.transpose(2, 1, 0, 3).reshape(nt, 128, L))
        _HYC[f"dft_{sq_}"] = np.stack(tabs, axis=0).astype(ml_dtypes.bfloat16)
    return _HYC


def host_inputs(inputs, b):
    f = lambda a: np.ascontiguousarray(a, dtype=np.float32)
    m = {}
    m["xin"] = f(np.concatenate([inputs["ctx"][b], inputs["x"][b]], axis=0))
    c2 = np.stack([inputs["c"][b], inputs["c_ctx"]], axis=0)
    m["c2T"] = f(c2.reshape(2, 8, 128).transpose(2, 1, 0))
    m["ident"] = np.eye(128, dtype=np.float32)
    m["ada_bT"] = f(np.asarray(inputs["ada_b"]).reshape(DEPTH, 48, 128).transpose(0, 2, 1))
    w_in = np.asarray(inputs["w_in"])
    wp = np.zeros((DEPTH, D, ZROWS), np.float32)
    wp[:, :, 0:960] = w_in[:, :, 0:960]
    wp[:, :, 1024:2048] = w_in[:, :, 960:1984]
    wp[:, :, 2048:2064] = w_in[:, :, 1984:2000]
    wp[:, :, 2176:2944] = w_in[:, :, 2000:2768]
    wp[:, :, 2944:3200] = w_in[:, :, 2768:3024]
    m["w_in_p"] = wp
    def st(a):
        a = np.asarray(a).reshape(DEPTH, 2, 8, 2, 64)
        return f(a.transpose(0, 3, 4, 1, 2).reshape(DEPTH, 128, 16))
    m["s5_lreT"] = st(inputs["s5_lre"])
    m["s5_limT"] = st(inputs["s5_lim"])
    m["s5_dtT"] = st(np.broadcast_to(np.asarray(inputs["s5_logstep"])[..., None], (DEPTH, 2, 16, 64)))
    m["s5_dT"] = f(np.asarray(inputs["s5_d"]).reshape(DEPTH, 2, 128).transpose(0, 2, 1))
    m["s5_glbT"] = f(np.asarray(inputs["s5_glu_b"]).reshape(DEPTH, 2, 128).transpose(0, 2, 1))
    m["s5_glu_w"] = f(inputs["s5_glu_w"])

    def bT(a):
        a = np.asarray(a)
        o = np.zeros((DEPTH, 2, 8, 128, 128), np.float32)
        for j in range(8):
            for gg in range(2):
                g = 2 * j + gg
                r0 = (g % 8) * 16
                o[:, :, j, r0:r0 + 16, gg * 64:(gg + 1) * 64] = a[:, :, g].transpose(0, 1, 3, 2)
        return o.reshape(DEPTH, 16, 128, 128)

    def cT(a, sign=1.0):
        a = np.asarray(a)
        o = np.zeros((DEPTH, 2, 8, 128, 128), np.float32)
        for j in range(8):
            for gg in range(2):
                g = 2 * j + gg
                c0 = (g % 8) * 16
                o[:, :, j, gg * 64:(gg + 1) * 64, c0:c0 + 16] = a[:, :, g].transpose(0, 1, 3, 2)
        return o.reshape(DEPTH, 16, 128, 128)
    m["s5_breT"] = bT(inputs["s5_bre"])
    m["s5_bimT"] = bT(inputs["s5_bim"])
    m["s5_creT"] = cT(inputs["s5_cre"])
    m["s5_cimT"] = cT(inputs["s5_cim"])
    m.update(_hy_consts())
    import ml_dtypes
    w2, a2, g2 = np.asarray(inputs["rw_w2"]), np.asarray(inputs["rw_a2"]), np.asarray(inputs["rw_g2"])
    w2p = np.zeros((DEPTH, 4, 128, 128), np.float32)
    a2p = np.zeros((DEPTH, 4, 128, 128), np.float32)
    g2p = np.zeros((DEPTH, 2, 128, 128), np.float32)
    for d in range(2):
        for cc in range(2):
            w2p[:, d * 2 + cc, d * 32:(d + 1) * 32, :] = w2[:, d, :, cc * 128:(cc + 1) * 128]
            a2p[:, d * 2 + cc, 64 + d * 32:64 + (d + 1) * 32, :] = a2[:, d, :, cc * 128:(cc + 1) * 128]
    for cc in range(2):
        g2p[:, cc, 0:64, :] = g2[:, :, cc * 128:(cc + 1) * 128]
    m["rw_w2p"], m["rw_a2p"], m["rw_g2p"] = w2p, a2p, g2p
    cols = []
    for nm in ("rw_w0", "rw_a0"):
        a_ = np.asarray(inputs[nm])
        for d in range(2):
            for cc in range(2):
                cols.append(a_[:, d, cc * 128:(cc + 1) * 128])
    for nm in ("rw_kk", "rw_ka", "rw_rk", "rw_gn_g", "rw_gn_b"):
        a_ = np.asarray(inputs[nm]).reshape(DEPTH, 256)
        for cc in range(2):
            cols.append(a_[:, cc * 128:(cc + 1) * 128])
    m["rw_pT"] = f(np.stack(cols, axis=-1))
    blk = np.zeros((128, 128), np.float32)
    blk[0:64, 0:64] = 1.0
    blk[64:128, 64:128] = 1.0
    m["blk64"] = blk
    m["gd_normT"] = f(np.tile(np.asarray(inputs["gd_norm"]), (1, 2)))
    for k in ("gd_conv", "gd_dtb", "gd_alog"):
        m[k] = f(inputs[k])
    sel = np.zeros((6, 128), np.float32)
    for q in range(6):
        sel[q, (q % 2) * 64:(q % 2 + 1) * 64] = 1.0
    m["scan_sel"] = sel.astype(ml_dtypes.bfloat16)
    for k in ("hy_conv", "hy_conv_b", "hy_w1", "hy_w2", "hy_w3", "hy_b1", "hy_f1", "hy_b2", "hy_f2", "hy_skip"):
        m[k] = f(inputs[k])
    for k in ("ada_w", "ada_b", "w_out", "ln_g", "ln_b", "router_w", "router_b", "ex_w1", "ex_w3", "ex_w2",
              "sh_w1", "sh_w3", "sh_w2"):
        m[k] = f(inputs[k])
    return m


def kernel(**inputs):
    nc = build_program()
    names = set(LAST_INPUT_NAMES)
    in_maps = [{k: v for k, v in host_inputs(inputs, b).items() if k in names} for b in range(8)]
    res = run_bass_kernel_spmd(nc, in_maps, core_ids=list(range(8)))
    return np.stack([r["out"] for r in res.results], axis=0)
```
